# Optimizing a Trainium2 kernel written in Bass

```python
import jax
import jax.numpy as jnp
from jax import lax
import numpy as np

D_MODEL = 1024
BATCH = 16
SEQ = 2048
DEPTH = 1

GRID_W = 64
CTX_LEN = 256
NA_HEADS = 8
NA_HEAD_DIM = 64
NA_WIDTH = NA_HEADS * NA_HEAD_DIM
NA_WIN_H = 8
NA_WIN_W = 16
NA_QBLOCK = 16
NA_KBLOCK = NA_QBLOCK + NA_WIN_W
GLA_HEADS = 4
GLA_DK = D_MODEL // 2
GLA_DV = D_MODEL
GLA_HK = GLA_DK // GLA_HEADS
GLA_HV = GLA_DV // GLA_HEADS
GLA_GATE_RANK = 16
GLA_GATE_NORM = 16.0
GLA_CHUNK = 64
ROPE_BASE = 10000.0
N_GROUPS = 4
EXPERTS_PER_GROUP = 8
N_EXPERTS = N_GROUPS * EXPERTS_PER_GROUP
TOP_K = 2
D_EXPERT = D_MODEL // 2
MOE_BLOCK = 128
D_IN = 3 * NA_WIDTH + 2 * GLA_DK + 2 * GLA_DV + 2 * GLA_GATE_RANK + 2 * D_MODEL
EPS = 1e-6
NEG_INF = -1e30

kernel_name = "hybrid_na_gla_hmoe_dit_block"


def rmsnorm(x, g):
    xf = x.astype(jnp.float32)
    xf = xf * lax.rsqrt(jnp.mean(xf * xf, axis=-1, keepdims=True) + EPS)
    return xf.astype(x.dtype) * g


def split_proj(p):
    sizes = (NA_WIDTH, NA_WIDTH, NA_WIDTH, GLA_DK, GLA_DK, GLA_DV, GLA_DV, 2 * GLA_GATE_RANK, D_MODEL, D_MODEL)
    return jnp.split(p, np.cumsum(sizes)[:-1].tolist(), axis=-1)


def heads(t, n):
    return t.reshape(t.shape[0], t.shape[1], n, t.shape[-1] // n)


def _rotate_half(x, ang):
    m = x.shape[-1] // 2
    cos = jnp.cos(ang)[:, None, :].astype(x.dtype)
    sin = jnp.sin(ang)[:, None, :].astype(x.dtype)
    x1, x2 = x[..., :m], x[..., m:]
    return jnp.concatenate([x1 * cos - x2 * sin, x1 * sin + x2 * cos], axis=-1)


def rope_2d(x, pos_r, pos_c):
    half = x.shape[-1] // 2
    nf = half // 2
    inv = ROPE_BASE ** (-jnp.arange(nf, dtype=jnp.float32) / nf)
    return jnp.concatenate([_rotate_half(x[..., :half], pos_r[:, None] * inv),
                            _rotate_half(x[..., half:], pos_c[:, None] * inv)], axis=-1)


def na_latent(q, k, v, k_ctx, v_ctx, rpb):
    B_, S, H, dh = q.shape
    rows = S // GRID_W
    kh = min(NA_WIN_H, rows)
    nb = GRID_W // NA_QBLOCK
    kc0 = [min(max(j * NA_QBLOCK - NA_WIN_W // 2, 0), GRID_W - NA_KBLOCK) for j in range(nb)]
    qcol = np.arange(GRID_W).reshape(nb, NA_QBLOCK)
    kcol = np.array(kc0)[:, None] + np.arange(NA_KBLOCK)[None, :]
    cstart = np.clip(qcol - NA_WIN_W // 2, 0, GRID_W - NA_WIN_W)
    col_valid = (kcol[:, None, :] >= cstart[:, :, None]) & (kcol[:, None, :] < cstart[:, :, None] + NA_WIN_W)
    col_idx = np.clip(kcol[:, None, :] - qcol[:, :, None] + NA_WIN_W - 1, 0, 2 * NA_WIN_W - 2)
    col_valid = jnp.asarray(col_valid)[:, :, None, :]
    scale = dh ** -0.5
    k_grid = k.reshape(B_, rows, GRID_W, H, dh)
    v_grid = v.reshape(B_, rows, GRID_W, H, dh)
    q_rows = q.reshape(B_, rows, nb, NA_QBLOCK, H, dh).transpose(1, 0, 2, 3, 4, 5)

    def row_block(inp):
        r, q_r = inp
        rs = jnp.clip(r - kh // 2, 0, rows - kh)
        k_r = lax.dynamic_slice_in_dim(k_grid, rs, kh, axis=1)
        v_r = lax.dynamic_slice_in_dim(v_grid, rs, kh, axis=1)
        k_blk = jnp.stack([k_r[:, :, c0:c0 + NA_KBLOCK] for c0 in kc0], axis=1)
        v_blk = jnp.stack([v_r[:, :, c0:c0 + NA_KBLOCK] for c0 in kc0], axis=1)
        row_idx = rs + jnp.arange(kh) - r + NA_WIN_H - 1
        bias = rpb[:, row_idx][:, :, col_idx].transpose(0, 2, 3, 1, 4)
        s_win = jnp.einsum('bjqhd,bjikhd->bhjqik', q_r, k_blk).astype(jnp.float32) * scale
        s_win = jnp.where(col_valid, s_win + bias.astype(jnp.float32), NEG_INF)
        s_win = s_win.reshape(B_, H, nb, NA_QBLOCK, kh * NA_KBLOCK)
        s_ctx = jnp.einsum('bjqhd,bkhd->bhjqk', q_r, k_ctx).astype(jnp.float32) * scale
        p = jax.nn.softmax(jnp.concatenate([s_win, s_ctx], axis=-1), axis=-1).astype(v.dtype)
        p_win = p[..., :kh * NA_KBLOCK].reshape(B_, H, nb, NA_QBLOCK, kh, NA_KBLOCK)
        p_ctx = p[..., kh * NA_KBLOCK:]
        return (jnp.einsum('bhjqik,bjikhd->bjqhd', p_win, v_blk)
                + jnp.einsum('bhjqk,bkhd->bjqhd', p_ctx, v_ctx))

    o = lax.map(row_block, (jnp.arange(rows), q_rows))
    return o.transpose(1, 0, 2, 3, 4, 5).reshape(B_, S, H * dh)


def na_context(q, k, v):
    B_, T, H, dh = q.shape
    s = jnp.einsum('bqhd,bkhd->bhqk', q, k).astype(jnp.float32) * dh ** -0.5
    p = jax.nn.softmax(s, axis=-1).astype(v.dtype)
    return jnp.einsum('bhqk,bkhd->bqhd', p, v).reshape(B_, T, H * dh)


def gla_chunked(q, k, v, g, s0):
    B_, T, H, _ = q.shape
    dv = v.shape[-1]
    n = T // GLA_CHUNK

    def to_chunks(a):
        return a.astype(jnp.float32).reshape(B_, n, GLA_CHUNK, H, a.shape[-1]).transpose(1, 0, 3, 2, 4)

    tril = jnp.tril(jnp.ones((GLA_CHUNK, GLA_CHUNK), dtype=bool))

    def step(s, inp):
        qc, kc, vc, gc = inp
        b = jnp.cumsum(gc, axis=-2)
        b_last = b[..., -1:, :]
        qe = qc * jnp.exp(b)
        ke = kc * jnp.exp(-b)
        att = jnp.where(tril, jnp.einsum('bhtd,bhsd->bhts', qe, ke), 0.0)
        o = jnp.einsum('bhts,bhse->bhte', att, vc) + jnp.einsum('bhtd,bhde->bhte', qe, s)
        s = (jnp.exp(b_last[..., 0, :])[..., None] * s
             + jnp.einsum('bhsd,bhse->bhde', kc * jnp.exp(b_last - b), vc))
        return s, o

    s_fin, o = lax.scan(step, s0, (to_chunks(q), to_chunks(k), to_chunks(v), to_chunks(g)))
    return o.transpose(1, 0, 3, 2, 4).reshape(B_, T, H, dv), s_fin


def gla_bidir(q, k, v, g_f, g_b, s_f0, s_b0):
    o_f, s_f = gla_chunked(q, k, v, g_f, s_f0)
    o_b, s_b = gla_chunked(q[:, ::-1], k[:, ::-1], v[:, ::-1], g_b[:, ::-1], s_b0)
    return o_f + o_b[:, ::-1], s_f, s_b


def hybrid_mixer(a_lat, a_ctx, pos_r, pos_c, w_in, w_gla_a2, b_gla_a2, gla_norm_g, na_rpb,
                 w_na_o, w_gla_o, w_out, need_ctx_out):
    B_ = a_lat.shape[0]
    P_lat = split_proj(a_lat @ w_in)
    P_ctx = split_proj(a_ctx @ w_in)

    def gla_streams(P, rotary):
        q = heads(P[3], GLA_HEADS) * (GLA_HK ** -0.5)
        k = heads(P[4], GLA_HEADS)
        if rotary:
            q = rope_2d(q, pos_r, pos_c)
            k = rope_2d(k, pos_r, pos_c)
        v = heads(P[5], GLA_HEADS)
        a = P[7]
        z_f = (a[..., :GLA_GATE_RANK] @ w_gla_a2[0] + b_gla_a2[0]).astype(jnp.float32)
        z_b = (a[..., GLA_GATE_RANK:] @ w_gla_a2[1] + b_gla_a2[1]).astype(jnp.float32)
        g_f = heads(jax.nn.log_sigmoid(z_f) / GLA_GATE_NORM, GLA_HEADS)
        g_b = heads(jax.nn.log_sigmoid(z_b) / GLA_GATE_NORM, GLA_HEADS)
        return q, k, v, g_f, g_b

    def gla_readout(o, P):
        o = rmsnorm(o.astype(P[6].dtype), gla_norm_g) * jax.nn.silu(heads(P[6], GLA_HEADS))
        return o.reshape(o.shape[0], o.shape[1], GLA_DV)

    def merge(P, o_na, o_gla):
        y = jax.nn.sigmoid(P[8]) * (o_na @ w_na_o) + jax.nn.sigmoid(P[9]) * (o_gla @ w_gla_o)
        return y @ w_out

    q_na, k_na, v_na = [heads(t, NA_HEADS) for t in P_lat[:3]]
    qc_na, kc_na, vc_na = [heads(t, NA_HEADS) for t in P_ctx[:3]]
    o_na = na_latent(q_na, k_na, v_na, kc_na, vc_na, na_rpb)

    s0 = jnp.zeros((B_, GLA_HEADS, GLA_HK, GLA_HV), jnp.float32)
    oc_gla, s_f, s_b = gla_bidir(*gla_streams(P_ctx, False), s0, s0)
    o_gla, _, _ = gla_bidir(*gla_streams(P_lat, True), s_f, s_b)

    y_lat = merge(P_lat, o_na, gla_readout(o_gla, P_lat))
    y_ctx = None
    if need_ctx_out:
        y_ctx = merge(P_ctx, na_context(qc_na, kc_na, vc_na), gla_readout(oc_gla, P_ctx))
    return y_lat, y_ctx


def hier_moe(x2d, w_group, b_group, w_expert, b_expert, w_gate, w_up, w_down):
    T, D = x2d.shape
    lg = (x2d @ w_group).astype(jnp.float32) + b_group
    grp = jnp.argmax(lg, axis=-1)
    p_grp = jnp.take_along_axis(jax.nn.softmax(lg, axis=-1), grp[:, None], axis=-1)
    le = ((x2d @ w_expert).astype(jnp.float32) + b_expert).reshape(T, N_GROUPS, EXPERTS_PER_GROUP)
    le_g = jnp.take_along_axis(le, grp[:, None, None], axis=1)[:, 0]
    top_p, top_i = lax.top_k(jax.nn.softmax(le_g, axis=-1), TOP_K)
    weights = p_grp * top_p / jnp.sum(top_p, axis=-1, keepdims=True)
    expert_id = grp[:, None] * EXPERTS_PER_GROUP + top_i

    ids = expert_id.reshape(-1).astype(jnp.int32)
    wts = weights.reshape(-1)
    tok = jnp.repeat(jnp.arange(T, dtype=jnp.int32), TOP_K)
    order = jnp.argsort(ids)
    ids_s, tok_s, w_s = ids[order], tok[order], wts[order]
    counts = jax.ops.segment_sum(jnp.ones_like(ids), ids, num_segments=N_EXPERTS)
    padded = (counts + MOE_BLOCK - 1) // MOE_BLOCK * MOE_BLOCK
    start = jnp.cumsum(counts) - counts
    pend = jnp.cumsum(padded)
    pstart = pend - padded
    dest = pstart[ids_s] + (jnp.arange(T * TOP_K, dtype=jnp.int32) - start[ids_s])
    n_pad = (T * TOP_K + N_EXPERTS * (MOE_BLOCK - 1) + MOE_BLOCK - 1) // MOE_BLOCK * MOE_BLOCK
    n_blk = n_pad // MOE_BLOCK
    tok_buf = jnp.full((n_pad,), T, jnp.int32).at[dest].set(tok_s)
    w_buf = jnp.zeros((n_pad,), jnp.float32).at[dest].set(w_s)
    blk_start = jnp.arange(n_blk, dtype=jnp.int32) * MOE_BLOCK
    blk_expert = jnp.clip(jnp.searchsorted(pend, blk_start, side='right'), 0, N_EXPERTS - 1)
    x_pad = jnp.concatenate([x2d, jnp.zeros((1, D), x2d.dtype)], axis=0)
    xb = x_pad[tok_buf].reshape(n_blk, MOE_BLOCK, D)

    def expert_block(inp):
        xe, e = inp
        hdn = jax.nn.silu(xe @ w_gate[e]) * (xe @ w_up[e])
        return hdn @ w_down[e]

    yb = lax.map(expert_block, (xb, blk_expert)).reshape(n_pad, D)
    y = jax.ops.segment_sum(yb * w_buf[:, None].astype(yb.dtype), tok_buf, num_segments=T + 1)
    return y[:T]


def setup_inputs(seed: int = 0) -> dict:
    key = jax.random.key(seed)
    ks = jax.random.split(key, 24)
    f32 = jnp.float32
    L = DEPTH

    def nrm(k, shape, scale):
        return jax.random.normal(k, shape, f32) * scale

    return {
        "x": nrm(ks[0], (BATCH, SEQ, D_MODEL), 1.0),
        "c": nrm(ks[1], (BATCH, D_MODEL), 1.0),
        "ctx": nrm(ks[2], (BATCH, CTX_LEN, D_MODEL), 1.0),
        "c_ctx": nrm(ks[3], (D_MODEL,), 1.0),
        "w_mod": nrm(ks[4], (L, D_MODEL, 6 * D_MODEL), 0.5 * D_MODEL ** -0.5),
        "b_mod": nrm(ks[5], (L, 6 * D_MODEL), 0.02),
        "norm_attn_g": 1.0 + nrm(ks[6], (L, D_MODEL), 0.02),
        "norm_ffn_g": 1.0 + nrm(ks[7], (L, D_MODEL), 0.02),
        "w_in": nrm(ks[8], (L, D_MODEL, D_IN), D_MODEL ** -0.5),
        "w_gla_a2": nrm(ks[9], (L, 2, GLA_GATE_RANK, GLA_DK), GLA_GATE_RANK ** -0.5),
        "b_gla_a2": nrm(ks[10], (L, 2, GLA_DK), 0.1),
        "gla_norm_g": 1.0 + nrm(ks[11], (L, GLA_HV), 0.02),
        "na_rpb": nrm(ks[12], (L, NA_HEADS, 2 * NA_WIN_H - 1, 2 * NA_WIN_W - 1), 0.1),
        "w_na_o": nrm(ks[13], (L, NA_WIDTH, D_MODEL), NA_WIDTH ** -0.5),
        "w_gla_o": nrm(ks[14], (L, GLA_DV, D_MODEL), GLA_DV ** -0.5),
        "w_out": nrm(ks[15], (L, D_MODEL, D_MODEL), D_MODEL ** -0.5),
        "w_group": nrm(ks[16], (L, D_MODEL, N_GROUPS), D_MODEL ** -0.5),
        "b_group": nrm(ks[17], (L, N_GROUPS), 0.01),
        "w_expert": nrm(ks[18], (L, D_MODEL, N_EXPERTS), D_MODEL ** -0.5),
        "b_expert": nrm(ks[19], (L, N_EXPERTS), 0.01),
        "w_exp_gate": nrm(ks[20], (L, N_EXPERTS, D_MODEL, D_EXPERT), D_MODEL ** -0.5),
        "w_exp_up": nrm(ks[21], (L, N_EXPERTS, D_MODEL, D_EXPERT), D_MODEL ** -0.5),
        "w_exp_down": nrm(ks[22], (L, N_EXPERTS, D_EXPERT, D_MODEL), D_EXPERT ** -0.5),
        "final_norm_g": 1.0 + nrm(ks[23], (D_MODEL,), 0.02),
    }


def reference(x, c, ctx, c_ctx, w_mod, b_mod, norm_attn_g, norm_ffn_g, w_in, w_gla_a2, b_gla_a2,
              gla_norm_g, na_rpb, w_na_o, w_gla_o, w_out, w_group, b_group, w_expert, b_expert,
              w_exp_gate, w_exp_up, w_exp_down, final_norm_g):
    B_, S, D = x.shape
    t = jnp.arange(S)
    pos_r = (t // GRID_W).astype(jnp.float32)
    pos_c = (t % GRID_W).astype(jnp.float32)
    h, hc = x, ctx
    for l in range(DEPTH):
        ctx_continues = l < DEPTH - 1
        mod = jax.nn.silu(c) @ w_mod[l] + b_mod[l]
        sh1, sc1, g1, sh2, sc2, g2 = jnp.split(mod[:, None, :], 6, axis=-1)
        csh1, csc1, cg1, csh2, csc2, cg2 = jnp.split(jax.nn.silu(c_ctx) @ w_mod[l] + b_mod[l], 6, axis=-1)
        a_lat = rmsnorm(h, norm_attn_g[l]) * (1 + sc1) + sh1
        a_ctx = rmsnorm(hc, norm_attn_g[l]) * (1 + csc1) + csh1
        y_lat, y_ctx = hybrid_mixer(a_lat, a_ctx, pos_r, pos_c, w_in[l], w_gla_a2[l], b_gla_a2[l],
                                    gla_norm_g[l], na_rpb[l], w_na_o[l], w_gla_o[l], w_out[l],
                                    ctx_continues)
        h = h + g1 * y_lat
        f_lat = rmsnorm(h, norm_ffn_g[l]) * (1 + sc2) + sh2
        h = h + g2 * hier_moe(f_lat.reshape(-1, D), w_group[l], b_group[l], w_expert[l], b_expert[l],
                              w_exp_gate[l], w_exp_up[l], w_exp_down[l]).reshape(B_, S, D)
        if ctx_continues:
            hc = hc + cg1 * y_ctx
            f_ctx = rmsnorm(hc, norm_ffn_g[l]) * (1 + csc2) + csh2
            hc = hc + cg2 * hier_moe(f_ctx.reshape(-1, D), w_group[l], b_group[l], w_expert[l], b_expert[l],
                                     w_exp_gate[l], w_exp_up[l], w_exp_down[l]).reshape(hc.shape)
    return rmsnorm(h, final_norm_g)
```

```python
import os
import numpy as np
import concourse.bass as bass
import concourse.mybir as mybir
from concourse.bass_utils import run_bass_kernel_spmd
from contextlib import ExitStack

F32 = mybir.dt.float32
BF16 = mybir.dt.bfloat16
I32 = mybir.dt.int32
ALU = mybir.AluOpType
AF = mybir.ActivationFunctionType
AX = mybir.AxisListType

D = 1024
S = 2048
CT = 256
NT = 16
NTC = 2
NTA = NT + NTC
GW = 64
EPS = 1e-6
NEXP = 32
DE = 512
C_NAQ, C_NAK, C_NAV, C_GQ, C_GK, C_GV, C_GG, C_AL, C_M8, C_M9 = 0, 512, 1024, 1536, 2048, 2560, 3584, 4608, 4640, 5664
MASKV = -30000.0


class Sched:
    ENGS = ("pe", "act", "dve", "pool", "sp")
    HND = {"pe": "tensor", "act": "scalar", "dve": "vector", "pool": "gpsimd", "sp": "sync"}

    def __init__(self, nc, es, n_dma_sems=32):
        self.nc = nc
        self.sem = {e: es.enter_context(nc.semaphore("s_" + e)) for e in self.ENGS}
        self.cnt = {e: 0 for e in self.ENGS}
        self.pending = {e: False for e in self.ENGS}
        self.waited = {e: {} for e in self.ENGS}
        self.dsem = [es.enter_context(nc.semaphore("s_dma%d" % i)) for i in range(n_dma_sems)]
        self.dcnt = [0] * n_dma_sems
        self.dnext = 0
        self.lastw = {}
        self.reads = {}
        self.semobj = {}
        for e in self.ENGS:
            self.semobj[("e", e)] = self.sem[e]
        for i, sm in enumerate(self.dsem):
            self.semobj[("d", i)] = sm
        self.n_ins = 0
        self.n_wait = 0

    def _deps(self, eng, reads, writes, excl=()):
        toks = {}

        def add(t):
            if t is None:
                return
            k, v = t
            if toks.get(k, -1) < v:
                toks[k] = v
        for b in reads:
            add(self.lastw.get(b))
        for b in writes:
            add(self.lastw.get(b))
            for t in self.reads.get(b, ()):
                add(t)
        for b in excl:
            t = self.lastw.get(b)
            if t is not None and t[0] != ("e", eng):
                add(t)
        out = []
        for k, v in toks.items():
            if k == ("e", eng):
                if eng == "pe":
                    continue
                if v <= self.cnt[eng] - 2:
                    continue
            if self.waited[eng].get(k, -1) >= v:
                continue
            self.waited[eng][k] = v
            out.append((k, v))
        return out

    def _commit(self, tok, reads, writes):
        for b in writes:
            self.lastw[b] = tok
            self.reads[b] = []
        for b in reads:
            self.reads.setdefault(b, []).append(tok)

    def check_deadlock(self):
        pos = {e: 0 for e in self.ENGS}
        val = {}
        prog = True
        while prog:
            prog = False
            for e in self.ENGS:
                lst = self.log[e]
                while pos[e] < len(lst):
                    waits, inc = lst[pos[e]]
                    if all(val.get(k, 0) >= v for k, v in waits):
                        if inc is not None:
                            val[inc[0]] = val.get(inc[0], 0) + inc[1]
                        pos[e] += 1
                        prog = True
                    else:
                        break
        stuck = {e: (pos[e], len(self.log[e])) for e in self.ENGS if pos[e] < len(self.log[e])}
        for e in stuck:
            waits, inc = self.log[e][pos[e]]
            print("STUCK", e, pos[e], [(k, v, val.get(k, 0)) for k, v in waits])
        return not stuck

    def _emit1(self, eng, waits, fn, inc):
        if not hasattr(self, "log"):
            self.log = {e: [] for e in self.ENGS}
        self.log[eng].append((list(waits), inc if fn is not None else None))
        engh = getattr(self.nc, self.HND[eng])
        for k, v in waits:
            engh.wait_ge(self.semobj[k], v)
            self.n_wait += 1
        if fn is None:
            return
        ins = fn(engh)
        if inc is not None:
            ins.then_inc(self.semobj[inc[0]], inc[1])
        self.n_ins += 1

    def op(self, eng, fn, reads=(), writes=(), inc=True, excl=()):
        waits = self._deps(eng, reads, writes, excl)
        if inc:
            self.cnt[eng] += 1
            tok = (("e", eng), self.cnt[eng])
            self.pending[eng] = False
        else:
            tok = (("e", eng), self.cnt[eng] + 1)
            self.pending[eng] = True
        self._emit1(eng, waits, fn, (("e", eng), 1) if inc else None)
        self._commit(tok, reads, writes)
        for b_ in excl:
            self.lastw[b_] = tok
        return tok

    def dma(self, eng, out, in_, reads=(), writes=(), **kw):
        i = self.dnext
        self.dnext = (self.dnext + 1) % len(self.dsem)
        k = ("d", i)
        waits = self._deps(eng, reads, writes)
        if self.dcnt[i] > 0 and self.waited[eng].get(k, -1) < self.dcnt[i]:
            self.waited[eng][k] = self.dcnt[i]
            waits.append((k, self.dcnt[i]))
        self.dcnt[i] += 16
        tok = (k, self.dcnt[i])
        self._emit1(eng, waits, lambda e: e.dma_start(out=out, in_=in_, **kw), (k, 16))
        self._commit(tok, reads, writes)
        return tok

    def barrier(self):
        assert not any(self.pending.values())
        allt = [(("e", e), self.cnt[e]) for e in self.ENGS if self.cnt[e] > 0]
        allt += [(("d", i), c) for i, c in enumerate(self.dcnt) if c > 0]
        for e in self.ENGS:
            waits = []
            for k, v in allt:
                if k == ("e", e):
                    continue
                if self.waited[e].get(k, -1) >= v:
                    continue
                self.waited[e][k] = v
                waits.append((k, v))
            self._emit1(e, waits, None, None)
        self.lastw = {}
        self.reads = {}


def build_nc(NS=2, dbg=None, stop_after=None):
    nc = bass.Bass("TRN2", target_bir_lowering=False)

    def din(name, shape, dt=F32):
        return nc.dram_tensor(name, list(shape), dt, kind="ExternalInput").ap()

    x_d = din("x", [NS, S, D])
    ctx_d = din("ctx", [NS, CT, D])
    cc_d = din("cc", [NS + 1, D])
    w_mod_d = din("w_mod", [D, 6 * D])
    b_mod_d = din("b_mod", [6 * D])
    g_attn_d = din("g_attn", [D])
    g_ffn_d = din("g_ffn", [D])
    g_fin_d = din("g_fin", [D])
    gla_g_d = din("gla_g", [256])
    w_in_d = din("w_in", [D, 6688])
    w_rope_d = din("w_rope", [D, 1024])
    w_a2b_d = din("w_a2b", [17, 2, 512])
    w_nao_d = din("w_na_o", [512, D])
    w_glo_d = din("w_gla_o", [D, D])
    w_out_d = din("w_out", [D, D])
    w_rt_d = din("w_rt", [D, 36])
    b_rt_d = din("b_rt", [36])
    w_eg_d = din("w_eg", [NEXP, D, DE])
    w_eu_d = din("w_eu", [NEXP, D, DE])
    w_ed_d = din("w_ed", [NEXP, DE, D])
    ident_d = din("ident", [128, 128])
    tri_d = din("tri", [4, 128, 128])
    cos_d = din("rope_cos", [128, S])
    sin_d = din("rope_sin", [128, S])
    nab_d = din("na_bias", [4, 128, 8, 2, 256])
    nam_d = din("na_mask", [128, 8, 256])
    y_d = nc.dram_tensor("y", [NS, S, D], F32, kind="ExternalOutput").ap()
    modv_d = nc.dram_tensor("modv", [NS + 1, 6 * D], F32).ap()
    dbg_d = {}
    if dbg:
        for name, shape in dbg.items():
            dbg_d[name] = nc.dram_tensor("dbg_" + name, list(shape), F32, kind="ExternalOutput").ap()

    with ExitStack() as es:
        s = Sched(nc, es)

        used_names = {}

        def uniq(name):
            k = used_names.get(name, 0)
            used_names[name] = k + 1
            return name if k == 0 else "%s_r%d" % (name, k)

        def sb(st, name, shape, dt):
            return st.enter_context(nc.sbuf_tensor(uniq(name), list(shape), dt))

        def pst(st, name, shape, dt=F32):
            return st.enter_context(nc.psum_tensor(uniq(name), list(shape), dt))

        def mm(out, lhsT, rhs, start, stop, reads, writes, inc=None, x=()):
            if inc is None:
                inc = stop
            s.op("pe", lambda e: e.matmul(out, lhsT, rhs, start=start, stop=stop),
                 reads=reads, writes=writes, inc=inc, excl=x)

        def tr(out, in_, idn, reads, writes, x=()):
            s.op("pe", lambda e: e.transpose(out, in_, idn), reads=reads, writes=writes, excl=x)

        def act(out, in_, func, reads, writes, x=(), **kw):
            s.op("act", lambda e: e.activation(out=out, in_=in_, func=func, **kw), reads=reads, writes=writes, excl=x)

        def tt(eng, out, in0, in1, op, reads, writes, x=()):
            s.op(eng, lambda e: e.tensor_tensor(out=out, in0=in0, in1=in1, op=op), reads=reads, writes=writes, excl=x)

        def ts(eng, out, in0, s1, s2, op0, op1, reads, writes, x=()):
            if s2 is None:
                s.op(eng, lambda e: e.tensor_scalar(out=out, in0=in0, scalar1=s1, scalar2=None, op0=op0),
                     reads=reads, writes=writes, excl=x)
            else:
                s.op(eng, lambda e: e.tensor_scalar(out=out, in0=in0, scalar1=s1, scalar2=s2, op0=op0, op1=op1),
                     reads=reads, writes=writes, excl=x)

        def stt(eng, out, in0, scalar, in1, op0, op1, reads, writes, x=()):
            s.op(eng, lambda e: e.scalar_tensor_tensor(out=out, in0=in0, scalar=scalar, in1=in1, op0=op0, op1=op1),
                 reads=reads, writes=writes, excl=x)

        def cp(eng, out, in_, reads, writes, x=()):
            if eng == "act":
                s.op("act", lambda e: e.copy(out=out, in_=in_), reads=reads, writes=writes, excl=x)
            else:
                s.op(eng, lambda e: e.tensor_copy(out=out, in_=in_), reads=reads, writes=writes, excl=x)

        def dump(name, src_ap, reads, dst=None):
            if name in dbg_d:
                s.dma("pool", dbg_d[name] if dst is None else dst, src_ap, reads=reads)

        def wview(ap2d):
            return ap2d.rearrange("(kt p) n -> p kt n", p=128)

        ident_f = sb(es, "ident_f", [128, 128], F32)
        ident_b = sb(es, "ident_b", [128, 128], BF16)
        tri_f = sb(es, "tri_f", [128, 4, 128], F32)
        ones_b = sb(es, "ones_b", [128, 128], BF16)
        eps_c = sb(es, "eps_c", [128, 1], F32)
        one_c = sb(es, "one_c", [128, 1], F32)
        s.dma("sp", ident_f[:], ident_d, writes=["ident_f"])
        s.dma("pool", ident_b[:], ident_d, writes=["ident_b"])
        s.dma("sp", tri_f[:], tri_d.rearrange("m s t -> s m t"), writes=["tri"])
        s.op("pool", lambda e: e.memset(ones_b[:], 1.0), writes=["ones_b"])
        s.op("pool", lambda e: e.memset(eps_c[:], EPS), writes=["eps_c"])
        s.op("pool", lambda e: e.memset(one_c[:], 1.0), writes=["one_c"])

        with ExitStack() as st:
            ccs = sb(st, "ccs", [NS + 1, D], F32)
            scT = sb(st, "scT", [128, 8, NS + 1], F32)
            modsb = sb(st, "modsb", [NS + 1, 6 * D], F32)
            bmod = sb(st, "bmod", [NS + 1, 6 * D], F32)
            wm = [sb(st, "wm%d" % i, [128, 8, 512], F32) for i in range(2)]
            psA = pst(st, "p0a", [128, 512])
            psB = [pst(st, "p0b%d" % i, [128, 512]) for i in range(2)]
            R = NS + 1
            s.dma("sp", ccs[:], cc_d, writes=["ccs"])
            s.dma("sp", bmod[:], b_mod_d.partition_broadcast(R), writes=["bmod"])
            act(ccs[:], ccs[:], AF.Silu, ["ccs"], ["ccs"])
            for kt in range(8):
                tr(psA[:, kt * R:(kt + 1) * R], ccs[:, kt * 128:(kt + 1) * 128], ident_f[0:R, 0:R],
                   ["ccs", "ident_f"], ["p0a"])
            cp("dve", scT[:].rearrange("p k r -> p (k r)"), psA[:, 0:8 * R], ["p0a"], ["scT"])
            for cb in range(12):
                w = wm[cb % 2]
                s.dma("sp", w[:], wview(w_mod_d[:, cb * 512:(cb + 1) * 512]), writes=["wm%d" % (cb % 2)])
                ps = psB[cb % 2]
                for kt in range(8):
                    mm(ps[0:R, :], scT[:, kt, :], w[:, kt, :], kt == 0, kt == 7,
                       ["scT", "wm%d" % (cb % 2)], ["p0b%d" % (cb % 2)])
                tt("dve", modsb[:, cb * 512:(cb + 1) * 512], ps[0:R, :], bmod[:, cb * 512:(cb + 1) * 512], ALU.add,
                   ["p0b%d" % (cb % 2), "bmod"], ["modsb"])
            s.dma("sp", modv_d, modsb[:], reads=["modsb"], writes=["modv"])
            dump("modv", modsb[:], ["modsb"])
            s.barrier()
        if stop_after == "P0":
            s.barrier()
            return nc

        QSCALE = 128.0 ** -0.5
        h1_d = nc.dram_tensor("h1_scr", [NS, S, D], F32).ap()

        def bank_set(st, pfx):
            return [pst(st, "%s%d" % (pfx, i), [128, 512]) for i in range(7)]

        for smp in range(NS):
            with ExitStack() as sa:
                aT = sb(sa, "aT", [128, 8, S], BF16)
                acT = sb(sa, "acT", [128, 8, CT], BF16)
                oglT = sb(sa, "oglT", [128, 8, S], BF16)

                with ExitStack() as st:
                    s1b = sb(st, "s1b", [128, D], F32)
                    sh1b = sb(st, "sh1b", [128, D], F32)
                    s1c = sb(st, "s1c", [128, D], F32)
                    sh1c = sb(st, "sh1c", [128, D], F32)
                    gab = sb(st, "gab", [128, D], F32)
                    tmpv = sb(st, "tmpv", [128, D], F32)
                    xt = [sb(st, "xt%d" % i, [128, D], F32) for i in range(2)]
                    xm = [sb(st, "xm%d" % i, [128, D], F32) for i in range(2)]
                    xn = [sb(st, "xn%d" % i, [128, D], BF16) for i in range(2)]
                    stat = [sb(st, "stat%d" % i, [128, 4], F32) for i in range(2)]
                    psT = [pst(st, "p1t%d" % i, [128, 8, 128], BF16) for i in range(2)]
                    s.dma("sp", gab[:], g_attn_d.partition_broadcast(128), writes=["gab"])
                    for row, s1, sh, nm in ((smp, s1b, sh1b, "l"), (NS, s1c, sh1c, "c")):
                        s.dma("sp", tmpv[:], modv_d[row, D:2 * D].partition_broadcast(128), writes=["tmpv"])
                        stt("dve", s1[:], tmpv[:], 1.0, gab[:], ALU.add, ALU.mult, ["tmpv", "gab"], ["s1" + nm])
                        s.dma("sp", sh[:], modv_d[row, 0:D].partition_broadcast(128), writes=["sh1" + nm])
                    for i in range(NTA):
                        b = i % 2
                        lat = i < NT
                        src = x_d[smp, i * 128:(i + 1) * 128, :] if lat else ctx_d[smp, (i - NT) * 128:(i - NT + 1) * 128, :]
                        nm = "l" if lat else "c"
                        s1, sh = (s1b, sh1b) if lat else (s1c, sh1c)
                        s.dma("sp", xt[b][:], src, writes=["xt%d" % b])
                        s.op("pool", lambda e, b=b: e.memset(stat[b][:], 0.0), writes=["stat%d" % b])
                        act(xm[b][:], xt[b][:], AF.Square, ["xt%d" % b, "stat%d" % b], ["xm%d" % b, "stat%d" % b],
                            accum_out=stat[b][:, 0:1])
                        act(stat[b][:, 1:2], stat[b][:, 0:1], AF.Sqrt, ["stat%d" % b, "eps_c"], ["stat%d" % b],
                            scale=1.0 / D, bias=eps_c[:, 0:1])
                        s.op("dve", lambda e, b=b: e.reciprocal(out=stat[b][:, 2:3], in_=stat[b][:, 1:2]),
                             reads=["stat%d" % b], writes=["stat%d" % b])
                        stt("dve", xm[b][:], xt[b][:], stat[b][:, 2:3], s1[:], ALU.mult, ALU.mult,
                            ["xt%d" % b, "stat%d" % b, "s1" + nm], ["xm%d" % b])
                        tt("pool", xn[b][:], xm[b][:], sh[:], ALU.add, ["xm%d" % b, "sh1" + nm], ["xn%d" % b])
                        for kt in range(8):
                            tr(psT[b][:, kt, :], xn[b][:, kt * 128:(kt + 1) * 128], ident_b[:],
                               ["xn%d" % b, "ident_b"], ["p1t%d" % b])
                        if lat:
                            cp("act", aT[:, :, i * 128:(i + 1) * 128], psT[b][:, :, :], ["p1t%d" % b], ["aT"])
                        else:
                            cp("act", acT[:, :, (i - NT) * 128:(i - NT + 1) * 128], psT[b][:, :, :], ["p1t%d" % b], ["acT"])
                    if smp == 0:
                        dump("aT", aT[:, :, 0:256], ["aT"])
                    s.barrier()
                if stop_after == "P1":
                    s.barrier()
                    return nc

                with ExitStack() as st:
                    ropeC = sb(st, "ropeC", [128, S], BF16)
                    ropeS = sb(st, "ropeS", [128, S], BF16)
                    alT = [sb(st, "alT%d" % d_, [16, S + CT], BF16) for d_ in range(2)]
                    wal = sb(st, "wal", [128, 8, 32], BF16)
                    wa2 = sb(st, "wa2", [16, 2, 512], BF16)
                    ba2 = sb(st, "ba2", [1, 2, 512], BF16)
                    gnb = sb(st, "gnb", [128, 256], F32)
                    wq = sb(st, "wq", [128, 8, 128], BF16)
                    wqs = sb(st, "wqs", [128, 8, 128], BF16)
                    wk = sb(st, "wk", [128, 8, 128], BF16)
                    wks = sb(st, "wks", [128, 8, 128], BF16)
                    wv = sb(st, "wv", [128, 8, 256], BF16)
                    wg = sb(st, "wg", [128, 8, 256], BF16)
                    qrT = sb(st, "qrT", [128, S], BF16)
                    krT = sb(st, "krT", [128, S], BF16)
                    krk = sb(st, "krk", [128, NTA, 128], BF16)
                    vtk = sb(st, "vtk", [128, NTA, 256], BF16)
                    sgt = sb(st, "sgt", [128, NT, 256], BF16)
                    qe = [sb(st, "qe%d" % d_, [128, S], BF16) for d_ in range(2)]
                    ke = [sb(st, "ke%d" % d_, [128, S], BF16) for d_ in range(2)]
                    kd = [sb(st, "kd%d" % d_, [128, NTA, 128], BF16) for d_ in range(2)]
                    dec = sb(st, "dec", [128, 2, NTA, 2], F32)
                    oacc = sb(st, "oacc", [128, NT, 256], F32)
                    S32 = [sb(st, "S32_%d" % d_, [128, 256], F32) for d_ in range(2)]
                    S16 = [sb(st, "S16_%d" % d_, [128, 256], BF16) for d_ in range(2)]
                    t1 = [sb(st, "t1_%d" % i, [128, 512], F32) for i in range(2)]
                    t2 = [sb(st, "t2_%d" % i, [128, 512], F32) for i in range(2)]
                    az = [sb(st, "az%d" % i, [128, 128], F32) for i in range(2)]
                    ex = [sb(st, "ex%d" % i, [128, 128], F32) for i in range(2)]
                    rz = [sb(st, "rz%d" % i, [128, 128], F32) for i in range(2)]
                    Lt = [sb(st, "Lt%d" % i, [128, 128], F32) for i in range(2)]
                    Dt = [sb(st, "Dt%d" % i, [128, 128], F32) for i in range(2)]
                    Di = [sb(st, "Di%d" % i, [128, 128], F32) for i in range(2)]
                    EK = [sb(st, "EK%d" % i, [128, 128], F32) for i in range(2)]
                    att = [sb(st, "att%d" % i, [128, 128], BF16) for i in range(2)]
                    junk = sb(st, "junk2", [128, 256], F32)
                    onr = [sb(st, "onr%d" % i, [128, 256], F32) for i in range(2)]
                    rbf = [sb(st, "rbf%d" % i, [128, 256], BF16) for i in range(2)]
                    st2 = [sb(st, "st2_%d" % i, [128, 4], F32) for i in range(2)]
                    B = [pst(st, "p2b%d" % i, [128, 512]) for i in range(6)]
                    BK = ["p2B%d" % i for i in range(6)]
                    pT = [pst(st, "p2t%d" % i, [128, 8, 128], BF16) for i in range(2)]
                    TK = ["p2T0", "p2T1"]

                    s.dma("pool", ropeC[:], cos_d, writes=["ropeC"])
                    s.dma("pool", ropeS[:], sin_d, writes=["ropeS"])
                    s.dma("pool", wal[:], wview(w_in_d[:, C_AL:C_AL + 32]), writes=["wal"])
                    s.dma("pool", wa2[:], w_a2b_d[0:16], writes=["wa2"])
                    s.dma("pool", ba2[:], w_a2b_d[16:17], writes=["ba2"])
                    s.dma("sp", gnb[:], gla_g_d.partition_broadcast(128), writes=["gnb"])
                    for tb in range(5):
                        if tb < 4:
                            rhs_of = lambda kt, tb=tb: aT[:, kt, tb * 512:(tb + 1) * 512]
                            n, c0, rk = 512, tb * 512, "aT"
                        else:
                            rhs_of = lambda kt: acT[:, kt, :]
                            n, c0, rk = CT, S, "acT"
                        for d_ in range(2):
                            for kt in range(8):
                                mm(B[d_][0:16, 0:n], wal[:, kt, d_ * 16:(d_ + 1) * 16], rhs_of(kt), kt == 0, kt == 7,
                                   ["wal", rk], [], x=[BK[d_]])
                            cp("act" if d_ == 0 else "dve", alT[d_][:, c0:c0 + n], B[d_][0:16, 0:n], [], ["alT%d" % d_], x=[BK[d_]])
                    if stop_after == "P2a":
                        s.barrier()
                        return nc

                    for h in range(4):
                        s.dma("pool", wq[:], wview(w_in_d[:, C_GQ + 128 * h:C_GQ + 128 * (h + 1)]), writes=["wq"])
                        s.dma("pool", wqs[:], wview(w_rope_d[:, 128 * h:128 * (h + 1)]), writes=["wqs"])
                        s.dma("pool", wk[:], wview(w_in_d[:, C_GK + 128 * h:C_GK + 128 * (h + 1)]), writes=["wk"])
                        s.dma("pool", wks[:], wview(w_rope_d[:, 512 + 128 * h:512 + 128 * (h + 1)]), writes=["wks"])
                        s.dma("pool", wv[:], wview(w_in_d[:, C_GV + 256 * h:C_GV + 256 * (h + 1)]), writes=["wv"])
                        s.dma("pool", wg[:], wview(w_in_d[:, C_GG + 256 * h:C_GG + 256 * (h + 1)]), writes=["wg"])
                        for tb in range(4):
                            cols = slice(tb * 512, (tb + 1) * 512)
                            for bi, (w, wn) in enumerate(((wq, "wq"), (wqs, "wqs"), (wk, "wk"), (wks, "wks"))):
                                for kt in range(8):
                                    mm(B[bi][:, :], w[:, kt, :], aT[:, kt, cols], kt == 0, kt == 7, [wn, "aT"], [], x=[BK[bi]])
                            stt("dve", t1[0][:], B[0][:, :], QSCALE, ropeC[:, cols], ALU.mult, ALU.mult, ["ropeC"], ["t1_0"], x=[BK[0]])
                            stt("dve", t2[0][:], B[1][:, :], QSCALE, ropeS[:, cols], ALU.mult, ALU.mult, ["ropeS"], ["t2_0"], x=[BK[1]])
                            tt("pool", qrT[:, cols], t1[0][:], t2[0][:], ALU.add, ["t1_0", "t2_0"], ["qrT"])
                            tt("dve", t1[1][:], B[2][:, :], ropeC[:, cols], ALU.mult, ["ropeC"], ["t1_1"], x=[BK[2]])
                            tt("dve", t2[1][:], B[3][:, :], ropeS[:, cols], ALU.mult, ["ropeS"], ["t2_1"], x=[BK[3]])
                            tt("pool", krT[:, cols], t1[1][:], t2[1][:], ALU.add, ["t1_1", "t2_1"], ["krT"])
                        if stop_after == "P2b":
                            s.barrier()
                            return nc
                        for i in range(NTA):
                            lat = i < NT
                            bv = 4 + (i % 2)
                            bg = 2 + (i % 2)
                            srcT, rk, c0 = (aT, "aT", i * 128) if lat else (acT, "acT", (i - NT) * 128)
                            for kt in range(8):
                                mm(B[bv][:, 0:256], srcT[:, kt, c0:c0 + 128], wv[:, kt, :], kt == 0, kt == 7, [rk, "wv"], [], x=[BK[bv]])
                            cp("act", vtk[:, i, :], B[bv][:, 0:256], [], ["vtk"], x=[BK[bv]])
                            if lat:
                                for kt in range(8):
                                    mm(B[bg][:, 0:256], srcT[:, kt, c0:c0 + 128], wg[:, kt, :], kt == 0, kt == 7, [rk, "wg"], [], x=[BK[bg]])
                                act(sgt[:, i, :], B[bg][:, 0:256], AF.Silu, [], ["sgt"], x=[BK[bg]])
                            else:
                                for kt in range(8):
                                    mm(B[bg][:, 0:128], srcT[:, kt, c0:c0 + 128], wk[:, kt, :], kt == 0, kt == 7, [rk, "wk"], [], x=[BK[bg]])
                                cp("dve", krk[:, i, :], B[bg][:, 0:128], [], ["krk"], x=[BK[bg]])
                        for i in range(NT):
                            u = i % 2
                            tr(pT[u][:, 0, :], krT[:, i * 128:(i + 1) * 128], ident_b[:], ["krT", "ident_b"], [], x=[TK[u]])
                            cp("dve", krk[:, i, :], pT[u][:, 0, :], [], ["krk"], x=[TK[u]])
                        if stop_after == "P2c":
                            s.barrier()
                            return nc
                        for i in range(NTA):
                            lat = i < NT
                            tokc = slice(i * 128, (i + 1) * 128)
                            for d_ in range(2):
                                u = d_
                                bz, bb, be = B[d_], B[2 + d_], B[4 + d_]
                                xz, xb, xe = [BK[d_]], [BK[2 + d_]], [BK[4 + d_]]
                                hc = slice(128 * h, 128 * (h + 1))
                                mm(bz[:, 0:128], alT[d_][0:16, tokc], wa2[0:16, d_, hc], True, False, ["alT%d" % d_, "wa2"], [], inc=False, x=xz)
                                mm(bz[:, 0:128], ones_b[0:1, 0:128], ba2[0:1, d_, hc], False, True, ["ones_b", "ba2"], [], x=xz)
                                act(az[u][:], bz[:, 0:128], AF.Abs, [], ["az%d" % u], x=xz)
                                ts("dve", rz[u][:], bz[:, 0:128], -1.0, 0.0, ALU.mult, ALU.max, [], ["rz%d" % u], x=xz)
                                act(ex[u][:], az[u][:], AF.Exp, ["az%d" % u], ["ex%d" % u], scale=-1.0)
                                act(ex[u][:], ex[u][:], AF.Ln, ["ex%d" % u, "one_c"], ["ex%d" % u], bias=one_c[:, 0:1])
                                tt("pool", Lt[u][:], rz[u][:], ex[u][:], ALU.add, ["rz%d" % u, "ex%d" % u], ["Lt%d" % u])
                                mm(bb[:, 0:128], Lt[u][:], tri_f[:, 2 * d_, :], True, True, ["Lt%d" % u, "tri"], [], x=xb)
                                mm(be[:, 0:128], tri_f[:, 2 * d_ + 1, :], Lt[u][:], True, True, ["Lt%d" % u, "tri"], [], x=xe)
                                act(Dt[u][:], bb[:, 0:128], AF.Exp, [], ["Dt%d" % u], scale=-1.0 / 16, x=xb)
                                if lat:
                                    act(Di[u][:], bb[:, 0:128], AF.Exp, [], ["Di%d" % u], scale=1.0 / 16, x=xb)
                                act(EK[u][:], be[:, 0:128], AF.Exp, [], ["EK%d" % u], scale=-1.0 / 16, x=xe)
                                if d_ == 0:
                                    dsrc = Dt[u][:, 63:128:64]
                                else:
                                    dsrc = Dt[u][:, 0:128:64]
                                cp("pool", dec[:, d_, i, :], dsrc, ["Dt%d" % u], ["dec"])
                                tt("pool", kd[d_][:, i, :], krk[:, i, :], EK[u][:], ALU.mult, ["krk", "EK%d" % u], ["kd%d" % d_])
                                if lat:
                                    tt("dve", qe[d_][:, tokc], qrT[:, tokc], Dt[u][:], ALU.mult, ["qrT", "Dt%d" % u], ["qe%d" % d_])
                                    tt("dve", ke[d_][:, tokc], krT[:, tokc], Di[u][:], ALU.mult, ["krT", "Di%d" % u], ["ke%d" % d_])
                        if stop_after == "P2d":
                            s.barrier()
                            return nc
                        for d_ in range(2):
                            s.op("pool", lambda e, d_=d_: e.memset(S32[d_][:], 0.0), writes=["S32_%d" % d_])
                            s.op("pool", lambda e, d_=d_: e.memset(S16[d_][:], 0.0), writes=["S16_%d" % d_])

                        def upd(d_, i, half):
                            rows = slice(64 * half, 64 * half + 64)
                            pkv = B[4 + d_][:, 0:256]
                            xk = [BK[4 + d_]]
                            mm(pkv, kd[d_][rows, i, :], vtk[rows, i, :], True, True, ["kd%d" % d_, "vtk"], [], x=xk)
                            stt("dve", S32[d_][:], S32[d_][:], dec[:, d_, i, half:half + 1], pkv, ALU.mult, ALU.add,
                                ["S32_%d" % d_, "dec"], ["S32_%d" % d_], x=xk)
                            cp("act", S16[d_][:], S32[d_][:], ["S32_%d" % d_], ["S16_%d" % d_])

                        for i in (NT, NT + 1):
                            for half in (0, 1):
                                upd(0, i, half)
                        for i in (NT + 1, NT):
                            for half in (1, 0):
                                upd(1, i, half)
                        if smp == 0 and h == 0:
                            dump("s_f", S32[0][:], ["S32_0"])
                            dump("s_b", S32[1][:], ["S32_1"])
                        if stop_after == "P2e":
                            s.barrier()
                            return nc

                        done = [0] * NT

                        def lat_front(d_, i):
                            tokc = slice(i * 128, (i + 1) * 128)
                            pa = B[d_][:, 0:128]
                            po = B[2 + d_][:, 0:256]
                            mm(pa, ke[d_][:, tokc], qe[d_][:, tokc], True, True, ["ke%d" % d_, "qe%d" % d_], [], x=[BK[d_]])
                            tt("dve", att[d_][:], pa, tri_f[:, 2 * d_, :], ALU.mult, ["tri"], ["att%d" % d_], x=[BK[d_]])
                            mm(po, att[d_][:], vtk[:, i, :], True, False, ["att%d" % d_, "vtk"], [], inc=False, x=[BK[2 + d_]])

                        def lat_half(d_, i, half, last):
                            po = B[2 + d_][:, 0:256]
                            rows = slice(64 * half, 64 * half + 64)
                            c0 = i * 128 + 64 * half
                            mm(po[rows, :], qe[d_][:, c0:c0 + 64], S16[d_][:], False, last, ["qe%d" % d_, "S16_%d" % d_],
                               [], inc=True, x=[BK[2 + d_]])
                            upd(d_, i, half)

                        def lat_back(d_, i):
                            po = B[2 + d_][:, 0:256]
                            xo = [BK[2 + d_]]
                            tokc = slice(i * 128, (i + 1) * 128)
                            if done[i] == 0:
                                cp("act", oacc[:, i, :], po, [], ["oacc%d" % i], x=xo)
                                done[i] = 1
                                return
                            u = i % 2
                            tt("dve", oacc[:, i, :], po, oacc[:, i, :], ALU.add, ["oacc%d" % i], ["oacc%d" % i], x=xo)
                            s.op("pool", lambda e, u=u: e.memset(st2[u][:], 0.0), writes=["st2_%d" % u])
                            act(junk[:], oacc[:, i, :], AF.Square, ["oacc%d" % i, "st2_%d" % u], ["junk2", "st2_%d" % u],
                                accum_out=st2[u][:, 0:1])
                            act(st2[u][:, 1:2], st2[u][:, 0:1], AF.Sqrt, ["st2_%d" % u, "eps_c"], ["st2_%d" % u],
                                scale=1.0 / 256, bias=eps_c[:, 0:1])
                            s.op("dve", lambda e, u=u: e.reciprocal(out=st2[u][:, 2:3], in_=st2[u][:, 1:2]),
                                 reads=["st2_%d" % u], writes=["st2_%d" % u])
                            stt("dve", onr[u][:], oacc[:, i, :], st2[u][:, 2:3], gnb[:], ALU.mult, ALU.mult,
                                ["oacc%d" % i, "st2_%d" % u, "gnb"], ["onr%d" % u])
                            tt("pool", rbf[u][:], onr[u][:], sgt[:, i, :], ALU.mult, ["onr%d" % u, "sgt"], ["rbf%d" % u])
                            for j in range(2):
                                tr(pT[u][:, j, :], rbf[u][:, j * 128:(j + 1) * 128], ident_b[:], ["rbf%d" % u, "ident_b"], [], x=[TK[u]])
                            cp("act", oglT[:, 2 * h:2 * h + 2, tokc], pT[u][:, 0:2, :], [], ["oglT"], x=[TK[u]])

                        for j in range(NT):
                            fi, bi_ = j, NT - 1 - j
                            lat_front(0, fi)
                            lat_front(1, bi_)
                            lat_half(0, fi, 0, False)
                            lat_half(1, bi_, 1, False)
                            lat_half(0, fi, 1, True)
                            lat_half(1, bi_, 0, True)
                            lat_back(0, fi)
                            lat_back(1, bi_)
                    if smp == 0:
                        dump("oglT", oglT[:, :, :], ["oglT"])
                    s.barrier()
                    print("P2 done: deadlock-free", s.check_deadlock(), s.n_ins, s.n_wait, s.cnt)
                if stop_after == "P2":
                    s.barrier()
                    return nc

                onaT = sb(sa, "onaT", [128, 4, S], BF16)
                with ExitStack() as st:
                    wq3 = sb(st, "wq3", [128, 8, 128], BF16)
                    wk3 = sb(st, "wk3", [128, 8, 128], BF16)
                    wv3 = sb(st, "wv3", [128, 8, 128], BF16)
                    nam = sb(st, "nam", [128, 8, 256], F32)
                    nabt = [sb(st, "nabt%d" % i, [128, 2, 256], F32) for i in range(2)]
                    BMp = sb(st, "BMp", [128, 8, 2, 256], BF16)
                    qT3 = sb(st, "qT3", [128, S], BF16)
                    kT3 = sb(st, "kT3", [128, S + CT], BF16)
                    Ve = sb(st, "Ve", [128, 16, 128], BF16)
                    Vo = sb(st, "Vo", [128, 15, 128], BF16)
                    Vc = sb(st, "Vc", [128, 2, 128], BF16)
                    PT = [sb(st, "PT%d" % i, [128, 6, 2, 64], BF16) for i in range(2)]
                    rden = [sb(st, "rden%d" % i, [128, 128], F32) for i in range(2)]
                    SB = [pst(st, "p3s%d" % i, [128, 512]) for i in range(4)]
                    SK = ["p3S%d" % i for i in range(4)]
                    OB = [pst(st, "p3o%d" % i, [128, 512]) for i in range(2)]
                    OK_ = ["p3O%d" % i for i in range(2)]
                    PB = [pst(st, "p3p%d" % i, [128, 512]) for i in range(2)]
                    PK = ["p3P%d" % i for i in range(2)]
                    s.dma("sp", nam[:], nam_d, writes=["nam"])
                    for pr in range(4):
                        s.dma("pool", wq3[:], wview(w_in_d[:, C_NAQ + 128 * pr:C_NAQ + 128 * (pr + 1)]), writes=["wq3"])
                        s.dma("pool", wk3[:], wview(w_in_d[:, C_NAK + 128 * pr:C_NAK + 128 * (pr + 1)]), writes=["wk3"])
                        s.dma("pool", wv3[:], wview(w_in_d[:, C_NAV + 128 * pr:C_NAV + 128 * (pr + 1)]), writes=["wv3"])
                        for p in range(8):
                            s.dma("sp", nabt[p % 2][:], nab_d[pr, :, p, :, :], writes=["nabt%d" % (p % 2)])
                            for hh in range(2):
                                stt("dve", BMp[:, p, hh, :], nabt[p % 2][:, hh, :], 8.0, nam[:, p, :], ALU.mult, ALU.add,
                                    ["nabt%d" % (p % 2), "nam"], ["BMp"])
                        cnt_p = [0]

                        def proj(dst, lhs_fn, rhs_fn, m, n, rk, dk):
                            u = cnt_p[0] % 2
                            cnt_p[0] += 1
                            for kt in range(8):
                                mm(PB[u][0:m, 0:n], lhs_fn(kt), rhs_fn(kt), kt == 0, kt == 7, rk, [], x=[PK[u]])
                            cp("act" if u == 0 else "dve", dst, PB[u][0:m, 0:n], [], [dk], x=[PK[u]])

                        for tb in range(4):
                            cols = slice(tb * 512, (tb + 1) * 512)
                            proj(qT3[:, cols], lambda kt: wq3[:, kt, :], lambda kt, cols=cols: aT[:, kt, cols], 128, 512, ["wq3", "aT"], "qT3")
                            proj(kT3[:, cols], lambda kt: wk3[:, kt, :], lambda kt, cols=cols: aT[:, kt, cols], 128, 512, ["wk3", "aT"], "kT3")
                        proj(kT3[:, S:S + CT], lambda kt: wk3[:, kt, :], lambda kt: acT[:, kt, :], 128, CT, ["wk3", "acT"], "kT3")
                        for i in range(16):
                            proj(Ve[:, i, :], lambda kt, i=i: aT[:, kt, i * 128:(i + 1) * 128], lambda kt: wv3[:, kt, :], 128, 128, ["wv3", "aT"], "Ve")
                        for i in range(15):
                            proj(Vo[:, i, :], lambda kt, i=i: aT[:, kt, 64 + i * 128:64 + (i + 1) * 128], lambda kt: wv3[:, kt, :], 128, 128, ["wv3", "aT"], "Vo")
                        for i in range(2):
                            proj(Vc[:, i, :], lambda kt, i=i: acT[:, kt, i * 128:(i + 1) * 128], lambda kt: wv3[:, kt, :], 128, 128, ["wv3", "acT"], "Vc")

                        def scores(r):
                            rs = min(max(r - 4, 0), 24)
                            p = r - rs
                            buf = r % 2
                            qc = slice(r * 64, r * 64 + 64)
                            for hh in range(2):
                                pr_ = slice(64 * hh, 64 * hh + 64)
                                bank = SB[buf * 2 + hh]
                                xk = [SK[buf * 2 + hh]]
                                mm(bank[:, 0:256], ident_b[:], BMp[:, p, hh, :], True, False, ["ident_b", "BMp"], [], inc=False, x=xk)
                                for j in range(4):
                                    kc0 = (rs + 2 * j) * 64
                                    mm(bank[:, j * 64:(j + 1) * 64], kT3[pr_, kc0:kc0 + 128], qT3[pr_, qc], False, j == 3,
                                       ["kT3", "qT3"], [], inc=False, x=xk)
                                for jc in range(2):
                                    mm(bank[:, 256 + jc * 64:256 + (jc + 1) * 64], kT3[pr_, S + jc * 128:S + (jc + 1) * 128], qT3[pr_, qc],
                                       True, True, ["kT3", "qT3"], [], inc=(jc == 1), x=xk)
                                act(PT[buf][:, :, hh, :], bank[:, 0:384].rearrange("p (j q) -> p j q", q=64), AF.Exp,
                                    [], ["PT%d" % buf], scale=0.125, x=xk)

                        def pv(r):
                            rs = min(max(r - 4, 0), 24)
                            buf = r % 2
                            qc = slice(r * 64, r * 64 + 64)
                            ob = OB[buf]
                            xo = [OK_[buf]]
                            for hh in range(2):
                                hs = slice(64 * hh, 64 * hh + 64)
                                for j in range(6):
                                    if j < 4:
                                        if rs % 2 == 0:
                                            Vt, vk = Ve[:, (rs + 2 * j) // 2, hs], "Ve"
                                        else:
                                            Vt, vk = Vo[:, (rs + 2 * j - 1) // 2, hs], "Vo"
                                    else:
                                        Vt, vk = Vc[:, j - 4, hs], "Vc"
                                    mm(ob[hs, 0:64], Vt, PT[buf][:, j, hh, :], j == 0, j == 5, [vk, "PT%d" % buf], [],
                                       inc=(j == 5), x=xo)
                            for j in range(6):
                                mm(ob[:, 64:192], ones_b[:, :], PT[buf][:, j, :, :].rearrange("p h q -> p (h q)"), j == 0, j == 5,
                                   ["ones_b", "PT%d" % buf], [], inc=(j == 5), x=xo)
                            s.op("dve", lambda e: e.reciprocal(out=rden[buf][:], in_=ob[:, 64:192]), reads=[], writes=["rden%d" % buf], excl=xo)
                            for hh in range(2):
                                hs = slice(64 * hh, 64 * hh + 64)
                                tt("dve", onaT[hs, pr, qc], ob[hs, 0:64], rden[buf][hs, 64 * hh:64 * hh + 64], ALU.mult,
                                   ["rden%d" % buf], ["onaT"], x=xo)

                        for r in range(32):
                            scores(r)
                            if r > 0:
                                pv(r - 1)
                        pv(31)
                    if smp == 0:
                        dump("onaT", onaT[:, :, :], ["onaT"])
                    s.barrier()
                    print("P3 done: deadlock-free", s.check_deadlock(), s.n_ins, s.n_wait, s.cnt)
                if stop_after == "P3":
                    s.barrier()
                    return nc

                UT = sb(sa, "UT", [128, 8, S], BF16)
                with ExitStack() as st:
                    wna = [sb(st, "wna%d" % i, [128, 4, 128], BF16) for i in range(2)]
                    wgl = [sb(st, "wgl%d" % i, [128, 8, 128], BF16) for i in range(2)]
                    w8 = [sb(st, "w8_%d" % i, [128, 8, 128], BF16) for i in range(2)]
                    w9 = [sb(st, "w9_%d" % i, [128, 8, 128], BF16) for i in range(2)]
                    sg8 = [sb(st, "sg8_%d" % i, [128, 512], F32) for i in range(2)]
                    sg9 = [sb(st, "sg9_%d" % i, [128, 512], F32) for i in range(2)]
                    t1m = [sb(st, "t1m%d" % i, [128, 512], F32) for i in range(2)]
                    t2m = [sb(st, "t2m%d" % i, [128, 512], F32) for i in range(2)]
                    MB = [pst(st, "p4b%d" % i, [128, 512]) for i in range(8)]
                    MK = ["p4B%d" % i for i in range(8)]
                    for nt in range(8):
                        wb = nt % 2
                        ncol = slice(nt * 128, (nt + 1) * 128)
                        s.dma("pool", wna[wb][:], wview(w_nao_d[:, ncol]), writes=["wna%d" % wb])
                        s.dma("pool", wgl[wb][:], wview(w_glo_d[:, ncol]), writes=["wgl%d" % wb])
                        s.dma("pool", w8[wb][:], wview(w_in_d[:, C_M8 + nt * 128:C_M8 + (nt + 1) * 128]), writes=["w8_%d" % wb])
                        s.dma("pool", w9[wb][:], wview(w_in_d[:, C_M9 + nt * 128:C_M9 + (nt + 1) * 128]), writes=["w9_%d" % wb])
                        for tb in range(4):
                            cols = slice(tb * 512, (tb + 1) * 512)
                            u = (nt * 4 + tb) % 2
                            bA, bG8, bB, bG9 = [MB[4 * u + q] for q in range(4)]
                            kA, kG8, kB, kG9 = [[MK[4 * u + q]] for q in range(4)]
                            for kt in range(4):
                                mm(bA[:, :], wna[wb][:, kt, :], onaT[:, kt, cols], kt == 0, kt == 3, ["wna%d" % wb, "onaT"], [], x=kA)
                            for kt in range(8):
                                mm(bG8[:, :], w8[wb][:, kt, :], aT[:, kt, cols], kt == 0, kt == 7, ["w8_%d" % wb, "aT"], [], x=kG8)
                            for kt in range(8):
                                mm(bB[:, :], wgl[wb][:, kt, :], oglT[:, kt, cols], kt == 0, kt == 7, ["wgl%d" % wb, "oglT"], [], x=kB)
                            for kt in range(8):
                                mm(bG9[:, :], w9[wb][:, kt, :], aT[:, kt, cols], kt == 0, kt == 7, ["w9_%d" % wb, "aT"], [], x=kG9)
                            act(sg8[u][:], bG8[:, :], AF.Sigmoid, [], ["sg8_%d" % u], x=kG8)
                            act(sg9[u][:], bG9[:, :], AF.Sigmoid, [], ["sg9_%d" % u], x=kG9)
                            tt("dve", t1m[u][:], bA[:, :], sg8[u][:], ALU.mult, ["sg8_%d" % u], ["t1m%d" % u], x=kA)
                            tt("dve", t2m[u][:], bB[:, :], sg9[u][:], ALU.mult, ["sg9_%d" % u], ["t2m%d" % u], x=kB)
                            tt("pool", UT[:, nt, cols], t1m[u][:], t2m[u][:], ALU.add, ["t1m%d" % u, "t2m%d" % u], ["UT"])
                    if smp == 0:
                        dump("UT", UT[:, :, 0:256], ["UT"])
                    s.barrier()
                with ExitStack() as st:
                    wo = sb(st, "wo", [128, 8, D], BF16)
                    wtmp = [sb(st, "wtmp%d" % i, [128, D], F32) for i in range(2)]
                    g1b = sb(st, "g1b", [128, D], F32)
                    xt2 = [sb(st, "xt2_%d" % i, [128, D], F32) for i in range(2)]
                    h1t = [sb(st, "h1t%d" % i, [128, D], F32) for i in range(2)]
                    MB = [pst(st, "p4c%d" % i, [128, 512]) for i in range(4)]
                    MK = ["p4C%d" % i for i in range(4)]
                    s.dma("sp", g1b[:], modv_d[smp, 2 * D:3 * D].partition_broadcast(128), writes=["g1b"])
                    for kt in range(8):
                        s.dma("sp", wtmp[kt % 2][:], w_out_d[kt * 128:(kt + 1) * 128, :], writes=["wtmp%d" % (kt % 2)])
                        tt("pool", wo[:, kt, :], wtmp[kt % 2][:], g1b[:], ALU.mult, ["wtmp%d" % (kt % 2), "g1b"], ["wo"])
                    for i in range(NT):
                        u = i % 2
                        tokc = slice(i * 128, (i + 1) * 128)
                        s.dma("sp", xt2[u][:], x_d[smp, tokc, :], writes=["xt2_%d" % u])
                        for half in range(2):
                            hcol = slice(half * 512, (half + 1) * 512)
                            bk = MB[2 * u + half]
                            xk = [MK[2 * u + half]]
                            for kt in range(8):
                                mm(bk[:, :], UT[:, kt, tokc], wo[:, kt, hcol], kt == 0, kt == 7, ["UT", "wo"], [], x=xk)
                            tt("dve", h1t[u][:, hcol], bk[:, :], xt2[u][:, hcol], ALU.add, ["xt2_%d" % u], ["h1t%d" % u], x=xk)
                        s.dma("sp", h1_d[smp, tokc, :], h1t[u][:], reads=["h1t%d" % u], writes=["h1d"])
                        if smp == 0 and i < 2 and "h1" in dbg_d:
                            dump("h1", h1t[u][:], ["h1t%d" % u], dst=dbg_d["h1"][i * 128:(i + 1) * 128, :])
                    s.barrier()
                    print("P4 done: deadlock-free", s.check_deadlock(), s.n_ins, s.n_wait, s.cnt)
                if stop_after == "P4":
                    s.barrier()
                    return nc
            with ExitStack() as sm:
                hres = sb(sm, "hres", [128, NT, D], F32)
                fT = sb(sm, "fT", [128, 8, S], BF16)
                Wtok = sb(sm, "Wtok", [128, NT, 32], F32)
                g2b = sb(sm, "g2b", [128, D], F32)
                gfb = sb(sm, "gfb", [128, D], F32)
                s.dma("sp", g2b[:], modv_d[smp, 5 * D:6 * D].partition_broadcast(128), writes=["g2b"])
                s.dma("sp", gfb[:], g_fin_d.partition_broadcast(128), writes=["gfb"])
                with ExitStack() as st:
                    s2b = sb(st, "s2b", [128, D], F32)
                    sh2b = sb(st, "sh2b", [128, D], F32)
                    gfn = sb(st, "gfn", [128, D], F32)
                    wrt = sb(st, "wrt", [128, 8, 36], F32)
                    brt = sb(st, "brt", [1, 36], F32)
                    ones_f = sb(st, "ones_f", [1, 128], F32)
                    xm3 = [sb(st, "xm3_%d" % i, [128, D], F32) for i in range(2)]
                    junk3 = sb(st, "junk3", [128, D], F32)
                    fT32 = [sb(st, "fT32_%d" % i, [128, 8, 128], F32) for i in range(2)]
                    R = [sb(st, "R%d" % i, [128, 96], F32) for i in range(2)]
                    st3 = [sb(st, "st3_%d" % i, [128, 4], F32) for i in range(2)]
                    pf = [[pst(st, "p5f%d_%d" % (i, j), [128, 4, 128]) for j in range(2)] for i in range(2)]
                    pfk = [["p5F%d_%d" % (i, j) for j in range(2)] for i in range(2)]
                    pl = [pst(st, "p5l%d" % i, [128, 512]) for i in range(2)]
                    plk = ["p5L%d" % i for i in range(2)]
                    s.dma("sp", gfn[:], g_ffn_d.partition_broadcast(128), writes=["gfn"])
                    s.dma("sp", s2b[:], modv_d[smp, 4 * D:5 * D].partition_broadcast(128), writes=["s2b"])
                    stt("dve", s2b[:], s2b[:], 1.0, gfn[:], ALU.add, ALU.mult, ["s2b", "gfn"], ["s2b"])
                    s.dma("sp", sh2b[:], modv_d[smp, 3 * D:4 * D].partition_broadcast(128), writes=["sh2b"])
                    s.dma("sp", wrt[:], wview(w_rt_d), writes=["wrt"])
                    s.dma("sp", brt[:], b_rt_d.partition_broadcast(1), writes=["brt"])
                    s.op("pool", lambda e: e.memset(ones_f[:], 1.0), writes=["ones_f"])
                    for i in range(NT):
                        u = i % 2
                        tokc = slice(i * 128, (i + 1) * 128)
                        hk = "hres%d" % i
                        Rk = "R%d" % u
                        Ru = R[u]
                        s.dma("sp", hres[:, i, :], h1_d[smp, tokc, :], reads=["h1d"], writes=[hk])
                        s.op("pool", lambda e, u=u: e.memset(st3[u][:], 0.0), writes=["st3_%d" % u])
                        act(junk3[:], hres[:, i, :], AF.Square, [hk, "st3_%d" % u], ["junk3", "st3_%d" % u], accum_out=st3[u][:, 0:1])
                        act(st3[u][:, 1:2], st3[u][:, 0:1], AF.Sqrt, ["st3_%d" % u, "eps_c"], ["st3_%d" % u], scale=1.0 / D, bias=eps_c[:, 0:1])
                        s.op("dve", lambda e, u=u: e.reciprocal(out=st3[u][:, 2:3], in_=st3[u][:, 1:2]), reads=["st3_%d" % u], writes=["st3_%d" % u])
                        stt("dve", xm3[u][:], hres[:, i, :], st3[u][:, 2:3], s2b[:], ALU.mult, ALU.mult, [hk, "st3_%d" % u, "s2b"], ["xm3_%d" % u])
                        tt("pool", xm3[u][:], xm3[u][:], sh2b[:], ALU.add, ["xm3_%d" % u, "sh2b"], ["xm3_%d" % u])
                        for kt in range(8):
                            tr(pf[u][kt // 4][:, kt % 4, :], xm3[u][:, kt * 128:(kt + 1) * 128], ident_f[:], ["xm3_%d" % u, "ident_f"], [], x=[pfk[u][kt // 4]])
                        for j in range(2):
                            cp("act", fT[:, 4 * j:4 * j + 4, tokc], pf[u][j][:, :, :], [], ["fT"], x=[pfk[u][j]])
                            cp("dve", fT32[u][:, 4 * j:4 * j + 4, :], pf[u][j][:, :, :], [], ["fT32_%d" % u], x=[pfk[u][j]])
                        for kt in range(8):
                            mm(pl[u][:, 0:36], fT32[u][:, kt, :], wrt[:, kt, :], kt == 0, False, ["fT32_%d" % u, "wrt"], [], inc=False, x=[plk[u]])
                        mm(pl[u][:, 0:36], ones_f[0:1, :], brt[0:1, :], False, True, ["ones_f", "brt"], [], x=[plk[u]])
                        dv = lambda fn, rd=(), x=(): s.op("dve", fn, reads=[Rk] + list(rd), writes=[Rk], excl=x)
                        dv(lambda e: e.tensor_copy(out=Ru[:, 0:36], in_=pl[u][:, 0:36]), x=[plk[u]])
                        dv(lambda e: e.reduce_max(out=Ru[:, 36:37], in_=Ru[:, 0:4], axis=AX.X))
                        dv(lambda e: e.tensor_scalar(out=Ru[:, 40:44], in0=Ru[:, 0:4], scalar1=Ru[:, 36:37], scalar2=None, op0=ALU.is_equal))
                        dv(lambda e: e.tensor_scalar(out=Ru[:, 37:38], in0=Ru[:, 36:37], scalar1=-1.0, scalar2=None, op0=ALU.mult))
                        s.op("pool", lambda e: e.memset(Ru[:, 38:39], 0.0), reads=[Rk], writes=[Rk])
                        act(Ru[:, 44:48], Ru[:, 0:4], AF.Exp, [Rk], [Rk], bias=Ru[:, 37:38], accum_out=Ru[:, 38:39])
                        dv(lambda e: e.reciprocal(out=Ru[:, 39:40], in_=Ru[:, 38:39]))
                        dv(lambda e: e.tensor_scalar(out=Ru[:, 48:56], in0=Ru[:, 4:12], scalar1=Ru[:, 40:41], scalar2=None, op0=ALU.mult))
                        for g in range(1, 4):
                            dv(lambda e, g=g: e.scalar_tensor_tensor(out=Ru[:, 48:56], in0=Ru[:, 4 + 8 * g:12 + 8 * g], scalar=Ru[:, 40 + g:41 + g],
                                                                      in1=Ru[:, 48:56], op0=ALU.mult, op1=ALU.add))
                        dv(lambda e: e.reduce_max(out=Ru[:, 56:57], in_=Ru[:, 48:56], axis=AX.X))
                        dv(lambda e: e.tensor_scalar(out=Ru[:, 64:72], in0=Ru[:, 48:56], scalar1=Ru[:, 56:57], scalar2=None, op0=ALU.is_equal))
                        dv(lambda e: e.scalar_tensor_tensor(out=Ru[:, 72:80], in0=Ru[:, 64:72], scalar=-1e30, in1=Ru[:, 48:56], op0=ALU.mult, op1=ALU.add))
                        dv(lambda e: e.reduce_max(out=Ru[:, 57:58], in_=Ru[:, 72:80], axis=AX.X))
                        dv(lambda e: e.tensor_scalar(out=Ru[:, 80:88], in0=Ru[:, 72:80], scalar1=Ru[:, 57:58], scalar2=None, op0=ALU.is_equal))
                        dv(lambda e: e.tensor_tensor(out=Ru[:, 58:59], in0=Ru[:, 57:58], in1=Ru[:, 56:57], op=ALU.subtract))
                        act(Ru[:, 59:60], Ru[:, 58:59], AF.Exp, [Rk], [Rk])
                        dv(lambda e: e.tensor_scalar(out=Ru[:, 60:61], in0=Ru[:, 59:60], scalar1=1.0, scalar2=None, op0=ALU.add))
                        dv(lambda e: e.reciprocal(out=Ru[:, 61:62], in_=Ru[:, 60:61]))
                        dv(lambda e: e.tensor_tensor(out=Ru[:, 62:63], in0=Ru[:, 61:62], in1=Ru[:, 39:40], op=ALU.mult))
                        dv(lambda e: e.tensor_tensor(out=Ru[:, 63:64], in0=Ru[:, 62:63], in1=Ru[:, 59:60], op=ALU.mult))
                        dv(lambda e: e.tensor_scalar(out=Ru[:, 88:96], in0=Ru[:, 64:72], scalar1=Ru[:, 62:63], scalar2=None, op0=ALU.mult))
                        dv(lambda e: e.scalar_tensor_tensor(out=Ru[:, 88:96], in0=Ru[:, 80:88], scalar=Ru[:, 63:64], in1=Ru[:, 88:96], op0=ALU.mult, op1=ALU.add))
                        for g in range(4):
                            s.op("dve", lambda e, g=g: e.tensor_scalar(out=Wtok[:, i, 8 * g:8 * g + 8], in0=Ru[:, 88:96], scalar1=Ru[:, 40 + g:41 + g],
                                                                         scalar2=None, op0=ALU.mult), reads=[Rk], writes=["Wtok"])
                    if smp == 0:
                        dump("Wtok", Wtok[:, 0:2, :], ["Wtok"])
                        dump("fT", fT[:, :, 0:256], ["fT"])
                    s.barrier()
                    print("P5 done: deadlock-free", s.check_deadlock(), s.n_ins, s.n_wait, s.cnt)
                if stop_after == "P5":
                    s.barrier()
                    return nc
                with ExitStack() as st:
                    weg = [sb(st, "weg%d" % i, [128, 8, DE], BF16) for i in range(2)]
                    weu = [sb(st, "weu%d" % i, [128, 8, DE], BF16) for i in range(2)]
                    wed = [sb(st, "wed%d" % i, [128, 4, D], BF16) for i in range(2)]
                    hT = [sb(st, "hT%d" % i, [128, 4, 512], BF16) for i in range(2)]
                    sgm = [sb(st, "sgm%d" % i, [128, 512], F32) for i in range(2)]
                    GB = [pst(st, "p6g%d" % i, [128, 512]) for i in range(2)]
                    UB = [pst(st, "p6u%d" % i, [128, 512]) for i in range(2)]
                    YB = [pst(st, "p6y%d" % i, [128, 512]) for i in range(4)]
                    GK = ["p6G%d" % i for i in range(2)]
                    UK = ["p6U%d" % i for i in range(2)]
                    YK = ["p6Y%d" % i for i in range(4)]
                    n_exp = NEXP if not os.environ.get("LIM_E") else int(os.environ["LIM_E"])
                    for e_ in range(n_exp):
                        wb = e_ % 2
                        s.dma("pool", weg[wb][:], wview(w_eg_d[e_]), writes=["weg%d" % wb])
                        s.dma("pool", weu[wb][:], wview(w_eu_d[e_]), writes=["weu%d" % wb])
                        s.dma("pool", wed[wb][:], wview(w_ed_d[e_]), writes=["wed%d" % wb])
                        for kt in range(4):
                            tt("pool", wed[wb][:, kt, :], wed[wb][:, kt, :], g2b[:], ALU.mult, ["wed%d" % wb, "g2b"], ["wed%d" % wb])
                        for tb in range(4):
                            cols = slice(tb * 512, (tb + 1) * 512)
                            hb = tb % 2
                            for nt in range(4):
                                u = (tb * 4 + nt) % 2
                                ncol = slice(nt * 128, (nt + 1) * 128)
                                for kt in range(8):
                                    mm(GB[u][:, :], weg[wb][:, kt, ncol], fT[:, kt, cols], kt == 0, kt == 7, ["weg%d" % wb, "fT"], [], x=[GK[u]])
                                for kt in range(8):
                                    mm(UB[u][:, :], weu[wb][:, kt, ncol], fT[:, kt, cols], kt == 0, kt == 7, ["weu%d" % wb, "fT"], [], x=[UK[u]])
                                act(sgm[u][:], GB[u][:, :], AF.Silu, [], ["sgm%d" % u], x=[GK[u]])
                                tt("dve", hT[hb][:, nt, :], sgm[u][:], UB[u][:, :], ALU.mult, ["sgm%d" % u], ["hT%d" % hb], x=[UK[u]])
                            for ti in range(4):
                                i = tb * 4 + ti
                                for half in range(2):
                                    yb = (ti * 2 + half) % 4
                                    hcol = slice(half * 512, (half + 1) * 512)
                                    for kt in range(4):
                                        mm(YB[yb][:, :], hT[hb][:, kt, ti * 128:(ti + 1) * 128], wed[wb][:, kt, hcol], kt == 0, kt == 3,
                                           ["hT%d" % hb, "wed%d" % wb], [], x=[YK[yb]])
                                    stt("dve", hres[:, i, hcol], YB[yb][:, :], Wtok[:, i, e_:e_ + 1], hres[:, i, hcol], ALU.mult, ALU.add,
                                        ["Wtok", "hres%d" % i], ["hres%d" % i], x=[YK[yb]])
                    s.barrier()
                    print("P6 done: deadlock-free", s.check_deadlock(), s.n_ins, s.n_wait, s.cnt)
                with ExitStack() as st:
                    ot = [sb(st, "ot%d" % i, [128, D], F32) for i in range(2)]
                    junk4 = sb(st, "junk4", [128, D], F32)
                    st4 = [sb(st, "st4_%d" % i, [128, 4], F32) for i in range(2)]
                    for i in range(NT):
                        u = i % 2
                        tokc = slice(i * 128, (i + 1) * 128)
                        hk = "hres%d" % i
                        s.op("pool", lambda e, u=u: e.memset(st4[u][:], 0.0), writes=["st4_%d" % u])
                        act(junk4[:], hres[:, i, :], AF.Square, [hk, "st4_%d" % u], ["junk4", "st4_%d" % u], accum_out=st4[u][:, 0:1])
                        act(st4[u][:, 1:2], st4[u][:, 0:1], AF.Sqrt, ["st4_%d" % u, "eps_c"], ["st4_%d" % u], scale=1.0 / D, bias=eps_c[:, 0:1])
                        s.op("dve", lambda e, u=u: e.reciprocal(out=st4[u][:, 2:3], in_=st4[u][:, 1:2]), reads=["st4_%d" % u], writes=["st4_%d" % u])
                        stt("dve", ot[u][:], hres[:, i, :], st4[u][:, 2:3], gfb[:], ALU.mult, ALU.mult, [hk, "st4_%d" % u, "gfb"], ["ot%d" % u])
                        s.dma("sp", y_d[smp, tokc, :], ot[u][:], reads=["ot%d" % u])
                    s.barrier()
        s.barrier()
    return nc


def _consts():
    ident = np.eye(128, dtype=np.float32)
    idx = np.arange(128)
    same = (idx[:, None] // 64) == (idx[None, :] // 64)
    tri = np.zeros((4, 128, 128), np.float32)
    tri[0] = same & (idx[:, None] <= idx[None, :])
    tri[1] = same & (idx[:, None] > idx[None, :])
    tri[2] = same & (idx[:, None] >= idx[None, :])
    tri[3] = same & (idx[:, None] < idx[None, :])
    t = np.arange(S)
    pos_r = (t // GW).astype(np.float32)
    pos_c = (t % GW).astype(np.float32)
    inv = (10000.0 ** (-np.arange(32, dtype=np.float32) / 32)).astype(np.float32)
    ang = np.zeros((128, S), np.float32)
    ang[0:32] = inv[:, None] * pos_r[None, :]
    ang[32:64] = inv[:, None] * pos_r[None, :]
    ang[64:96] = inv[:, None] * pos_c[None, :]
    ang[96:128] = inv[:, None] * pos_c[None, :]
    cos = np.cos(ang).astype(np.float32)
    sin = np.sin(ang).astype(np.float32)
    sgn = np.ones((128, 1), np.float32)
    sgn[0:32] = -1
    sgn[64:96] = -1
    sin = sin * sgn
    perm = np.concatenate([np.arange(32, 64), np.arange(0, 32), np.arange(96, 128), np.arange(64, 96)])
    return ident, tri, cos, sin, perm


def _na_tables(rpb):
    a = (np.arange(128) // 64)[:, None, None, None]
    kc = (np.arange(128) % 64)[:, None, None, None]
    p = np.arange(8)[None, :, None, None]
    j = np.arange(4)[None, None, :, None]
    qc = np.arange(64)[None, None, None, :]
    ridx = 2 * j + a - p + 7
    cstart = np.clip(qc - 8, 0, 48)
    valid = (kc >= cstart) & (kc < cstart + 16) & (ridx >= 0) & (ridx <= 14)
    valid = np.broadcast_to(valid, (128, 8, 4, 64))
    cidx = np.clip(kc - qc + 15, 0, 30)
    ridx_c = np.clip(ridx, 0, 14)
    ridx_b = np.broadcast_to(ridx_c, (128, 8, 4, 64))
    cidx_b = np.broadcast_to(cidx, (128, 8, 4, 64))
    g = rpb[:, ridx_b, cidx_b]
    g = np.where(valid[None], g, np.float32(0.0)).astype(np.float32)
    g = g.reshape(4, 2, 128, 8, 256).transpose(0, 2, 3, 1, 4)
    mask = np.where(valid, np.float32(0.0), np.float32(MASKV)).astype(np.float32).reshape(128, 8, 256)
    return np.ascontiguousarray(g), np.ascontiguousarray(mask)


_NC_CACHE = {}


def kernel(x, c, ctx, c_ctx, w_mod, b_mod, norm_attn_g, norm_ffn_g, w_in, w_gla_a2, b_gla_a2,
           gla_norm_g, na_rpb, w_na_o, w_gla_o, w_out, w_group, b_group, w_expert, b_expert,
           w_exp_gate, w_exp_up, w_exp_down, final_norm_g):
    f = lambda a: np.ascontiguousarray(np.asarray(a, dtype=np.float32))
    x, c, ctx, c_ctx = f(x), f(c), f(ctx), f(c_ctx)
    ident, tri, cos, sin, perm = _consts()
    w_in0 = f(w_in)[0]
    gq = w_in0[:, C_GQ:C_GQ + 512].reshape(D, 4, 128)[:, :, perm].reshape(D, 512)
    gk = w_in0[:, C_GK:C_GK + 512].reshape(D, 4, 128)[:, :, perm].reshape(D, 512)
    w_rope = np.ascontiguousarray(np.concatenate([gq, gk], axis=1))
    w_a2b = np.ascontiguousarray(np.concatenate(
        [f(w_gla_a2)[0].transpose(1, 0, 2), f(b_gla_a2)[0][None]], axis=0))
    nab, nam = _na_tables(f(na_rpb)[0])
    w_rt = np.ascontiguousarray(np.concatenate([f(w_group)[0], f(w_expert)[0]], axis=1))
    b_rt = np.ascontiguousarray(np.concatenate([f(b_group)[0], f(b_expert)[0]], axis=0))
    shared = {
        "w_mod": f(w_mod)[0], "b_mod": f(b_mod)[0], "g_attn": f(norm_attn_g)[0], "g_ffn": f(norm_ffn_g)[0],
        "g_fin": f(final_norm_g), "gla_g": f(gla_norm_g)[0], "w_in": w_in0, "w_rope": w_rope, "w_a2b": w_a2b,
        "w_na_o": f(w_na_o)[0], "w_gla_o": f(w_gla_o)[0], "w_out": f(w_out)[0], "w_rt": w_rt, "b_rt": b_rt,
        "w_eg": f(w_exp_gate)[0], "w_eu": f(w_exp_up)[0], "w_ed": f(w_exp_down)[0],
        "ident": ident, "tri": tri, "rope_cos": cos, "rope_sin": sin, "na_bias": nab, "na_mask": nam,
    }
    n = 8
    NS = x.shape[0] // n
    if "nc" not in _NC_CACHE:
        _NC_CACHE["nc"] = build_nc(NS)
    nc = _NC_CACHE["nc"]
    in_maps = []
    for i in range(n):
        m = dict(shared)
        m["x"] = x[i * NS:(i + 1) * NS]
        m["ctx"] = ctx[i * NS:(i + 1) * NS]
        m["cc"] = np.ascontiguousarray(np.concatenate([c[i * NS:(i + 1) * NS], c_ctx[None]], axis=0))
        in_maps.append(m)
    res = run_bass_kernel_spmd(nc, in_maps, core_ids=list(range(n)))
    return np.concatenate([r["y"] for r in res.results], axis=0)
```

```python
import os
import numpy as np
import concourse.bass as bass
import concourse.mybir as mybir
from concourse.bass_utils import run_bass_kernel_spmd
from contextlib import ExitStack

F32 = mybir.dt.float32
BF16 = mybir.dt.bfloat16
I32 = mybir.dt.int32
ALU = mybir.AluOpType
AF = mybir.ActivationFunctionType
AX = mybir.AxisListType

D = 1024
S = 2048
CT = 256
NT = 16
NTC = 2
NTA = NT + NTC
GW = 64
EPS = 1e-6
NEXP = 32
DE = 512
C_NAQ, C_NAK, C_NAV, C_GQ, C_GK, C_GV, C_GG, C_AL, C_M8, C_M9 = 0, 512, 1024, 1536, 2048, 2560, 3584, 4608, 4640, 5664
MASKV = -30000.0
MOE_CAP = 640


class Sched:
    ENGS = ("pe", "act", "dve", "pool", "sp")
    HND = {"pe": "tensor", "act": "scalar", "dve": "vector", "pool": "gpsimd", "sp": "sync"}

    def __init__(self, nc, es, n_dma_sems=32):
        self.nc = nc
        self.sem = {e: es.enter_context(nc.semaphore("s_" + e)) for e in self.ENGS}
        self.cnt = {e: 0 for e in self.ENGS}
        self.pending = {e: False for e in self.ENGS}
        self.waited = {e: {} for e in self.ENGS}
        self.dsem = [es.enter_context(nc.semaphore("s_dma%d" % i)) for i in range(n_dma_sems)]
        self.dcnt = [0] * n_dma_sems
        self.dnext = 0
        self.lastw = {}
        self.reads = {}
        self.semobj = {}
        for e in self.ENGS:
            self.semobj[("e", e)] = self.sem[e]
        for i, sm in enumerate(self.dsem):
            self.semobj[("d", i)] = sm
        self.n_ins = 0
        self.n_wait = 0

    def _deps(self, eng, reads, writes, excl=()):
        toks = {}

        def add(t):
            if t is None:
                return
            k, v = t
            if toks.get(k, -1) < v:
                toks[k] = v
        for b in reads:
            add(self.lastw.get(b))
        for b in writes:
            add(self.lastw.get(b))
            for t in self.reads.get(b, ()):
                add(t)
        for b in excl:
            t = self.lastw.get(b)
            if t is not None and t[0] != ("e", eng):
                add(t)
        out = []
        for k, v in toks.items():
            if k == ("e", eng):
                if eng == "pe":
                    continue
                if v <= self.cnt[eng] - 2:
                    continue
            if self.waited[eng].get(k, -1) >= v:
                continue
            self.waited[eng][k] = v
            out.append((k, v))
        return out

    def _commit(self, tok, reads, writes):
        for b in writes:
            self.lastw[b] = tok
            self.reads[b] = []
        for b in reads:
            self.reads.setdefault(b, []).append(tok)

    def check_deadlock(self):
        pos = {e: 0 for e in self.ENGS}
        val = {}
        prog = True
        while prog:
            prog = False
            for e in self.ENGS:
                lst = self.log[e]
                while pos[e] < len(lst):
                    waits, inc = lst[pos[e]]
                    if all(val.get(k, 0) >= v for k, v in waits):
                        if inc is not None:
                            val[inc[0]] = val.get(inc[0], 0) + inc[1]
                        pos[e] += 1
                        prog = True
                    else:
                        break
        stuck = {e: (pos[e], len(self.log[e])) for e in self.ENGS if pos[e] < len(self.log[e])}
        for e in stuck:
            waits, inc = self.log[e][pos[e]]
            print("STUCK", e, pos[e], [(k, v, val.get(k, 0)) for k, v in waits])
        return not stuck

    def _emit1(self, eng, waits, fn, inc):
        if not hasattr(self, "log"):
            self.log = {e: [] for e in self.ENGS}
        self.log[eng].append((list(waits), inc if fn is not None else None))
        engh = getattr(self.nc, self.HND[eng])
        for k, v in waits:
            engh.wait_ge(self.semobj[k], v)
            self.n_wait += 1
        if fn is None:
            return
        ins = fn(engh)
        if inc is not None:
            ins.then_inc(self.semobj[inc[0]], inc[1])
        self.n_ins += 1

    def op(self, eng, fn, reads=(), writes=(), inc=True, excl=()):
        waits = self._deps(eng, reads, writes, excl)
        if inc:
            self.cnt[eng] += 1
            tok = (("e", eng), self.cnt[eng])
            self.pending[eng] = False
        else:
            tok = (("e", eng), self.cnt[eng] + 1)
            self.pending[eng] = True
        self._emit1(eng, waits, fn, (("e", eng), 1) if inc else None)
        self._commit(tok, reads, writes)
        for b_ in excl:
            self.lastw[b_] = tok
        return tok

    def dma(self, eng, out, in_, reads=(), writes=(), **kw):
        i = self.dnext
        self.dnext = (self.dnext + 1) % len(self.dsem)
        k = ("d", i)
        waits = self._deps(eng, reads, writes)
        if self.dcnt[i] > 0 and self.waited[eng].get(k, -1) < self.dcnt[i]:
            self.waited[eng][k] = self.dcnt[i]
            waits.append((k, self.dcnt[i]))
        self.dcnt[i] += 16
        tok = (k, self.dcnt[i])
        self._emit1(eng, waits, lambda e: e.dma_start(out=out, in_=in_, **kw), (k, 16))
        self._commit(tok, reads, writes)
        return tok

    def barrier(self):
        assert not any(self.pending.values())
        allt = [(("e", e), self.cnt[e]) for e in self.ENGS if self.cnt[e] > 0]
        allt += [(("d", i), c) for i, c in enumerate(self.dcnt) if c > 0]
        for e in self.ENGS:
            waits = []
            for k, v in allt:
                if k == ("e", e):
                    continue
                if self.waited[e].get(k, -1) >= v:
                    continue
                self.waited[e][k] = v
                waits.append((k, v))
            self._emit1(e, waits, None, None)
        self.lastw = {}
        self.reads = {}


def build_nc(NS=2, dbg=None, stop_after=None):
    nc = bass.Bass("TRN2", target_bir_lowering=False)

    def din(name, shape, dt=F32):
        return nc.dram_tensor(name, list(shape), dt, kind="ExternalInput").ap()

    x_d = din("x", [NS, S, D])
    ctx_d = din("ctx", [NS, CT, D])
    cc_d = din("cc", [NS + 1, D])
    w_mod_d = din("w_mod", [D, 6 * D])
    b_mod_d = din("b_mod", [6 * D])
    g_attn_d = din("g_attn", [D])
    g_ffn_d = din("g_ffn", [D])
    g_fin_d = din("g_fin", [D])
    gla_g_d = din("gla_g", [256])
    w_in_d = din("w_in", [D, 6688])
    w_rope_d = din("w_rope", [D, 1024])
    w_a2b_d = din("w_a2b", [17, 2, 512])
    w_nao_d = din("w_na_o", [512, D])
    w_glo_d = din("w_gla_o", [D, D])
    w_out_d = din("w_out", [D, D])
    w_rt_d = din("w_rt", [D, 36])
    b_rt_d = din("b_rt", [36])
    w_eg_d = din("w_eg", [NEXP, D, DE])
    w_eu_d = din("w_eu", [NEXP, D, DE])
    w_ed_d = din("w_ed", [NEXP, DE, D])
    ident_d = din("ident", [128, 128])
    tri_d = din("tri", [6, 128, 128])
    ebase_d = din("ebase", [128, 32])
    cos_d = din("rope_cos", [128, S])
    sin_d = din("rope_sin", [128, S])
    nab_d = din("na_bias", [4, 128, 8, 2, 256])
    nam_d = din("na_mask", [128, 8, 256])
    y_d = nc.dram_tensor("y", [NS, S, D], F32, kind="ExternalOutput").ap()
    modv_d = nc.dram_tensor("modv", [NS + 1, 6 * D], F32).ap()
    dbg_d = {}
    if dbg:
        for name, shape in dbg.items():
            dbg_d[name] = nc.dram_tensor("dbg_" + name, list(shape), F32, kind="ExternalOutput").ap()

    with ExitStack() as es:
        s = Sched(nc, es)

        used_names = {}

        def uniq(name):
            k = used_names.get(name, 0)
            used_names[name] = k + 1
            return name if k == 0 else "%s_r%d" % (name, k)

        def sb(st, name, shape, dt):
            return st.enter_context(nc.sbuf_tensor(uniq(name), list(shape), dt))

        def pst(st, name, shape, dt=F32):
            return st.enter_context(nc.psum_tensor(uniq(name), list(shape), dt))

        def mm(out, lhsT, rhs, start, stop, reads, writes, inc=None, x=()):
            if inc is None:
                inc = stop
            s.op("pe", lambda e: e.matmul(out, lhsT, rhs, start=start, stop=stop),
                 reads=reads, writes=writes, inc=inc, excl=x)

        def tr(out, in_, idn, reads, writes, x=()):
            s.op("pe", lambda e: e.transpose(out, in_, idn), reads=reads, writes=writes, excl=x)

        def act(out, in_, func, reads, writes, x=(), **kw):
            s.op("act", lambda e: e.activation(out=out, in_=in_, func=func, **kw), reads=reads, writes=writes, excl=x)

        def tt(eng, out, in0, in1, op, reads, writes, x=()):
            s.op(eng, lambda e: e.tensor_tensor(out=out, in0=in0, in1=in1, op=op), reads=reads, writes=writes, excl=x)

        def ts(eng, out, in0, s1, s2, op0, op1, reads, writes, x=()):
            if s2 is None:
                s.op(eng, lambda e: e.tensor_scalar(out=out, in0=in0, scalar1=s1, scalar2=None, op0=op0),
                     reads=reads, writes=writes, excl=x)
            else:
                s.op(eng, lambda e: e.tensor_scalar(out=out, in0=in0, scalar1=s1, scalar2=s2, op0=op0, op1=op1),
                     reads=reads, writes=writes, excl=x)

        def stt(eng, out, in0, scalar, in1, op0, op1, reads, writes, x=()):
            s.op(eng, lambda e: e.scalar_tensor_tensor(out=out, in0=in0, scalar=scalar, in1=in1, op0=op0, op1=op1),
                 reads=reads, writes=writes, excl=x)

        def cp(eng, out, in_, reads, writes, x=()):
            if eng == "act":
                s.op("act", lambda e: e.copy(out=out, in_=in_), reads=reads, writes=writes, excl=x)
            else:
                s.op(eng, lambda e: e.tensor_copy(out=out, in_=in_), reads=reads, writes=writes, excl=x)

        def dump(name, src_ap, reads, dst=None):
            if name in dbg_d:
                s.dma("pool", dbg_d[name] if dst is None else dst, src_ap, reads=reads)

        def wview(ap2d):
            return ap2d.rearrange("(kt p) n -> p kt n", p=128)

        ident_f = sb(es, "ident_f", [128, 128], F32)
        ident_b = sb(es, "ident_b", [128, 128], BF16)
        tri_f = sb(es, "tri_f", [128, 4, 128], F32)
        ones_b = sb(es, "ones_b", [128, 128], BF16)
        eps_c = sb(es, "eps_c", [128, 1], F32)
        one_c = sb(es, "one_c", [128, 1], F32)
        s.dma("sp", ident_f[:], ident_d, writes=["ident_f"])
        s.dma("pool", ident_b[:], ident_d, writes=["ident_b"])
        s.dma("sp", tri_f[:], tri_d[0:4].rearrange("m s t -> s m t"), writes=["tri"])
        s.op("pool", lambda e: e.memset(ones_b[:], 1.0), writes=["ones_b"])
        s.op("pool", lambda e: e.memset(eps_c[:], EPS), writes=["eps_c"])
        s.op("pool", lambda e: e.memset(one_c[:], 1.0), writes=["one_c"])

        with ExitStack() as st:
            ccs = sb(st, "ccs", [NS + 1, D], F32)
            scT = sb(st, "scT", [128, 8, NS + 1], F32)
            modsb = sb(st, "modsb", [NS + 1, 6 * D], F32)
            bmod = sb(st, "bmod", [NS + 1, 6 * D], F32)
            wm = [sb(st, "wm%d" % i, [128, 8, 512], F32) for i in range(2)]
            psA = pst(st, "p0a", [128, 512])
            psB = [pst(st, "p0b%d" % i, [128, 512]) for i in range(2)]
            R = NS + 1
            s.dma("sp", ccs[:], cc_d, writes=["ccs"])
            s.dma("sp", bmod[:], b_mod_d.partition_broadcast(R), writes=["bmod"])
            act(ccs[:], ccs[:], AF.Silu, ["ccs"], ["ccs"])
            for kt in range(8):
                tr(psA[:, kt * R:(kt + 1) * R], ccs[:, kt * 128:(kt + 1) * 128], ident_f[0:R, 0:R],
                   ["ccs", "ident_f"], ["p0a"])
            cp("dve", scT[:].rearrange("p k r -> p (k r)"), psA[:, 0:8 * R], ["p0a"], ["scT"])
            for cb in range(12):
                w = wm[cb % 2]
                s.dma("sp", w[:], wview(w_mod_d[:, cb * 512:(cb + 1) * 512]), writes=["wm%d" % (cb % 2)])
                ps = psB[cb % 2]
                for kt in range(8):
                    mm(ps[0:R, :], scT[:, kt, :], w[:, kt, :], kt == 0, kt == 7,
                       ["scT", "wm%d" % (cb % 2)], ["p0b%d" % (cb % 2)])
                tt("dve", modsb[:, cb * 512:(cb + 1) * 512], ps[0:R, :], bmod[:, cb * 512:(cb + 1) * 512], ALU.add,
                   ["p0b%d" % (cb % 2), "bmod"], ["modsb"])
            s.dma("sp", modv_d, modsb[:], reads=["modsb"], writes=["modv"])
            dump("modv", modsb[:], ["modsb"])
            s.barrier()
        if stop_after == "P0":
            s.barrier()
            return nc

        QSCALE = 128.0 ** -0.5
        bc_reg = nc.gpsimd.alloc_register("bcreg")
        nc.gpsimd.reg_mov(bc_reg, NEXP * MOE_CAP - 1)
        h1_d = nc.dram_tensor("h1_scr", [NS, S, D], F32).ap()

        def bank_set(st, pfx):
            return [pst(st, "%s%d" % (pfx, i), [128, 512]) for i in range(7)]

        for smp in range(NS):
            with ExitStack() as sa:
                aT = sb(sa, "aT", [128, 8, S], BF16)
                acT = sb(sa, "acT", [128, 8, CT], BF16)
                oglT = sb(sa, "oglT", [128, 8, S], BF16)

                with ExitStack() as st:
                    s1b = sb(st, "s1b", [128, D], F32)
                    sh1b = sb(st, "sh1b", [128, D], F32)
                    s1c = sb(st, "s1c", [128, D], F32)
                    sh1c = sb(st, "sh1c", [128, D], F32)
                    gab = sb(st, "gab", [128, D], F32)
                    tmpv = sb(st, "tmpv", [128, D], F32)
                    xt = [sb(st, "xt%d" % i, [128, D], F32) for i in range(2)]
                    xm = [sb(st, "xm%d" % i, [128, D], F32) for i in range(2)]
                    xn = [sb(st, "xn%d" % i, [128, D], BF16) for i in range(2)]
                    stat = [sb(st, "stat%d" % i, [128, 4], F32) for i in range(2)]
                    psT = [pst(st, "p1t%d" % i, [128, 8, 128], BF16) for i in range(2)]
                    s.dma("sp", gab[:], g_attn_d.partition_broadcast(128), writes=["gab"])
                    for row, s1, sh, nm in ((smp, s1b, sh1b, "l"), (NS, s1c, sh1c, "c")):
                        s.dma("sp", tmpv[:], modv_d[row, D:2 * D].partition_broadcast(128), writes=["tmpv"])
                        stt("dve", s1[:], tmpv[:], 1.0, gab[:], ALU.add, ALU.mult, ["tmpv", "gab"], ["s1" + nm])
                        s.dma("sp", sh[:], modv_d[row, 0:D].partition_broadcast(128), writes=["sh1" + nm])
                    for i in range(NTA):
                        b = i % 2
                        lat = i < NT
                        src = x_d[smp, i * 128:(i + 1) * 128, :] if lat else ctx_d[smp, (i - NT) * 128:(i - NT + 1) * 128, :]
                        nm = "l" if lat else "c"
                        s1, sh = (s1b, sh1b) if lat else (s1c, sh1c)
                        s.dma("sp", xt[b][:], src, writes=["xt%d" % b])
                        s.op("pool", lambda e, b=b: e.memset(stat[b][:], 0.0), writes=["stat%d" % b])
                        act(xm[b][:], xt[b][:], AF.Square, ["xt%d" % b, "stat%d" % b], ["xm%d" % b, "stat%d" % b],
                            accum_out=stat[b][:, 0:1])
                        act(stat[b][:, 1:2], stat[b][:, 0:1], AF.Sqrt, ["stat%d" % b, "eps_c"], ["stat%d" % b],
                            scale=1.0 / D, bias=eps_c[:, 0:1])
                        s.op("dve", lambda e, b=b: e.reciprocal(out=stat[b][:, 2:3], in_=stat[b][:, 1:2]),
                             reads=["stat%d" % b], writes=["stat%d" % b])
                        stt("dve", xm[b][:], xt[b][:], stat[b][:, 2:3], s1[:], ALU.mult, ALU.mult,
                            ["xt%d" % b, "stat%d" % b, "s1" + nm], ["xm%d" % b])
                        tt("pool", xn[b][:], xm[b][:], sh[:], ALU.add, ["xm%d" % b, "sh1" + nm], ["xn%d" % b])
                        for kt in range(8):
                            tr(psT[b][:, kt, :], xn[b][:, kt * 128:(kt + 1) * 128], ident_b[:],
                               ["xn%d" % b, "ident_b"], ["p1t%d" % b])
                        if lat:
                            cp("act", aT[:, :, i * 128:(i + 1) * 128], psT[b][:, :, :], ["p1t%d" % b], ["aT"])
                        else:
                            cp("act", acT[:, :, (i - NT) * 128:(i - NT + 1) * 128], psT[b][:, :, :], ["p1t%d" % b], ["acT"])
                    if smp == 0:
                        dump("aT", aT[:, :, 0:256], ["aT"])
                    s.barrier()
                if stop_after == "P1":
                    s.barrier()
                    return nc

                with ExitStack() as st:
                    ropeC = sb(st, "ropeC", [128, S], BF16)
                    ropeS = sb(st, "ropeS", [128, S], BF16)
                    alT = [sb(st, "alT%d" % d_, [16, S + CT], BF16) for d_ in range(2)]
                    wal = sb(st, "wal", [128, 8, 32], BF16)
                    wa2 = sb(st, "wa2", [16, 2, 512], BF16)
                    ba2 = sb(st, "ba2", [1, 2, 512], BF16)
                    gnb = sb(st, "gnb", [128, 256], F32)
                    wq = sb(st, "wq", [128, 8, 128], BF16)
                    wqs = sb(st, "wqs", [128, 8, 128], BF16)
                    wk = sb(st, "wk", [128, 8, 128], BF16)
                    wks = sb(st, "wks", [128, 8, 128], BF16)
                    wv = sb(st, "wv", [128, 8, 256], BF16)
                    wg = sb(st, "wg", [128, 8, 256], BF16)
                    qrT = sb(st, "qrT", [128, S], BF16)
                    krT = sb(st, "krT", [128, S], BF16)
                    krk = sb(st, "krk", [128, NTA, 128], BF16)
                    vtk = sb(st, "vtk", [128, NTA, 256], BF16)
                    sgt = sb(st, "sgt", [128, NT, 256], BF16)
                    qe = [sb(st, "qe%d" % d_, [128, S], BF16) for d_ in range(2)]
                    ke = [sb(st, "ke%d" % d_, [128, S], BF16) for d_ in range(2)]
                    kd = [sb(st, "kd%d" % d_, [128, NTA, 128], BF16) for d_ in range(2)]
                    dec = sb(st, "dec", [128, 2, NTA, 2], F32)
                    oacc = sb(st, "oacc", [128, NT, 256], F32)
                    S32 = [sb(st, "S32_%d" % d_, [128, 256], F32) for d_ in range(2)]
                    S16 = [sb(st, "S16_%d" % d_, [128, 256], BF16) for d_ in range(2)]
                    t1 = [sb(st, "t1_%d" % i, [128, 512], F32) for i in range(2)]
                    t2 = [sb(st, "t2_%d" % i, [128, 512], F32) for i in range(2)]
                    az = [sb(st, "az%d" % i, [128, 128], F32) for i in range(2)]
                    ex = [sb(st, "ex%d" % i, [128, 128], F32) for i in range(2)]
                    rz = [sb(st, "rz%d" % i, [128, 128], F32) for i in range(2)]
                    Lt = [sb(st, "Lt%d" % i, [128, 128], F32) for i in range(2)]
                    Dt = [sb(st, "Dt%d" % i, [128, 128], F32) for i in range(2)]
                    Di = [sb(st, "Di%d" % i, [128, 128], F32) for i in range(2)]
                    EK = [sb(st, "EK%d" % i, [128, 128], F32) for i in range(2)]
                    att = [sb(st, "att%d" % i, [128, 128], BF16) for i in range(2)]
                    junk = sb(st, "junk2", [128, 256], F32)
                    onr = [sb(st, "onr%d" % i, [128, 256], F32) for i in range(2)]
                    rbf = [sb(st, "rbf%d" % i, [128, 256], BF16) for i in range(2)]
                    st2 = [sb(st, "st2_%d" % i, [128, 4], F32) for i in range(2)]
                    B = [pst(st, "p2b%d" % i, [128, 512]) for i in range(6)]
                    BK = ["p2B%d" % i for i in range(6)]
                    pT = [pst(st, "p2t%d" % i, [128, 8, 128], BF16) for i in range(2)]
                    TK = ["p2T0", "p2T1"]

                    s.dma("pool", ropeC[:], cos_d, writes=["ropeC"])
                    s.dma("pool", ropeS[:], sin_d, writes=["ropeS"])
                    s.dma("pool", wal[:], wview(w_in_d[:, C_AL:C_AL + 32]), writes=["wal"])
                    s.dma("pool", wa2[:], w_a2b_d[0:16], writes=["wa2"])
                    s.dma("pool", ba2[:], w_a2b_d[16:17], writes=["ba2"])
                    s.dma("sp", gnb[:], gla_g_d.partition_broadcast(128), writes=["gnb"])
                    for tb in range(5):
                        if tb < 4:
                            rhs_of = lambda kt, tb=tb: aT[:, kt, tb * 512:(tb + 1) * 512]
                            n, c0, rk = 512, tb * 512, "aT"
                        else:
                            rhs_of = lambda kt: acT[:, kt, :]
                            n, c0, rk = CT, S, "acT"
                        for d_ in range(2):
                            for kt in range(8):
                                mm(B[d_][0:16, 0:n], wal[:, kt, d_ * 16:(d_ + 1) * 16], rhs_of(kt), kt == 0, kt == 7,
                                   ["wal", rk], [], x=[BK[d_]])
                            cp("act" if d_ == 0 else "dve", alT[d_][:, c0:c0 + n], B[d_][0:16, 0:n], [], ["alT%d" % d_], x=[BK[d_]])
                    if stop_after == "P2a":
                        s.barrier()
                        return nc

                    for h in range(4):
                        s.dma("pool", wq[:], wview(w_in_d[:, C_GQ + 128 * h:C_GQ + 128 * (h + 1)]), writes=["wq"])
                        s.dma("pool", wqs[:], wview(w_rope_d[:, 128 * h:128 * (h + 1)]), writes=["wqs"])
                        s.dma("pool", wk[:], wview(w_in_d[:, C_GK + 128 * h:C_GK + 128 * (h + 1)]), writes=["wk"])
                        s.dma("pool", wks[:], wview(w_rope_d[:, 512 + 128 * h:512 + 128 * (h + 1)]), writes=["wks"])
                        s.dma("pool", wv[:], wview(w_in_d[:, C_GV + 256 * h:C_GV + 256 * (h + 1)]), writes=["wv"])
                        s.dma("pool", wg[:], wview(w_in_d[:, C_GG + 256 * h:C_GG + 256 * (h + 1)]), writes=["wg"])
                        for tb in range(4):
                            cols = slice(tb * 512, (tb + 1) * 512)
                            for bi, (w, wn) in enumerate(((wq, "wq"), (wqs, "wqs"), (wk, "wk"), (wks, "wks"))):
                                for kt in range(8):
                                    mm(B[bi][:, :], w[:, kt, :], aT[:, kt, cols], kt == 0, kt == 7, [wn, "aT"], [], x=[BK[bi]])
                            stt("dve", t1[0][:], B[0][:, :], QSCALE, ropeC[:, cols], ALU.mult, ALU.mult, ["ropeC"], ["t1_0"], x=[BK[0]])
                            stt("dve", t2[0][:], B[1][:, :], QSCALE, ropeS[:, cols], ALU.mult, ALU.mult, ["ropeS"], ["t2_0"], x=[BK[1]])
                            tt("pool", qrT[:, cols], t1[0][:], t2[0][:], ALU.add, ["t1_0", "t2_0"], ["qrT"])
                            tt("dve", t1[1][:], B[2][:, :], ropeC[:, cols], ALU.mult, ["ropeC"], ["t1_1"], x=[BK[2]])
                            tt("dve", t2[1][:], B[3][:, :], ropeS[:, cols], ALU.mult, ["ropeS"], ["t2_1"], x=[BK[3]])
                            tt("pool", krT[:, cols], t1[1][:], t2[1][:], ALU.add, ["t1_1", "t2_1"], ["krT"])
                        if stop_after == "P2b":
                            s.barrier()
                            return nc
                        for i in range(NTA):
                            lat = i < NT
                            bv = 4 + (i % 2)
                            bg = 2 + (i % 2)
                            srcT, rk, c0 = (aT, "aT", i * 128) if lat else (acT, "acT", (i - NT) * 128)
                            for kt in range(8):
                                mm(B[bv][:, 0:256], srcT[:, kt, c0:c0 + 128], wv[:, kt, :], kt == 0, kt == 7, [rk, "wv"], [], x=[BK[bv]])
                            cp("act", vtk[:, i, :], B[bv][:, 0:256], [], ["vtk"], x=[BK[bv]])
                            if lat:
                                for kt in range(8):
                                    mm(B[bg][:, 0:256], srcT[:, kt, c0:c0 + 128], wg[:, kt, :], kt == 0, kt == 7, [rk, "wg"], [], x=[BK[bg]])
                                act(sgt[:, i, :], B[bg][:, 0:256], AF.Silu, [], ["sgt"], x=[BK[bg]])
                            else:
                                for kt in range(8):
                                    mm(B[bg][:, 0:128], srcT[:, kt, c0:c0 + 128], wk[:, kt, :], kt == 0, kt == 7, [rk, "wk"], [], x=[BK[bg]])
                                cp("dve", krk[:, i, :], B[bg][:, 0:128], [], ["krk"], x=[BK[bg]])
                        for i in range(NT):
                            u = i % 2
                            tr(pT[u][:, 0, :], krT[:, i * 128:(i + 1) * 128], ident_b[:], ["krT", "ident_b"], [], x=[TK[u]])
                            cp("dve", krk[:, i, :], pT[u][:, 0, :], [], ["krk"], x=[TK[u]])
                        if stop_after == "P2c":
                            s.barrier()
                            return nc
                        for i in range(NTA):
                            lat = i < NT
                            tokc = slice(i * 128, (i + 1) * 128)
                            for d_ in range(2):
                                u = d_
                                bz, bb, be = B[d_], B[2 + d_], B[4 + d_]
                                xz, xb, xe = [BK[d_]], [BK[2 + d_]], [BK[4 + d_]]
                                hc = slice(128 * h, 128 * (h + 1))
                                mm(bz[:, 0:128], alT[d_][0:16, tokc], wa2[0:16, d_, hc], True, False, ["alT%d" % d_, "wa2"], [], inc=False, x=xz)
                                mm(bz[:, 0:128], ones_b[0:1, 0:128], ba2[0:1, d_, hc], False, True, ["ones_b", "ba2"], [], x=xz)
                                act(az[u][:], bz[:, 0:128], AF.Abs, [], ["az%d" % u], x=xz)
                                ts("dve", rz[u][:], bz[:, 0:128], -1.0, 0.0, ALU.mult, ALU.max, [], ["rz%d" % u], x=xz)
                                act(ex[u][:], az[u][:], AF.Exp, ["az%d" % u], ["ex%d" % u], scale=-1.0)
                                act(ex[u][:], ex[u][:], AF.Ln, ["ex%d" % u, "one_c"], ["ex%d" % u], bias=one_c[:, 0:1])
                                tt("pool", Lt[u][:], rz[u][:], ex[u][:], ALU.add, ["rz%d" % u, "ex%d" % u], ["Lt%d" % u])
                                mm(bb[:, 0:128], Lt[u][:], tri_f[:, 2 * d_, :], True, True, ["Lt%d" % u, "tri"], [], x=xb)
                                mm(be[:, 0:128], tri_f[:, 2 * d_ + 1, :], Lt[u][:], True, True, ["Lt%d" % u, "tri"], [], x=xe)
                                act(Dt[u][:], bb[:, 0:128], AF.Exp, [], ["Dt%d" % u], scale=-1.0 / 16, x=xb)
                                if lat:
                                    act(Di[u][:], bb[:, 0:128], AF.Exp, [], ["Di%d" % u], scale=1.0 / 16, x=xb)
                                act(EK[u][:], be[:, 0:128], AF.Exp, [], ["EK%d" % u], scale=-1.0 / 16, x=xe)
                                if d_ == 0:
                                    dsrc = Dt[u][:, 63:128:64]
                                else:
                                    dsrc = Dt[u][:, 0:128:64]
                                cp("pool", dec[:, d_, i, :], dsrc, ["Dt%d" % u], ["dec"])
                                tt("pool", kd[d_][:, i, :], krk[:, i, :], EK[u][:], ALU.mult, ["krk", "EK%d" % u], ["kd%d" % d_])
                                if lat:
                                    tt("dve", qe[d_][:, tokc], qrT[:, tokc], Dt[u][:], ALU.mult, ["qrT", "Dt%d" % u], ["qe%d" % d_])
                                    tt("dve", ke[d_][:, tokc], krT[:, tokc], Di[u][:], ALU.mult, ["krT", "Di%d" % u], ["ke%d" % d_])
                        if stop_after == "P2d":
                            s.barrier()
                            return nc
                        for d_ in range(2):
                            s.op("pool", lambda e, d_=d_: e.memset(S32[d_][:], 0.0), writes=["S32_%d" % d_])
                            s.op("pool", lambda e, d_=d_: e.memset(S16[d_][:], 0.0), writes=["S16_%d" % d_])

                        def upd(d_, i, half):
                            rows = slice(64 * half, 64 * half + 64)
                            pkv = B[4 + d_][:, 0:256]
                            xk = [BK[4 + d_]]
                            mm(pkv, kd[d_][rows, i, :], vtk[rows, i, :], True, True, ["kd%d" % d_, "vtk"], [], x=xk)
                            stt("dve", S32[d_][:], S32[d_][:], dec[:, d_, i, half:half + 1], pkv, ALU.mult, ALU.add,
                                ["S32_%d" % d_, "dec"], ["S32_%d" % d_], x=xk)
                            cp("act", S16[d_][:], S32[d_][:], ["S32_%d" % d_], ["S16_%d" % d_])

                        for i in (NT, NT + 1):
                            for half in (0, 1):
                                upd(0, i, half)
                        for i in (NT + 1, NT):
                            for half in (1, 0):
                                upd(1, i, half)
                        if smp == 0 and h == 0:
                            dump("s_f", S32[0][:], ["S32_0"])
                            dump("s_b", S32[1][:], ["S32_1"])
                        if stop_after == "P2e":
                            s.barrier()
                            return nc

                        done = [0] * NT

                        def lat_front(d_, i):
                            tokc = slice(i * 128, (i + 1) * 128)
                            pa = B[d_][:, 0:128]
                            po = B[2 + d_][:, 0:256]
                            mm(pa, ke[d_][:, tokc], qe[d_][:, tokc], True, True, ["ke%d" % d_, "qe%d" % d_], [], x=[BK[d_]])
                            tt("dve", att[d_][:], pa, tri_f[:, 2 * d_, :], ALU.mult, ["tri"], ["att%d" % d_], x=[BK[d_]])
                            mm(po, att[d_][:], vtk[:, i, :], True, False, ["att%d" % d_, "vtk"], [], inc=False, x=[BK[2 + d_]])

                        def lat_half(d_, i, half, last):
                            po = B[2 + d_][:, 0:256]
                            rows = slice(64 * half, 64 * half + 64)
                            c0 = i * 128 + 64 * half
                            mm(po[rows, :], qe[d_][:, c0:c0 + 64], S16[d_][:], False, last, ["qe%d" % d_, "S16_%d" % d_],
                               [], inc=True, x=[BK[2 + d_]])
                            upd(d_, i, half)

                        def lat_back(d_, i):
                            po = B[2 + d_][:, 0:256]
                            xo = [BK[2 + d_]]
                            tokc = slice(i * 128, (i + 1) * 128)
                            if done[i] == 0:
                                cp("act", oacc[:, i, :], po, [], ["oacc%d" % i], x=xo)
                                done[i] = 1
                                return
                            u = i % 2
                            tt("dve", oacc[:, i, :], po, oacc[:, i, :], ALU.add, ["oacc%d" % i], ["oacc%d" % i], x=xo)
                            s.op("pool", lambda e, u=u: e.memset(st2[u][:], 0.0), writes=["st2_%d" % u])
                            act(junk[:], oacc[:, i, :], AF.Square, ["oacc%d" % i, "st2_%d" % u], ["junk2", "st2_%d" % u],
                                accum_out=st2[u][:, 0:1])
                            act(st2[u][:, 1:2], st2[u][:, 0:1], AF.Sqrt, ["st2_%d" % u, "eps_c"], ["st2_%d" % u],
                                scale=1.0 / 256, bias=eps_c[:, 0:1])
                            s.op("dve", lambda e, u=u: e.reciprocal(out=st2[u][:, 2:3], in_=st2[u][:, 1:2]),
                                 reads=["st2_%d" % u], writes=["st2_%d" % u])
                            stt("dve", onr[u][:], oacc[:, i, :], st2[u][:, 2:3], gnb[:], ALU.mult, ALU.mult,
                                ["oacc%d" % i, "st2_%d" % u, "gnb"], ["onr%d" % u])
                            tt("pool", rbf[u][:], onr[u][:], sgt[:, i, :], ALU.mult, ["onr%d" % u, "sgt"], ["rbf%d" % u])
                            for j in range(2):
                                tr(pT[u][:, j, :], rbf[u][:, j * 128:(j + 1) * 128], ident_b[:], ["rbf%d" % u, "ident_b"], [], x=[TK[u]])
                            cp("act", oglT[:, 2 * h:2 * h + 2, tokc], pT[u][:, 0:2, :], [], ["oglT"], x=[TK[u]])

                        for j in range(NT):
                            fi, bi_ = j, NT - 1 - j
                            lat_front(0, fi)
                            lat_front(1, bi_)
                            lat_half(0, fi, 0, False)
                            lat_half(1, bi_, 1, False)
                            lat_half(0, fi, 1, True)
                            lat_half(1, bi_, 0, True)
                            lat_back(0, fi)
                            lat_back(1, bi_)
                    if smp == 0:
                        dump("oglT", oglT[:, :, :], ["oglT"])
                    s.barrier()
                    print("P2 done: deadlock-free", s.check_deadlock(), s.n_ins, s.n_wait, s.cnt)
                if stop_after == "P2":
                    s.barrier()
                    return nc

                onaT = sb(sa, "onaT", [128, 4, S], BF16)
                with ExitStack() as st:
                    wq3 = sb(st, "wq3", [128, 8, 128], BF16)
                    wk3 = sb(st, "wk3", [128, 8, 128], BF16)
                    wv3 = sb(st, "wv3", [128, 8, 128], BF16)
                    nam = sb(st, "nam", [128, 8, 256], F32)
                    nabt = [sb(st, "nabt%d" % i, [128, 2, 256], F32) for i in range(2)]
                    BMp = sb(st, "BMp", [128, 8, 2, 256], BF16)
                    qT3 = sb(st, "qT3", [128, S], BF16)
                    kT3 = sb(st, "kT3", [128, S + CT], BF16)
                    Ve = sb(st, "Ve", [128, 16, 128], BF16)
                    Vo = sb(st, "Vo", [128, 15, 128], BF16)
                    Vc = sb(st, "Vc", [128, 2, 128], BF16)
                    PT = [sb(st, "PT%d" % i, [128, 6, 2, 64], BF16) for i in range(2)]
                    rden = [sb(st, "rden%d" % i, [128, 128], F32) for i in range(2)]
                    SB = [pst(st, "p3s%d" % i, [128, 512]) for i in range(4)]
                    SK = ["p3S%d" % i for i in range(4)]
                    OB = [pst(st, "p3o%d" % i, [128, 512]) for i in range(2)]
                    OK_ = ["p3O%d" % i for i in range(2)]
                    PB = [pst(st, "p3p%d" % i, [128, 512]) for i in range(2)]
                    PK = ["p3P%d" % i for i in range(2)]
                    s.dma("sp", nam[:], nam_d, writes=["nam"])
                    for pr in range(4):
                        s.dma("pool", wq3[:], wview(w_in_d[:, C_NAQ + 128 * pr:C_NAQ + 128 * (pr + 1)]), writes=["wq3"])
                        s.dma("pool", wk3[:], wview(w_in_d[:, C_NAK + 128 * pr:C_NAK + 128 * (pr + 1)]), writes=["wk3"])
                        s.dma("pool", wv3[:], wview(w_in_d[:, C_NAV + 128 * pr:C_NAV + 128 * (pr + 1)]), writes=["wv3"])
                        for p in range(8):
                            s.dma("sp", nabt[p % 2][:], nab_d[pr, :, p, :, :], writes=["nabt%d" % (p % 2)])
                            for hh in range(2):
                                stt("dve", BMp[:, p, hh, :], nabt[p % 2][:, hh, :], 8.0, nam[:, p, :], ALU.mult, ALU.add,
                                    ["nabt%d" % (p % 2), "nam"], ["BMp"])
                        cnt_p = [0]

                        def proj(dst, lhs_fn, rhs_fn, m, n, rk, dk):
                            u = cnt_p[0] % 2
                            cnt_p[0] += 1
                            for kt in range(8):
                                mm(PB[u][0:m, 0:n], lhs_fn(kt), rhs_fn(kt), kt == 0, kt == 7, rk, [], x=[PK[u]])
                            cp("act" if u == 0 else "dve", dst, PB[u][0:m, 0:n], [], [dk], x=[PK[u]])

                        for tb in range(4):
                            cols = slice(tb * 512, (tb + 1) * 512)
                            proj(qT3[:, cols], lambda kt: wq3[:, kt, :], lambda kt, cols=cols: aT[:, kt, cols], 128, 512, ["wq3", "aT"], "qT3")
                            proj(kT3[:, cols], lambda kt: wk3[:, kt, :], lambda kt, cols=cols: aT[:, kt, cols], 128, 512, ["wk3", "aT"], "kT3")
                        proj(kT3[:, S:S + CT], lambda kt: wk3[:, kt, :], lambda kt: acT[:, kt, :], 128, CT, ["wk3", "acT"], "kT3")
                        for i in range(16):
                            proj(Ve[:, i, :], lambda kt, i=i: aT[:, kt, i * 128:(i + 1) * 128], lambda kt: wv3[:, kt, :], 128, 128, ["wv3", "aT"], "Ve")
                        for i in range(15):
                            proj(Vo[:, i, :], lambda kt, i=i: aT[:, kt, 64 + i * 128:64 + (i + 1) * 128], lambda kt: wv3[:, kt, :], 128, 128, ["wv3", "aT"], "Vo")
                        for i in range(2):
                            proj(Vc[:, i, :], lambda kt, i=i: acT[:, kt, i * 128:(i + 1) * 128], lambda kt: wv3[:, kt, :], 128, 128, ["wv3", "acT"], "Vc")

                        def scores(r):
                            rs = min(max(r - 4, 0), 24)
                            p = r - rs
                            buf = r % 2
                            qc = slice(r * 64, r * 64 + 64)
                            for hh in range(2):
                                pr_ = slice(64 * hh, 64 * hh + 64)
                                bank = SB[buf * 2 + hh]
                                xk = [SK[buf * 2 + hh]]
                                mm(bank[:, 0:256], ident_b[:], BMp[:, p, hh, :], True, False, ["ident_b", "BMp"], [], inc=False, x=xk)
                                for j in range(4):
                                    kc0 = (rs + 2 * j) * 64
                                    mm(bank[:, j * 64:(j + 1) * 64], kT3[pr_, kc0:kc0 + 128], qT3[pr_, qc], False, j == 3,
                                       ["kT3", "qT3"], [], inc=False, x=xk)
                                for jc in range(2):
                                    mm(bank[:, 256 + jc * 64:256 + (jc + 1) * 64], kT3[pr_, S + jc * 128:S + (jc + 1) * 128], qT3[pr_, qc],
                                       True, True, ["kT3", "qT3"], [], inc=(jc == 1), x=xk)
                                act(PT[buf][:, :, hh, :], bank[:, 0:384].rearrange("p (j q) -> p j q", q=64), AF.Exp,
                                    [], ["PT%d" % buf], scale=0.125, x=xk)

                        def pv(r):
                            rs = min(max(r - 4, 0), 24)
                            buf = r % 2
                            qc = slice(r * 64, r * 64 + 64)
                            ob = OB[buf]
                            xo = [OK_[buf]]
                            for hh in range(2):
                                hs = slice(64 * hh, 64 * hh + 64)
                                for j in range(6):
                                    if j < 4:
                                        if rs % 2 == 0:
                                            Vt, vk = Ve[:, (rs + 2 * j) // 2, hs], "Ve"
                                        else:
                                            Vt, vk = Vo[:, (rs + 2 * j - 1) // 2, hs], "Vo"
                                    else:
                                        Vt, vk = Vc[:, j - 4, hs], "Vc"
                                    mm(ob[hs, 0:64], Vt, PT[buf][:, j, hh, :], j == 0, j == 5, [vk, "PT%d" % buf], [],
                                       inc=(j == 5), x=xo)
                            for j in range(6):
                                mm(ob[:, 64:192], ones_b[:, :], PT[buf][:, j, :, :].rearrange("p h q -> p (h q)"), j == 0, j == 5,
                                   ["ones_b", "PT%d" % buf], [], inc=(j == 5), x=xo)
                            s.op("dve", lambda e: e.reciprocal(out=rden[buf][:], in_=ob[:, 64:192]), reads=[], writes=["rden%d" % buf], excl=xo)
                            for hh in range(2):
                                hs = slice(64 * hh, 64 * hh + 64)
                                tt("dve", onaT[hs, pr, qc], ob[hs, 0:64], rden[buf][hs, 64 * hh:64 * hh + 64], ALU.mult,
                                   ["rden%d" % buf], ["onaT"], x=xo)

                        for r in range(32):
                            scores(r)
                            if r > 0:
                                pv(r - 1)
                        pv(31)
                    if smp == 0:
                        dump("onaT", onaT[:, :, :], ["onaT"])
                    s.barrier()
                    print("P3 done: deadlock-free", s.check_deadlock(), s.n_ins, s.n_wait, s.cnt)
                if stop_after == "P3":
                    s.barrier()
                    return nc

                UT = sb(sa, "UT", [128, 8, S], BF16)
                with ExitStack() as st:
                    wna = [sb(st, "wna%d" % i, [128, 4, 128], BF16) for i in range(2)]
                    wgl = [sb(st, "wgl%d" % i, [128, 8, 128], BF16) for i in range(2)]
                    w8 = [sb(st, "w8_%d" % i, [128, 8, 128], BF16) for i in range(2)]
                    w9 = [sb(st, "w9_%d" % i, [128, 8, 128], BF16) for i in range(2)]
                    sg8 = [sb(st, "sg8_%d" % i, [128, 512], F32) for i in range(2)]
                    sg9 = [sb(st, "sg9_%d" % i, [128, 512], F32) for i in range(2)]
                    t1m = [sb(st, "t1m%d" % i, [128, 512], F32) for i in range(2)]
                    t2m = [sb(st, "t2m%d" % i, [128, 512], F32) for i in range(2)]
                    MB = [pst(st, "p4b%d" % i, [128, 512]) for i in range(8)]
                    MK = ["p4B%d" % i for i in range(8)]
                    for nt in range(8):
                        wb = nt % 2
                        ncol = slice(nt * 128, (nt + 1) * 128)
                        s.dma("pool", wna[wb][:], wview(w_nao_d[:, ncol]), writes=["wna%d" % wb])
                        s.dma("pool", wgl[wb][:], wview(w_glo_d[:, ncol]), writes=["wgl%d" % wb])
                        s.dma("pool", w8[wb][:], wview(w_in_d[:, C_M8 + nt * 128:C_M8 + (nt + 1) * 128]), writes=["w8_%d" % wb])
                        s.dma("pool", w9[wb][:], wview(w_in_d[:, C_M9 + nt * 128:C_M9 + (nt + 1) * 128]), writes=["w9_%d" % wb])
                        for tb in range(4):
                            cols = slice(tb * 512, (tb + 1) * 512)
                            u = (nt * 4 + tb) % 2
                            bA, bG8, bB, bG9 = [MB[4 * u + q] for q in range(4)]
                            kA, kG8, kB, kG9 = [[MK[4 * u + q]] for q in range(4)]
                            for kt in range(4):
                                mm(bA[:, :], wna[wb][:, kt, :], onaT[:, kt, cols], kt == 0, kt == 3, ["wna%d" % wb, "onaT"], [], x=kA)
                            for kt in range(8):
                                mm(bG8[:, :], w8[wb][:, kt, :], aT[:, kt, cols], kt == 0, kt == 7, ["w8_%d" % wb, "aT"], [], x=kG8)
                            for kt in range(8):
                                mm(bB[:, :], wgl[wb][:, kt, :], oglT[:, kt, cols], kt == 0, kt == 7, ["wgl%d" % wb, "oglT"], [], x=kB)
                            for kt in range(8):
                                mm(bG9[:, :], w9[wb][:, kt, :], aT[:, kt, cols], kt == 0, kt == 7, ["w9_%d" % wb, "aT"], [], x=kG9)
                            act(sg8[u][:], bG8[:, :], AF.Sigmoid, [], ["sg8_%d" % u], x=kG8)
                            act(sg9[u][:], bG9[:, :], AF.Sigmoid, [], ["sg9_%d" % u], x=kG9)
                            tt("dve", t1m[u][:], bA[:, :], sg8[u][:], ALU.mult, ["sg8_%d" % u], ["t1m%d" % u], x=kA)
                            tt("dve", t2m[u][:], bB[:, :], sg9[u][:], ALU.mult, ["sg9_%d" % u], ["t2m%d" % u], x=kB)
                            tt("pool", UT[:, nt, cols], t1m[u][:], t2m[u][:], ALU.add, ["t1m%d" % u, "t2m%d" % u], ["UT"])
                    if smp == 0:
                        dump("UT", UT[:, :, 0:256], ["UT"])
                    s.barrier()
                with ExitStack() as st:
                    wo = sb(st, "wo", [128, 8, D], BF16)
                    wtmp = [sb(st, "wtmp%d" % i, [128, D], F32) for i in range(2)]
                    g1b = sb(st, "g1b", [128, D], F32)
                    xt2 = [sb(st, "xt2_%d" % i, [128, D], F32) for i in range(2)]
                    h1t = [sb(st, "h1t%d" % i, [128, D], F32) for i in range(2)]
                    MB = [pst(st, "p4c%d" % i, [128, 512]) for i in range(4)]
                    MK = ["p4C%d" % i for i in range(4)]
                    s.dma("sp", g1b[:], modv_d[smp, 2 * D:3 * D].partition_broadcast(128), writes=["g1b"])
                    for kt in range(8):
                        s.dma("sp", wtmp[kt % 2][:], w_out_d[kt * 128:(kt + 1) * 128, :], writes=["wtmp%d" % (kt % 2)])
                        tt("pool", wo[:, kt, :], wtmp[kt % 2][:], g1b[:], ALU.mult, ["wtmp%d" % (kt % 2), "g1b"], ["wo"])
                    for i in range(NT):
                        u = i % 2
                        tokc = slice(i * 128, (i + 1) * 128)
                        s.dma("sp", xt2[u][:], x_d[smp, tokc, :], writes=["xt2_%d" % u])
                        for half in range(2):
                            hcol = slice(half * 512, (half + 1) * 512)
                            bk = MB[2 * u + half]
                            xk = [MK[2 * u + half]]
                            for kt in range(8):
                                mm(bk[:, :], UT[:, kt, tokc], wo[:, kt, hcol], kt == 0, kt == 7, ["UT", "wo"], [], x=xk)
                            tt("dve", h1t[u][:, hcol], bk[:, :], xt2[u][:, hcol], ALU.add, ["xt2_%d" % u], ["h1t%d" % u], x=xk)
                        s.dma("sp", h1_d[smp, tokc, :], h1t[u][:], reads=["h1t%d" % u], writes=["h1d"])
                        if smp == 0 and i < 2 and "h1" in dbg_d:
                            dump("h1", h1t[u][:], ["h1t%d" % u], dst=dbg_d["h1"][i * 128:(i + 1) * 128, :])
                    s.barrier()
                    print("P4 done: deadlock-free", s.check_deadlock(), s.n_ins, s.n_wait, s.cnt)
                if stop_after == "P4":
                    s.barrier()
                    return nc
            CAP = MOE_CAP
            NB = CAP // 128
            NSLOT = NEXP * CAP
            Xd = nc.dram_tensor("xd_scr%d" % smp, [NSLOT, D], BF16).ap()
            Yd = nc.dram_tensor("yd_scr%d" % smp, [NSLOT, D], BF16).ap()

            ind_hist = []
            IND_DEPTH = int(os.environ.get("IND_DEPTH", "2"))

            def dma_fn(eng, fn, reads, writes):
                i_ = s.dnext
                s.dnext = (s.dnext + 1) % len(s.dsem)
                k_ = ("d", i_)
                waits = s._deps(eng, reads, writes)
                if s.dcnt[i_] > 0 and s.waited[eng].get(k_, -1) < s.dcnt[i_]:
                    s.waited[eng][k_] = s.dcnt[i_]
                    waits.append((k_, s.dcnt[i_]))
                if len(ind_hist) >= IND_DEPTH:
                    pk, pv = ind_hist[-IND_DEPTH]
                    if pk != k_ and s.waited[eng].get(pk, -1) < pv:
                        s.waited[eng][pk] = pv
                        waits.append((pk, pv))
                s.dcnt[i_] += 16
                tok = (k_, s.dcnt[i_])
                ind_hist.append(tok)
                s._emit1(eng, waits, fn, (k_, 16))
                s._commit(tok, reads, writes)

            with ExitStack() as sm:
                hres = sb(sm, "hres", [128, NT, D], F32)
                g2b = sb(sm, "g2b", [128, D], F32)
                gfb = sb(sm, "gfb", [128, D], F32)
                SLi = sb(sm, "SLi", [128, NT * 2], I32)
                w12 = sb(sm, "w12", [128, NT, 2], F32)
                s.dma("sp", g2b[:], modv_d[smp, 5 * D:6 * D].partition_broadcast(128), writes=["g2b"])
                s.dma("sp", gfb[:], g_fin_d.partition_broadcast(128), writes=["gfb"])
                with ExitStack() as st:
                    fbf = sb(st, "fbf", [128, NT, D], BF16)
                    M1a = sb(st, "M1a", [128, NT, 32], F32)
                    M2a = sb(st, "M2a", [128, NT, 32], F32)
                    Ma = sb(st, "Ma", [128, NT, 32], F32)
                    SLf = sb(st, "SLf", [128, NT, 2], F32)
                    ebase = sb(st, "ebase_sb", [128, 32], F32)
                    triS = sb(st, "triS", [128, 128], F32)
                    ones128 = sb(st, "ones128", [128, 128], F32)
                    s2b = sb(st, "s2b", [128, D], F32)
                    sh2b = sb(st, "sh2b", [128, D], F32)
                    gfn = sb(st, "gfn", [128, D], F32)
                    wrt = sb(st, "wrt", [128, 8, 36], F32)
                    brt = sb(st, "brt", [1, 36], F32)
                    ones_f = sb(st, "ones_f", [1, 128], F32)
                    xm3 = [sb(st, "xm3_%d" % i, [128, D], F32) for i in range(2)]
                    junk3 = sb(st, "junk3", [128, D], F32)
                    fT32 = [sb(st, "fT32_%d" % i, [128, 8, 128], F32) for i in range(2)]
                    R = [sb(st, "R%d" % i, [128, 96], F32) for i in range(2)]
                    Q = [sb(st, "Q%d" % i, [128, 4, 32], F32) for i in range(2)]
                    st3 = [sb(st, "st3_%d" % i, [128, 4], F32) for i in range(2)]
                    pf = [[pst(st, "p5f%d_%d" % (i, j), [128, 4, 128]) for j in range(2)] for i in range(2)]
                    pfk = [["p5F%d_%d" % (i, j) for j in range(2)] for i in range(2)]
                    pl = [pst(st, "p5l%d" % i, [128, 512]) for i in range(2)]
                    plk = ["p5L%d" % i for i in range(2)]
                    s.dma("sp", gfn[:], g_ffn_d.partition_broadcast(128), writes=["gfn"])
                    s.dma("sp", s2b[:], modv_d[smp, 4 * D:5 * D].partition_broadcast(128), writes=["s2b"])
                    stt("dve", s2b[:], s2b[:], 1.0, gfn[:], ALU.add, ALU.mult, ["s2b", "gfn"], ["s2b"])
                    s.dma("sp", sh2b[:], modv_d[smp, 3 * D:4 * D].partition_broadcast(128), writes=["sh2b"])
                    s.dma("sp", wrt[:], wview(w_rt_d), writes=["wrt"])
                    s.dma("sp", brt[:], b_rt_d.partition_broadcast(1), writes=["brt"])
                    s.dma("sp", ebase[:], ebase_d, writes=["ebase"])
                    s.dma("sp", triS[:], tri_d[4], writes=["triS"])
                    s.op("pool", lambda e: e.memset(ones_f[:], 1.0), writes=["ones_f"])
                    s.op("pool", lambda e: e.memset(ones128[:], 1.0), writes=["ones128"])
                    for i in range(NT):
                        u = i % 2
                        tokc = slice(i * 128, (i + 1) * 128)
                        hk = "hres%d" % i
                        Rk = "R%d" % u
                        Ru = R[u]
                        s.dma("sp", hres[:, i, :], h1_d[smp, tokc, :], reads=["h1d"], writes=[hk])
                        s.op("pool", lambda e, u=u: e.memset(st3[u][:], 0.0), writes=["st3_%d" % u])
                        act(junk3[:], hres[:, i, :], AF.Square, [hk, "st3_%d" % u], ["junk3", "st3_%d" % u], accum_out=st3[u][:, 0:1])
                        act(st3[u][:, 1:2], st3[u][:, 0:1], AF.Sqrt, ["st3_%d" % u, "eps_c"], ["st3_%d" % u], scale=1.0 / D, bias=eps_c[:, 0:1])
                        s.op("dve", lambda e, u=u: e.reciprocal(out=st3[u][:, 2:3], in_=st3[u][:, 1:2]), reads=["st3_%d" % u], writes=["st3_%d" % u])
                        stt("dve", xm3[u][:], hres[:, i, :], st3[u][:, 2:3], s2b[:], ALU.mult, ALU.mult, [hk, "st3_%d" % u, "s2b"], ["xm3_%d" % u])
                        tt("pool", xm3[u][:], xm3[u][:], sh2b[:], ALU.add, ["xm3_%d" % u, "sh2b"], ["xm3_%d" % u])
                        cp("pool", fbf[:, i, :], xm3[u][:], ["xm3_%d" % u], ["fbf%d" % i])
                        for kt in range(8):
                            tr(pf[u][kt // 4][:, kt % 4, :], xm3[u][:, kt * 128:(kt + 1) * 128], ident_f[:], ["xm3_%d" % u, "ident_f"], [], x=[pfk[u][kt // 4]])
                        for j in range(2):
                            cp("act" if j == 0 else "dve", fT32[u][:, 4 * j:4 * j + 4, :], pf[u][j][:, :, :], [], ["fT32_%d" % u], x=[pfk[u][j]])
                        for kt in range(8):
                            mm(pl[u][:, 0:36], fT32[u][:, kt, :], wrt[:, kt, :], kt == 0, False, ["fT32_%d" % u, "wrt"], [], inc=False, x=[plk[u]])
                        mm(pl[u][:, 0:36], ones_f[0:1, :], brt[0:1, :], False, True, ["ones_f", "brt"], [], x=[plk[u]])
                        dv = lambda fn, rd=(), wr=(), x=(): s.op("dve", fn, reads=[Rk] + list(rd), writes=[Rk] + list(wr), excl=x)
                        dv(lambda e: e.tensor_copy(out=Ru[:, 0:36], in_=pl[u][:, 0:36]), x=[plk[u]])
                        dv(lambda e: e.reduce_max(out=Ru[:, 36:37], in_=Ru[:, 0:4], axis=AX.X))
                        dv(lambda e: e.tensor_scalar(out=Ru[:, 40:44], in0=Ru[:, 0:4], scalar1=Ru[:, 36:37], scalar2=None, op0=ALU.is_equal))
                        dv(lambda e: e.tensor_scalar(out=Ru[:, 37:38], in0=Ru[:, 36:37], scalar1=-1.0, scalar2=None, op0=ALU.mult))
                        s.op("pool", lambda e: e.memset(Ru[:, 38:39], 0.0), reads=[Rk], writes=[Rk])
                        act(Ru[:, 44:48], Ru[:, 0:4], AF.Exp, [Rk], [Rk], bias=Ru[:, 37:38], accum_out=Ru[:, 38:39])
                        dv(lambda e: e.reciprocal(out=Ru[:, 39:40], in_=Ru[:, 38:39]))
                        dv(lambda e: e.tensor_scalar(out=Ru[:, 48:56], in0=Ru[:, 4:12], scalar1=Ru[:, 40:41], scalar2=None, op0=ALU.mult))
                        for g in range(1, 4):
                            dv(lambda e, g=g: e.scalar_tensor_tensor(out=Ru[:, 48:56], in0=Ru[:, 4 + 8 * g:12 + 8 * g], scalar=Ru[:, 40 + g:41 + g],
                                                                      in1=Ru[:, 48:56], op0=ALU.mult, op1=ALU.add))
                        dv(lambda e: e.reduce_max(out=Ru[:, 56:57], in_=Ru[:, 48:56], axis=AX.X))
                        dv(lambda e: e.tensor_scalar(out=Ru[:, 64:72], in0=Ru[:, 48:56], scalar1=Ru[:, 56:57], scalar2=None, op0=ALU.is_equal))
                        dv(lambda e: e.scalar_tensor_tensor(out=Ru[:, 72:80], in0=Ru[:, 64:72], scalar=-1e30, in1=Ru[:, 48:56], op0=ALU.mult, op1=ALU.add))
                        dv(lambda e: e.reduce_max(out=Ru[:, 57:58], in_=Ru[:, 72:80], axis=AX.X))
                        dv(lambda e: e.tensor_scalar(out=Ru[:, 80:88], in0=Ru[:, 72:80], scalar1=Ru[:, 57:58], scalar2=None, op0=ALU.is_equal))
                        dv(lambda e: e.tensor_tensor(out=Ru[:, 58:59], in0=Ru[:, 57:58], in1=Ru[:, 56:57], op=ALU.subtract))
                        act(Ru[:, 59:60], Ru[:, 58:59], AF.Exp, [Rk], [Rk])
                        dv(lambda e: e.tensor_scalar(out=Ru[:, 60:61], in0=Ru[:, 59:60], scalar1=1.0, scalar2=None, op0=ALU.add))
                        dv(lambda e: e.reciprocal(out=Ru[:, 61:62], in_=Ru[:, 60:61]))
                        dv(lambda e: e.tensor_tensor(out=w12[:, i, 0:1], in0=Ru[:, 61:62], in1=Ru[:, 39:40], op=ALU.mult), wr=["w12"])
                        dv(lambda e: e.tensor_tensor(out=w12[:, i, 1:2], in0=w12[:, i, 0:1], in1=Ru[:, 59:60], op=ALU.mult), rd=["w12"], wr=["w12"])
                        for g in range(4):
                            dv(lambda e, g=g: e.tensor_scalar(out=M1a[:, i, 8 * g:8 * g + 8], in0=Ru[:, 64:72], scalar1=Ru[:, 40 + g:41 + g],
                                                              scalar2=None, op0=ALU.mult), wr=["M1a"])
                            dv(lambda e, g=g: e.tensor_scalar(out=M2a[:, i, 8 * g:8 * g + 8], in0=Ru[:, 80:88], scalar1=Ru[:, 40 + g:41 + g],
                                                              scalar2=None, op0=ALU.mult), wr=["M2a"])
                        tt("pool", Ma[:, i, :], M1a[:, i, :], M2a[:, i, :], ALU.add, ["M1a", "M2a"], ["Ma"])
                    for i in range(NT):
                        u = i % 2
                        Qk = "Q%d" % u
                        Qu = Q[u]
                        mm(pl[u][:, 0:32], triS[:], Ma[:, i, :], True, i == 0, ["triS", "Ma"], [], inc=(i == 0), x=[plk[u]])
                        for i2 in range(i):
                            mm(pl[u][:, 0:32], ones128[:], Ma[:, i2, :], False, i2 == i - 1, ["ones128", "Ma"], [], inc=(i2 == i - 1), x=[plk[u]])
                        dq = lambda fn, rd=(), wr=(), x=(): s.op("dve", fn, reads=[Qk] + list(rd), writes=[Qk] + list(wr), excl=x)
                        dq(lambda e: e.tensor_copy(out=Qu[:, 0, :], in_=pl[u][:, 0:32]), x=[plk[u]])
                        dq(lambda e: e.tensor_scalar(out=Qu[:, 1, :], in0=Qu[:, 0, :], scalar1=float(CAP), scalar2=None, op0=ALU.is_lt))
                        dq(lambda e: e.tensor_tensor(out=Qu[:, 0, :], in0=Qu[:, 0, :], in1=ebase[:], op=ALU.add), rd=["ebase"])
                        for k_, Mk, mk in ((0, M1a, "M1a"), (1, M2a, "M2a")):
                            dq(lambda e, Mk=Mk: e.tensor_tensor(out=Qu[:, 2, :], in0=Mk[:, i, :], in1=Qu[:, 0, :], op=ALU.mult), rd=[mk])
                            dq(lambda e: e.reduce_sum(out=Qu[:, 3, 0:1], in_=Qu[:, 2, :], axis=AX.X))
                            dq(lambda e, Mk=Mk: e.tensor_tensor(out=Qu[:, 2, :], in0=Mk[:, i, :], in1=Qu[:, 1, :], op=ALU.mult), rd=[mk])
                            dq(lambda e: e.reduce_sum(out=Qu[:, 3, 1:2], in_=Qu[:, 2, :], axis=AX.X))
                            dq(lambda e, k_=k_: e.tensor_tensor(out=w12[:, i, k_:k_ + 1], in0=w12[:, i, k_:k_ + 1], in1=Qu[:, 3, 1:2], op=ALU.mult),
                               rd=["w12"], wr=["w12"])
                            dq(lambda e: e.tensor_scalar(out=Qu[:, 3, 2:3], in0=Qu[:, 3, 1:2], scalar1=-1.0e6, scalar2=1.0e6, op0=ALU.mult, op1=ALU.add))
                            dq(lambda e, k_=k_: e.tensor_tensor(out=SLf[:, i, k_:k_ + 1], in0=Qu[:, 3, 0:1], in1=Qu[:, 3, 2:3], op=ALU.add), wr=["SLf"])
                        s.op("dve", lambda e: e.tensor_copy(out=SLi[:, 2 * i:2 * i + 2], in_=SLf[:, i, :]), reads=["SLf"], writes=["SLi"])
                        for k_ in range(2):
                            dma_fn("pool", lambda e, k_=k_: e.indirect_dma_start(
                                out=Xd[:, :], out_offset=bass.IndirectOffsetOnAxis(ap=SLi[:, 2 * i + k_:2 * i + k_ + 1], axis=0),
                                in_=fbf[:, i, :], in_offset=None, bounds_check=bc_reg, oob_is_err=False),
                                ["SLi", "fbf%d" % i], ["Xd"])
                    if smp == 0:
                        dump("SLf", SLf[:, :, :], ["SLf"])
                        dump("w12", w12[:, :, :], ["w12"])
                    s.barrier()
                    print("P5 done: deadlock-free", s.check_deadlock(), s.n_ins, s.n_wait, s.cnt)
                if stop_after == "P5":
                    s.barrier()
                    return nc
                chunks = [(c0, min(c0 + 512, CAP)) for c0 in range(0, CAP, 512)]
                with ExitStack() as st:
                    weg = [sb(st, "weg%d" % i, [128, 8, DE], BF16) for i in range(2)]
                    weu = [sb(st, "weu%d" % i, [128, 8, DE], BF16) for i in range(2)]
                    wed = [sb(st, "wed%d" % i, [128, 4, D], BF16) for i in range(2)]
                    XeT = [sb(st, "XeT%d" % i, [128, 8, CAP], BF16) for i in range(2)]
                    hT = [sb(st, "hT%d" % i, [128, 4, CAP], BF16) for i in range(2)]
                    xe = [sb(st, "xe%d" % i, [128, D], BF16) for i in range(2)]
                    ye = [sb(st, "ye%d" % i, [128, D], BF16) for i in range(2)]
                    sgm = [sb(st, "sgm%d" % i, [128, 512], F32) for i in range(2)]
                    TX = [pst(st, "p6t%d" % i, [128, 8, 128], BF16) for i in range(2)]
                    GB = [pst(st, "p6g%d" % i, [128, 512]) for i in range(2)]
                    UB = [pst(st, "p6u%d" % i, [128, 512]) for i in range(2)]
                    YB = [pst(st, "p6y%d" % i, [128, 512]) for i in range(2)]
                    TXK = ["p6T%d" % i for i in range(2)]
                    GK = ["p6G%d" % i for i in range(2)]
                    UK = ["p6U%d" % i for i in range(2)]
                    YK = ["p6Y%d" % i for i in range(2)]
                    cnt6 = [0, 0, 0]
                    for e_ in range(NEXP):
                        wb = e_ % 2
                        s.dma("pool", weg[wb][:], wview(w_eg_d[e_]), writes=["weg%d" % wb])
                        s.dma("pool", weu[wb][:], wview(w_eu_d[e_]), writes=["weu%d" % wb])
                        s.dma("pool", wed[wb][:], wview(w_ed_d[e_]), writes=["wed%d" % wb])
                        for kt in range(4):
                            tt("pool", wed[wb][:, kt, :], wed[wb][:, kt, :], g2b[:], ALU.mult, ["wed%d" % wb, "g2b"], ["wed%d" % wb])
                        for blk in range(NB):
                            u = cnt6[0] % 2
                            cnt6[0] += 1
                            r0 = e_ * CAP + blk * 128
                            s.dma("sp", xe[u][:], Xd[r0:r0 + 128, :], writes=["xe%d" % u])
                            for kt in range(8):
                                tr(TX[u][:, kt, :], xe[u][:, kt * 128:(kt + 1) * 128], ident_b[:], ["xe%d" % u, "ident_b"], [], x=[TXK[u]])
                            cp("act" if u == 0 else "dve", XeT[wb][:, :, blk * 128:(blk + 1) * 128], TX[u][:, :, :], [], ["XeT%d" % wb], x=[TXK[u]])
                        for nt in range(4):
                            ncol = slice(nt * 128, (nt + 1) * 128)
                            for (c0, c1) in chunks:
                                u = cnt6[1] % 2
                                cnt6[1] += 1
                                n = c1 - c0
                                for kt in range(8):
                                    mm(GB[u][:, 0:n], weg[wb][:, kt, ncol], XeT[wb][:, kt, c0:c1], kt == 0, kt == 7, ["weg%d" % wb, "XeT%d" % wb], [], x=[GK[u]])
                                for kt in range(8):
                                    mm(UB[u][:, 0:n], weu[wb][:, kt, ncol], XeT[wb][:, kt, c0:c1], kt == 0, kt == 7, ["weu%d" % wb, "XeT%d" % wb], [], x=[UK[u]])
                                act(sgm[u][:, 0:n], GB[u][:, 0:n], AF.Silu, [], ["sgm%d" % u], x=[GK[u]])
                                tt("dve", hT[wb][:, nt, c0:c1], sgm[u][:, 0:n], UB[u][:, 0:n], ALU.mult, ["sgm%d" % u], ["hT%d" % wb], x=[UK[u]])
                        for blk in range(NB):
                            yu = blk % 2
                            r0 = e_ * CAP + blk * 128
                            for half in range(2):
                                u = cnt6[2] % 2
                                cnt6[2] += 1
                                hcol = slice(half * 512, (half + 1) * 512)
                                for kt in range(4):
                                    mm(YB[u][:, :], hT[wb][:, kt, blk * 128:(blk + 1) * 128], wed[wb][:, kt, hcol], kt == 0, kt == 3,
                                       ["hT%d" % wb, "wed%d" % wb], [], x=[YK[u]])
                                cp("act" if half == 0 else "dve", ye[yu][:, hcol], YB[u][:, :], [], ["ye%d" % yu], x=[YK[u]])
                            s.dma("sp", Yd[r0:r0 + 128, :], ye[yu][:], reads=["ye%d" % yu])
                    s.barrier()
                    print("P6 done: deadlock-free", s.check_deadlock(), s.n_ins, s.n_wait, s.cnt)
                with ExitStack() as st:
                    yk = [[sb(st, "yk%d_%d" % (k_, i), [128, D], BF16) for i in range(2)] for k_ in range(2)]
                    ot = [sb(st, "ot%d" % i, [128, D], F32) for i in range(2)]
                    junk4 = sb(st, "junk4", [128, D], F32)
                    st4 = [sb(st, "st4_%d" % i, [128, 4], F32) for i in range(2)]
                    for k_ in range(2):
                        for i in range(2):
                            s.op("pool", lambda e, k_=k_, i=i: e.memset(yk[k_][i][:], 0.0), writes=["yk%d_%d" % (k_, i)])
                    for i in range(NT):
                        u = i % 2
                        tokc = slice(i * 128, (i + 1) * 128)
                        hk = "hres%d" % i
                        for k_ in range(2):
                            dma_fn("pool", lambda e, k_=k_, u=u, i=i: e.indirect_dma_start(
                                out=yk[k_][u][:, :], out_offset=None, in_=Yd[:, :],
                                in_offset=bass.IndirectOffsetOnAxis(ap=SLi[:, 2 * i + k_:2 * i + k_ + 1], axis=0), bounds_check=bc_reg, oob_is_err=False),
                                ["SLi"], ["yk%d_%d" % (k_, u)])
                            stt("dve", hres[:, i, :], yk[k_][u][:], w12[:, i, k_:k_ + 1], hres[:, i, :], ALU.mult, ALU.add,
                                ["yk%d_%d" % (k_, u), "w12", hk], [hk])
                        s.op("pool", lambda e, u=u: e.memset(st4[u][:], 0.0), writes=["st4_%d" % u])
                        act(junk4[:], hres[:, i, :], AF.Square, [hk, "st4_%d" % u], ["junk4", "st4_%d" % u], accum_out=st4[u][:, 0:1])
                        act(st4[u][:, 1:2], st4[u][:, 0:1], AF.Sqrt, ["st4_%d" % u, "eps_c"], ["st4_%d" % u], scale=1.0 / D, bias=eps_c[:, 0:1])
                        s.op("dve", lambda e, u=u: e.reciprocal(out=st4[u][:, 2:3], in_=st4[u][:, 1:2]), reads=["st4_%d" % u], writes=["st4_%d" % u])
                        stt("dve", ot[u][:], hres[:, i, :], st4[u][:, 2:3], gfb[:], ALU.mult, ALU.mult, [hk, "st4_%d" % u, "gfb"], ["ot%d" % u])
                        s.dma("sp", y_d[smp, tokc, :], ot[u][:], reads=["ot%d" % u])
                    s.barrier()
        s.barrier()
    return nc


def _consts():
    ident = np.eye(128, dtype=np.float32)
    idx = np.arange(128)
    same = (idx[:, None] // 64) == (idx[None, :] // 64)
    tri = np.zeros((6, 128, 128), np.float32)
    tri[0] = same & (idx[:, None] <= idx[None, :])
    tri[1] = same & (idx[:, None] > idx[None, :])
    tri[2] = same & (idx[:, None] >= idx[None, :])
    tri[3] = same & (idx[:, None] < idx[None, :])
    tri[4] = idx[:, None] < idx[None, :]
    tri[5] = 1.0
    t = np.arange(S)
    pos_r = (t // GW).astype(np.float32)
    pos_c = (t % GW).astype(np.float32)
    inv = (10000.0 ** (-np.arange(32, dtype=np.float32) / 32)).astype(np.float32)
    ang = np.zeros((128, S), np.float32)
    ang[0:32] = inv[:, None] * pos_r[None, :]
    ang[32:64] = inv[:, None] * pos_r[None, :]
    ang[64:96] = inv[:, None] * pos_c[None, :]
    ang[96:128] = inv[:, None] * pos_c[None, :]
    cos = np.cos(ang).astype(np.float32)
    sin = np.sin(ang).astype(np.float32)
    sgn = np.ones((128, 1), np.float32)
    sgn[0:32] = -1
    sgn[64:96] = -1
    sin = sin * sgn
    perm = np.concatenate([np.arange(32, 64), np.arange(0, 32), np.arange(96, 128), np.arange(64, 96)])
    return ident, tri, cos, sin, perm


def _na_tables(rpb):
    a = (np.arange(128) // 64)[:, None, None, None]
    kc = (np.arange(128) % 64)[:, None, None, None]
    p = np.arange(8)[None, :, None, None]
    j = np.arange(4)[None, None, :, None]
    qc = np.arange(64)[None, None, None, :]
    ridx = 2 * j + a - p + 7
    cstart = np.clip(qc - 8, 0, 48)
    valid = (kc >= cstart) & (kc < cstart + 16) & (ridx >= 0) & (ridx <= 14)
    valid = np.broadcast_to(valid, (128, 8, 4, 64))
    cidx = np.clip(kc - qc + 15, 0, 30)
    ridx_c = np.clip(ridx, 0, 14)
    ridx_b = np.broadcast_to(ridx_c, (128, 8, 4, 64))
    cidx_b = np.broadcast_to(cidx, (128, 8, 4, 64))
    g = rpb[:, ridx_b, cidx_b]
    g = np.where(valid[None], g, np.float32(0.0)).astype(np.float32)
    g = g.reshape(4, 2, 128, 8, 256).transpose(0, 2, 3, 1, 4)
    mask = np.where(valid, np.float32(0.0), np.float32(MASKV)).astype(np.float32).reshape(128, 8, 256)
    return np.ascontiguousarray(g), np.ascontiguousarray(mask)


_NC_CACHE = {}


def kernel(x, c, ctx, c_ctx, w_mod, b_mod, norm_attn_g, norm_ffn_g, w_in, w_gla_a2, b_gla_a2,
           gla_norm_g, na_rpb, w_na_o, w_gla_o, w_out, w_group, b_group, w_expert, b_expert,
           w_exp_gate, w_exp_up, w_exp_down, final_norm_g):
    f = lambda a: np.ascontiguousarray(np.asarray(a, dtype=np.float32))
    x, c, ctx, c_ctx = f(x), f(c), f(ctx), f(c_ctx)
    ident, tri, cos, sin, perm = _consts()
    w_in0 = f(w_in)[0]
    gq = w_in0[:, C_GQ:C_GQ + 512].reshape(D, 4, 128)[:, :, perm].reshape(D, 512)
    gk = w_in0[:, C_GK:C_GK + 512].reshape(D, 4, 128)[:, :, perm].reshape(D, 512)
    w_rope = np.ascontiguousarray(np.concatenate([gq, gk], axis=1))
    w_a2b = np.ascontiguousarray(np.concatenate(
        [f(w_gla_a2)[0].transpose(1, 0, 2), f(b_gla_a2)[0][None]], axis=0))
    nab, nam = _na_tables(f(na_rpb)[0])
    w_rt = np.ascontiguousarray(np.concatenate([f(w_group)[0], f(w_expert)[0]], axis=1))
    b_rt = np.ascontiguousarray(np.concatenate([f(b_group)[0], f(b_expert)[0]], axis=0))
    shared = {
        "w_mod": f(w_mod)[0], "b_mod": f(b_mod)[0], "g_attn": f(norm_attn_g)[0], "g_ffn": f(norm_ffn_g)[0],
        "g_fin": f(final_norm_g), "gla_g": f(gla_norm_g)[0], "w_in": w_in0, "w_rope": w_rope, "w_a2b": w_a2b,
        "w_na_o": f(w_na_o)[0], "w_gla_o": f(w_gla_o)[0], "w_out": f(w_out)[0], "w_rt": w_rt, "b_rt": b_rt,
        "w_eg": f(w_exp_gate)[0], "w_eu": f(w_exp_up)[0], "w_ed": f(w_exp_down)[0],
        "ident": ident, "tri": tri, "rope_cos": cos, "rope_sin": sin, "na_bias": nab, "na_mask": nam,
        "ebase": np.ascontiguousarray(np.broadcast_to((np.arange(NEXP, dtype=np.float32) * MOE_CAP)[None, :], (128, NEXP))),
    }
    n = 8
    NS = x.shape[0] // n
    if "nc" not in _NC_CACHE:
        _NC_CACHE["nc"] = build_nc(NS)
    nc = _NC_CACHE["nc"]
    in_maps = []
    for i in range(n):
        m = dict(shared)
        m["x"] = x[i * NS:(i + 1) * NS]
        m["ctx"] = ctx[i * NS:(i + 1) * NS]
        m["cc"] = np.ascontiguousarray(np.concatenate([c[i * NS:(i + 1) * NS], c_ctx[None]], axis=0))
        in_maps.append(m)
    res = run_bass_kernel_spmd(nc, in_maps, core_ids=list(range(n)))
    return np.concatenate([r["y"] for r in res.results], axis=0)
```

```python
import os
import numpy as np
import concourse.bass as bass
import concourse.mybir as mybir
from concourse.bass_utils import run_bass_kernel_spmd
from contextlib import ExitStack

F32 = mybir.dt.float32
BF16 = mybir.dt.bfloat16
I32 = mybir.dt.int32
ALU = mybir.AluOpType
AF = mybir.ActivationFunctionType
AX = mybir.AxisListType

D = 1024
S = 2048
CT = 256
NT = 16
NTC = 2
NTA = NT + NTC
GW = 64
EPS = 1e-6
NEXP = 32
DE = 512
C_NAQ, C_NAK, C_NAV, C_GQ, C_GK, C_GV, C_GG, C_AL, C_M8, C_M9 = 0, 512, 1024, 1536, 2048, 2560, 3584, 4608, 4640, 5664
MASKV = -30000.0
MOE_CAP = 640


class Sched:
    ENGS = ("pe", "act", "dve", "pool", "sp")
    HND = {"pe": "tensor", "act": "scalar", "dve": "vector", "pool": "gpsimd", "sp": "sync"}

    def __init__(self, nc, es, n_dma_sems=40):
        self.nc = nc
        self.sem = {e: es.enter_context(nc.semaphore("s_" + e)) for e in self.ENGS}
        self.cnt = {e: 0 for e in self.ENGS}
        self.pending = {e: False for e in self.ENGS}
        self.waited = {e: {} for e in self.ENGS}
        self.dsem = [es.enter_context(nc.semaphore("s_dma%d" % i)) for i in range(n_dma_sems)]
        self.dcnt = [0] * n_dma_sems
        self.dnext = 0
        self.n_fg = n_dma_sems - 8
        self.bnext = 0
        self.lastw = {}
        self.reads = {}
        self.semobj = {}
        for e in self.ENGS:
            self.semobj[("e", e)] = self.sem[e]
        for i, sm in enumerate(self.dsem):
            self.semobj[("d", i)] = sm
        self.n_ins = 0
        self.n_wait = 0

    def _deps(self, eng, reads, writes, excl=()):
        toks = {}

        def add(t):
            if t is None:
                return
            k, v = t
            if toks.get(k, -1) < v:
                toks[k] = v
        for b in reads:
            add(self.lastw.get(b))
        for b in writes:
            add(self.lastw.get(b))
            for t in self.reads.get(b, ()):
                add(t)
        for b in excl:
            t = self.lastw.get(b)
            if t is not None and t[0] != ("e", eng):
                add(t)
        out = []
        for k, v in toks.items():
            if k == ("e", eng):
                if eng == "pe":
                    continue
                if v <= self.cnt[eng] - 2:
                    continue
            if self.waited[eng].get(k, -1) >= v:
                continue
            self.waited[eng][k] = v
            out.append((k, v))
        return out

    def _commit(self, tok, reads, writes):
        for b in writes:
            self.lastw[b] = tok
            self.reads[b] = []
        for b in reads:
            self.reads.setdefault(b, []).append(tok)

    def check_deadlock(self):
        pos = {e: 0 for e in self.ENGS}
        val = {}
        prog = True
        while prog:
            prog = False
            for e in self.ENGS:
                lst = self.log[e]
                while pos[e] < len(lst):
                    waits, inc = lst[pos[e]]
                    if all(val.get(k, 0) >= v for k, v in waits):
                        if inc is not None:
                            val[inc[0]] = val.get(inc[0], 0) + inc[1]
                        pos[e] += 1
                        prog = True
                    else:
                        break
        stuck = {e: (pos[e], len(self.log[e])) for e in self.ENGS if pos[e] < len(self.log[e])}
        for e in stuck:
            waits, inc = self.log[e][pos[e]]
            print("STUCK", e, pos[e], [(k, v, val.get(k, 0)) for k, v in waits])
        return not stuck

    def _emit1(self, eng, waits, fn, inc):
        if not hasattr(self, "log"):
            self.log = {e: [] for e in self.ENGS}
        self.log[eng].append((list(waits), inc if fn is not None else None))
        engh = getattr(self.nc, self.HND[eng])
        for k, v in waits:
            engh.wait_ge(self.semobj[k], v)
            self.n_wait += 1
        if fn is None:
            return
        ins = fn(engh)
        if inc is not None:
            ins.then_inc(self.semobj[inc[0]], inc[1])
        self.n_ins += 1

    def op(self, eng, fn, reads=(), writes=(), inc=True, excl=()):
        waits = self._deps(eng, reads, writes, excl)
        if inc:
            self.cnt[eng] += 1
            tok = (("e", eng), self.cnt[eng])
            self.pending[eng] = False
        else:
            tok = (("e", eng), self.cnt[eng] + 1)
            self.pending[eng] = True
        self._emit1(eng, waits, fn, (("e", eng), 1) if inc else None)
        self._commit(tok, reads, writes)
        for b_ in excl:
            self.lastw[b_] = tok
        return tok

    def dma(self, eng, out, in_, reads=(), writes=(), bg=False, **kw):
        if bg:
            i = self.n_fg + self.bnext
            self.bnext = (self.bnext + 1) % 8
        else:
            i = self.dnext
            self.dnext = (self.dnext + 1) % self.n_fg
        k = ("d", i)
        waits = self._deps(eng, reads, writes)
        if self.dcnt[i] > 0 and self.waited[eng].get(k, -1) < self.dcnt[i]:
            self.waited[eng][k] = self.dcnt[i]
            waits.append((k, self.dcnt[i]))
        self.dcnt[i] += 16
        tok = (k, self.dcnt[i])
        self._emit1(eng, waits, lambda e: e.dma_start(out=out, in_=in_, **kw), (k, 16))
        self._commit(tok, reads, writes)
        return tok

    def barrier(self, final=False):
        assert not any(self.pending.values())
        allt = [(("e", e), self.cnt[e]) for e in self.ENGS if self.cnt[e] > 0]
        allt += [(("d", i), c) for i, c in enumerate(self.dcnt) if c > 0 and (i < self.n_fg or final)]
        for e in self.ENGS:
            waits = []
            for k, v in allt:
                if k == ("e", e):
                    continue
                if self.waited[e].get(k, -1) >= v:
                    continue
                self.waited[e][k] = v
                waits.append((k, v))
            self._emit1(e, waits, None, None)
        keep = {k: t for k, t in self.lastw.items() if t[0][0] == "d" and t[0][1] >= self.n_fg}
        self.lastw = {} if final else keep
        self.reads = {}


def build_nc(NS=2, dbg=None, stop_after=None):
    nc = bass.Bass("TRN2", target_bir_lowering=False)

    def din(name, shape, dt=F32):
        return nc.dram_tensor(name, list(shape), dt, kind="ExternalInput").ap()

    x_d = din("x", [NS, S, D])
    ctx_d = din("ctx", [NS, CT, D])
    cc_d = din("cc", [NS + 1, D])
    w_mod_d = din("w_mod", [D, 6 * D])
    b_mod_d = din("b_mod", [6 * D])
    g_attn_d = din("g_attn", [D])
    g_ffn_d = din("g_ffn", [D])
    g_fin_d = din("g_fin", [D])
    gla_g_d = din("gla_g", [256])
    w_in_d = din("w_in", [D, 6688])
    w_rope_d = din("w_rope", [D, 1024])
    w_a2b_d = din("w_a2b", [17, 2, 512])
    w_nao_d = din("w_na_o", [512, D])
    w_glo_d = din("w_gla_o", [D, D])
    w_out_d = din("w_out", [D, D])
    w_rt_d = din("w_rt", [D, 36])
    b_rt_d = din("b_rt", [36])
    w_eg_d = din("w_eg", [NEXP, D, DE])
    w_eu_d = din("w_eu", [NEXP, D, DE])
    w_ed_d = din("w_ed", [NEXP, DE, D])
    ident_d = din("ident", [128, 128])
    tri_d = din("tri", [6, 128, 128])
    ebase_d = din("ebase", [128, 32])
    cos_d = din("rope_cos", [128, S])
    sin_d = din("rope_sin", [128, S])
    nab_d = din("na_bias", [4, 128, 8, 2, 256])
    nam_d = din("na_mask", [128, 8, 256])
    y_d = nc.dram_tensor("y", [NS, S, D], F32, kind="ExternalOutput").ap()
    modv_d = nc.dram_tensor("modv", [NS + 1, 6 * D], F32).ap()
    dbg_d = {}
    if dbg:
        for name, shape in dbg.items():
            dbg_d[name] = nc.dram_tensor("dbg_" + name, list(shape), F32, kind="ExternalOutput").ap()

    with ExitStack() as es:
        s = Sched(nc, es)

        used_names = {}

        def uniq(name):
            k = used_names.get(name, 0)
            used_names[name] = k + 1
            return name if k == 0 else "%s_r%d" % (name, k)

        def sb(st, name, shape, dt):
            return st.enter_context(nc.sbuf_tensor(uniq(name), list(shape), dt))

        def pst(st, name, shape, dt=F32):
            return st.enter_context(nc.psum_tensor(uniq(name), list(shape), dt))

        def mm(out, lhsT, rhs, start, stop, reads, writes, inc=None, x=()):
            if inc is None:
                inc = stop
            s.op("pe", lambda e: e.matmul(out, lhsT, rhs, start=start, stop=stop),
                 reads=reads, writes=writes, inc=inc, excl=x)

        def tr(out, in_, idn, reads, writes, x=()):
            s.op("pe", lambda e: e.transpose(out, in_, idn), reads=reads, writes=writes, excl=x)

        def act(out, in_, func, reads, writes, x=(), **kw):
            s.op("act", lambda e: e.activation(out=out, in_=in_, func=func, **kw), reads=reads, writes=writes, excl=x)

        def tt(eng, out, in0, in1, op, reads, writes, x=()):
            s.op(eng, lambda e: e.tensor_tensor(out=out, in0=in0, in1=in1, op=op), reads=reads, writes=writes, excl=x)

        def ts(eng, out, in0, s1, s2, op0, op1, reads, writes, x=()):
            if s2 is None:
                s.op(eng, lambda e: e.tensor_scalar(out=out, in0=in0, scalar1=s1, scalar2=None, op0=op0),
                     reads=reads, writes=writes, excl=x)
            else:
                s.op(eng, lambda e: e.tensor_scalar(out=out, in0=in0, scalar1=s1, scalar2=s2, op0=op0, op1=op1),
                     reads=reads, writes=writes, excl=x)

        def stt(eng, out, in0, scalar, in1, op0, op1, reads, writes, x=()):
            s.op(eng, lambda e: e.scalar_tensor_tensor(out=out, in0=in0, scalar=scalar, in1=in1, op0=op0, op1=op1),
                 reads=reads, writes=writes, excl=x)

        def cp(eng, out, in_, reads, writes, x=()):
            if eng == "act":
                s.op("act", lambda e: e.copy(out=out, in_=in_), reads=reads, writes=writes, excl=x)
            else:
                s.op(eng, lambda e: e.tensor_copy(out=out, in_=in_), reads=reads, writes=writes, excl=x)

        def dump(name, src_ap, reads, dst=None):
            if name in dbg_d:
                s.dma("pool", dbg_d[name] if dst is None else dst, src_ap, reads=reads)

        def wview(ap2d):
            return ap2d.rearrange("(kt p) n -> p kt n", p=128)

        ident_f = sb(es, "ident_f", [128, 128], F32)
        ident_b = sb(es, "ident_b", [128, 128], BF16)
        tri_f = sb(es, "tri_f", [128, 4, 128], F32)
        ones_b = sb(es, "ones_b", [128, 128], BF16)
        eps_c = sb(es, "eps_c", [128, 1], F32)
        one_c = sb(es, "one_c", [128, 1], F32)
        s.dma("sp", ident_f[:], ident_d, writes=["ident_f"])
        s.dma("pool", ident_b[:], ident_d, writes=["ident_b"])
        s.dma("sp", tri_f[:], tri_d[0:4].rearrange("m s t -> s m t"), writes=["tri"])
        s.op("pool", lambda e: e.memset(ones_b[:], 1.0), writes=["ones_b"])
        s.op("pool", lambda e: e.memset(eps_c[:], EPS), writes=["eps_c"])
        s.op("pool", lambda e: e.memset(one_c[:], 1.0), writes=["one_c"])

        with ExitStack() as st:
            ccs = sb(st, "ccs", [NS + 1, D], F32)
            scT = sb(st, "scT", [128, 8, NS + 1], F32)
            modsb = sb(st, "modsb", [NS + 1, 6 * D], F32)
            bmod = sb(st, "bmod", [NS + 1, 6 * D], F32)
            wm = [sb(st, "wm%d" % i, [128, 8, 512], F32) for i in range(2)]
            psA = pst(st, "p0a", [128, 512])
            psB = [pst(st, "p0b%d" % i, [128, 512]) for i in range(2)]
            R = NS + 1
            s.dma("sp", ccs[:], cc_d, writes=["ccs"])
            s.dma("sp", bmod[:], b_mod_d.partition_broadcast(R), writes=["bmod"])
            act(ccs[:], ccs[:], AF.Silu, ["ccs"], ["ccs"])
            for kt in range(8):
                tr(psA[:, kt * R:(kt + 1) * R], ccs[:, kt * 128:(kt + 1) * 128], ident_f[0:R, 0:R],
                   ["ccs", "ident_f"], ["p0a"])
            cp("dve", scT[:].rearrange("p k r -> p (k r)"), psA[:, 0:8 * R], ["p0a"], ["scT"])
            for cb in range(12):
                w = wm[cb % 2]
                s.dma("sp", w[:], wview(w_mod_d[:, cb * 512:(cb + 1) * 512]), writes=["wm%d" % (cb % 2)])
                ps = psB[cb % 2]
                for kt in range(8):
                    mm(ps[0:R, :], scT[:, kt, :], w[:, kt, :], kt == 0, kt == 7,
                       ["scT", "wm%d" % (cb % 2)], ["p0b%d" % (cb % 2)])
                tt("dve", modsb[:, cb * 512:(cb + 1) * 512], ps[0:R, :], bmod[:, cb * 512:(cb + 1) * 512], ALU.add,
                   ["p0b%d" % (cb % 2), "bmod"], ["modsb"])
            s.dma("sp", modv_d, modsb[:], reads=["modsb"], writes=["modv"])
            dump("modv", modsb[:], ["modsb"])
            s.barrier()
        if stop_after == "P0":
            s.barrier()
            return nc

        QSCALE = 128.0 ** -0.5
        weg_b = nc.dram_tensor("weg_bf", [NEXP, D, DE], BF16).ap()
        weu_b = nc.dram_tensor("weu_bf", [NEXP, D, DE], BF16).ap()
        wed_b = nc.dram_tensor("wed_bf", [NEXP, DE, D], BF16).ap()

        def _precast_gen():
            for e_ in range(NEXP):
                for nm, dst, srcw, r in (("g", weg_b, w_eg_d, 4), ("u", weu_b, w_eu_d, 4), ("d", wed_b, w_ed_d, 2)):
                    s.dma("pool", dst[e_].rearrange("(a r) n -> a (r n)", r=r), srcw[e_].rearrange("(a r) n -> a (r n)", r=r),
                          writes=["wb%s%d" % (nm, e_)], bg=True)
                    yield
        _pc = _precast_gen()

        def precast(n):
            for _ in range(n):
                next(_pc, None)
        bc_reg = nc.gpsimd.alloc_register("bcreg")
        nc.gpsimd.reg_mov(bc_reg, NEXP * MOE_CAP - 1)
        h1_d = nc.dram_tensor("h1_scr", [NS, S, D], F32).ap()

        def bank_set(st, pfx):
            return [pst(st, "%s%d" % (pfx, i), [128, 512]) for i in range(7)]

        for smp in range(NS):
            with ExitStack() as sa:
                aT = sb(sa, "aT", [128, 8, S], BF16)
                acT = sb(sa, "acT", [128, 8, CT], BF16)
                oglT = sb(sa, "oglT", [128, 8, S], BF16)

                with ExitStack() as st:
                    s1b = sb(st, "s1b", [128, D], F32)
                    sh1b = sb(st, "sh1b", [128, D], F32)
                    s1c = sb(st, "s1c", [128, D], F32)
                    sh1c = sb(st, "sh1c", [128, D], F32)
                    gab = sb(st, "gab", [128, D], F32)
                    tmpv = sb(st, "tmpv", [128, D], F32)
                    xt = [sb(st, "xt%d" % i, [128, D], F32) for i in range(2)]
                    xm = [sb(st, "xm%d" % i, [128, D], F32) for i in range(2)]
                    xn = [sb(st, "xn%d" % i, [128, D], BF16) for i in range(2)]
                    stat = [sb(st, "stat%d" % i, [128, 4], F32) for i in range(2)]
                    psT = [pst(st, "p1t%d" % i, [128, 8, 128], BF16) for i in range(2)]
                    s.dma("sp", gab[:], g_attn_d.partition_broadcast(128), writes=["gab"])
                    for row, s1, sh, nm in ((smp, s1b, sh1b, "l"), (NS, s1c, sh1c, "c")):
                        s.dma("sp", tmpv[:], modv_d[row, D:2 * D].partition_broadcast(128), writes=["tmpv"])
                        stt("dve", s1[:], tmpv[:], 1.0, gab[:], ALU.add, ALU.mult, ["tmpv", "gab"], ["s1" + nm])
                        s.dma("sp", sh[:], modv_d[row, 0:D].partition_broadcast(128), writes=["sh1" + nm])
                    for i in range(NTA):
                        b = i % 2
                        lat = i < NT
                        src = x_d[smp, i * 128:(i + 1) * 128, :] if lat else ctx_d[smp, (i - NT) * 128:(i - NT + 1) * 128, :]
                        nm = "l" if lat else "c"
                        s1, sh = (s1b, sh1b) if lat else (s1c, sh1c)
                        s.dma("sp", xt[b][:], src, writes=["xt%d" % b])
                        precast(2)
                        s.op("pool", lambda e, b=b: e.memset(stat[b][:], 0.0), writes=["stat%d" % b])
                        act(xm[b][:], xt[b][:], AF.Square, ["xt%d" % b, "stat%d" % b], ["xm%d" % b, "stat%d" % b],
                            accum_out=stat[b][:, 0:1])
                        act(stat[b][:, 1:2], stat[b][:, 0:1], AF.Sqrt, ["stat%d" % b, "eps_c"], ["stat%d" % b],
                            scale=1.0 / D, bias=eps_c[:, 0:1])
                        s.op("dve", lambda e, b=b: e.reciprocal(out=stat[b][:, 2:3], in_=stat[b][:, 1:2]),
                             reads=["stat%d" % b], writes=["stat%d" % b])
                        stt("dve", xm[b][:], xt[b][:], stat[b][:, 2:3], s1[:], ALU.mult, ALU.mult,
                            ["xt%d" % b, "stat%d" % b, "s1" + nm], ["xm%d" % b])
                        tt("pool", xn[b][:], xm[b][:], sh[:], ALU.add, ["xm%d" % b, "sh1" + nm], ["xn%d" % b])
                        for kt in range(8):
                            tr(psT[b][:, kt, :], xn[b][:, kt * 128:(kt + 1) * 128], ident_b[:],
                               ["xn%d" % b, "ident_b"], ["p1t%d" % b])
                        if lat:
                            cp("act", aT[:, :, i * 128:(i + 1) * 128], psT[b][:, :, :], ["p1t%d" % b], ["aT"])
                        else:
                            cp("act", acT[:, :, (i - NT) * 128:(i - NT + 1) * 128], psT[b][:, :, :], ["p1t%d" % b], ["acT"])
                    if smp == 0:
                        dump("aT", aT[:, :, 0:256], ["aT"])
                    s.barrier()
                if stop_after == "P1":
                    s.barrier()
                    return nc

                with ExitStack() as st:
                    ropeC = sb(st, "ropeC", [128, S], BF16)
                    ropeS = sb(st, "ropeS", [128, S], BF16)
                    alT = [sb(st, "alT%d" % d_, [16, S + CT], BF16) for d_ in range(2)]
                    wal = sb(st, "wal", [128, 8, 32], BF16)
                    wa2 = sb(st, "wa2", [16, 2, 512], BF16)
                    ba2 = sb(st, "ba2", [1, 2, 512], BF16)
                    gnb = sb(st, "gnb", [128, 256], F32)
                    wq = sb(st, "wq", [128, 8, 128], BF16)
                    wqs = sb(st, "wqs", [128, 8, 128], BF16)
                    wk = sb(st, "wk", [128, 8, 128], BF16)
                    wks = sb(st, "wks", [128, 8, 128], BF16)
                    wv = sb(st, "wv", [128, 8, 256], BF16)
                    wg = sb(st, "wg", [128, 8, 256], BF16)
                    qrT = sb(st, "qrT", [128, S], BF16)
                    krT = sb(st, "krT", [128, S], BF16)
                    krk = sb(st, "krk", [128, NTA, 128], BF16)
                    vtk = sb(st, "vtk", [128, NTA, 256], BF16)
                    sgt = sb(st, "sgt", [128, NT, 256], BF16)
                    qe = [sb(st, "qe%d" % d_, [128, S], BF16) for d_ in range(2)]
                    ke = [sb(st, "ke%d" % d_, [128, S], BF16) for d_ in range(2)]
                    kd = [sb(st, "kd%d" % d_, [128, NTA, 128], BF16) for d_ in range(2)]
                    dec = sb(st, "dec", [128, 2, NTA, 2], F32)
                    oacc = sb(st, "oacc", [128, NT, 256], F32)
                    S32 = [sb(st, "S32_%d" % d_, [128, 256], F32) for d_ in range(2)]
                    S16 = [sb(st, "S16_%d" % d_, [128, 256], BF16) for d_ in range(2)]
                    t1 = [sb(st, "t1_%d" % i, [128, 512], F32) for i in range(2)]
                    t2 = [sb(st, "t2_%d" % i, [128, 512], F32) for i in range(2)]
                    az = [sb(st, "az%d" % i, [128, 128], F32) for i in range(2)]
                    ex = [sb(st, "ex%d" % i, [128, 128], F32) for i in range(2)]
                    rz = [sb(st, "rz%d" % i, [128, 128], F32) for i in range(2)]
                    Lt = [sb(st, "Lt%d" % i, [128, 128], F32) for i in range(2)]
                    Dt = [sb(st, "Dt%d" % i, [128, 128], F32) for i in range(2)]
                    Di = [sb(st, "Di%d" % i, [128, 128], F32) for i in range(2)]
                    EK = [sb(st, "EK%d" % i, [128, 128], F32) for i in range(2)]
                    att = [sb(st, "att%d" % i, [128, 128], BF16) for i in range(2)]
                    junk = sb(st, "junk2", [128, 256], F32)
                    onr = [sb(st, "onr%d" % i, [128, 256], F32) for i in range(2)]
                    rbf = [sb(st, "rbf%d" % i, [128, 256], BF16) for i in range(2)]
                    st2 = [sb(st, "st2_%d" % i, [128, 4], F32) for i in range(2)]
                    B = [pst(st, "p2b%d" % i, [128, 512]) for i in range(6)]
                    BK = ["p2B%d" % i for i in range(6)]
                    pT = [pst(st, "p2t%d" % i, [128, 8, 128], BF16) for i in range(2)]
                    TK = ["p2T0", "p2T1"]

                    s.dma("pool", ropeC[:], cos_d, writes=["ropeC"])
                    s.dma("pool", ropeS[:], sin_d, writes=["ropeS"])
                    s.dma("pool", wal[:], wview(w_in_d[:, C_AL:C_AL + 32]), writes=["wal"])
                    s.dma("pool", wa2[:], w_a2b_d[0:16], writes=["wa2"])
                    s.dma("pool", ba2[:], w_a2b_d[16:17], writes=["ba2"])
                    s.dma("sp", gnb[:], gla_g_d.partition_broadcast(128), writes=["gnb"])
                    for tb in range(5):
                        if tb < 4:
                            rhs_of = lambda kt, tb=tb: aT[:, kt, tb * 512:(tb + 1) * 512]
                            n, c0, rk = 512, tb * 512, "aT"
                        else:
                            rhs_of = lambda kt: acT[:, kt, :]
                            n, c0, rk = CT, S, "acT"
                        for d_ in range(2):
                            for kt in range(8):
                                mm(B[d_][0:16, 0:n], wal[:, kt, d_ * 16:(d_ + 1) * 16], rhs_of(kt), kt == 0, kt == 7,
                                   ["wal", rk], [], x=[BK[d_]])
                            cp("act" if d_ == 0 else "dve", alT[d_][:, c0:c0 + n], B[d_][0:16, 0:n], [], ["alT%d" % d_], x=[BK[d_]])
                    if stop_after == "P2a":
                        s.barrier()
                        return nc

                    for h in range(4):
                        precast(8)
                        s.dma("pool", wq[:], wview(w_in_d[:, C_GQ + 128 * h:C_GQ + 128 * (h + 1)]), writes=["wq"])
                        s.dma("pool", wqs[:], wview(w_rope_d[:, 128 * h:128 * (h + 1)]), writes=["wqs"])
                        s.dma("pool", wk[:], wview(w_in_d[:, C_GK + 128 * h:C_GK + 128 * (h + 1)]), writes=["wk"])
                        s.dma("pool", wks[:], wview(w_rope_d[:, 512 + 128 * h:512 + 128 * (h + 1)]), writes=["wks"])
                        s.dma("pool", wv[:], wview(w_in_d[:, C_GV + 256 * h:C_GV + 256 * (h + 1)]), writes=["wv"])
                        s.dma("pool", wg[:], wview(w_in_d[:, C_GG + 256 * h:C_GG + 256 * (h + 1)]), writes=["wg"])
                        for tb in range(4):
                            cols = slice(tb * 512, (tb + 1) * 512)
                            for bi, (w, wn) in enumerate(((wq, "wq"), (wqs, "wqs"), (wk, "wk"), (wks, "wks"))):
                                for kt in range(8):
                                    mm(B[bi][:, :], w[:, kt, :], aT[:, kt, cols], kt == 0, kt == 7, [wn, "aT"], [], x=[BK[bi]])
                            stt("dve", t1[0][:], B[0][:, :], QSCALE, ropeC[:, cols], ALU.mult, ALU.mult, ["ropeC"], ["t1_0"], x=[BK[0]])
                            stt("dve", t2[0][:], B[1][:, :], QSCALE, ropeS[:, cols], ALU.mult, ALU.mult, ["ropeS"], ["t2_0"], x=[BK[1]])
                            tt("pool", qrT[:, cols], t1[0][:], t2[0][:], ALU.add, ["t1_0", "t2_0"], ["qrT"])
                            tt("dve", t1[1][:], B[2][:, :], ropeC[:, cols], ALU.mult, ["ropeC"], ["t1_1"], x=[BK[2]])
                            tt("dve", t2[1][:], B[3][:, :], ropeS[:, cols], ALU.mult, ["ropeS"], ["t2_1"], x=[BK[3]])
                            tt("pool", krT[:, cols], t1[1][:], t2[1][:], ALU.add, ["t1_1", "t2_1"], ["krT"])
                        if stop_after == "P2b":
                            s.barrier()
                            return nc
                        for i in range(NTA):
                            lat = i < NT
                            bv = 4 + (i % 2)
                            bg = 2 + (i % 2)
                            srcT, rk, c0 = (aT, "aT", i * 128) if lat else (acT, "acT", (i - NT) * 128)
                            for kt in range(8):
                                mm(B[bv][:, 0:256], srcT[:, kt, c0:c0 + 128], wv[:, kt, :], kt == 0, kt == 7, [rk, "wv"], [], x=[BK[bv]])
                            cp("act", vtk[:, i, :], B[bv][:, 0:256], [], ["vtk"], x=[BK[bv]])
                            if lat:
                                for kt in range(8):
                                    mm(B[bg][:, 0:256], srcT[:, kt, c0:c0 + 128], wg[:, kt, :], kt == 0, kt == 7, [rk, "wg"], [], x=[BK[bg]])
                                act(sgt[:, i, :], B[bg][:, 0:256], AF.Silu, [], ["sgt"], x=[BK[bg]])
                            else:
                                for kt in range(8):
                                    mm(B[bg][:, 0:128], srcT[:, kt, c0:c0 + 128], wk[:, kt, :], kt == 0, kt == 7, [rk, "wk"], [], x=[BK[bg]])
                                cp("dve", krk[:, i, :], B[bg][:, 0:128], [], ["krk"], x=[BK[bg]])
                        for i in range(NT):
                            u = i % 2
                            tr(pT[u][:, 0, :], krT[:, i * 128:(i + 1) * 128], ident_b[:], ["krT", "ident_b"], [], x=[TK[u]])
                            cp("dve", krk[:, i, :], pT[u][:, 0, :], [], ["krk"], x=[TK[u]])
                        if stop_after == "P2c":
                            s.barrier()
                            return nc
                        for i in range(NTA):
                            lat = i < NT
                            tokc = slice(i * 128, (i + 1) * 128)
                            for d_ in range(2):
                                u = d_
                                bz, bb, be = B[d_], B[2 + d_], B[4 + d_]
                                xz, xb, xe = [BK[d_]], [BK[2 + d_]], [BK[4 + d_]]
                                hc = slice(128 * h, 128 * (h + 1))
                                mm(bz[:, 0:128], alT[d_][0:16, tokc], wa2[0:16, d_, hc], True, False, ["alT%d" % d_, "wa2"], [], inc=False, x=xz)
                                mm(bz[:, 0:128], ones_b[0:1, 0:128], ba2[0:1, d_, hc], False, True, ["ones_b", "ba2"], [], x=xz)
                                act(az[u][:], bz[:, 0:128], AF.Abs, [], ["az%d" % u], x=xz)
                                ts("dve", rz[u][:], bz[:, 0:128], -1.0, 0.0, ALU.mult, ALU.max, [], ["rz%d" % u], x=xz)
                                act(ex[u][:], az[u][:], AF.Exp, ["az%d" % u], ["ex%d" % u], scale=-1.0)
                                act(ex[u][:], ex[u][:], AF.Ln, ["ex%d" % u, "one_c"], ["ex%d" % u], bias=one_c[:, 0:1])
                                tt("pool", Lt[u][:], rz[u][:], ex[u][:], ALU.add, ["rz%d" % u, "ex%d" % u], ["Lt%d" % u])
                                mm(bb[:, 0:128], Lt[u][:], tri_f[:, 2 * d_, :], True, True, ["Lt%d" % u, "tri"], [], x=xb)
                                mm(be[:, 0:128], tri_f[:, 2 * d_ + 1, :], Lt[u][:], True, True, ["Lt%d" % u, "tri"], [], x=xe)
                                act(Dt[u][:], bb[:, 0:128], AF.Exp, [], ["Dt%d" % u], scale=-1.0 / 16, x=xb)
                                if lat:
                                    act(Di[u][:], bb[:, 0:128], AF.Exp, [], ["Di%d" % u], scale=1.0 / 16, x=xb)
                                act(EK[u][:], be[:, 0:128], AF.Exp, [], ["EK%d" % u], scale=-1.0 / 16, x=xe)
                                if d_ == 0:
                                    dsrc = Dt[u][:, 63:128:64]
                                else:
                                    dsrc = Dt[u][:, 0:128:64]
                                cp("pool", dec[:, d_, i, :], dsrc, ["Dt%d" % u], ["dec"])
                                tt("pool", kd[d_][:, i, :], krk[:, i, :], EK[u][:], ALU.mult, ["krk", "EK%d" % u], ["kd%d" % d_])
                                if lat:
                                    tt("dve", qe[d_][:, tokc], qrT[:, tokc], Dt[u][:], ALU.mult, ["qrT", "Dt%d" % u], ["qe%d" % d_])
                                    tt("dve", ke[d_][:, tokc], krT[:, tokc], Di[u][:], ALU.mult, ["krT", "Di%d" % u], ["ke%d" % d_])
                        if stop_after == "P2d":
                            s.barrier()
                            return nc
                        for d_ in range(2):
                            s.op("pool", lambda e, d_=d_: e.memset(S32[d_][:], 0.0), writes=["S32_%d" % d_])
                            s.op("pool", lambda e, d_=d_: e.memset(S16[d_][:], 0.0), writes=["S16_%d" % d_])

                        def upd(d_, i, half):
                            rows = slice(64 * half, 64 * half + 64)
                            pkv = B[4 + d_][:, 0:256]
                            xk = [BK[4 + d_]]
                            mm(pkv, kd[d_][rows, i, :], vtk[rows, i, :], True, True, ["kd%d" % d_, "vtk"], [], x=xk)
                            stt("dve", S16[d_][:], S32[d_][:], dec[:, d_, i, half:half + 1], pkv, ALU.mult, ALU.add,
                                ["S32_%d" % d_, "dec"], ["S16_%d" % d_], x=xk)
                            stt("dve", S32[d_][:], S32[d_][:], dec[:, d_, i, half:half + 1], pkv, ALU.mult, ALU.add,
                                ["S32_%d" % d_, "dec"], ["S32_%d" % d_], x=xk)

                        for i in (NT, NT + 1):
                            for half in (0, 1):
                                upd(0, i, half)
                        for i in (NT + 1, NT):
                            for half in (1, 0):
                                upd(1, i, half)
                        if smp == 0 and h == 0:
                            dump("s_f", S32[0][:], ["S32_0"])
                            dump("s_b", S32[1][:], ["S32_1"])
                        if stop_after == "P2e":
                            s.barrier()
                            return nc

                        done = [0] * NT

                        def lat_front(d_, i):
                            tokc = slice(i * 128, (i + 1) * 128)
                            pa = B[d_][:, 0:128]
                            po = B[2 + d_][:, 0:256]
                            mm(pa, ke[d_][:, tokc], qe[d_][:, tokc], True, True, ["ke%d" % d_, "qe%d" % d_], [], x=[BK[d_]])
                            tt("dve", att[d_][:], pa, tri_f[:, 2 * d_, :], ALU.mult, ["tri"], ["att%d" % d_], x=[BK[d_]])
                            mm(po, att[d_][:], vtk[:, i, :], True, False, ["att%d" % d_, "vtk"], [], inc=False, x=[BK[2 + d_]])

                        def lat_half(d_, i, half, last):
                            po = B[2 + d_][:, 0:256]
                            rows = slice(64 * half, 64 * half + 64)
                            c0 = i * 128 + 64 * half
                            mm(po[rows, :], qe[d_][:, c0:c0 + 64], S16[d_][:], False, last, ["qe%d" % d_, "S16_%d" % d_],
                               [], inc=True, x=[BK[2 + d_]])
                            upd(d_, i, half)

                        def lat_back(d_, i):
                            po = B[2 + d_][:, 0:256]
                            xo = [BK[2 + d_]]
                            tokc = slice(i * 128, (i + 1) * 128)
                            if done[i] == 0:
                                cp("act", oacc[:, i, :], po, [], ["oacc%d" % i], x=xo)
                                done[i] = 1
                                return
                            u = i % 2
                            tt("dve", oacc[:, i, :], po, oacc[:, i, :], ALU.add, ["oacc%d" % i], ["oacc%d" % i], x=xo)
                            s.op("pool", lambda e, u=u: e.memset(st2[u][:], 0.0), writes=["st2_%d" % u])
                            act(junk[:], oacc[:, i, :], AF.Square, ["oacc%d" % i, "st2_%d" % u], ["junk2", "st2_%d" % u],
                                accum_out=st2[u][:, 0:1])
                            act(st2[u][:, 1:2], st2[u][:, 0:1], AF.Sqrt, ["st2_%d" % u, "eps_c"], ["st2_%d" % u],
                                scale=1.0 / 256, bias=eps_c[:, 0:1])
                            s.op("dve", lambda e, u=u: e.reciprocal(out=st2[u][:, 2:3], in_=st2[u][:, 1:2]),
                                 reads=["st2_%d" % u], writes=["st2_%d" % u])
                            stt("dve", onr[u][:], oacc[:, i, :], st2[u][:, 2:3], gnb[:], ALU.mult, ALU.mult,
                                ["oacc%d" % i, "st2_%d" % u, "gnb"], ["onr%d" % u])
                            tt("pool", rbf[u][:], onr[u][:], sgt[:, i, :], ALU.mult, ["onr%d" % u, "sgt"], ["rbf%d" % u])
                            for j in range(2):
                                tr(pT[u][:, j, :], rbf[u][:, j * 128:(j + 1) * 128], ident_b[:], ["rbf%d" % u, "ident_b"], [], x=[TK[u]])
                            cp("act", oglT[:, 2 * h:2 * h + 2, tokc], pT[u][:, 0:2, :], [], ["oglT"], x=[TK[u]])

                        for j in range(NT):
                            fi, bi_ = j, NT - 1 - j
                            lat_front(0, fi)
                            lat_front(1, bi_)
                            lat_half(0, fi, 0, False)
                            lat_half(1, bi_, 1, False)
                            lat_half(0, fi, 1, True)
                            lat_half(1, bi_, 0, True)
                            lat_back(0, fi)
                            lat_back(1, bi_)
                    if smp == 0:
                        dump("oglT", oglT[:, :, :], ["oglT"])
                    s.barrier()
                    print("P2 done: deadlock-free", s.check_deadlock(), s.n_ins, s.n_wait, s.cnt)
                if stop_after == "P2":
                    s.barrier()
                    return nc

                onaT = sb(sa, "onaT", [128, 4, S], BF16)
                with ExitStack() as st:
                    wq3 = sb(st, "wq3", [128, 8, 128], BF16)
                    wk3 = sb(st, "wk3", [128, 8, 128], BF16)
                    wv3 = sb(st, "wv3", [128, 8, 128], BF16)
                    nam = sb(st, "nam", [128, 8, 256], F32)
                    nabt = [sb(st, "nabt%d" % i, [128, 2, 256], F32) for i in range(2)]
                    BMp = sb(st, "BMp", [128, 8, 2, 256], BF16)
                    qT3 = sb(st, "qT3", [128, S], BF16)
                    kT3 = sb(st, "kT3", [128, S + CT], BF16)
                    Ve = sb(st, "Ve", [128, 16, 128], BF16)
                    Vo = sb(st, "Vo", [128, 15, 128], BF16)
                    Vc = sb(st, "Vc", [128, 2, 128], BF16)
                    PT = [sb(st, "PT%d" % i, [128, 6, 2, 64], BF16) for i in range(2)]
                    rden = [sb(st, "rden%d" % i, [128, 128], F32) for i in range(2)]
                    SB = [pst(st, "p3s%d" % i, [128, 512]) for i in range(4)]
                    SK = ["p3S%d" % i for i in range(4)]
                    OB = [pst(st, "p3o%d" % i, [128, 512]) for i in range(2)]
                    OK_ = ["p3O%d" % i for i in range(2)]
                    PB = [pst(st, "p3p%d" % i, [128, 512]) for i in range(2)]
                    PK = ["p3P%d" % i for i in range(2)]
                    s.dma("sp", nam[:], nam_d, writes=["nam"])
                    for pr in range(4):
                        precast(8)
                        s.dma("pool", wq3[:], wview(w_in_d[:, C_NAQ + 128 * pr:C_NAQ + 128 * (pr + 1)]), writes=["wq3"])
                        s.dma("pool", wk3[:], wview(w_in_d[:, C_NAK + 128 * pr:C_NAK + 128 * (pr + 1)]), writes=["wk3"])
                        s.dma("pool", wv3[:], wview(w_in_d[:, C_NAV + 128 * pr:C_NAV + 128 * (pr + 1)]), writes=["wv3"])
                        for p in range(8):
                            s.dma("sp", nabt[p % 2][:], nab_d[pr, :, p, :, :], writes=["nabt%d" % (p % 2)])
                            for hh in range(2):
                                stt("dve", BMp[:, p, hh, :], nabt[p % 2][:, hh, :], 8.0, nam[:, p, :], ALU.mult, ALU.add,
                                    ["nabt%d" % (p % 2), "nam"], ["BMp"])
                        cnt_p = [0]

                        def proj(dst, lhs_fn, rhs_fn, m, n, rk, dk):
                            u = cnt_p[0] % 2
                            cnt_p[0] += 1
                            for kt in range(8):
                                mm(PB[u][0:m, 0:n], lhs_fn(kt), rhs_fn(kt), kt == 0, kt == 7, rk, [], x=[PK[u]])
                            cp("act" if u == 0 else "dve", dst, PB[u][0:m, 0:n], [], [dk], x=[PK[u]])

                        for tb in range(4):
                            cols = slice(tb * 512, (tb + 1) * 512)
                            proj(qT3[:, cols], lambda kt: wq3[:, kt, :], lambda kt, cols=cols: aT[:, kt, cols], 128, 512, ["wq3", "aT"], "qT3")
                            proj(kT3[:, cols], lambda kt: wk3[:, kt, :], lambda kt, cols=cols: aT[:, kt, cols], 128, 512, ["wk3", "aT"], "kT3")
                        proj(kT3[:, S:S + CT], lambda kt: wk3[:, kt, :], lambda kt: acT[:, kt, :], 128, CT, ["wk3", "acT"], "kT3")
                        for i in range(16):
                            proj(Ve[:, i, :], lambda kt, i=i: aT[:, kt, i * 128:(i + 1) * 128], lambda kt: wv3[:, kt, :], 128, 128, ["wv3", "aT"], "Ve")
                        for i in range(15):
                            proj(Vo[:, i, :], lambda kt, i=i: aT[:, kt, 64 + i * 128:64 + (i + 1) * 128], lambda kt: wv3[:, kt, :], 128, 128, ["wv3", "aT"], "Vo")
                        for i in range(2):
                            proj(Vc[:, i, :], lambda kt, i=i: acT[:, kt, i * 128:(i + 1) * 128], lambda kt: wv3[:, kt, :], 128, 128, ["wv3", "acT"], "Vc")

                        def scores(r):
                            rs = min(max(r - 4, 0), 24)
                            p = r - rs
                            buf = r % 2
                            qc = slice(r * 64, r * 64 + 64)
                            for hh in range(2):
                                pr_ = slice(64 * hh, 64 * hh + 64)
                                bank = SB[buf * 2 + hh]
                                xk = [SK[buf * 2 + hh]]
                                mm(bank[:, 0:256], ident_b[:], BMp[:, p, hh, :], True, False, ["ident_b", "BMp"], [], inc=False, x=xk)
                                for j in range(4):
                                    kc0 = (rs + 2 * j) * 64
                                    mm(bank[:, j * 64:(j + 1) * 64], kT3[pr_, kc0:kc0 + 128], qT3[pr_, qc], False, j == 3,
                                       ["kT3", "qT3"], [], inc=False, x=xk)
                                for jc in range(2):
                                    mm(bank[:, 256 + jc * 64:256 + (jc + 1) * 64], kT3[pr_, S + jc * 128:S + (jc + 1) * 128], qT3[pr_, qc],
                                       True, True, ["kT3", "qT3"], [], inc=(jc == 1), x=xk)
                                act(PT[buf][:, :, hh, :], bank[:, 0:384].rearrange("p (j q) -> p j q", q=64), AF.Exp,
                                    [], ["PT%d" % buf], scale=0.125, x=xk)

                        def pv(r):
                            rs = min(max(r - 4, 0), 24)
                            buf = r % 2
                            qc = slice(r * 64, r * 64 + 64)
                            ob = OB[buf]
                            xo = [OK_[buf]]
                            for hh in range(2):
                                hs = slice(64 * hh, 64 * hh + 64)
                                for j in range(6):
                                    if j < 4:
                                        if rs % 2 == 0:
                                            Vt, vk = Ve[:, (rs + 2 * j) // 2, hs], "Ve"
                                        else:
                                            Vt, vk = Vo[:, (rs + 2 * j - 1) // 2, hs], "Vo"
                                    else:
                                        Vt, vk = Vc[:, j - 4, hs], "Vc"
                                    mm(ob[hs, 0:64], Vt, PT[buf][:, j, hh, :], j == 0, j == 5, [vk, "PT%d" % buf], [],
                                       inc=(j == 5), x=xo)
                            for j in range(6):
                                mm(ob[:, 64:192], ones_b[:, :], PT[buf][:, j, :, :].rearrange("p h q -> p (h q)"), j == 0, j == 5,
                                   ["ones_b", "PT%d" % buf], [], inc=(j == 5), x=xo)
                            s.op("dve", lambda e: e.reciprocal(out=rden[buf][:], in_=ob[:, 64:192]), reads=[], writes=["rden%d" % buf], excl=xo)
                            for hh in range(2):
                                hs = slice(64 * hh, 64 * hh + 64)
                                tt("dve", onaT[hs, pr, qc], ob[hs, 0:64], rden[buf][hs, 64 * hh:64 * hh + 64], ALU.mult,
                                   ["rden%d" % buf], ["onaT"], x=xo)

                        for r in range(32):
                            scores(r)
                            if r > 0:
                                pv(r - 1)
                        pv(31)
                    if smp == 0:
                        dump("onaT", onaT[:, :, :], ["onaT"])
                    s.barrier()
                    print("P3 done: deadlock-free", s.check_deadlock(), s.n_ins, s.n_wait, s.cnt)
                if stop_after == "P3":
                    s.barrier()
                    return nc

                UT = sb(sa, "UT", [128, 8, S], BF16)
                with ExitStack() as st:
                    wna = [sb(st, "wna%d" % i, [128, 4, 128], BF16) for i in range(2)]
                    wgl = [sb(st, "wgl%d" % i, [128, 8, 128], BF16) for i in range(2)]
                    w8 = [sb(st, "w8_%d" % i, [128, 8, 128], BF16) for i in range(2)]
                    w9 = [sb(st, "w9_%d" % i, [128, 8, 128], BF16) for i in range(2)]
                    sg8 = [sb(st, "sg8_%d" % i, [128, 512], F32) for i in range(2)]
                    sg9 = [sb(st, "sg9_%d" % i, [128, 512], F32) for i in range(2)]
                    t1m = [sb(st, "t1m%d" % i, [128, 512], F32) for i in range(2)]
                    t2m = [sb(st, "t2m%d" % i, [128, 512], F32) for i in range(2)]
                    MB = [pst(st, "p4b%d" % i, [128, 512]) for i in range(8)]
                    MK = ["p4B%d" % i for i in range(8)]
                    for nt in range(8):
                        wb = nt % 2
                        ncol = slice(nt * 128, (nt + 1) * 128)
                        s.dma("pool", wna[wb][:], wview(w_nao_d[:, ncol]), writes=["wna%d" % wb])
                        s.dma("pool", wgl[wb][:], wview(w_glo_d[:, ncol]), writes=["wgl%d" % wb])
                        s.dma("pool", w8[wb][:], wview(w_in_d[:, C_M8 + nt * 128:C_M8 + (nt + 1) * 128]), writes=["w8_%d" % wb])
                        s.dma("pool", w9[wb][:], wview(w_in_d[:, C_M9 + nt * 128:C_M9 + (nt + 1) * 128]), writes=["w9_%d" % wb])
                        for tb in range(4):
                            cols = slice(tb * 512, (tb + 1) * 512)
                            u = (nt * 4 + tb) % 2
                            bA, bG8, bB, bG9 = [MB[4 * u + q] for q in range(4)]
                            kA, kG8, kB, kG9 = [[MK[4 * u + q]] for q in range(4)]
                            for kt in range(4):
                                mm(bA[:, :], wna[wb][:, kt, :], onaT[:, kt, cols], kt == 0, kt == 3, ["wna%d" % wb, "onaT"], [], x=kA)
                            for kt in range(8):
                                mm(bG8[:, :], w8[wb][:, kt, :], aT[:, kt, cols], kt == 0, kt == 7, ["w8_%d" % wb, "aT"], [], x=kG8)
                            for kt in range(8):
                                mm(bB[:, :], wgl[wb][:, kt, :], oglT[:, kt, cols], kt == 0, kt == 7, ["wgl%d" % wb, "oglT"], [], x=kB)
                            for kt in range(8):
                                mm(bG9[:, :], w9[wb][:, kt, :], aT[:, kt, cols], kt == 0, kt == 7, ["w9_%d" % wb, "aT"], [], x=kG9)
                            act(sg8[u][:], bG8[:, :], AF.Sigmoid, [], ["sg8_%d" % u], x=kG8)
                            act(sg9[u][:], bG9[:, :], AF.Sigmoid, [], ["sg9_%d" % u], x=kG9)
                            tt("dve", t1m[u][:], bA[:, :], sg8[u][:], ALU.mult, ["sg8_%d" % u], ["t1m%d" % u], x=kA)
                            tt("dve", t2m[u][:], bB[:, :], sg9[u][:], ALU.mult, ["sg9_%d" % u], ["t2m%d" % u], x=kB)
                            tt("pool", UT[:, nt, cols], t1m[u][:], t2m[u][:], ALU.add, ["t1m%d" % u, "t2m%d" % u], ["UT"])
                    if smp == 0:
                        dump("UT", UT[:, :, 0:256], ["UT"])
                    s.barrier()
                with ExitStack() as st:
                    wo = sb(st, "wo", [128, 8, D], BF16)
                    wtmp = [sb(st, "wtmp%d" % i, [128, D], F32) for i in range(2)]
                    g1b = sb(st, "g1b", [128, D], F32)
                    xt2 = [sb(st, "xt2_%d" % i, [128, D], F32) for i in range(2)]
                    h1t = [sb(st, "h1t%d" % i, [128, D], F32) for i in range(2)]
                    MB = [pst(st, "p4c%d" % i, [128, 512]) for i in range(4)]
                    MK = ["p4C%d" % i for i in range(4)]
                    s.dma("sp", g1b[:], modv_d[smp, 2 * D:3 * D].partition_broadcast(128), writes=["g1b"])
                    for kt in range(8):
                        s.dma("sp", wtmp[kt % 2][:], w_out_d[kt * 128:(kt + 1) * 128, :], writes=["wtmp%d" % (kt % 2)])
                        tt("pool", wo[:, kt, :], wtmp[kt % 2][:], g1b[:], ALU.mult, ["wtmp%d" % (kt % 2), "g1b"], ["wo"])
                    for i in range(NT):
                        u = i % 2
                        tokc = slice(i * 128, (i + 1) * 128)
                        s.dma("sp", xt2[u][:], x_d[smp, tokc, :], writes=["xt2_%d" % u])
                        for half in range(2):
                            hcol = slice(half * 512, (half + 1) * 512)
                            bk = MB[2 * u + half]
                            xk = [MK[2 * u + half]]
                            for kt in range(8):
                                mm(bk[:, :], UT[:, kt, tokc], wo[:, kt, hcol], kt == 0, kt == 7, ["UT", "wo"], [], x=xk)
                            tt("dve", h1t[u][:, hcol], bk[:, :], xt2[u][:, hcol], ALU.add, ["xt2_%d" % u], ["h1t%d" % u], x=xk)
                        s.dma("sp", h1_d[smp, tokc, :], h1t[u][:], reads=["h1t%d" % u], writes=["h1d"])
                        if smp == 0 and i < 2 and "h1" in dbg_d:
                            dump("h1", h1t[u][:], ["h1t%d" % u], dst=dbg_d["h1"][i * 128:(i + 1) * 128, :])
                    s.barrier()
                    print("P4 done: deadlock-free", s.check_deadlock(), s.n_ins, s.n_wait, s.cnt)
                if stop_after == "P4":
                    s.barrier()
                    return nc
            CAP = MOE_CAP
            NB = CAP // 128
            NSLOT = NEXP * CAP
            Xd = nc.dram_tensor("xd_scr%d" % smp, [NSLOT, D], BF16).ap()
            Yd = nc.dram_tensor("yd_scr%d" % smp, [NSLOT, D], BF16).ap()

            ind_hist = []
            IND_DEPTH = int(os.environ.get("IND_DEPTH", "1000"))

            def dma_fn(eng, fn, reads, writes):
                i_ = s.dnext
                s.dnext = (s.dnext + 1) % s.n_fg
                k_ = ("d", i_)
                waits = s._deps(eng, reads, writes)
                if s.dcnt[i_] > 0 and s.waited[eng].get(k_, -1) < s.dcnt[i_]:
                    s.waited[eng][k_] = s.dcnt[i_]
                    waits.append((k_, s.dcnt[i_]))
                if len(ind_hist) >= IND_DEPTH:
                    pk, pv = ind_hist[-IND_DEPTH]
                    if pk != k_ and s.waited[eng].get(pk, -1) < pv:
                        s.waited[eng][pk] = pv
                        waits.append((pk, pv))
                s.dcnt[i_] += 16
                tok = (k_, s.dcnt[i_])
                ind_hist.append(tok)
                s._emit1(eng, waits, fn, (k_, 16))
                s._commit(tok, reads, writes)

            with ExitStack() as sm:
                hres = sb(sm, "hres", [128, NT, D], F32)
                g2b = sb(sm, "g2b", [128, D], F32)
                gfb = sb(sm, "gfb", [128, D], F32)
                SLi = sb(sm, "SLi", [128, NT * 2], I32)
                w12 = sb(sm, "w12", [128, NT, 2], F32)
                s.dma("sp", g2b[:], modv_d[smp, 5 * D:6 * D].partition_broadcast(128), writes=["g2b"])
                s.dma("sp", gfb[:], g_fin_d.partition_broadcast(128), writes=["gfb"])
                with ExitStack() as st:
                    fbf = sb(st, "fbf", [128, NT, D], BF16)
                    M1a = sb(st, "M1a", [128, NT, 32], F32)
                    M2a = sb(st, "M2a", [128, NT, 32], F32)
                    Ma = sb(st, "Ma", [128, NT, 32], F32)
                    SLf = sb(st, "SLf", [128, NT, 2], F32)
                    ebase = sb(st, "ebase_sb", [128, 32], F32)
                    triS = sb(st, "triS", [128, 128], F32)
                    ones128 = sb(st, "ones128", [128, 128], F32)
                    s2b = sb(st, "s2b", [128, D], F32)
                    sh2b = sb(st, "sh2b", [128, D], F32)
                    gfn = sb(st, "gfn", [128, D], F32)
                    wrt = sb(st, "wrt", [128, 8, 36], F32)
                    brt = sb(st, "brt", [1, 36], F32)
                    ones_f = sb(st, "ones_f", [1, 128], F32)
                    xm3 = [sb(st, "xm3_%d" % i, [128, D], F32) for i in range(2)]
                    junk3 = sb(st, "junk3", [128, D], F32)
                    fT32 = [sb(st, "fT32_%d" % i, [128, 8, 128], F32) for i in range(2)]
                    R = [sb(st, "R%d" % i, [128, 96], F32) for i in range(2)]
                    Q = [sb(st, "Q%d" % i, [128, 4, 32], F32) for i in range(2)]
                    st3 = [sb(st, "st3_%d" % i, [128, 4], F32) for i in range(2)]
                    pf = [[pst(st, "p5f%d_%d" % (i, j), [128, 4, 128]) for j in range(2)] for i in range(2)]
                    pfk = [["p5F%d_%d" % (i, j) for j in range(2)] for i in range(2)]
                    pl = [pst(st, "p5l%d" % i, [128, 512]) for i in range(2)]
                    plk = ["p5L%d" % i for i in range(2)]
                    s.dma("sp", gfn[:], g_ffn_d.partition_broadcast(128), writes=["gfn"])
                    s.dma("sp", s2b[:], modv_d[smp, 4 * D:5 * D].partition_broadcast(128), writes=["s2b"])
                    stt("dve", s2b[:], s2b[:], 1.0, gfn[:], ALU.add, ALU.mult, ["s2b", "gfn"], ["s2b"])
                    s.dma("sp", sh2b[:], modv_d[smp, 3 * D:4 * D].partition_broadcast(128), writes=["sh2b"])
                    s.dma("sp", wrt[:], wview(w_rt_d), writes=["wrt"])
                    s.dma("sp", brt[:], b_rt_d.partition_broadcast(1), writes=["brt"])
                    s.dma("sp", ebase[:], ebase_d, writes=["ebase"])
                    s.dma("sp", triS[:], tri_d[4], writes=["triS"])
                    s.op("pool", lambda e: e.memset(ones_f[:], 1.0), writes=["ones_f"])
                    s.op("pool", lambda e: e.memset(ones128[:], 1.0), writes=["ones128"])
                    for i in range(NT):
                        u = i % 2
                        tokc = slice(i * 128, (i + 1) * 128)
                        hk = "hres%d" % i
                        Rk = "R%d" % u
                        Ru = R[u]
                        s.dma("sp", hres[:, i, :], h1_d[smp, tokc, :], reads=["h1d"], writes=[hk])
                        s.op("pool", lambda e, u=u: e.memset(st3[u][:], 0.0), writes=["st3_%d" % u])
                        act(junk3[:], hres[:, i, :], AF.Square, [hk, "st3_%d" % u], ["junk3", "st3_%d" % u], accum_out=st3[u][:, 0:1])
                        act(st3[u][:, 1:2], st3[u][:, 0:1], AF.Sqrt, ["st3_%d" % u, "eps_c"], ["st3_%d" % u], scale=1.0 / D, bias=eps_c[:, 0:1])
                        s.op("dve", lambda e, u=u: e.reciprocal(out=st3[u][:, 2:3], in_=st3[u][:, 1:2]), reads=["st3_%d" % u], writes=["st3_%d" % u])
                        stt("dve", xm3[u][:], hres[:, i, :], st3[u][:, 2:3], s2b[:], ALU.mult, ALU.mult, [hk, "st3_%d" % u, "s2b"], ["xm3_%d" % u])
                        tt("pool", xm3[u][:], xm3[u][:], sh2b[:], ALU.add, ["xm3_%d" % u, "sh2b"], ["xm3_%d" % u])
                        cp("pool", fbf[:, i, :], xm3[u][:], ["xm3_%d" % u], ["fbf%d" % i])
                        for kt in range(8):
                            tr(pf[u][kt // 4][:, kt % 4, :], xm3[u][:, kt * 128:(kt + 1) * 128], ident_f[:], ["xm3_%d" % u, "ident_f"], [], x=[pfk[u][kt // 4]])
                        for j in range(2):
                            cp("act" if j == 0 else "dve", fT32[u][:, 4 * j:4 * j + 4, :], pf[u][j][:, :, :], [], ["fT32_%d" % u], x=[pfk[u][j]])
                        for kt in range(8):
                            mm(pl[u][:, 0:36], fT32[u][:, kt, :], wrt[:, kt, :], kt == 0, False, ["fT32_%d" % u, "wrt"], [], inc=False, x=[plk[u]])
                        mm(pl[u][:, 0:36], ones_f[0:1, :], brt[0:1, :], False, True, ["ones_f", "brt"], [], x=[plk[u]])
                        dv = lambda fn, rd=(), wr=(), x=(): s.op("dve", fn, reads=[Rk] + list(rd), writes=[Rk] + list(wr), excl=x)
                        dv(lambda e: e.tensor_copy(out=Ru[:, 0:36], in_=pl[u][:, 0:36]), x=[plk[u]])
                        dv(lambda e: e.reduce_max(out=Ru[:, 36:37], in_=Ru[:, 0:4], axis=AX.X))
                        dv(lambda e: e.tensor_scalar(out=Ru[:, 40:44], in0=Ru[:, 0:4], scalar1=Ru[:, 36:37], scalar2=None, op0=ALU.is_equal))
                        dv(lambda e: e.tensor_scalar(out=Ru[:, 37:38], in0=Ru[:, 36:37], scalar1=-1.0, scalar2=None, op0=ALU.mult))
                        s.op("pool", lambda e: e.memset(Ru[:, 38:39], 0.0), reads=[Rk], writes=[Rk])
                        act(Ru[:, 44:48], Ru[:, 0:4], AF.Exp, [Rk], [Rk], bias=Ru[:, 37:38], accum_out=Ru[:, 38:39])
                        dv(lambda e: e.reciprocal(out=Ru[:, 39:40], in_=Ru[:, 38:39]))
                        dv(lambda e: e.tensor_scalar(out=Ru[:, 48:56], in0=Ru[:, 4:12], scalar1=Ru[:, 40:41], scalar2=None, op0=ALU.mult))
                        for g in range(1, 4):
                            dv(lambda e, g=g: e.scalar_tensor_tensor(out=Ru[:, 48:56], in0=Ru[:, 4 + 8 * g:12 + 8 * g], scalar=Ru[:, 40 + g:41 + g],
                                                                      in1=Ru[:, 48:56], op0=ALU.mult, op1=ALU.add))
                        dv(lambda e: e.reduce_max(out=Ru[:, 56:57], in_=Ru[:, 48:56], axis=AX.X))
                        dv(lambda e: e.tensor_scalar(out=Ru[:, 64:72], in0=Ru[:, 48:56], scalar1=Ru[:, 56:57], scalar2=None, op0=ALU.is_equal))
                        dv(lambda e: e.scalar_tensor_tensor(out=Ru[:, 72:80], in0=Ru[:, 64:72], scalar=-1e30, in1=Ru[:, 48:56], op0=ALU.mult, op1=ALU.add))
                        dv(lambda e: e.reduce_max(out=Ru[:, 57:58], in_=Ru[:, 72:80], axis=AX.X))
                        dv(lambda e: e.tensor_scalar(out=Ru[:, 80:88], in0=Ru[:, 72:80], scalar1=Ru[:, 57:58], scalar2=None, op0=ALU.is_equal))
                        dv(lambda e: e.tensor_tensor(out=Ru[:, 58:59], in0=Ru[:, 57:58], in1=Ru[:, 56:57], op=ALU.subtract))
                        act(Ru[:, 59:60], Ru[:, 58:59], AF.Exp, [Rk], [Rk])
                        dv(lambda e: e.tensor_scalar(out=Ru[:, 60:61], in0=Ru[:, 59:60], scalar1=1.0, scalar2=None, op0=ALU.add))
                        dv(lambda e: e.reciprocal(out=Ru[:, 61:62], in_=Ru[:, 60:61]))
                        dv(lambda e: e.tensor_tensor(out=w12[:, i, 0:1], in0=Ru[:, 61:62], in1=Ru[:, 39:40], op=ALU.mult), wr=["w12"])
                        dv(lambda e: e.tensor_tensor(out=w12[:, i, 1:2], in0=w12[:, i, 0:1], in1=Ru[:, 59:60], op=ALU.mult), rd=["w12"], wr=["w12"])
                        for g in range(4):
                            dv(lambda e, g=g: e.tensor_scalar(out=M1a[:, i, 8 * g:8 * g + 8], in0=Ru[:, 64:72], scalar1=Ru[:, 40 + g:41 + g],
                                                              scalar2=None, op0=ALU.mult), wr=["M1a"])
                            dv(lambda e, g=g: e.tensor_scalar(out=M2a[:, i, 8 * g:8 * g + 8], in0=Ru[:, 80:88], scalar1=Ru[:, 40 + g:41 + g],
                                                              scalar2=None, op0=ALU.mult), wr=["M2a"])
                        tt("pool", Ma[:, i, :], M1a[:, i, :], M2a[:, i, :], ALU.add, ["M1a", "M2a"], ["Ma"])
                    for i in range(NT):
                        u = i % 2
                        Qk = "Q%d" % u
                        Qu = Q[u]
                        mm(pl[u][:, 0:32], triS[:], Ma[:, i, :], True, i == 0, ["triS", "Ma"], [], inc=(i == 0), x=[plk[u]])
                        for i2 in range(i):
                            mm(pl[u][:, 0:32], ones128[:], Ma[:, i2, :], False, i2 == i - 1, ["ones128", "Ma"], [], inc=(i2 == i - 1), x=[plk[u]])
                        dq = lambda fn, rd=(), wr=(), x=(): s.op("dve", fn, reads=[Qk] + list(rd), writes=[Qk] + list(wr), excl=x)
                        dq(lambda e: e.tensor_copy(out=Qu[:, 0, :], in_=pl[u][:, 0:32]), x=[plk[u]])
                        dq(lambda e: e.tensor_scalar(out=Qu[:, 1, :], in0=Qu[:, 0, :], scalar1=float(CAP), scalar2=None, op0=ALU.is_lt))
                        dq(lambda e: e.tensor_tensor(out=Qu[:, 0, :], in0=Qu[:, 0, :], in1=ebase[:], op=ALU.add), rd=["ebase"])
                        for k_, Mk, mk in ((0, M1a, "M1a"), (1, M2a, "M2a")):
                            dq(lambda e, Mk=Mk: e.tensor_tensor(out=Qu[:, 2, :], in0=Mk[:, i, :], in1=Qu[:, 0, :], op=ALU.mult), rd=[mk])
                            dq(lambda e: e.reduce_sum(out=Qu[:, 3, 0:1], in_=Qu[:, 2, :], axis=AX.X))
                            dq(lambda e, Mk=Mk: e.tensor_tensor(out=Qu[:, 2, :], in0=Mk[:, i, :], in1=Qu[:, 1, :], op=ALU.mult), rd=[mk])
                            dq(lambda e: e.reduce_sum(out=Qu[:, 3, 1:2], in_=Qu[:, 2, :], axis=AX.X))
                            dq(lambda e, k_=k_: e.tensor_tensor(out=w12[:, i, k_:k_ + 1], in0=w12[:, i, k_:k_ + 1], in1=Qu[:, 3, 1:2], op=ALU.mult),
                               rd=["w12"], wr=["w12"])
                            dq(lambda e: e.tensor_scalar(out=Qu[:, 3, 2:3], in0=Qu[:, 3, 1:2], scalar1=-1.0e6, scalar2=1.0e6, op0=ALU.mult, op1=ALU.add))
                            dq(lambda e, k_=k_: e.tensor_tensor(out=SLf[:, i, k_:k_ + 1], in0=Qu[:, 3, 0:1], in1=Qu[:, 3, 2:3], op=ALU.add), wr=["SLf"])
                        s.op("dve", lambda e: e.tensor_copy(out=SLi[:, 2 * i:2 * i + 2], in_=SLf[:, i, :]), reads=["SLf"], writes=["SLi"])
                        for k_ in range(2):
                            dma_fn("pool", lambda e, k_=k_: e.indirect_dma_start(
                                out=Xd[:, :], out_offset=bass.IndirectOffsetOnAxis(ap=SLi[:, 2 * i + k_:2 * i + k_ + 1], axis=0),
                                in_=fbf[:, i, :], in_offset=None, bounds_check=bc_reg, oob_is_err=False),
                                ["SLi", "fbf%d" % i], ["Xd"])
                    if smp == 0:
                        dump("SLf", SLf[:, :, :], ["SLf"])
                        dump("w12", w12[:, :, :], ["w12"])
                    s.barrier()
                    print("P5 done: deadlock-free", s.check_deadlock(), s.n_ins, s.n_wait, s.cnt)
                if stop_after == "P5":
                    s.barrier()
                    return nc
                chunks = [(c0, min(c0 + 512, CAP)) for c0 in range(0, CAP, 512)]
                with ExitStack() as st:
                    weg = [sb(st, "weg%d" % i, [128, 8, DE], BF16) for i in range(2)]
                    weu = [sb(st, "weu%d" % i, [128, 8, DE], BF16) for i in range(2)]
                    wed = [sb(st, "wed%d" % i, [128, 4, D], BF16) for i in range(2)]
                    XeT = [sb(st, "XeT%d" % i, [128, 8, CAP], BF16) for i in range(2)]
                    hT = [sb(st, "hT%d" % i, [128, 4, CAP], BF16) for i in range(2)]
                    xe = [sb(st, "xe%d" % i, [128, D], BF16) for i in range(2)]
                    ye = [sb(st, "ye%d" % i, [128, D], BF16) for i in range(2)]
                    sgm = [sb(st, "sgm%d" % i, [128, 512], F32) for i in range(2)]
                    TX = [pst(st, "p6t%d" % i, [128, 8, 128], BF16) for i in range(2)]
                    GB = [pst(st, "p6g%d" % i, [128, 512]) for i in range(2)]
                    UB = [pst(st, "p6u%d" % i, [128, 512]) for i in range(2)]
                    YB = [pst(st, "p6y%d" % i, [128, 512]) for i in range(2)]
                    TXK = ["p6T%d" % i for i in range(2)]
                    GK = ["p6G%d" % i for i in range(2)]
                    UK = ["p6U%d" % i for i in range(2)]
                    YK = ["p6Y%d" % i for i in range(2)]
                    cnt6 = [0, 0, 0]

                    def load_w(e_):
                        wb = e_ % 2
                        s.dma("sp", weg[wb][:], wview(weg_b[e_]), reads=["wbg%d" % e_], writes=["weg%d" % wb])
                        s.dma("sp", weu[wb][:], wview(weu_b[e_]), reads=["wbu%d" % e_], writes=["weu%d" % wb])
                        s.dma("sp", wed[wb][:], wview(wed_b[e_]), reads=["wbd%d" % e_], writes=["wed%d" % wb])
                        for kt in range(4):
                            tt("pool", wed[wb][:, kt, :], wed[wb][:, kt, :], g2b[:], ALU.mult, ["wed%d" % wb, "g2b"], ["wed%d" % wb])

                    def ph_T(e_):
                        wb = e_ % 2
                        for blk in range(NB):
                            u = cnt6[0] % 2
                            cnt6[0] += 1
                            r0 = e_ * CAP + blk * 128
                            s.dma("sp", xe[u][:], Xd[r0:r0 + 128, :], writes=["xe%d" % u])
                            for kt in range(8):
                                tr(TX[u][:, kt, :], xe[u][:, kt * 128:(kt + 1) * 128], ident_b[:], ["xe%d" % u, "ident_b"], [], x=[TXK[u]])
                            cp("act" if u == 0 else "dve", XeT[wb][:, :, blk * 128:(blk + 1) * 128], TX[u][:, :, :], [], ["XeT%d" % wb], x=[TXK[u]])

                    def ph_GU(e_):
                        wb = e_ % 2
                        for nt in range(4):
                            ncol = slice(nt * 128, (nt + 1) * 128)
                            for (c0, c1) in chunks:
                                u = cnt6[1] % 2
                                cnt6[1] += 1
                                n = c1 - c0
                                for kt in range(8):
                                    mm(GB[u][:, 0:n], weg[wb][:, kt, ncol], XeT[wb][:, kt, c0:c1], kt == 0, kt == 7, ["weg%d" % wb, "XeT%d" % wb], [], x=[GK[u]])
                                for kt in range(8):
                                    mm(UB[u][:, 0:n], weu[wb][:, kt, ncol], XeT[wb][:, kt, c0:c1], kt == 0, kt == 7, ["weu%d" % wb, "XeT%d" % wb], [], x=[UK[u]])
                                act(sgm[u][:, 0:n], GB[u][:, 0:n], AF.Silu, [], ["sgm%d" % u], x=[GK[u]])
                                tt("dve", hT[wb][:, nt, c0:c1], sgm[u][:, 0:n], UB[u][:, 0:n], ALU.mult, ["sgm%d" % u], ["hT%d" % wb], x=[UK[u]])

                    def ph_D(e_):
                        wb = e_ % 2
                        for blk in range(NB):
                            yu = blk % 2
                            r0 = e_ * CAP + blk * 128
                            for half in range(2):
                                u = cnt6[2] % 2
                                cnt6[2] += 1
                                hcol = slice(half * 512, (half + 1) * 512)
                                for kt in range(4):
                                    mm(YB[u][:, :], hT[wb][:, kt, blk * 128:(blk + 1) * 128], wed[wb][:, kt, hcol], kt == 0, kt == 3,
                                       ["hT%d" % wb, "wed%d" % wb], [], x=[YK[u]])
                                cp("act" if half == 0 else "dve", ye[yu][:, hcol], YB[u][:, :], [], ["ye%d" % yu], x=[YK[u]])
                            s.dma("pool", Yd[r0:r0 + 128, :], ye[yu][:], reads=["ye%d" % yu])

                    precast(96)
                    load_w(0)
                    ph_T(0)
                    for e_ in range(NEXP):
                        if e_ + 1 < NEXP:
                            load_w(e_ + 1)
                        ph_GU(e_)
                        if e_ + 1 < NEXP:
                            ph_T(e_ + 1)
                        ph_D(e_)
                    s.barrier()
                    print("P6 done: deadlock-free", s.check_deadlock(), s.n_ins, s.n_wait, s.cnt)
                with ExitStack() as st:
                    yk = [[sb(st, "yk%d_%d" % (k_, i), [128, D], BF16) for i in range(2)] for k_ in range(2)]
                    ot = [sb(st, "ot%d" % i, [128, D], F32) for i in range(2)]
                    junk4 = sb(st, "junk4", [128, D], F32)
                    st4 = [sb(st, "st4_%d" % i, [128, 4], F32) for i in range(2)]
                    for k_ in range(2):
                        for i in range(2):
                            s.op("pool", lambda e, k_=k_, i=i: e.memset(yk[k_][i][:], 0.0), writes=["yk%d_%d" % (k_, i)])
                    for i in range(NT):
                        u = i % 2
                        tokc = slice(i * 128, (i + 1) * 128)
                        hk = "hres%d" % i
                        for k_ in range(2):
                            dma_fn("pool", lambda e, k_=k_, u=u, i=i: e.indirect_dma_start(
                                out=yk[k_][u][:, :], out_offset=None, in_=Yd[:, :],
                                in_offset=bass.IndirectOffsetOnAxis(ap=SLi[:, 2 * i + k_:2 * i + k_ + 1], axis=0), bounds_check=bc_reg, oob_is_err=False),
                                ["SLi"], ["yk%d_%d" % (k_, u)])
                            stt("dve", hres[:, i, :], yk[k_][u][:], w12[:, i, k_:k_ + 1], hres[:, i, :], ALU.mult, ALU.add,
                                ["yk%d_%d" % (k_, u), "w12", hk], [hk])
                        s.op("pool", lambda e, u=u: e.memset(st4[u][:], 0.0), writes=["st4_%d" % u])
                        act(junk4[:], hres[:, i, :], AF.Square, [hk, "st4_%d" % u], ["junk4", "st4_%d" % u], accum_out=st4[u][:, 0:1])
                        act(st4[u][:, 1:2], st4[u][:, 0:1], AF.Sqrt, ["st4_%d" % u, "eps_c"], ["st4_%d" % u], scale=1.0 / D, bias=eps_c[:, 0:1])
                        s.op("dve", lambda e, u=u: e.reciprocal(out=st4[u][:, 2:3], in_=st4[u][:, 1:2]), reads=["st4_%d" % u], writes=["st4_%d" % u])
                        stt("dve", ot[u][:], hres[:, i, :], st4[u][:, 2:3], gfb[:], ALU.mult, ALU.mult, [hk, "st4_%d" % u, "gfb"], ["ot%d" % u])
                        s.dma("sp", y_d[smp, tokc, :], ot[u][:], reads=["ot%d" % u])
                    s.barrier()
        s.barrier(final=True)
    return nc


def _consts():
    ident = np.eye(128, dtype=np.float32)
    idx = np.arange(128)
    same = (idx[:, None] // 64) == (idx[None, :] // 64)
    tri = np.zeros((6, 128, 128), np.float32)
    tri[0] = same & (idx[:, None] <= idx[None, :])
    tri[1] = same & (idx[:, None] > idx[None, :])
    tri[2] = same & (idx[:, None] >= idx[None, :])
    tri[3] = same & (idx[:, None] < idx[None, :])
    tri[4] = idx[:, None] < idx[None, :]
    tri[5] = 1.0
    t = np.arange(S)
    pos_r = (t // GW).astype(np.float32)
    pos_c = (t % GW).astype(np.float32)
    inv = (10000.0 ** (-np.arange(32, dtype=np.float32) / 32)).astype(np.float32)
    ang = np.zeros((128, S), np.float32)
    ang[0:32] = inv[:, None] * pos_r[None, :]
    ang[32:64] = inv[:, None] * pos_r[None, :]
    ang[64:96] = inv[:, None] * pos_c[None, :]
    ang[96:128] = inv[:, None] * pos_c[None, :]
    cos = np.cos(ang).astype(np.float32)
    sin = np.sin(ang).astype(np.float32)
    sgn = np.ones((128, 1), np.float32)
    sgn[0:32] = -1
    sgn[64:96] = -1
    sin = sin * sgn
    perm = np.concatenate([np.arange(32, 64), np.arange(0, 32), np.arange(96, 128), np.arange(64, 96)])
    return ident, tri, cos, sin, perm


def _na_tables(rpb):
    a = (np.arange(128) // 64)[:, None, None, None]
    kc = (np.arange(128) % 64)[:, None, None, None]
    p = np.arange(8)[None, :, None, None]
    j = np.arange(4)[None, None, :, None]
    qc = np.arange(64)[None, None, None, :]
    ridx = 2 * j + a - p + 7
    cstart = np.clip(qc - 8, 0, 48)
    valid = (kc >= cstart) & (kc < cstart + 16) & (ridx >= 0) & (ridx <= 14)
    valid = np.broadcast_to(valid, (128, 8, 4, 64))
    cidx = np.clip(kc - qc + 15, 0, 30)
    ridx_c = np.clip(ridx, 0, 14)
    ridx_b = np.broadcast_to(ridx_c, (128, 8, 4, 64))
    cidx_b = np.broadcast_to(cidx, (128, 8, 4, 64))
    g = rpb[:, ridx_b, cidx_b]
    g = np.where(valid[None], g, np.float32(0.0)).astype(np.float32)
    g = g.reshape(4, 2, 128, 8, 256).transpose(0, 2, 3, 1, 4)
    mask = np.where(valid, np.float32(0.0), np.float32(MASKV)).astype(np.float32).reshape(128, 8, 256)
    return np.ascontiguousarray(g), np.ascontiguousarray(mask)


_NC_CACHE = {}


def kernel(x, c, ctx, c_ctx, w_mod, b_mod, norm_attn_g, norm_ffn_g, w_in, w_gla_a2, b_gla_a2,
           gla_norm_g, na_rpb, w_na_o, w_gla_o, w_out, w_group, b_group, w_expert, b_expert,
           w_exp_gate, w_exp_up, w_exp_down, final_norm_g):
    f = lambda a: np.ascontiguousarray(np.asarray(a, dtype=np.float32))
    x, c, ctx, c_ctx = f(x), f(c), f(ctx), f(c_ctx)
    ident, tri, cos, sin, perm = _consts()
    w_in0 = f(w_in)[0]
    gq = w_in0[:, C_GQ:C_GQ + 512].reshape(D, 4, 128)[:, :, perm].reshape(D, 512)
    gk = w_in0[:, C_GK:C_GK + 512].reshape(D, 4, 128)[:, :, perm].reshape(D, 512)
    w_rope = np.ascontiguousarray(np.concatenate([gq, gk], axis=1))
    w_a2b = np.ascontiguousarray(np.concatenate(
        [f(w_gla_a2)[0].transpose(1, 0, 2), f(b_gla_a2)[0][None]], axis=0))
    nab, nam = _na_tables(f(na_rpb)[0])
    w_rt = np.ascontiguousarray(np.concatenate([f(w_group)[0], f(w_expert)[0]], axis=1))
    b_rt = np.ascontiguousarray(np.concatenate([f(b_group)[0], f(b_expert)[0]], axis=0))
    shared = {
        "w_mod": f(w_mod)[0], "b_mod": f(b_mod)[0], "g_attn": f(norm_attn_g)[0], "g_ffn": f(norm_ffn_g)[0],
        "g_fin": f(final_norm_g), "gla_g": f(gla_norm_g)[0], "w_in": w_in0, "w_rope": w_rope, "w_a2b": w_a2b,
        "w_na_o": f(w_na_o)[0], "w_gla_o": f(w_gla_o)[0], "w_out": f(w_out)[0], "w_rt": w_rt, "b_rt": b_rt,
        "w_eg": f(w_exp_gate)[0], "w_eu": f(w_exp_up)[0], "w_ed": f(w_exp_down)[0],
        "ident": ident, "tri": tri, "rope_cos": cos, "rope_sin": sin, "na_bias": nab, "na_mask": nam,
        "ebase": np.ascontiguousarray(np.broadcast_to((np.arange(NEXP, dtype=np.float32) * MOE_CAP)[None, :], (128, NEXP))),
    }
    n = 8
    NS = x.shape[0] // n
    if "nc" not in _NC_CACHE:
        _NC_CACHE["nc"] = build_nc(NS)
    nc = _NC_CACHE["nc"]
    in_maps = []
    for i in range(n):
        m = dict(shared)
        m["x"] = x[i * NS:(i + 1) * NS]
        m["ctx"] = ctx[i * NS:(i + 1) * NS]
        m["cc"] = np.ascontiguousarray(np.concatenate([c[i * NS:(i + 1) * NS], c_ctx[None]], axis=0))
        in_maps.append(m)
    res = run_bass_kernel_spmd(nc, in_maps, core_ids=list(range(n)))
    return np.concatenate([r["y"] for r in res.results], axis=0)
```

```python
import os
import numpy as np
import concourse.bass as bass
import concourse.mybir as mybir
from concourse.bass_utils import run_bass_kernel_spmd
from contextlib import ExitStack

F32 = mybir.dt.float32
BF16 = mybir.dt.bfloat16
I32 = mybir.dt.int32
ALU = mybir.AluOpType
AF = mybir.ActivationFunctionType
AX = mybir.AxisListType

D = 1024
S = 2048
CT = 256
NT = 16
NTC = 2
NTA = NT + NTC
GW = 64
EPS = 1e-6
NEXP = 32
DE = 512
C_NAQ, C_NAK, C_NAV, C_GQ, C_GK, C_GV, C_GG, C_AL, C_M8, C_M9 = 0, 512, 1024, 1536, 2048, 2560, 3584, 4608, 4640, 5664
MASKV = -30000.0
MOE_CAP = 640


class Sched:
    ENGS = ("pe", "act", "dve", "pool", "sp")
    HND = {"pe": "tensor", "act": "scalar", "dve": "vector", "pool": "gpsimd", "sp": "sync"}

    def __init__(self, nc, es, n_dma_sems=40):
        self.nc = nc
        self.sem = {e: es.enter_context(nc.semaphore("s_" + e)) for e in self.ENGS}
        self.cnt = {e: 0 for e in self.ENGS}
        self.pending = {e: False for e in self.ENGS}
        self.waited = {e: {} for e in self.ENGS}
        self.dsem = [es.enter_context(nc.semaphore("s_dma%d" % i)) for i in range(n_dma_sems)]
        self.dcnt = [0] * n_dma_sems
        self.dnext = 0
        self.n_fg = n_dma_sems - 8
        self.bnext = 0
        self.lastw = {}
        self.reads = {}
        self.semobj = {}
        for e in self.ENGS:
            self.semobj[("e", e)] = self.sem[e]
        for i, sm in enumerate(self.dsem):
            self.semobj[("d", i)] = sm
        self.n_ins = 0
        self.n_wait = 0

    def _deps(self, eng, reads, writes, excl=()):
        toks = {}

        def add(t):
            if t is None:
                return
            k, v = t
            if toks.get(k, -1) < v:
                toks[k] = v
        for b in reads:
            add(self.lastw.get(b))
        for b in writes:
            add(self.lastw.get(b))
            for t in self.reads.get(b, ()):
                add(t)
        for b in excl:
            t = self.lastw.get(b)
            if t is not None and t[0] != ("e", eng):
                add(t)
        out = []
        for k, v in toks.items():
            if k == ("e", eng):
                if eng == "pe":
                    continue
                if v <= self.cnt[eng] - 2:
                    continue
            if self.waited[eng].get(k, -1) >= v:
                continue
            self.waited[eng][k] = v
            out.append((k, v))
        return out

    def _commit(self, tok, reads, writes):
        for b in writes:
            self.lastw[b] = tok
            self.reads[b] = []
        for b in reads:
            self.reads.setdefault(b, []).append(tok)

    def check_deadlock(self):
        pos = {e: 0 for e in self.ENGS}
        val = {}
        prog = True
        while prog:
            prog = False
            for e in self.ENGS:
                lst = self.log[e]
                while pos[e] < len(lst):
                    waits, inc = lst[pos[e]]
                    if all(val.get(k, 0) >= v for k, v in waits):
                        if inc is not None:
                            val[inc[0]] = val.get(inc[0], 0) + inc[1]
                        pos[e] += 1
                        prog = True
                    else:
                        break
        stuck = {e: (pos[e], len(self.log[e])) for e in self.ENGS if pos[e] < len(self.log[e])}
        for e in stuck:
            waits, inc = self.log[e][pos[e]]
            print("STUCK", e, pos[e], [(k, v, val.get(k, 0)) for k, v in waits])
        return not stuck

    def _emit1(self, eng, waits, fn, inc):
        if not hasattr(self, "log"):
            self.log = {e: [] for e in self.ENGS}
        self.log[eng].append((list(waits), inc if fn is not None else None))
        engh = getattr(self.nc, self.HND[eng])
        for k, v in waits:
            engh.wait_ge(self.semobj[k], v)
            self.n_wait += 1
        if fn is None:
            return
        ins = fn(engh)
        if inc is not None:
            ins.then_inc(self.semobj[inc[0]], inc[1])
        self.n_ins += 1

    def op(self, eng, fn, reads=(), writes=(), inc=True, excl=()):
        waits = self._deps(eng, reads, writes, excl)
        if inc:
            self.cnt[eng] += 1
            tok = (("e", eng), self.cnt[eng])
            self.pending[eng] = False
        else:
            tok = (("e", eng), self.cnt[eng] + 1)
            self.pending[eng] = True
        self._emit1(eng, waits, fn, (("e", eng), 1) if inc else None)
        self._commit(tok, reads, writes)
        for b_ in excl:
            self.lastw[b_] = tok
        return tok

    def dma(self, eng, out, in_, reads=(), writes=(), bg=False, **kw):
        if bg:
            i = self.n_fg + self.bnext
            self.bnext = (self.bnext + 1) % 8
        else:
            i = self.dnext
            self.dnext = (self.dnext + 1) % self.n_fg
        k = ("d", i)
        waits = self._deps(eng, reads, writes)
        if self.dcnt[i] > 0 and self.waited[eng].get(k, -1) < self.dcnt[i]:
            self.waited[eng][k] = self.dcnt[i]
            waits.append((k, self.dcnt[i]))
        self.dcnt[i] += 16
        tok = (k, self.dcnt[i])
        self._emit1(eng, waits, lambda e: e.dma_start(out=out, in_=in_, **kw), (k, 16))
        self._commit(tok, reads, writes)
        return tok

    def barrier(self, final=False):
        assert not any(self.pending.values())
        allt = [(("e", e), self.cnt[e]) for e in self.ENGS if self.cnt[e] > 0]
        allt += [(("d", i), c) for i, c in enumerate(self.dcnt) if c > 0 and (i < self.n_fg or final)]
        for e in self.ENGS:
            waits = []
            for k, v in allt:
                if k == ("e", e):
                    continue
                if self.waited[e].get(k, -1) >= v:
                    continue
                self.waited[e][k] = v
                waits.append((k, v))
            self._emit1(e, waits, None, None)
        keep = {k: t for k, t in self.lastw.items() if t[0][0] == "d" and t[0][1] >= self.n_fg}
        self.lastw = {} if final else keep
        self.reads = {}


def build_nc(NS=2, dbg=None, stop_after=None):
    nc = bass.Bass("TRN2", target_bir_lowering=False)

    def din(name, shape, dt=F32):
        return nc.dram_tensor(name, list(shape), dt, kind="ExternalInput").ap()

    x_d = din("x", [NS, S, D])
    ctx_d = din("ctx", [NS, CT, D])
    cc_d = din("cc", [NS + 1, D])
    w_mod_d = din("w_mod", [D, 6 * D])
    b_mod_d = din("b_mod", [6 * D])
    g_attn_d = din("g_attn", [D])
    g_ffn_d = din("g_ffn", [D])
    g_fin_d = din("g_fin", [D])
    gla_g_d = din("gla_g", [256])
    w_in_d = din("w_in", [D, 6688])
    w_rope_d = din("w_rope", [D, 1024])
    w_a2b_d = din("w_a2b", [17, 2, 512])
    w_nao_d = din("w_na_o", [512, D])
    w_glo_d = din("w_gla_o", [D, D])
    w_out_d = din("w_out", [D, D])
    w_rt_d = din("w_rt", [D, 36])
    b_rt_d = din("b_rt", [36])
    w_eg_d = din("w_eg", [NEXP, D, DE])
    w_eu_d = din("w_eu", [NEXP, D, DE])
    w_ed_d = din("w_ed", [NEXP, DE, D])
    ident_d = din("ident", [128, 128])
    tri_d = din("tri", [6, 128, 128])
    ebase_d = din("ebase", [128, 32])
    cos_d = din("rope_cos", [128, S])
    sin_d = din("rope_sin", [128, S])
    nab_d = din("na_bias", [4, 128, 8, 2, 256])
    nam_d = din("na_mask", [128, 8, 256])
    y_d = nc.dram_tensor("y", [NS, S, D], F32, kind="ExternalOutput").ap()
    modv_d = nc.dram_tensor("modv", [NS + 1, 6 * D], F32).ap()
    dbg_d = {}
    if dbg:
        for name, shape in dbg.items():
            dbg_d[name] = nc.dram_tensor("dbg_" + name, list(shape), F32, kind="ExternalOutput").ap()

    with ExitStack() as es:
        s = Sched(nc, es)

        used_names = {}

        def uniq(name):
            k = used_names.get(name, 0)
            used_names[name] = k + 1
            return name if k == 0 else "%s_r%d" % (name, k)

        def sb(st, name, shape, dt):
            return st.enter_context(nc.sbuf_tensor(uniq(name), list(shape), dt))

        def pst(st, name, shape, dt=F32):
            return st.enter_context(nc.psum_tensor(uniq(name), list(shape), dt))

        def mm(out, lhsT, rhs, start, stop, reads, writes, inc=None, x=()):
            if inc is None:
                inc = stop
            s.op("pe", lambda e: e.matmul(out, lhsT, rhs, start=start, stop=stop),
                 reads=reads, writes=writes, inc=inc, excl=x)

        def tr(out, in_, idn, reads, writes, x=()):
            s.op("pe", lambda e: e.transpose(out, in_, idn), reads=reads, writes=writes, excl=x)

        def act(out, in_, func, reads, writes, x=(), **kw):
            s.op("act", lambda e: e.activation(out=out, in_=in_, func=func, **kw), reads=reads, writes=writes, excl=x)

        def tt(eng, out, in0, in1, op, reads, writes, x=()):
            s.op(eng, lambda e: e.tensor_tensor(out=out, in0=in0, in1=in1, op=op), reads=reads, writes=writes, excl=x)

        def ts(eng, out, in0, s1, s2, op0, op1, reads, writes, x=()):
            if s2 is None:
                s.op(eng, lambda e: e.tensor_scalar(out=out, in0=in0, scalar1=s1, scalar2=None, op0=op0),
                     reads=reads, writes=writes, excl=x)
            else:
                s.op(eng, lambda e: e.tensor_scalar(out=out, in0=in0, scalar1=s1, scalar2=s2, op0=op0, op1=op1),
                     reads=reads, writes=writes, excl=x)

        def stt(eng, out, in0, scalar, in1, op0, op1, reads, writes, x=()):
            s.op(eng, lambda e: e.scalar_tensor_tensor(out=out, in0=in0, scalar=scalar, in1=in1, op0=op0, op1=op1),
                 reads=reads, writes=writes, excl=x)

        def cp(eng, out, in_, reads, writes, x=()):
            if eng == "act":
                s.op("act", lambda e: e.copy(out=out, in_=in_), reads=reads, writes=writes, excl=x)
            else:
                s.op(eng, lambda e: e.tensor_copy(out=out, in_=in_), reads=reads, writes=writes, excl=x)

        def dump(name, src_ap, reads, dst=None):
            if name in dbg_d:
                s.dma("pool", dbg_d[name] if dst is None else dst, src_ap, reads=reads)

        def wview(ap2d):
            return ap2d.rearrange("(kt p) n -> p kt n", p=128)

        ident_f = sb(es, "ident_f", [128, 128], F32)
        ident_b = sb(es, "ident_b", [128, 128], BF16)
        tri_f = sb(es, "tri_f", [128, 4, 128], F32)
        ones_b = sb(es, "ones_b", [128, 128], BF16)
        eps_c = sb(es, "eps_c", [128, 1], F32)
        one_c = sb(es, "one_c", [128, 1], F32)
        s.dma("sp", ident_f[:], ident_d, writes=["ident_f"])
        s.dma("pool", ident_b[:], ident_d, writes=["ident_b"])
        s.dma("sp", tri_f[:], tri_d[0:4].rearrange("m s t -> s m t"), writes=["tri"])
        s.op("pool", lambda e: e.memset(ones_b[:], 1.0), writes=["ones_b"])
        s.op("pool", lambda e: e.memset(eps_c[:], EPS), writes=["eps_c"])
        s.op("pool", lambda e: e.memset(one_c[:], 1.0), writes=["one_c"])

        with ExitStack() as st:
            ccs = sb(st, "ccs", [NS + 1, D], F32)
            scT = sb(st, "scT", [128, 8, NS + 1], F32)
            modsb = sb(st, "modsb", [NS + 1, 6 * D], F32)
            bmod = sb(st, "bmod", [NS + 1, 6 * D], F32)
            wm = [sb(st, "wm%d" % i, [128, 8, 512], F32) for i in range(2)]
            psA = pst(st, "p0a", [128, 512])
            psB = [pst(st, "p0b%d" % i, [128, 512]) for i in range(2)]
            R = NS + 1
            s.dma("sp", ccs[:], cc_d, writes=["ccs"])
            s.dma("sp", bmod[:], b_mod_d.partition_broadcast(R), writes=["bmod"])
            act(ccs[:], ccs[:], AF.Silu, ["ccs"], ["ccs"])
            for kt in range(8):
                tr(psA[:, kt * R:(kt + 1) * R], ccs[:, kt * 128:(kt + 1) * 128], ident_f[0:R, 0:R],
                   ["ccs", "ident_f"], ["p0a"])
            cp("dve", scT[:].rearrange("p k r -> p (k r)"), psA[:, 0:8 * R], ["p0a"], ["scT"])
            for cb in range(12):
                w = wm[cb % 2]
                s.dma("sp", w[:], wview(w_mod_d[:, cb * 512:(cb + 1) * 512]), writes=["wm%d" % (cb % 2)])
                ps = psB[cb % 2]
                for kt in range(8):
                    mm(ps[0:R, :], scT[:, kt, :], w[:, kt, :], kt == 0, kt == 7,
                       ["scT", "wm%d" % (cb % 2)], ["p0b%d" % (cb % 2)])
                tt("dve", modsb[:, cb * 512:(cb + 1) * 512], ps[0:R, :], bmod[:, cb * 512:(cb + 1) * 512], ALU.add,
                   ["p0b%d" % (cb % 2), "bmod"], ["modsb"])
            s.dma("sp", modv_d, modsb[:], reads=["modsb"], writes=["modv"])
            dump("modv", modsb[:], ["modsb"])
            s.barrier()
        if stop_after == "P0":
            s.barrier()
            return nc

        QSCALE = 128.0 ** -0.5
        weg_b = nc.dram_tensor("weg_bf", [NEXP, D, DE], BF16).ap()
        weu_b = nc.dram_tensor("weu_bf", [NEXP, D, DE], BF16).ap()
        wed_b = nc.dram_tensor("wed_bf", [NEXP, DE, D], BF16).ap()

        def _precast_gen():
            for e_ in range(NEXP):
                for nm, dst, srcw, r in (("g", weg_b, w_eg_d, 4), ("u", weu_b, w_eu_d, 4), ("d", wed_b, w_ed_d, 2)):
                    s.dma("pool", dst[e_].rearrange("(a r) n -> a (r n)", r=r), srcw[e_].rearrange("(a r) n -> a (r n)", r=r),
                          writes=["wb%s%d" % (nm, e_)], bg=True)
                    yield
        _pc = _precast_gen()

        def precast(n):
            for _ in range(n):
                next(_pc, None)
        bc_reg = nc.gpsimd.alloc_register("bcreg")
        nc.gpsimd.reg_mov(bc_reg, NEXP * MOE_CAP - 1)
        h1_d = nc.dram_tensor("h1_scr", [NS, S, D], F32).ap()

        def bank_set(st, pfx):
            return [pst(st, "%s%d" % (pfx, i), [128, 512]) for i in range(7)]

        for smp in range(NS):
            with ExitStack() as sa:
                aT = sb(sa, "aT", [128, 8, S], BF16)
                acT = sb(sa, "acT", [128, 8, CT], BF16)
                oglT = sb(sa, "oglT", [128, 8, S], BF16)

                with ExitStack() as st:
                    s1b = sb(st, "s1b", [128, D], F32)
                    sh1b = sb(st, "sh1b", [128, D], F32)
                    s1c = sb(st, "s1c", [128, D], F32)
                    sh1c = sb(st, "sh1c", [128, D], F32)
                    gab = sb(st, "gab", [128, D], F32)
                    tmpv = sb(st, "tmpv", [128, D], F32)
                    xt = [sb(st, "xt%d" % i, [128, D], F32) for i in range(2)]
                    xm = [sb(st, "xm%d" % i, [128, D], F32) for i in range(2)]
                    xn = [sb(st, "xn%d" % i, [128, D], BF16) for i in range(2)]
                    stat = [sb(st, "stat%d" % i, [128, 4], F32) for i in range(2)]
                    psT = [pst(st, "p1t%d" % i, [128, 8, 128], BF16) for i in range(2)]
                    s.dma("sp", gab[:], g_attn_d.partition_broadcast(128), writes=["gab"])
                    for row, s1, sh, nm in ((smp, s1b, sh1b, "l"), (NS, s1c, sh1c, "c")):
                        s.dma("sp", tmpv[:], modv_d[row, D:2 * D].partition_broadcast(128), writes=["tmpv"])
                        stt("dve", s1[:], tmpv[:], 1.0, gab[:], ALU.add, ALU.mult, ["tmpv", "gab"], ["s1" + nm])
                        s.dma("sp", sh[:], modv_d[row, 0:D].partition_broadcast(128), writes=["sh1" + nm])
                    for i in range(NTA):
                        b = i % 2
                        lat = i < NT
                        src = x_d[smp, i * 128:(i + 1) * 128, :] if lat else ctx_d[smp, (i - NT) * 128:(i - NT + 1) * 128, :]
                        nm = "l" if lat else "c"
                        s1, sh = (s1b, sh1b) if lat else (s1c, sh1c)
                        s.dma("sp", xt[b][:], src, writes=["xt%d" % b])
                        s.op("pool", lambda e, b=b: e.memset(stat[b][:], 0.0), writes=["stat%d" % b])
                        act(xm[b][:], xt[b][:], AF.Square, ["xt%d" % b, "stat%d" % b], ["xm%d" % b, "stat%d" % b],
                            accum_out=stat[b][:, 0:1])
                        act(stat[b][:, 1:2], stat[b][:, 0:1], AF.Sqrt, ["stat%d" % b, "eps_c"], ["stat%d" % b],
                            scale=1.0 / D, bias=eps_c[:, 0:1])
                        s.op("dve", lambda e, b=b: e.reciprocal(out=stat[b][:, 2:3], in_=stat[b][:, 1:2]),
                             reads=["stat%d" % b], writes=["stat%d" % b])
                        stt("dve", xm[b][:], xt[b][:], stat[b][:, 2:3], s1[:], ALU.mult, ALU.mult,
                            ["xt%d" % b, "stat%d" % b, "s1" + nm], ["xm%d" % b])
                        tt("pool", xn[b][:], xm[b][:], sh[:], ALU.add, ["xm%d" % b, "sh1" + nm], ["xn%d" % b])
                        for kt in range(8):
                            tr(psT[b][:, kt, :], xn[b][:, kt * 128:(kt + 1) * 128], ident_b[:],
                               ["xn%d" % b, "ident_b"], ["p1t%d" % b])
                        if lat:
                            cp("act", aT[:, :, i * 128:(i + 1) * 128], psT[b][:, :, :], ["p1t%d" % b], ["aT"])
                        else:
                            cp("act", acT[:, :, (i - NT) * 128:(i - NT + 1) * 128], psT[b][:, :, :], ["p1t%d" % b], ["acT"])
                    if smp == 0:
                        dump("aT", aT[:, :, 0:256], ["aT"])
                    s.barrier()
                if stop_after == "P1":
                    s.barrier()
                    return nc

                with ExitStack() as st:
                    ropeC = sb(st, "ropeC", [128, S], BF16)
                    ropeS = sb(st, "ropeS", [128, S], BF16)
                    alT = [sb(st, "alT%d" % d_, [16, S + CT], BF16) for d_ in range(2)]
                    wal = sb(st, "wal", [128, 8, 32], BF16)
                    wa2 = sb(st, "wa2", [16, 2, 512], BF16)
                    ba2 = sb(st, "ba2", [1, 2, 512], BF16)
                    gnb = sb(st, "gnb", [128, 256], F32)
                    wq = sb(st, "wq", [128, 8, 128], BF16)
                    wqs = sb(st, "wqs", [128, 8, 128], BF16)
                    wk = sb(st, "wk", [128, 8, 128], BF16)
                    wks = sb(st, "wks", [128, 8, 128], BF16)
                    wv = sb(st, "wv", [128, 8, 256], BF16)
                    wg = sb(st, "wg", [128, 8, 256], BF16)
                    qrT = sb(st, "qrT", [128, S], BF16)
                    krT = sb(st, "krT", [128, S], BF16)
                    krk = sb(st, "krk", [128, NTA, 128], BF16)
                    vtk = sb(st, "vtk", [128, NTA, 256], BF16)
                    sgt = sb(st, "sgt", [128, NT, 256], BF16)
                    qe = [sb(st, "qe%d" % d_, [128, S], BF16) for d_ in range(2)]
                    ke = [sb(st, "ke%d" % d_, [128, S], BF16) for d_ in range(2)]
                    kd = [sb(st, "kd%d" % d_, [128, NTA, 128], BF16) for d_ in range(2)]
                    dec = sb(st, "dec", [128, 2, NTA, 2], F32)
                    oacc = sb(st, "oacc", [128, NT, 256], F32)
                    S32 = [sb(st, "S32_%d" % d_, [128, 256], F32) for d_ in range(2)]
                    S16 = [[sb(st, "S16_%d_%d" % (d_, v_), [128, 256], BF16) for v_ in range(2)] for d_ in range(2)]
                    t1 = [sb(st, "t1_%d" % i, [128, 512], F32) for i in range(2)]
                    t2 = [sb(st, "t2_%d" % i, [128, 512], F32) for i in range(2)]
                    Dt = [sb(st, "Dt0", [128, 512], F32)]
                    Di = [sb(st, "Di0", [128, 512], F32)]
                    EK = [sb(st, "EK0", [128, 512], F32)]
                    att = [sb(st, "att%d" % i, [128, 128], BF16) for i in range(2)]
                    junk = sb(st, "junk2", [128, 256], F32)
                    onr = [sb(st, "onr%d" % i, [128, 256], F32) for i in range(2)]
                    rbf = [sb(st, "rbf%d" % i, [128, 256], BF16) for i in range(2)]
                    st2 = [sb(st, "st2_%d" % i, [128, 4], F32) for i in range(2)]
                    B = [pst(st, "p2b%d" % i, [128, 512]) for i in range(6)]
                    BK = ["p2B%d" % i for i in range(6)]
                    pT = [pst(st, "p2t%d" % i, [128, 8, 128], BF16) for i in range(2)]
                    TK = ["p2T0", "p2T1"]

                    s.dma("pool", ropeC[:], cos_d, writes=["ropeC"])
                    s.dma("pool", ropeS[:], sin_d, writes=["ropeS"])
                    s.dma("pool", wal[:], wview(w_in_d[:, C_AL:C_AL + 32]), writes=["wal"])
                    s.dma("pool", wa2[:], w_a2b_d[0:16], writes=["wa2"])
                    s.dma("pool", ba2[:], w_a2b_d[16:17], writes=["ba2"])
                    s.dma("sp", gnb[:], gla_g_d.partition_broadcast(128), writes=["gnb"])
                    for tb in range(5):
                        if tb < 4:
                            rhs_of = lambda kt, tb=tb: aT[:, kt, tb * 512:(tb + 1) * 512]
                            n, c0, rk = 512, tb * 512, "aT"
                        else:
                            rhs_of = lambda kt: acT[:, kt, :]
                            n, c0, rk = CT, S, "acT"
                        for d_ in range(2):
                            for kt in range(8):
                                mm(B[d_][0:16, 0:n], wal[:, kt, d_ * 16:(d_ + 1) * 16], rhs_of(kt), kt == 0, kt == 7,
                                   ["wal", rk], [], x=[BK[d_]])
                            cp("act" if d_ == 0 else "dve", alT[d_][:, c0:c0 + n], B[d_][0:16, 0:n], [], ["alT%d" % d_], x=[BK[d_]])
                    if stop_after == "P2a":
                        s.barrier()
                        return nc

                    def load_head_w(h):
                        s.dma("pool", wq[:], wview(w_in_d[:, C_GQ + 128 * h:C_GQ + 128 * (h + 1)]), writes=["wq"])
                        s.dma("pool", wqs[:], wview(w_rope_d[:, 128 * h:128 * (h + 1)]), writes=["wqs"])
                        s.dma("pool", wk[:], wview(w_in_d[:, C_GK + 128 * h:C_GK + 128 * (h + 1)]), writes=["wk"])
                        s.dma("pool", wks[:], wview(w_rope_d[:, 512 + 128 * h:512 + 128 * (h + 1)]), writes=["wks"])
                        s.dma("pool", wv[:], wview(w_in_d[:, C_GV + 256 * h:C_GV + 256 * (h + 1)]), writes=["wv"])
                        s.dma("pool", wg[:], wview(w_in_d[:, C_GG + 256 * h:C_GG + 256 * (h + 1)]), writes=["wg"])

                    load_head_w(0)
                    for h in range(4):
                        for tb in range(4):
                            cols = slice(tb * 512, (tb + 1) * 512)
                            for bi, (w, wn) in enumerate(((wq, "wq"), (wqs, "wqs"), (wk, "wk"), (wks, "wks"))):
                                for kt in range(8):
                                    mm(B[bi][:, :], w[:, kt, :], aT[:, kt, cols], kt == 0, kt == 7, [wn, "aT"], [], x=[BK[bi]])
                            stt("dve", t1[0][:], B[0][:, :], QSCALE, ropeC[:, cols], ALU.mult, ALU.mult, ["ropeC"], ["t1_0"], x=[BK[0]])
                            stt("dve", t2[0][:], B[1][:, :], QSCALE, ropeS[:, cols], ALU.mult, ALU.mult, ["ropeS"], ["t2_0"], x=[BK[1]])
                            tt("pool", qrT[:, cols], t1[0][:], t2[0][:], ALU.add, ["t1_0", "t2_0"], ["qrT"])
                            tt("dve", t1[1][:], B[2][:, :], ropeC[:, cols], ALU.mult, ["ropeC"], ["t1_1"], x=[BK[2]])
                            tt("dve", t2[1][:], B[3][:, :], ropeS[:, cols], ALU.mult, ["ropeS"], ["t2_1"], x=[BK[3]])
                            tt("pool", krT[:, cols], t1[1][:], t2[1][:], ALU.add, ["t1_1", "t2_1"], ["krT"])
                        if stop_after == "P2b":
                            s.barrier()
                            return nc
                        for i in range(NTA):
                            lat = i < NT
                            bv = 4 + (i % 2)
                            bg = 2 + (i % 2)
                            srcT, rk, c0 = (aT, "aT", i * 128) if lat else (acT, "acT", (i - NT) * 128)
                            for kt in range(8):
                                mm(B[bv][:, 0:256], srcT[:, kt, c0:c0 + 128], wv[:, kt, :], kt == 0, kt == 7, [rk, "wv"], [], x=[BK[bv]])
                            cp("act", vtk[:, i, :], B[bv][:, 0:256], [], ["vtk"], x=[BK[bv]])
                            if lat:
                                for kt in range(8):
                                    mm(B[bg][:, 0:256], srcT[:, kt, c0:c0 + 128], wg[:, kt, :], kt == 0, kt == 7, [rk, "wg"], [], x=[BK[bg]])
                                act(sgt[:, i, :], B[bg][:, 0:256], AF.Silu, [], ["sgt"], x=[BK[bg]])
                            else:
                                for kt in range(8):
                                    mm(B[bg][:, 0:128], srcT[:, kt, c0:c0 + 128], wk[:, kt, :], kt == 0, kt == 7, [rk, "wk"], [], x=[BK[bg]])
                                cp("dve", krk[:, i, :], B[bg][:, 0:128], [], ["krk"], x=[BK[bg]])
                        for i in range(NT):
                            u = i % 2
                            tr(pT[u][:, 0, :], krT[:, i * 128:(i + 1) * 128], ident_b[:], ["krT", "ident_b"], [], x=[TK[u]])
                            cp("dve", krk[:, i, :], pT[u][:, 0, :], [], ["krk"], x=[TK[u]])
                        if stop_after == "P2c":
                            s.barrier()
                            return nc
                        if h + 1 < 4:
                            load_head_w(h + 1)
                        precast(12)
                        groups = [(0, 4), (4, 4), (8, 4), (12, 4), (16, 2)]
                        hc = slice(128 * h, 128 * (h + 1))
                        for (t0, nt_) in groups:
                            lat = t0 < NT
                            n = nt_ * 128
                            gc = slice(t0 * 128, t0 * 128 + n)
                            for d_ in range(2):
                                bz, bb, be = B[d_], B[2 + d_], B[4 + d_]
                                xz, xb, xe_ = [BK[d_]], [BK[2 + d_]], [BK[4 + d_]]
                                az_, ex_, rz_, L_ = t1[0], t1[1], t2[0], t2[1]
                                for tl in range(nt_):
                                    tokc = slice((t0 + tl) * 128, (t0 + tl + 1) * 128)
                                    zc = slice(tl * 128, (tl + 1) * 128)
                                    mm(bz[:, zc], alT[d_][0:16, tokc], wa2[0:16, d_, hc], True, False, ["alT%d" % d_, "wa2"], [], inc=False, x=xz)
                                    mm(bz[:, zc], ones_b[0:1, 0:128], ba2[0:1, d_, hc], False, True, ["ones_b", "ba2"], [], x=xz)
                                act(az_[:, 0:n], bz[:, 0:n], AF.Abs, [], ["t1_0"], x=xz)
                                ts("dve", rz_[:, 0:n], bz[:, 0:n], -1.0, 0.0, ALU.mult, ALU.max, [], ["t2_0"], x=xz)
                                act(ex_[:, 0:n], az_[:, 0:n], AF.Exp, ["t1_0"], ["t1_1"], scale=-1.0)
                                act(ex_[:, 0:n], ex_[:, 0:n], AF.Ln, ["t1_1", "one_c"], ["t1_1"], bias=one_c[:, 0:1])
                                tt("pool", L_[:, 0:n], rz_[:, 0:n], ex_[:, 0:n], ALU.add, ["t2_0", "t1_1"], ["t2_1"])
                                for tl in range(nt_):
                                    zc = slice(tl * 128, (tl + 1) * 128)
                                    mm(bb[:, zc], L_[:, zc], tri_f[:, 2 * d_, :], True, True, ["t2_1", "tri"], [], x=xb)
                                for tl in range(nt_):
                                    zc = slice(tl * 128, (tl + 1) * 128)
                                    mm(be[:, zc], tri_f[:, 2 * d_ + 1, :], L_[:, zc], True, True, ["t2_1", "tri"], [], x=xe_)
                                act(Dt[0][:, 0:n], bb[:, 0:n], AF.Exp, [], ["Dt0"], scale=-1.0 / 16, x=xb)
                                if lat:
                                    act(Di[0][:, 0:n], bb[:, 0:n], AF.Exp, [], ["Di0"], scale=1.0 / 16, x=xb)
                                act(EK[0][:, 0:n], be[:, 0:n], AF.Exp, [], ["EK0"], scale=-1.0 / 16, x=xe_)
                                dsrc = Dt[0][:, 63:n:64] if d_ == 0 else Dt[0][:, 0:n:64]
                                cp("pool", dec[:, d_, t0:t0 + nt_, :].rearrange("p t c -> p (t c)"), dsrc, ["Dt0"], ["dec"])
                                tt("pool", kd[d_][:, t0:t0 + nt_, :], krk[:, t0:t0 + nt_, :], EK[0][:, 0:n].rearrange("p (t c) -> p t c", c=128),
                                   ALU.mult, ["krk", "EK0"], ["kd%d" % d_])
                                if lat:
                                    tt("dve", qe[d_][:, gc], qrT[:, gc], Dt[0][:, 0:n], ALU.mult, ["qrT", "Dt0"], ["qe%d" % d_])
                                    tt("dve", ke[d_][:, gc], krT[:, gc], Di[0][:, 0:n], ALU.mult, ["krT", "Di0"], ["ke%d" % d_])
                        if stop_after == "P2d":
                            s.barrier()
                            return nc
                        ver = [0, 0]
                        for d_ in range(2):
                            s.op("pool", lambda e, d_=d_: e.memset(S32[d_][:], 0.0), writes=["S32_%d" % d_])
                            s.op("pool", lambda e, d_=d_: e.memset(S16[d_][0][:], 0.0), writes=["S16_%d_0" % d_])

                        def kvmm(d_, i, half):
                            rows = slice(64 * half, 64 * half + 64)
                            bk = 2 + 2 * d_ + half
                            mm(B[bk][:, 0:256], kd[d_][rows, i, :], vtk[rows, i, :], True, True, ["kd%d" % d_, "vtk"], [], x=[BK[bk]])

                        def upd(d_, i, half):
                            bk = 2 + 2 * d_ + half
                            pkv = B[bk][:, 0:256]
                            nv = (ver[d_] + 1) % 2
                            stt("dve", S16[d_][nv][:], S32[d_][:], dec[:, d_, i, half:half + 1], pkv, ALU.mult, ALU.add,
                                ["S32_%d" % d_, "dec"], ["S16_%d_%d" % (d_, nv)], x=[BK[bk]])
                            stt("dve", S32[d_][:], S32[d_][:], dec[:, d_, i, half:half + 1], pkv, ALU.mult, ALU.add,
                                ["S32_%d" % d_, "dec"], ["S32_%d" % d_], x=[BK[bk]])
                            ver[d_] += 1

                        for i in (NT, NT + 1):
                            for half in (0, 1):
                                kvmm(0, i, half)
                                upd(0, i, half)
                        for i in (NT + 1, NT):
                            for half in (1, 0):
                                kvmm(1, i, half)
                                upd(1, i, half)
                        if smp == 0 and h == 0:
                            dump("s_f", S32[0][:], ["S32_0"])
                            dump("s_b", S32[1][:], ["S32_1"])
                        if stop_after == "P2e":
                            s.barrier()
                            return nc

                        done = [0] * NT

                        def lat_front(d_, i):
                            tokc = slice(i * 128, (i + 1) * 128)
                            pa = B[d_][:, 256:384]
                            po = B[d_][:, 0:256]
                            mm(pa, ke[d_][:, tokc], qe[d_][:, tokc], True, True, ["ke%d" % d_, "qe%d" % d_], [], x=[BK[d_]])
                            tt("dve", att[d_][:], pa, tri_f[:, 2 * d_, :], ALU.mult, ["tri"], ["att%d" % d_], x=[BK[d_]])
                            mm(po, att[d_][:], vtk[:, i, :], True, False, ["att%d" % d_, "vtk"], [], inc=False, x=[BK[d_]])

                        def lat_inter(d_, i, half, last):
                            po = B[d_][:, 0:256]
                            rows = slice(64 * half, 64 * half + 64)
                            c0 = i * 128 + 64 * half
                            cv = ver[d_] % 2
                            mm(po[rows, :], qe[d_][:, c0:c0 + 64], S16[d_][cv][:], False, last, ["qe%d" % d_, "S16_%d_%d" % (d_, cv)],
                               [], inc=True, x=[BK[d_]])

                        def lat_back(d_, i):
                            po = B[d_][:, 0:256]
                            xo = [BK[d_]]
                            tokc = slice(i * 128, (i + 1) * 128)
                            if done[i] == 0:
                                cp("act", oacc[:, i, :], po, [], ["oacc%d" % i], x=xo)
                                done[i] = 1
                                return
                            u = i % 2
                            tt("dve", oacc[:, i, :], po, oacc[:, i, :], ALU.add, ["oacc%d" % i], ["oacc%d" % i], x=xo)
                            s.op("pool", lambda e, u=u: e.memset(st2[u][:], 0.0), writes=["st2_%d" % u])
                            act(junk[:], oacc[:, i, :], AF.Square, ["oacc%d" % i, "st2_%d" % u], ["junk2", "st2_%d" % u],
                                accum_out=st2[u][:, 0:1])
                            act(st2[u][:, 1:2], st2[u][:, 0:1], AF.Sqrt, ["st2_%d" % u, "eps_c"], ["st2_%d" % u],
                                scale=1.0 / 256, bias=eps_c[:, 0:1])
                            s.op("dve", lambda e, u=u: e.reciprocal(out=st2[u][:, 2:3], in_=st2[u][:, 1:2]),
                                 reads=["st2_%d" % u], writes=["st2_%d" % u])
                            stt("dve", onr[u][:], oacc[:, i, :], st2[u][:, 2:3], gnb[:], ALU.mult, ALU.mult,
                                ["oacc%d" % i, "st2_%d" % u, "gnb"], ["onr%d" % u])
                            tt("pool", rbf[u][:], onr[u][:], sgt[:, i, :], ALU.mult, ["onr%d" % u, "sgt"], ["rbf%d" % u])
                            for j in range(2):
                                tr(pT[u][:, j, :], rbf[u][:, j * 128:(j + 1) * 128], ident_b[:], ["rbf%d" % u, "ident_b"], [], x=[TK[u]])
                            cp("act", oglT[:, 2 * h:2 * h + 2, tokc], pT[u][:, 0:2, :], [], ["oglT"], x=[TK[u]])

                        for j in range(NT):
                            fi, bi_ = j, NT - 1 - j
                            lat_front(0, fi)
                            lat_front(1, bi_)
                            kvmm(0, fi, 0)
                            kvmm(0, fi, 1)
                            kvmm(1, bi_, 1)
                            kvmm(1, bi_, 0)
                            lat_inter(0, fi, 0, False)
                            upd(0, fi, 0)
                            lat_inter(1, bi_, 1, False)
                            upd(1, bi_, 1)
                            lat_inter(0, fi, 1, True)
                            upd(0, fi, 1)
                            lat_inter(1, bi_, 0, True)
                            upd(1, bi_, 0)
                            lat_back(0, fi)
                            lat_back(1, bi_)
                    if smp == 0:
                        dump("oglT", oglT[:, :, :], ["oglT"])
                    s.barrier()
                    print("P2 done: deadlock-free", s.check_deadlock(), s.n_ins, s.n_wait, s.cnt)
                if stop_after == "P2":
                    s.barrier()
                    return nc

                onaT = sb(sa, "onaT", [128, 4, S], BF16)
                with ExitStack() as st:
                    wq3 = sb(st, "wq3", [128, 8, 128], BF16)
                    wk3 = sb(st, "wk3", [128, 8, 128], BF16)
                    wv3 = sb(st, "wv3", [128, 8, 128], BF16)
                    nam = sb(st, "nam", [128, 8, 256], F32)
                    nabt = [sb(st, "nabt%d" % i, [128, 2, 256], F32) for i in range(2)]
                    BMp = sb(st, "BMp", [128, 8, 2, 256], BF16)
                    qT3 = sb(st, "qT3", [128, S], BF16)
                    kT3 = sb(st, "kT3", [128, S + CT], BF16)
                    Ve = sb(st, "Ve", [128, 16, 128], BF16)
                    Vo = sb(st, "Vo", [128, 15, 128], BF16)
                    Vc = sb(st, "Vc", [128, 2, 128], BF16)
                    PT = [sb(st, "PT%d" % i, [128, 6, 2, 64], BF16) for i in range(2)]
                    rden = [sb(st, "rden%d" % i, [128, 128], F32) for i in range(2)]
                    SB = [pst(st, "p3s%d" % i, [128, 512]) for i in range(4)]
                    SK = ["p3S%d" % i for i in range(4)]
                    OB = [pst(st, "p3o%d" % i, [128, 512]) for i in range(2)]
                    OK_ = ["p3O%d" % i for i in range(2)]
                    PB = [pst(st, "p3p%d" % i, [128, 512]) for i in range(2)]
                    PK = ["p3P%d" % i for i in range(2)]
                    s.dma("sp", nam[:], nam_d, writes=["nam"])
                    for pr in range(4):
                        precast(12)
                        s.dma("pool", wq3[:], wview(w_in_d[:, C_NAQ + 128 * pr:C_NAQ + 128 * (pr + 1)]), writes=["wq3"])
                        s.dma("pool", wk3[:], wview(w_in_d[:, C_NAK + 128 * pr:C_NAK + 128 * (pr + 1)]), writes=["wk3"])
                        s.dma("pool", wv3[:], wview(w_in_d[:, C_NAV + 128 * pr:C_NAV + 128 * (pr + 1)]), writes=["wv3"])
                        for p in range(8):
                            s.dma("sp", nabt[p % 2][:], nab_d[pr, :, p, :, :], writes=["nabt%d" % (p % 2)])
                            for hh in range(2):
                                stt("dve", BMp[:, p, hh, :], nabt[p % 2][:, hh, :], 8.0, nam[:, p, :], ALU.mult, ALU.add,
                                    ["nabt%d" % (p % 2), "nam"], ["BMp"])
                        cnt_p = [0]

                        def proj(dst, lhs_fn, rhs_fn, m, n, rk, dk):
                            u = cnt_p[0] % 2
                            cnt_p[0] += 1
                            for kt in range(8):
                                mm(PB[u][0:m, 0:n], lhs_fn(kt), rhs_fn(kt), kt == 0, kt == 7, rk, [], x=[PK[u]])
                            cp("act" if u == 0 else "dve", dst, PB[u][0:m, 0:n], [], [dk], x=[PK[u]])

                        for tb in range(4):
                            cols = slice(tb * 512, (tb + 1) * 512)
                            proj(qT3[:, cols], lambda kt: wq3[:, kt, :], lambda kt, cols=cols: aT[:, kt, cols], 128, 512, ["wq3", "aT"], "qT3")
                            proj(kT3[:, cols], lambda kt: wk3[:, kt, :], lambda kt, cols=cols: aT[:, kt, cols], 128, 512, ["wk3", "aT"], "kT3")
                        proj(kT3[:, S:S + CT], lambda kt: wk3[:, kt, :], lambda kt: acT[:, kt, :], 128, CT, ["wk3", "acT"], "kT3")
                        for i in range(16):
                            proj(Ve[:, i, :], lambda kt, i=i: aT[:, kt, i * 128:(i + 1) * 128], lambda kt: wv3[:, kt, :], 128, 128, ["wv3", "aT"], "Ve")
                        for i in range(15):
                            proj(Vo[:, i, :], lambda kt, i=i: aT[:, kt, 64 + i * 128:64 + (i + 1) * 128], lambda kt: wv3[:, kt, :], 128, 128, ["wv3", "aT"], "Vo")
                        for i in range(2):
                            proj(Vc[:, i, :], lambda kt, i=i: acT[:, kt, i * 128:(i + 1) * 128], lambda kt: wv3[:, kt, :], 128, 128, ["wv3", "acT"], "Vc")

                        def scores(r):
                            rs = min(max(r - 4, 0), 24)
                            p = r - rs
                            buf = r % 2
                            qc = slice(r * 64, r * 64 + 64)
                            for hh in range(2):
                                pr_ = slice(64 * hh, 64 * hh + 64)
                                bank = SB[buf * 2 + hh]
                                xk = [SK[buf * 2 + hh]]
                                mm(bank[:, 0:256], ident_b[:], BMp[:, p, hh, :], True, False, ["ident_b", "BMp"], [], inc=False, x=xk)
                                for j in range(4):
                                    kc0 = (rs + 2 * j) * 64
                                    mm(bank[:, j * 64:(j + 1) * 64], kT3[pr_, kc0:kc0 + 128], qT3[pr_, qc], False, j == 3,
                                       ["kT3", "qT3"], [], inc=False, x=xk)
                                for jc in range(2):
                                    mm(bank[:, 256 + jc * 64:256 + (jc + 1) * 64], kT3[pr_, S + jc * 128:S + (jc + 1) * 128], qT3[pr_, qc],
                                       True, True, ["kT3", "qT3"], [], inc=(jc == 1), x=xk)
                                act(PT[buf][:, :, hh, :], bank[:, 0:384].rearrange("p (j q) -> p j q", q=64), AF.Exp,
                                    [], ["PT%d" % buf], scale=0.125, x=xk)

                        def pv(r):
                            rs = min(max(r - 4, 0), 24)
                            buf = r % 2
                            qc = slice(r * 64, r * 64 + 64)
                            ob = OB[buf]
                            xo = [OK_[buf]]
                            for hh in range(2):
                                hs = slice(64 * hh, 64 * hh + 64)
                                for j in range(6):
                                    if j < 4:
                                        if rs % 2 == 0:
                                            Vt, vk = Ve[:, (rs + 2 * j) // 2, hs], "Ve"
                                        else:
                                            Vt, vk = Vo[:, (rs + 2 * j - 1) // 2, hs], "Vo"
                                    else:
                                        Vt, vk = Vc[:, j - 4, hs], "Vc"
                                    mm(ob[hs, 0:64], Vt, PT[buf][:, j, hh, :], j == 0, j == 5, [vk, "PT%d" % buf], [],
                                       inc=(j == 5), x=xo)
                            for j in range(6):
                                mm(ob[:, 64:192], ones_b[:, :], PT[buf][:, j, :, :].rearrange("p h q -> p (h q)"), j == 0, j == 5,
                                   ["ones_b", "PT%d" % buf], [], inc=(j == 5), x=xo)
                            s.op("dve", lambda e: e.reciprocal(out=rden[buf][:], in_=ob[:, 64:192]), reads=[], writes=["rden%d" % buf], excl=xo)
                            for hh in range(2):
                                hs = slice(64 * hh, 64 * hh + 64)
                                tt("dve", onaT[hs, pr, qc], ob[hs, 0:64], rden[buf][hs, 64 * hh:64 * hh + 64], ALU.mult,
                                   ["rden%d" % buf], ["onaT"], x=xo)

                        for r in range(32):
                            scores(r)
                            if r > 0:
                                pv(r - 1)
                        pv(31)
                    if smp == 0:
                        dump("onaT", onaT[:, :, :], ["onaT"])
                    s.barrier()
                    print("P3 done: deadlock-free", s.check_deadlock(), s.n_ins, s.n_wait, s.cnt)
                if stop_after == "P3":
                    s.barrier()
                    return nc

                UT = sb(sa, "UT", [128, 8, S], BF16)
                with ExitStack() as st:
                    wna = [sb(st, "wna%d" % i, [128, 4, 128], BF16) for i in range(2)]
                    wgl = [sb(st, "wgl%d" % i, [128, 8, 128], BF16) for i in range(2)]
                    w8 = [sb(st, "w8_%d" % i, [128, 8, 128], BF16) for i in range(2)]
                    w9 = [sb(st, "w9_%d" % i, [128, 8, 128], BF16) for i in range(2)]
                    sg8 = [sb(st, "sg8_%d" % i, [128, 512], F32) for i in range(2)]
                    sg9 = [sb(st, "sg9_%d" % i, [128, 512], F32) for i in range(2)]
                    t1m = [sb(st, "t1m%d" % i, [128, 512], F32) for i in range(2)]
                    t2m = [sb(st, "t2m%d" % i, [128, 512], F32) for i in range(2)]
                    MB = [pst(st, "p4b%d" % i, [128, 512]) for i in range(8)]
                    MK = ["p4B%d" % i for i in range(8)]
                    for nt in range(8):
                        wb = nt % 2
                        ncol = slice(nt * 128, (nt + 1) * 128)
                        s.dma("pool", wna[wb][:], wview(w_nao_d[:, ncol]), writes=["wna%d" % wb])
                        s.dma("pool", wgl[wb][:], wview(w_glo_d[:, ncol]), writes=["wgl%d" % wb])
                        s.dma("pool", w8[wb][:], wview(w_in_d[:, C_M8 + nt * 128:C_M8 + (nt + 1) * 128]), writes=["w8_%d" % wb])
                        s.dma("pool", w9[wb][:], wview(w_in_d[:, C_M9 + nt * 128:C_M9 + (nt + 1) * 128]), writes=["w9_%d" % wb])
                        for tb in range(4):
                            cols = slice(tb * 512, (tb + 1) * 512)
                            u = (nt * 4 + tb) % 2
                            bA, bG8, bB, bG9 = [MB[4 * u + q] for q in range(4)]
                            kA, kG8, kB, kG9 = [[MK[4 * u + q]] for q in range(4)]
                            for kt in range(4):
                                mm(bA[:, :], wna[wb][:, kt, :], onaT[:, kt, cols], kt == 0, kt == 3, ["wna%d" % wb, "onaT"], [], x=kA)
                            for kt in range(8):
                                mm(bG8[:, :], w8[wb][:, kt, :], aT[:, kt, cols], kt == 0, kt == 7, ["w8_%d" % wb, "aT"], [], x=kG8)
                            for kt in range(8):
                                mm(bB[:, :], wgl[wb][:, kt, :], oglT[:, kt, cols], kt == 0, kt == 7, ["wgl%d" % wb, "oglT"], [], x=kB)
                            for kt in range(8):
                                mm(bG9[:, :], w9[wb][:, kt, :], aT[:, kt, cols], kt == 0, kt == 7, ["w9_%d" % wb, "aT"], [], x=kG9)
                            act(sg8[u][:], bG8[:, :], AF.Sigmoid, [], ["sg8_%d" % u], x=kG8)
                            act(sg9[u][:], bG9[:, :], AF.Sigmoid, [], ["sg9_%d" % u], x=kG9)
                            tt("dve", t1m[u][:], bA[:, :], sg8[u][:], ALU.mult, ["sg8_%d" % u], ["t1m%d" % u], x=kA)
                            tt("dve", t2m[u][:], bB[:, :], sg9[u][:], ALU.mult, ["sg9_%d" % u], ["t2m%d" % u], x=kB)
                            tt("pool", UT[:, nt, cols], t1m[u][:], t2m[u][:], ALU.add, ["t1m%d" % u, "t2m%d" % u], ["UT"])
                    if smp == 0:
                        dump("UT", UT[:, :, 0:256], ["UT"])
                    s.barrier()
                with ExitStack() as st:
                    wo = sb(st, "wo", [128, 8, D], BF16)
                    wtmp = [sb(st, "wtmp%d" % i, [128, D], F32) for i in range(2)]
                    g1b = sb(st, "g1b", [128, D], F32)
                    xt2 = [sb(st, "xt2_%d" % i, [128, D], F32) for i in range(2)]
                    h1t = [sb(st, "h1t%d" % i, [128, D], F32) for i in range(2)]
                    MB = [pst(st, "p4c%d" % i, [128, 512]) for i in range(4)]
                    MK = ["p4C%d" % i for i in range(4)]
                    s.dma("sp", g1b[:], modv_d[smp, 2 * D:3 * D].partition_broadcast(128), writes=["g1b"])
                    for kt in range(8):
                        s.dma("sp", wtmp[kt % 2][:], w_out_d[kt * 128:(kt + 1) * 128, :], writes=["wtmp%d" % (kt % 2)])
                        tt("pool", wo[:, kt, :], wtmp[kt % 2][:], g1b[:], ALU.mult, ["wtmp%d" % (kt % 2), "g1b"], ["wo"])
                    for i in range(NT):
                        u = i % 2
                        tokc = slice(i * 128, (i + 1) * 128)
                        s.dma("sp", xt2[u][:], x_d[smp, tokc, :], writes=["xt2_%d" % u])
                        for half in range(2):
                            hcol = slice(half * 512, (half + 1) * 512)
                            bk = MB[2 * u + half]
                            xk = [MK[2 * u + half]]
                            for kt in range(8):
                                mm(bk[:, :], UT[:, kt, tokc], wo[:, kt, hcol], kt == 0, kt == 7, ["UT", "wo"], [], x=xk)
                            tt("dve", h1t[u][:, hcol], bk[:, :], xt2[u][:, hcol], ALU.add, ["xt2_%d" % u], ["h1t%d" % u], x=xk)
                        s.dma("sp", h1_d[smp, tokc, :], h1t[u][:], reads=["h1t%d" % u], writes=["h1d"])
                        if smp == 0 and i < 2 and "h1" in dbg_d:
                            dump("h1", h1t[u][:], ["h1t%d" % u], dst=dbg_d["h1"][i * 128:(i + 1) * 128, :])
                    s.barrier()
                    print("P4 done: deadlock-free", s.check_deadlock(), s.n_ins, s.n_wait, s.cnt)
                if stop_after == "P4":
                    s.barrier()
                    return nc
            CAP = MOE_CAP
            NB = CAP // 128
            NSLOT = NEXP * CAP
            Xd = nc.dram_tensor("xd_scr%d" % smp, [NSLOT, D], BF16).ap()
            Yd = nc.dram_tensor("yd_scr%d" % smp, [NSLOT, D], BF16).ap()

            ind_hist = []
            IND_DEPTH = int(os.environ.get("IND_DEPTH", "1000"))

            def dma_fn(eng, fn, reads, writes):
                i_ = s.dnext
                s.dnext = (s.dnext + 1) % s.n_fg
                k_ = ("d", i_)
                waits = s._deps(eng, reads, writes)
                if s.dcnt[i_] > 0 and s.waited[eng].get(k_, -1) < s.dcnt[i_]:
                    s.waited[eng][k_] = s.dcnt[i_]
                    waits.append((k_, s.dcnt[i_]))
                if len(ind_hist) >= IND_DEPTH:
                    pk, pv = ind_hist[-IND_DEPTH]
                    if pk != k_ and s.waited[eng].get(pk, -1) < pv:
                        s.waited[eng][pk] = pv
                        waits.append((pk, pv))
                s.dcnt[i_] += 16
                tok = (k_, s.dcnt[i_])
                ind_hist.append(tok)
                s._emit1(eng, waits, fn, (k_, 16))
                s._commit(tok, reads, writes)

            with ExitStack() as sm:
                hres = sb(sm, "hres", [128, NT, D], F32)
                g2b = sb(sm, "g2b", [128, D], F32)
                gfb = sb(sm, "gfb", [128, D], F32)
                SLi = sb(sm, "SLi", [128, NT * 2], I32)
                w12 = sb(sm, "w12", [128, NT, 2], F32)
                s.dma("sp", g2b[:], modv_d[smp, 5 * D:6 * D].partition_broadcast(128), writes=["g2b"])
                s.dma("sp", gfb[:], g_fin_d.partition_broadcast(128), writes=["gfb"])
                with ExitStack() as st:
                    fbf = sb(st, "fbf", [128, NT, D], BF16)
                    M1a = sb(st, "M1a", [128, NT, 32], F32)
                    M2a = sb(st, "M2a", [128, NT, 32], F32)
                    Ma = sb(st, "Ma", [128, NT, 32], F32)
                    SLf = sb(st, "SLf", [128, NT, 2], F32)
                    ebase = sb(st, "ebase_sb", [128, 32], F32)
                    triS = sb(st, "triS", [128, 128], F32)
                    ones128 = sb(st, "ones128", [128, 128], F32)
                    s2b = sb(st, "s2b", [128, D], F32)
                    sh2b = sb(st, "sh2b", [128, D], F32)
                    gfn = sb(st, "gfn", [128, D], F32)
                    wrt = sb(st, "wrt", [128, 8, 36], F32)
                    brt = sb(st, "brt", [1, 36], F32)
                    ones_f = sb(st, "ones_f", [1, 128], F32)
                    xm3 = [sb(st, "xm3_%d" % i, [128, D], F32) for i in range(2)]
                    junk3 = sb(st, "junk3", [128, D], F32)
                    fT32 = [sb(st, "fT32_%d" % i, [128, 8, 128], F32) for i in range(2)]
                    R = [sb(st, "R%d" % i, [128, 96], F32) for i in range(2)]
                    Q = [sb(st, "Q%d" % i, [128, 4, 32], F32) for i in range(2)]
                    st3 = [sb(st, "st3_%d" % i, [128, 4], F32) for i in range(2)]
                    pf = [[pst(st, "p5f%d_%d" % (i, j), [128, 4, 128]) for j in range(2)] for i in range(2)]
                    pfk = [["p5F%d_%d" % (i, j) for j in range(2)] for i in range(2)]
                    pl = [pst(st, "p5l%d" % i, [128, 512]) for i in range(2)]
                    plk = ["p5L%d" % i for i in range(2)]
                    s.dma("sp", gfn[:], g_ffn_d.partition_broadcast(128), writes=["gfn"])
                    s.dma("sp", s2b[:], modv_d[smp, 4 * D:5 * D].partition_broadcast(128), writes=["s2b"])
                    stt("dve", s2b[:], s2b[:], 1.0, gfn[:], ALU.add, ALU.mult, ["s2b", "gfn"], ["s2b"])
                    s.dma("sp", sh2b[:], modv_d[smp, 3 * D:4 * D].partition_broadcast(128), writes=["sh2b"])
                    s.dma("sp", wrt[:], wview(w_rt_d), writes=["wrt"])
                    s.dma("sp", brt[:], b_rt_d.partition_broadcast(1), writes=["brt"])
                    s.dma("sp", ebase[:], ebase_d, writes=["ebase"])
                    s.dma("sp", triS[:], tri_d[4], writes=["triS"])
                    s.op("pool", lambda e: e.memset(ones_f[:], 1.0), writes=["ones_f"])
                    s.op("pool", lambda e: e.memset(ones128[:], 1.0), writes=["ones128"])
                    for i in range(NT):
                        u = i % 2
                        tokc = slice(i * 128, (i + 1) * 128)
                        hk = "hres%d" % i
                        Rk = "R%d" % u
                        Ru = R[u]
                        s.dma("sp", hres[:, i, :], h1_d[smp, tokc, :], reads=["h1d"], writes=[hk])
                        s.op("pool", lambda e, u=u: e.memset(st3[u][:], 0.0), writes=["st3_%d" % u])
                        act(junk3[:], hres[:, i, :], AF.Square, [hk, "st3_%d" % u], ["junk3", "st3_%d" % u], accum_out=st3[u][:, 0:1])
                        act(st3[u][:, 1:2], st3[u][:, 0:1], AF.Sqrt, ["st3_%d" % u, "eps_c"], ["st3_%d" % u], scale=1.0 / D, bias=eps_c[:, 0:1])
                        s.op("dve", lambda e, u=u: e.reciprocal(out=st3[u][:, 2:3], in_=st3[u][:, 1:2]), reads=["st3_%d" % u], writes=["st3_%d" % u])
                        stt("dve", xm3[u][:], hres[:, i, :], st3[u][:, 2:3], s2b[:], ALU.mult, ALU.mult, [hk, "st3_%d" % u, "s2b"], ["xm3_%d" % u])
                        tt("pool", xm3[u][:], xm3[u][:], sh2b[:], ALU.add, ["xm3_%d" % u, "sh2b"], ["xm3_%d" % u])
                        cp("pool", fbf[:, i, :], xm3[u][:], ["xm3_%d" % u], ["fbf%d" % i])
                        for kt in range(8):
                            tr(pf[u][kt // 4][:, kt % 4, :], xm3[u][:, kt * 128:(kt + 1) * 128], ident_f[:], ["xm3_%d" % u, "ident_f"], [], x=[pfk[u][kt // 4]])
                        for j in range(2):
                            cp("act" if j == 0 else "dve", fT32[u][:, 4 * j:4 * j + 4, :], pf[u][j][:, :, :], [], ["fT32_%d" % u], x=[pfk[u][j]])
                        for kt in range(8):
                            mm(pl[u][:, 0:36], fT32[u][:, kt, :], wrt[:, kt, :], kt == 0, False, ["fT32_%d" % u, "wrt"], [], inc=False, x=[plk[u]])
                        mm(pl[u][:, 0:36], ones_f[0:1, :], brt[0:1, :], False, True, ["ones_f", "brt"], [], x=[plk[u]])
                        dv = lambda fn, rd=(), wr=(), x=(): s.op("dve", fn, reads=[Rk] + list(rd), writes=[Rk] + list(wr), excl=x)
                        dv(lambda e: e.tensor_copy(out=Ru[:, 0:36], in_=pl[u][:, 0:36]), x=[plk[u]])
                        dv(lambda e: e.reduce_max(out=Ru[:, 36:37], in_=Ru[:, 0:4], axis=AX.X))
                        dv(lambda e: e.tensor_scalar(out=Ru[:, 40:44], in0=Ru[:, 0:4], scalar1=Ru[:, 36:37], scalar2=None, op0=ALU.is_equal))
                        dv(lambda e: e.tensor_scalar(out=Ru[:, 37:38], in0=Ru[:, 36:37], scalar1=-1.0, scalar2=None, op0=ALU.mult))
                        s.op("pool", lambda e: e.memset(Ru[:, 38:39], 0.0), reads=[Rk], writes=[Rk])
                        act(Ru[:, 44:48], Ru[:, 0:4], AF.Exp, [Rk], [Rk], bias=Ru[:, 37:38], accum_out=Ru[:, 38:39])
                        dv(lambda e: e.reciprocal(out=Ru[:, 39:40], in_=Ru[:, 38:39]))
                        dv(lambda e: e.tensor_scalar(out=Ru[:, 48:56], in0=Ru[:, 4:12], scalar1=Ru[:, 40:41], scalar2=None, op0=ALU.mult))
                        for g in range(1, 4):
                            dv(lambda e, g=g: e.scalar_tensor_tensor(out=Ru[:, 48:56], in0=Ru[:, 4 + 8 * g:12 + 8 * g], scalar=Ru[:, 40 + g:41 + g],
                                                                      in1=Ru[:, 48:56], op0=ALU.mult, op1=ALU.add))
                        dv(lambda e: e.reduce_max(out=Ru[:, 56:57], in_=Ru[:, 48:56], axis=AX.X))
                        dv(lambda e: e.tensor_scalar(out=Ru[:, 64:72], in0=Ru[:, 48:56], scalar1=Ru[:, 56:57], scalar2=None, op0=ALU.is_equal))
                        dv(lambda e: e.scalar_tensor_tensor(out=Ru[:, 72:80], in0=Ru[:, 64:72], scalar=-1e30, in1=Ru[:, 48:56], op0=ALU.mult, op1=ALU.add))
                        dv(lambda e: e.reduce_max(out=Ru[:, 57:58], in_=Ru[:, 72:80], axis=AX.X))
                        dv(lambda e: e.tensor_scalar(out=Ru[:, 80:88], in0=Ru[:, 72:80], scalar1=Ru[:, 57:58], scalar2=None, op0=ALU.is_equal))
                        dv(lambda e: e.tensor_tensor(out=Ru[:, 58:59], in0=Ru[:, 57:58], in1=Ru[:, 56:57], op=ALU.subtract))
                        act(Ru[:, 59:60], Ru[:, 58:59], AF.Exp, [Rk], [Rk])
                        dv(lambda e: e.tensor_scalar(out=Ru[:, 60:61], in0=Ru[:, 59:60], scalar1=1.0, scalar2=None, op0=ALU.add))
                        dv(lambda e: e.reciprocal(out=Ru[:, 61:62], in_=Ru[:, 60:61]))
                        dv(lambda e: e.tensor_tensor(out=w12[:, i, 0:1], in0=Ru[:, 61:62], in1=Ru[:, 39:40], op=ALU.mult), wr=["w12"])
                        dv(lambda e: e.tensor_tensor(out=w12[:, i, 1:2], in0=w12[:, i, 0:1], in1=Ru[:, 59:60], op=ALU.mult), rd=["w12"], wr=["w12"])
                        for g in range(4):
                            dv(lambda e, g=g: e.tensor_scalar(out=M1a[:, i, 8 * g:8 * g + 8], in0=Ru[:, 64:72], scalar1=Ru[:, 40 + g:41 + g],
                                                              scalar2=None, op0=ALU.mult), wr=["M1a"])
                            dv(lambda e, g=g: e.tensor_scalar(out=M2a[:, i, 8 * g:8 * g + 8], in0=Ru[:, 80:88], scalar1=Ru[:, 40 + g:41 + g],
                                                              scalar2=None, op0=ALU.mult), wr=["M2a"])
                        tt("pool", Ma[:, i, :], M1a[:, i, :], M2a[:, i, :], ALU.add, ["M1a", "M2a"], ["Ma"])
                    for i in range(NT):
                        u = i % 2
                        Qk = "Q%d" % u
                        Qu = Q[u]
                        mm(pl[u][:, 0:32], triS[:], Ma[:, i, :], True, i == 0, ["triS", "Ma"], [], inc=(i == 0), x=[plk[u]])
                        for i2 in range(i):
                            mm(pl[u][:, 0:32], ones128[:], Ma[:, i2, :], False, i2 == i - 1, ["ones128", "Ma"], [], inc=(i2 == i - 1), x=[plk[u]])
                        dq = lambda fn, rd=(), wr=(), x=(): s.op("dve", fn, reads=[Qk] + list(rd), writes=[Qk] + list(wr), excl=x)
                        dq(lambda e: e.tensor_copy(out=Qu[:, 0, :], in_=pl[u][:, 0:32]), x=[plk[u]])
                        dq(lambda e: e.tensor_scalar(out=Qu[:, 1, :], in0=Qu[:, 0, :], scalar1=float(CAP), scalar2=None, op0=ALU.is_lt))
                        dq(lambda e: e.tensor_tensor(out=Qu[:, 0, :], in0=Qu[:, 0, :], in1=ebase[:], op=ALU.add), rd=["ebase"])
                        for k_, Mk, mk in ((0, M1a, "M1a"), (1, M2a, "M2a")):
                            dq(lambda e, Mk=Mk: e.tensor_tensor(out=Qu[:, 2, :], in0=Mk[:, i, :], in1=Qu[:, 0, :], op=ALU.mult), rd=[mk])
                            dq(lambda e: e.reduce_sum(out=Qu[:, 3, 0:1], in_=Qu[:, 2, :], axis=AX.X))
                            dq(lambda e, Mk=Mk: e.tensor_tensor(out=Qu[:, 2, :], in0=Mk[:, i, :], in1=Qu[:, 1, :], op=ALU.mult), rd=[mk])
                            dq(lambda e: e.reduce_sum(out=Qu[:, 3, 1:2], in_=Qu[:, 2, :], axis=AX.X))
                            dq(lambda e, k_=k_: e.tensor_tensor(out=w12[:, i, k_:k_ + 1], in0=w12[:, i, k_:k_ + 1], in1=Qu[:, 3, 1:2], op=ALU.mult),
                               rd=["w12"], wr=["w12"])
                            dq(lambda e: e.tensor_scalar(out=Qu[:, 3, 2:3], in0=Qu[:, 3, 1:2], scalar1=-1.0e6, scalar2=1.0e6, op0=ALU.mult, op1=ALU.add))
                            dq(lambda e, k_=k_: e.tensor_tensor(out=SLf[:, i, k_:k_ + 1], in0=Qu[:, 3, 0:1], in1=Qu[:, 3, 2:3], op=ALU.add), wr=["SLf"])
                        s.op("dve", lambda e: e.tensor_copy(out=SLi[:, 2 * i:2 * i + 2], in_=SLf[:, i, :]), reads=["SLf"], writes=["SLi"])
                        for k_ in range(2):
                            dma_fn("pool", lambda e, k_=k_: e.indirect_dma_start(
                                out=Xd[:, :], out_offset=bass.IndirectOffsetOnAxis(ap=SLi[:, 2 * i + k_:2 * i + k_ + 1], axis=0),
                                in_=fbf[:, i, :], in_offset=None, bounds_check=bc_reg, oob_is_err=False),
                                ["SLi", "fbf%d" % i], ["Xd"])
                    if smp == 0:
                        dump("SLf", SLf[:, :, :], ["SLf"])
                        dump("w12", w12[:, :, :], ["w12"])
                    s.barrier()
                    print("P5 done: deadlock-free", s.check_deadlock(), s.n_ins, s.n_wait, s.cnt)
                if stop_after == "P5":
                    s.barrier()
                    return nc
                chunks = [(c0, min(c0 + 512, CAP)) for c0 in range(0, CAP, 512)]
                with ExitStack() as st:
                    weg = [sb(st, "weg%d" % i, [128, 8, DE], BF16) for i in range(2)]
                    weu = [sb(st, "weu%d" % i, [128, 8, DE], BF16) for i in range(2)]
                    wed = [sb(st, "wed%d" % i, [128, 4, D], BF16) for i in range(2)]
                    XeT = [sb(st, "XeT%d" % i, [128, 8, CAP], BF16) for i in range(2)]
                    hT = [sb(st, "hT%d" % i, [128, 4, CAP], BF16) for i in range(2)]
                    xe = [sb(st, "xe%d" % i, [128, D], BF16) for i in range(2)]
                    ye = [sb(st, "ye%d" % i, [128, D], BF16) for i in range(2)]
                    sgm = [sb(st, "sgm%d" % i, [128, 512], F32) for i in range(2)]
                    TX = [pst(st, "p6t%d" % i, [128, 8, 128], BF16) for i in range(2)]
                    GB = [pst(st, "p6g%d" % i, [128, 512]) for i in range(2)]
                    UB = [pst(st, "p6u%d" % i, [128, 512]) for i in range(2)]
                    YB = [pst(st, "p6y%d" % i, [128, 512]) for i in range(2)]
                    TXK = ["p6T%d" % i for i in range(2)]
                    GK = ["p6G%d" % i for i in range(2)]
                    UK = ["p6U%d" % i for i in range(2)]
                    YK = ["p6Y%d" % i for i in range(2)]
                    cnt6 = [0, 0, 0]

                    def load_w(e_):
                        wb = e_ % 2
                        s.dma("sp", weg[wb][:], wview(weg_b[e_]), reads=["wbg%d" % e_], writes=["weg%d" % wb])
                        s.dma("sp", weu[wb][:], wview(weu_b[e_]), reads=["wbu%d" % e_], writes=["weu%d" % wb])
                        s.dma("sp", wed[wb][:], wview(wed_b[e_]), reads=["wbd%d" % e_], writes=["wed%d" % wb])
                        for kt in range(4):
                            tt("pool", wed[wb][:, kt, :], wed[wb][:, kt, :], g2b[:], ALU.mult, ["wed%d" % wb, "g2b"], ["wed%d" % wb])

                    def ph_T(e_):
                        wb = e_ % 2
                        for blk in range(NB):
                            u = cnt6[0] % 2
                            cnt6[0] += 1
                            r0 = e_ * CAP + blk * 128
                            s.dma("sp", xe[u][:], Xd[r0:r0 + 128, :], writes=["xe%d" % u])
                            for kt in range(8):
                                tr(TX[u][:, kt, :], xe[u][:, kt * 128:(kt + 1) * 128], ident_b[:], ["xe%d" % u, "ident_b"], [], x=[TXK[u]])
                            cp("act" if u == 0 else "dve", XeT[wb][:, :, blk * 128:(blk + 1) * 128], TX[u][:, :, :], [], ["XeT%d" % wb], x=[TXK[u]])

                    def ph_GU(e_):
                        wb = e_ % 2
                        for nt in range(4):
                            ncol = slice(nt * 128, (nt + 1) * 128)
                            for (c0, c1) in chunks:
                                u = cnt6[1] % 2
                                cnt6[1] += 1
                                n = c1 - c0
                                for kt in range(8):
                                    mm(GB[u][:, 0:n], weg[wb][:, kt, ncol], XeT[wb][:, kt, c0:c1], kt == 0, kt == 7, ["weg%d" % wb, "XeT%d" % wb], [], x=[GK[u]])
                                for kt in range(8):
                                    mm(UB[u][:, 0:n], weu[wb][:, kt, ncol], XeT[wb][:, kt, c0:c1], kt == 0, kt == 7, ["weu%d" % wb, "XeT%d" % wb], [], x=[UK[u]])
                                act(sgm[u][:, 0:n], GB[u][:, 0:n], AF.Silu, [], ["sgm%d" % u], x=[GK[u]])
                                tt("dve", hT[wb][:, nt, c0:c1], sgm[u][:, 0:n], UB[u][:, 0:n], ALU.mult, ["sgm%d" % u], ["hT%d" % wb], x=[UK[u]])

                    def ph_D(e_):
                        wb = e_ % 2
                        for blk in range(NB):
                            yu = blk % 2
                            r0 = e_ * CAP + blk * 128
                            for half in range(2):
                                u = cnt6[2] % 2
                                cnt6[2] += 1
                                hcol = slice(half * 512, (half + 1) * 512)
                                for kt in range(4):
                                    mm(YB[u][:, :], hT[wb][:, kt, blk * 128:(blk + 1) * 128], wed[wb][:, kt, hcol], kt == 0, kt == 3,
                                       ["hT%d" % wb, "wed%d" % wb], [], x=[YK[u]])
                                cp("act" if half == 0 else "dve", ye[yu][:, hcol], YB[u][:, :], [], ["ye%d" % yu], x=[YK[u]])
                            s.dma("pool", Yd[r0:r0 + 128, :], ye[yu][:], reads=["ye%d" % yu])

                    precast(96)
                    load_w(0)
                    ph_T(0)
                    for e_ in range(NEXP):
                        if e_ + 1 < NEXP:
                            load_w(e_ + 1)
                        ph_GU(e_)
                        if e_ + 1 < NEXP:
                            ph_T(e_ + 1)
                        ph_D(e_)
                    s.barrier()
                    print("P6 done: deadlock-free", s.check_deadlock(), s.n_ins, s.n_wait, s.cnt)
                with ExitStack() as st:
                    yk = [[sb(st, "yk%d_%d" % (k_, i), [128, D], BF16) for i in range(2)] for k_ in range(2)]
                    ot = [sb(st, "ot%d" % i, [128, D], F32) for i in range(2)]
                    junk4 = sb(st, "junk4", [128, D], F32)
                    st4 = [sb(st, "st4_%d" % i, [128, 4], F32) for i in range(2)]
                    for k_ in range(2):
                        for i in range(2):
                            s.op("pool", lambda e, k_=k_, i=i: e.memset(yk[k_][i][:], 0.0), writes=["yk%d_%d" % (k_, i)])
                    for i in range(NT):
                        u = i % 2
                        tokc = slice(i * 128, (i + 1) * 128)
                        hk = "hres%d" % i
                        for k_ in range(2):
                            dma_fn("pool", lambda e, k_=k_, u=u, i=i: e.indirect_dma_start(
                                out=yk[k_][u][:, :], out_offset=None, in_=Yd[:, :],
                                in_offset=bass.IndirectOffsetOnAxis(ap=SLi[:, 2 * i + k_:2 * i + k_ + 1], axis=0), bounds_check=bc_reg, oob_is_err=False),
                                ["SLi"], ["yk%d_%d" % (k_, u)])
                            stt("dve", hres[:, i, :], yk[k_][u][:], w12[:, i, k_:k_ + 1], hres[:, i, :], ALU.mult, ALU.add,
                                ["yk%d_%d" % (k_, u), "w12", hk], [hk])
                        s.op("pool", lambda e, u=u: e.memset(st4[u][:], 0.0), writes=["st4_%d" % u])
                        act(junk4[:], hres[:, i, :], AF.Square, [hk, "st4_%d" % u], ["junk4", "st4_%d" % u], accum_out=st4[u][:, 0:1])
                        act(st4[u][:, 1:2], st4[u][:, 0:1], AF.Sqrt, ["st4_%d" % u, "eps_c"], ["st4_%d" % u], scale=1.0 / D, bias=eps_c[:, 0:1])
                        s.op("dve", lambda e, u=u: e.reciprocal(out=st4[u][:, 2:3], in_=st4[u][:, 1:2]), reads=["st4_%d" % u], writes=["st4_%d" % u])
                        stt("dve", ot[u][:], hres[:, i, :], st4[u][:, 2:3], gfb[:], ALU.mult, ALU.mult, [hk, "st4_%d" % u, "gfb"], ["ot%d" % u])
                        s.dma("sp", y_d[smp, tokc, :], ot[u][:], reads=["ot%d" % u])
                    s.barrier()
        s.barrier(final=True)
    return nc


def _consts():
    ident = np.eye(128, dtype=np.float32)
    idx = np.arange(128)
    same = (idx[:, None] // 64) == (idx[None, :] // 64)
    tri = np.zeros((6, 128, 128), np.float32)
    tri[0] = same & (idx[:, None] <= idx[None, :])
    tri[1] = same & (idx[:, None] > idx[None, :])
    tri[2] = same & (idx[:, None] >= idx[None, :])
    tri[3] = same & (idx[:, None] < idx[None, :])
    tri[4] = idx[:, None] < idx[None, :]
    tri[5] = 1.0
    t = np.arange(S)
    pos_r = (t // GW).astype(np.float32)
    pos_c = (t % GW).astype(np.float32)
    inv = (10000.0 ** (-np.arange(32, dtype=np.float32) / 32)).astype(np.float32)
    ang = np.zeros((128, S), np.float32)
    ang[0:32] = inv[:, None] * pos_r[None, :]
    ang[32:64] = inv[:, None] * pos_r[None, :]
    ang[64:96] = inv[:, None] * pos_c[None, :]
    ang[96:128] = inv[:, None] * pos_c[None, :]
    cos = np.cos(ang).astype(np.float32)
    sin = np.sin(ang).astype(np.float32)
    sgn = np.ones((128, 1), np.float32)
    sgn[0:32] = -1
    sgn[64:96] = -1
    sin = sin * sgn
    perm = np.concatenate([np.arange(32, 64), np.arange(0, 32), np.arange(96, 128), np.arange(64, 96)])
    return ident, tri, cos, sin, perm


def _na_tables(rpb):
    a = (np.arange(128) // 64)[:, None, None, None]
    kc = (np.arange(128) % 64)[:, None, None, None]
    p = np.arange(8)[None, :, None, None]
    j = np.arange(4)[None, None, :, None]
    qc = np.arange(64)[None, None, None, :]
    ridx = 2 * j + a - p + 7
    cstart = np.clip(qc - 8, 0, 48)
    valid = (kc >= cstart) & (kc < cstart + 16) & (ridx >= 0) & (ridx <= 14)
    valid = np.broadcast_to(valid, (128, 8, 4, 64))
    cidx = np.clip(kc - qc + 15, 0, 30)
    ridx_c = np.clip(ridx, 0, 14)
    ridx_b = np.broadcast_to(ridx_c, (128, 8, 4, 64))
    cidx_b = np.broadcast_to(cidx, (128, 8, 4, 64))
    g = rpb[:, ridx_b, cidx_b]
    g = np.where(valid[None], g, np.float32(0.0)).astype(np.float32)
    g = g.reshape(4, 2, 128, 8, 256).transpose(0, 2, 3, 1, 4)
    mask = np.where(valid, np.float32(0.0), np.float32(MASKV)).astype(np.float32).reshape(128, 8, 256)
    return np.ascontiguousarray(g), np.ascontiguousarray(mask)


_NC_CACHE = {}


def kernel(x, c, ctx, c_ctx, w_mod, b_mod, norm_attn_g, norm_ffn_g, w_in, w_gla_a2, b_gla_a2,
           gla_norm_g, na_rpb, w_na_o, w_gla_o, w_out, w_group, b_group, w_expert, b_expert,
           w_exp_gate, w_exp_up, w_exp_down, final_norm_g):
    f = lambda a: np.ascontiguousarray(np.asarray(a, dtype=np.float32))
    x, c, ctx, c_ctx = f(x), f(c), f(ctx), f(c_ctx)
    ident, tri, cos, sin, perm = _consts()
    w_in0 = f(w_in)[0]
    gq = w_in0[:, C_GQ:C_GQ + 512].reshape(D, 4, 128)[:, :, perm].reshape(D, 512)
    gk = w_in0[:, C_GK:C_GK + 512].reshape(D, 4, 128)[:, :, perm].reshape(D, 512)
    w_rope = np.ascontiguousarray(np.concatenate([gq, gk], axis=1))
    w_a2b = np.ascontiguousarray(np.concatenate(
        [f(w_gla_a2)[0].transpose(1, 0, 2), f(b_gla_a2)[0][None]], axis=0))
    nab, nam = _na_tables(f(na_rpb)[0])
    w_rt = np.ascontiguousarray(np.concatenate([f(w_group)[0], f(w_expert)[0]], axis=1))
    b_rt = np.ascontiguousarray(np.concatenate([f(b_group)[0], f(b_expert)[0]], axis=0))
    shared = {
        "w_mod": f(w_mod)[0], "b_mod": f(b_mod)[0], "g_attn": f(norm_attn_g)[0], "g_ffn": f(norm_ffn_g)[0],
        "g_fin": f(final_norm_g), "gla_g": f(gla_norm_g)[0], "w_in": w_in0, "w_rope": w_rope, "w_a2b": w_a2b,
        "w_na_o": f(w_na_o)[0], "w_gla_o": f(w_gla_o)[0], "w_out": f(w_out)[0], "w_rt": w_rt, "b_rt": b_rt,
        "w_eg": f(w_exp_gate)[0], "w_eu": f(w_exp_up)[0], "w_ed": f(w_exp_down)[0],
        "ident": ident, "tri": tri, "rope_cos": cos, "rope_sin": sin, "na_bias": nab, "na_mask": nam,
        "ebase": np.ascontiguousarray(np.broadcast_to((np.arange(NEXP, dtype=np.float32) * MOE_CAP)[None, :], (128, NEXP))),
    }
    n = 8
    NS = x.shape[0] // n
    if "nc" not in _NC_CACHE:
        _NC_CACHE["nc"] = build_nc(NS)
    nc = _NC_CACHE["nc"]
    in_maps = []
    for i in range(n):
        m = dict(shared)
        m["x"] = x[i * NS:(i + 1) * NS]
        m["ctx"] = ctx[i * NS:(i + 1) * NS]
        m["cc"] = np.ascontiguousarray(np.concatenate([c[i * NS:(i + 1) * NS], c_ctx[None]], axis=0))
        in_maps.append(m)
    res = run_bass_kernel_spmd(nc, in_maps, core_ids=list(range(n)))
    return np.concatenate([r["y"] for r in res.results], axis=0)
```

```python
import os
import numpy as np
import concourse.bass as bass
import concourse.mybir as mybir
from concourse.bass_utils import run_bass_kernel_spmd
from contextlib import ExitStack

F32 = mybir.dt.float32
BF16 = mybir.dt.bfloat16
I32 = mybir.dt.int32
ALU = mybir.AluOpType
AF = mybir.ActivationFunctionType
AX = mybir.AxisListType

D = 1024
S = 2048
CT = 256
NT = 16
NTC = 2
NTA = NT + NTC
GW = 64
EPS = 1e-6
NEXP = 32
DE = 512
C_NAQ, C_NAK, C_NAV, C_GQ, C_GK, C_GV, C_GG, C_AL, C_M8, C_M9 = 0, 512, 1024, 1536, 2048, 2560, 3584, 4608, 4640, 5664
MASKV = -30000.0
MOE_CAP = 640


class Sched:
    ENGS = ("pe", "act", "dve", "pool", "sp")
    HND = {"pe": "tensor", "act": "scalar", "dve": "vector", "pool": "gpsimd", "sp": "sync"}

    def __init__(self, nc, es, n_dma_sems=40):
        self.nc = nc
        self.sem = {e: es.enter_context(nc.semaphore("s_" + e)) for e in self.ENGS}
        self.cnt = {e: 0 for e in self.ENGS}
        self.pending = {e: False for e in self.ENGS}
        self.waited = {e: {} for e in self.ENGS}
        self.dsem = [es.enter_context(nc.semaphore("s_dma%d" % i)) for i in range(n_dma_sems)]
        self.dcnt = [0] * n_dma_sems
        self.dnext = 0
        self.n_fg = n_dma_sems - 8
        self.bnext = 0
        self.lastw = {}
        self.reads = {}
        self.semobj = {}
        for e in self.ENGS:
            self.semobj[("e", e)] = self.sem[e]
        for i, sm in enumerate(self.dsem):
            self.semobj[("d", i)] = sm
        self.n_ins = 0
        self.n_wait = 0

    def _deps(self, eng, reads, writes, excl=()):
        toks = {}

        def add(t):
            if t is None:
                return
            k, v = t
            if toks.get(k, -1) < v:
                toks[k] = v
        for b in reads:
            add(self.lastw.get(b))
        for b in writes:
            add(self.lastw.get(b))
            for t in self.reads.get(b, ()):
                add(t)
        for b in excl:
            t = self.lastw.get(b)
            if t is not None and t[0] != ("e", eng):
                add(t)
        out = []
        for k, v in toks.items():
            if k == ("e", eng):
                if eng == "pe":
                    continue
                if v <= self.cnt[eng] - 2:
                    continue
            if self.waited[eng].get(k, -1) >= v:
                continue
            self.waited[eng][k] = v
            out.append((k, v))
        return out

    def _commit(self, tok, reads, writes):
        for b in writes:
            self.lastw[b] = tok
            self.reads[b] = []
        for b in reads:
            self.reads.setdefault(b, []).append(tok)

    def check_deadlock(self):
        pos = {e: 0 for e in self.ENGS}
        val = {}
        prog = True
        while prog:
            prog = False
            for e in self.ENGS:
                lst = self.log[e]
                while pos[e] < len(lst):
                    waits, inc = lst[pos[e]]
                    if all(val.get(k, 0) >= v for k, v in waits):
                        if inc is not None:
                            val[inc[0]] = val.get(inc[0], 0) + inc[1]
                        pos[e] += 1
                        prog = True
                    else:
                        break
        stuck = {e: (pos[e], len(self.log[e])) for e in self.ENGS if pos[e] < len(self.log[e])}
        for e in stuck:
            waits, inc = self.log[e][pos[e]]
            print("STUCK", e, pos[e], [(k, v, val.get(k, 0)) for k, v in waits])
        return not stuck

    def _emit1(self, eng, waits, fn, inc):
        if not hasattr(self, "log"):
            self.log = {e: [] for e in self.ENGS}
        self.log[eng].append((list(waits), inc if fn is not None else None))
        engh = getattr(self.nc, self.HND[eng])
        for k, v in waits:
            engh.wait_ge(self.semobj[k], v)
            self.n_wait += 1
        if fn is None:
            return
        ins = fn(engh)
        if inc is not None:
            ins.then_inc(self.semobj[inc[0]], inc[1])
        self.n_ins += 1

    def op(self, eng, fn, reads=(), writes=(), inc=True, excl=()):
        waits = self._deps(eng, reads, writes, excl)
        if inc:
            self.cnt[eng] += 1
            tok = (("e", eng), self.cnt[eng])
            self.pending[eng] = False
        else:
            tok = (("e", eng), self.cnt[eng] + 1)
            self.pending[eng] = True
        self._emit1(eng, waits, fn, (("e", eng), 1) if inc else None)
        self._commit(tok, reads, writes)
        for b_ in excl:
            self.lastw[b_] = tok
        return tok

    def dma(self, eng, out, in_, reads=(), writes=(), bg=False, **kw):
        if bg:
            i = self.n_fg + self.bnext
            self.bnext = (self.bnext + 1) % 8
        else:
            i = self.dnext
            self.dnext = (self.dnext + 1) % self.n_fg
        k = ("d", i)
        waits = self._deps(eng, reads, writes)
        if self.dcnt[i] > 0 and self.waited[eng].get(k, -1) < self.dcnt[i]:
            self.waited[eng][k] = self.dcnt[i]
            waits.append((k, self.dcnt[i]))
        self.dcnt[i] += 16
        tok = (k, self.dcnt[i])
        self._emit1(eng, waits, lambda e: e.dma_start(out=out, in_=in_, **kw), (k, 16))
        self._commit(tok, reads, writes)
        return tok

    def barrier(self, final=False):
        assert not any(self.pending.values())
        allt = [(("e", e), self.cnt[e]) for e in self.ENGS if self.cnt[e] > 0]
        allt += [(("d", i), c) for i, c in enumerate(self.dcnt) if c > 0 and (i < self.n_fg or final)]
        for e in self.ENGS:
            waits = []
            for k, v in allt:
                if k == ("e", e):
                    continue
                if self.waited[e].get(k, -1) >= v:
                    continue
                self.waited[e][k] = v
                waits.append((k, v))
            self._emit1(e, waits, None, None)
        keep = {k: t for k, t in self.lastw.items() if t[0][0] == "d" and t[0][1] >= self.n_fg}
        self.lastw = {} if final else keep
        self.reads = {}


def build_nc(NS=2, dbg=None, stop_after=None):
    nc = bass.Bass("TRN2", target_bir_lowering=False)

    def din(name, shape, dt=F32):
        return nc.dram_tensor(name, list(shape), dt, kind="ExternalInput").ap()

    x_d = din("x", [NS, S, D])
    ctx_d = din("ctx", [NS, CT, D])
    cc_d = din("cc", [NS + 1, D])
    w_mod_d = din("w_mod", [D, 6 * D])
    b_mod_d = din("b_mod", [6 * D])
    g_attn_d = din("g_attn", [D])
    g_ffn_d = din("g_ffn", [D])
    g_fin_d = din("g_fin", [D])
    gla_g_d = din("gla_g", [256])
    w_in_d = din("w_in", [D, 6688])
    w_rope_d = din("w_rope", [D, 1024])
    w_a2b_d = din("w_a2b", [17, 2, 512])
    w_nao_d = din("w_na_o", [512, D])
    w_glo_d = din("w_gla_o", [D, D])
    w_out_d = din("w_out", [D, D])
    w_rt_d = din("w_rt", [D, 36])
    b_rt_d = din("b_rt", [36])
    w_eg_d = din("w_eg", [NEXP, D, DE])
    w_eu_d = din("w_eu", [NEXP, D, DE])
    w_ed_d = din("w_ed", [NEXP, DE, D])
    ident_d = din("ident", [128, 128])
    tri_d = din("tri", [6, 128, 128])
    ebase_d = din("ebase", [128, 32])
    cos_d = din("rope_cos", [128, S])
    sin_d = din("rope_sin", [128, S])
    nab_d = din("na_bias", [4, 128, 8, 2, 256])
    nam_d = din("na_mask", [128, 8, 256])
    y_d = nc.dram_tensor("y", [NS, S, D], F32, kind="ExternalOutput").ap()
    modv_d = nc.dram_tensor("modv", [NS + 1, 6 * D], F32).ap()
    dbg_d = {}
    if dbg:
        for name, shape in dbg.items():
            dbg_d[name] = nc.dram_tensor("dbg_" + name, list(shape), F32, kind="ExternalOutput").ap()

    with ExitStack() as es:
        s = Sched(nc, es)

        used_names = {}

        def uniq(name):
            k = used_names.get(name, 0)
            used_names[name] = k + 1
            return name if k == 0 else "%s_r%d" % (name, k)

        def sb(st, name, shape, dt):
            return st.enter_context(nc.sbuf_tensor(uniq(name), list(shape), dt))

        def pst(st, name, shape, dt=F32):
            return st.enter_context(nc.psum_tensor(uniq(name), list(shape), dt))

        def mm(out, lhsT, rhs, start, stop, reads, writes, inc=None, x=()):
            if inc is None:
                inc = stop
            s.op("pe", lambda e: e.matmul(out, lhsT, rhs, start=start, stop=stop),
                 reads=reads, writes=writes, inc=inc, excl=x)

        def tr(out, in_, idn, reads, writes, x=()):
            s.op("pe", lambda e: e.transpose(out, in_, idn), reads=reads, writes=writes, excl=x)

        def act(out, in_, func, reads, writes, x=(), **kw):
            s.op("act", lambda e: e.activation(out=out, in_=in_, func=func, **kw), reads=reads, writes=writes, excl=x)

        def tt(eng, out, in0, in1, op, reads, writes, x=()):
            s.op(eng, lambda e: e.tensor_tensor(out=out, in0=in0, in1=in1, op=op), reads=reads, writes=writes, excl=x)

        def ts(eng, out, in0, s1, s2, op0, op1, reads, writes, x=()):
            if s2 is None:
                s.op(eng, lambda e: e.tensor_scalar(out=out, in0=in0, scalar1=s1, scalar2=None, op0=op0),
                     reads=reads, writes=writes, excl=x)
            else:
                s.op(eng, lambda e: e.tensor_scalar(out=out, in0=in0, scalar1=s1, scalar2=s2, op0=op0, op1=op1),
                     reads=reads, writes=writes, excl=x)

        def stt(eng, out, in0, scalar, in1, op0, op1, reads, writes, x=()):
            s.op(eng, lambda e: e.scalar_tensor_tensor(out=out, in0=in0, scalar=scalar, in1=in1, op0=op0, op1=op1),
                 reads=reads, writes=writes, excl=x)

        def cp(eng, out, in_, reads, writes, x=()):
            if eng == "act":
                s.op("act", lambda e: e.copy(out=out, in_=in_), reads=reads, writes=writes, excl=x)
            else:
                s.op(eng, lambda e: e.tensor_copy(out=out, in_=in_), reads=reads, writes=writes, excl=x)

        def dump(name, src_ap, reads, dst=None):
            if name in dbg_d:
                s.dma("pool", dbg_d[name] if dst is None else dst, src_ap, reads=reads)

        def wview(ap2d):
            return ap2d.rearrange("(kt p) n -> p kt n", p=128)

        ident_f = sb(es, "ident_f", [128, 128], F32)
        ident_b = sb(es, "ident_b", [128, 128], BF16)
        tri_f = sb(es, "tri_f", [128, 4, 128], F32)
        ones_b = sb(es, "ones_b", [128, 128], BF16)
        eps_c = sb(es, "eps_c", [128, 1], F32)
        one_c = sb(es, "one_c", [128, 1], F32)
        s.dma("sp", ident_f[:], ident_d, writes=["ident_f"])
        s.dma("pool", ident_b[:], ident_d, writes=["ident_b"])
        s.dma("sp", tri_f[:], tri_d[0:4].rearrange("m s t -> s m t"), writes=["tri"])
        s.op("pool", lambda e: e.memset(ones_b[:], 1.0), writes=["ones_b"])
        s.op("pool", lambda e: e.memset(eps_c[:], EPS), writes=["eps_c"])
        s.op("pool", lambda e: e.memset(one_c[:], 1.0), writes=["one_c"])

        with ExitStack() as st:
            ccs = sb(st, "ccs", [NS + 1, D], F32)
            scT = sb(st, "scT", [128, 8, NS + 1], F32)
            modsb = sb(st, "modsb", [NS + 1, 6 * D], F32)
            bmod = sb(st, "bmod", [NS + 1, 6 * D], F32)
            wm = [sb(st, "wm%d" % i, [128, 8, 512], F32) for i in range(2)]
            psA = pst(st, "p0a", [128, 512])
            psB = [pst(st, "p0b%d" % i, [128, 512]) for i in range(2)]
            R = NS + 1
            s.dma("sp", ccs[:], cc_d, writes=["ccs"])
            s.dma("sp", bmod[:], b_mod_d.partition_broadcast(R), writes=["bmod"])
            act(ccs[:], ccs[:], AF.Silu, ["ccs"], ["ccs"])
            for kt in range(8):
                tr(psA[:, kt * R:(kt + 1) * R], ccs[:, kt * 128:(kt + 1) * 128], ident_f[0:R, 0:R],
                   ["ccs", "ident_f"], ["p0a"])
            cp("dve", scT[:].rearrange("p k r -> p (k r)"), psA[:, 0:8 * R], ["p0a"], ["scT"])
            for cb in range(12):
                w = wm[cb % 2]
                s.dma("sp", w[:], wview(w_mod_d[:, cb * 512:(cb + 1) * 512]), writes=["wm%d" % (cb % 2)])
                ps = psB[cb % 2]
                for kt in range(8):
                    mm(ps[0:R, :], scT[:, kt, :], w[:, kt, :], kt == 0, kt == 7,
                       ["scT", "wm%d" % (cb % 2)], ["p0b%d" % (cb % 2)])
                tt("dve", modsb[:, cb * 512:(cb + 1) * 512], ps[0:R, :], bmod[:, cb * 512:(cb + 1) * 512], ALU.add,
                   ["p0b%d" % (cb % 2), "bmod"], ["modsb"])
            s.dma("sp", modv_d, modsb[:], reads=["modsb"], writes=["modv"])
            dump("modv", modsb[:], ["modsb"])
            s.barrier()
        if stop_after == "P0":
            s.barrier()
            return nc

        QSCALE = 128.0 ** -0.5
        weg_b = nc.dram_tensor("weg_bf", [NEXP, D, DE], BF16).ap()
        weu_b = nc.dram_tensor("weu_bf", [NEXP, D, DE], BF16).ap()
        wed_b = nc.dram_tensor("wed_bf", [NEXP, DE, D], BF16).ap()

        def _precast_gen():
            for e_ in range(NEXP):
                for nm, dst, srcw, r in (("g", weg_b, w_eg_d, 4), ("u", weu_b, w_eu_d, 4), ("d", wed_b, w_ed_d, 2)):
                    s.dma("pool", dst[e_].rearrange("(a r) n -> a (r n)", r=r), srcw[e_].rearrange("(a r) n -> a (r n)", r=r),
                          writes=["wb%s%d" % (nm, e_)], bg=True)
                    yield
        _pc = _precast_gen()

        def precast(n):
            for _ in range(n):
                next(_pc, None)
        bc_reg = nc.gpsimd.alloc_register("bcreg")
        nc.gpsimd.reg_mov(bc_reg, NEXP * MOE_CAP - 1)
        h1_d = nc.dram_tensor("h1_scr", [NS, S, D], F32).ap()

        def bank_set(st, pfx):
            return [pst(st, "%s%d" % (pfx, i), [128, 512]) for i in range(7)]

        for smp in range(NS):
            with ExitStack() as sa:
                aT = sb(sa, "aT", [128, 8, S], BF16)
                acT = sb(sa, "acT", [128, 8, CT], BF16)
                oglT = sb(sa, "oglT", [128, 8, S], BF16)

                with ExitStack() as st:
                    s1b = sb(st, "s1b", [128, D], F32)
                    sh1b = sb(st, "sh1b", [128, D], F32)
                    s1c = sb(st, "s1c", [128, D], F32)
                    sh1c = sb(st, "sh1c", [128, D], F32)
                    gab = sb(st, "gab", [128, D], F32)
                    tmpv = sb(st, "tmpv", [128, D], F32)
                    xt = [sb(st, "xt%d" % i, [128, D], F32) for i in range(2)]
                    xm = [sb(st, "xm%d" % i, [128, D], F32) for i in range(2)]
                    xn = [sb(st, "xn%d" % i, [128, D], BF16) for i in range(2)]
                    stat = [sb(st, "stat%d" % i, [128, 4], F32) for i in range(2)]
                    psT = [pst(st, "p1t%d" % i, [128, 8, 128], BF16) for i in range(2)]
                    s.dma("sp", gab[:], g_attn_d.partition_broadcast(128), writes=["gab"])
                    for row, s1, sh, nm in ((smp, s1b, sh1b, "l"), (NS, s1c, sh1c, "c")):
                        s.dma("sp", tmpv[:], modv_d[row, D:2 * D].partition_broadcast(128), writes=["tmpv"])
                        stt("dve", s1[:], tmpv[:], 1.0, gab[:], ALU.add, ALU.mult, ["tmpv", "gab"], ["s1" + nm])
                        s.dma("sp", sh[:], modv_d[row, 0:D].partition_broadcast(128), writes=["sh1" + nm])
                    for i in range(NTA):
                        b = i % 2
                        lat = i < NT
                        src = x_d[smp, i * 128:(i + 1) * 128, :] if lat else ctx_d[smp, (i - NT) * 128:(i - NT + 1) * 128, :]
                        nm = "l" if lat else "c"
                        s1, sh = (s1b, sh1b) if lat else (s1c, sh1c)
                        s.dma("sp", xt[b][:], src, writes=["xt%d" % b])
                        s.op("pool", lambda e, b=b: e.memset(stat[b][:], 0.0), writes=["stat%d" % b])
                        act(xm[b][:], xt[b][:], AF.Square, ["xt%d" % b, "stat%d" % b], ["xm%d" % b, "stat%d" % b],
                            accum_out=stat[b][:, 0:1])
                        act(stat[b][:, 1:2], stat[b][:, 0:1], AF.Sqrt, ["stat%d" % b, "eps_c"], ["stat%d" % b],
                            scale=1.0 / D, bias=eps_c[:, 0:1])
                        s.op("dve", lambda e, b=b: e.reciprocal(out=stat[b][:, 2:3], in_=stat[b][:, 1:2]),
                             reads=["stat%d" % b], writes=["stat%d" % b])
                        stt("dve", xm[b][:], xt[b][:], stat[b][:, 2:3], s1[:], ALU.mult, ALU.mult,
                            ["xt%d" % b, "stat%d" % b, "s1" + nm], ["xm%d" % b])
                        tt("pool", xn[b][:], xm[b][:], sh[:], ALU.add, ["xm%d" % b, "sh1" + nm], ["xn%d" % b])
                        for kt in range(8):
                            tr(psT[b][:, kt, :], xn[b][:, kt * 128:(kt + 1) * 128], ident_b[:],
                               ["xn%d" % b, "ident_b"], ["p1t%d" % b])
                        if lat:
                            cp("act", aT[:, :, i * 128:(i + 1) * 128], psT[b][:, :, :], ["p1t%d" % b], ["aT"])
                        else:
                            cp("act", acT[:, :, (i - NT) * 128:(i - NT + 1) * 128], psT[b][:, :, :], ["p1t%d" % b], ["acT"])
                    if smp == 0:
                        dump("aT", aT[:, :, 0:256], ["aT"])
                    s.barrier()
                if stop_after == "P1":
                    s.barrier()
                    return nc

                with ExitStack() as st:
                    ropeC = sb(st, "ropeC", [128, S], BF16)
                    ropeS = sb(st, "ropeS", [128, S], BF16)
                    alT = [sb(st, "alT%d" % d_, [16, S + CT], BF16) for d_ in range(2)]
                    wal = sb(st, "wal", [128, 8, 32], BF16)
                    wa2 = sb(st, "wa2", [16, 2, 512], BF16)
                    ba2 = sb(st, "ba2", [1, 2, 512], BF16)
                    gnb = sb(st, "gnb", [128, 256], F32)
                    wq = sb(st, "wq", [128, 8, 128], BF16)
                    wqs = sb(st, "wqs", [128, 8, 128], BF16)
                    wk = sb(st, "wk", [128, 8, 128], BF16)
                    wks = sb(st, "wks", [128, 8, 128], BF16)
                    wv = sb(st, "wv", [128, 8, 256], BF16)
                    wg = sb(st, "wg", [128, 8, 256], BF16)
                    qrT = sb(st, "qrT", [128, S], BF16)
                    krT = sb(st, "krT", [128, S], BF16)
                    krk = sb(st, "krk", [128, NTA, 128], BF16)
                    vtk = sb(st, "vtk", [128, NTA, 256], BF16)
                    sgt = sb(st, "sgt", [128, NT, 256], BF16)
                    qe = [sb(st, "qe%d" % d_, [128, S], BF16) for d_ in range(2)]
                    ke = [sb(st, "ke%d" % d_, [128, S], BF16) for d_ in range(2)]
                    kd = [sb(st, "kd%d" % d_, [128, NTA, 128], BF16) for d_ in range(2)]
                    dec = sb(st, "dec", [128, 2, NTA, 2], F32)
                    oacc = sb(st, "oacc", [128, NT, 256], F32)
                    S32 = [sb(st, "S32_%d" % d_, [128, 256], F32) for d_ in range(2)]
                    S16 = [[sb(st, "S16_%d_%d" % (d_, v_), [128, 256], BF16) for v_ in range(2)] for d_ in range(2)]
                    t1 = [sb(st, "t1_%d" % i, [128, 512], F32) for i in range(2)]
                    t2 = [sb(st, "t2_%d" % i, [128, 512], F32) for i in range(2)]
                    Dt = [sb(st, "Dt0", [128, 512], F32)]
                    Di = [sb(st, "Di0", [128, 512], F32)]
                    EK = [sb(st, "EK0", [128, 512], F32)]
                    att = [sb(st, "att%d" % i, [128, 128], BF16) for i in range(2)]
                    junk = sb(st, "junk2", [128, 256], F32)
                    onr = [sb(st, "onr%d" % i, [128, 256], F32) for i in range(2)]
                    rbf = [sb(st, "rbf%d" % i, [128, 256], BF16) for i in range(2)]
                    st2 = [sb(st, "st2_%d" % i, [128, 4], F32) for i in range(2)]
                    B = [pst(st, "p2b%d" % i, [128, 512]) for i in range(6)]
                    BK = ["p2B%d" % i for i in range(6)]
                    pT = [pst(st, "p2t%d" % i, [128, 8, 128], BF16) for i in range(2)]
                    TK = ["p2T0", "p2T1"]

                    s.dma("pool", ropeC[:], cos_d, writes=["ropeC"])
                    s.dma("pool", ropeS[:], sin_d, writes=["ropeS"])
                    s.dma("pool", wal[:], wview(w_in_d[:, C_AL:C_AL + 32]), writes=["wal"])
                    s.dma("pool", wa2[:], w_a2b_d[0:16], writes=["wa2"])
                    s.dma("pool", ba2[:], w_a2b_d[16:17], writes=["ba2"])
                    s.dma("sp", gnb[:], gla_g_d.partition_broadcast(128), writes=["gnb"])
                    for tb in range(5):
                        if tb < 4:
                            rhs_of = lambda kt, tb=tb: aT[:, kt, tb * 512:(tb + 1) * 512]
                            n, c0, rk = 512, tb * 512, "aT"
                        else:
                            rhs_of = lambda kt: acT[:, kt, :]
                            n, c0, rk = CT, S, "acT"
                        for d_ in range(2):
                            for kt in range(8):
                                mm(B[d_][0:16, 0:n], wal[:, kt, d_ * 16:(d_ + 1) * 16], rhs_of(kt), kt == 0, kt == 7,
                                   ["wal", rk], [], x=[BK[d_]])
                            cp("act" if d_ == 0 else "dve", alT[d_][:, c0:c0 + n], B[d_][0:16, 0:n], [], ["alT%d" % d_], x=[BK[d_]])
                    if stop_after == "P2a":
                        s.barrier()
                        return nc

                    def load_head_w(h):
                        s.dma("pool", wq[:], wview(w_in_d[:, C_GQ + 128 * h:C_GQ + 128 * (h + 1)]), writes=["wq"])
                        s.dma("pool", wqs[:], wview(w_rope_d[:, 128 * h:128 * (h + 1)]), writes=["wqs"])
                        s.dma("pool", wk[:], wview(w_in_d[:, C_GK + 128 * h:C_GK + 128 * (h + 1)]), writes=["wk"])
                        s.dma("pool", wks[:], wview(w_rope_d[:, 512 + 128 * h:512 + 128 * (h + 1)]), writes=["wks"])
                        s.dma("pool", wv[:], wview(w_in_d[:, C_GV + 256 * h:C_GV + 256 * (h + 1)]), writes=["wv"])
                        s.dma("pool", wg[:], wview(w_in_d[:, C_GG + 256 * h:C_GG + 256 * (h + 1)]), writes=["wg"])

                    load_head_w(0)
                    for h in range(4):
                        for tb in range(4):
                            cols = slice(tb * 512, (tb + 1) * 512)
                            for bi, (w, wn) in enumerate(((wq, "wq"), (wqs, "wqs"), (wk, "wk"), (wks, "wks"))):
                                for kt in range(8):
                                    mm(B[bi][:, :], w[:, kt, :], aT[:, kt, cols], kt == 0, kt == 7, [wn, "aT"], [], x=[BK[bi]])
                            stt("dve", t1[0][:], B[0][:, :], QSCALE, ropeC[:, cols], ALU.mult, ALU.mult, ["ropeC"], ["t1_0"], x=[BK[0]])
                            stt("dve", t2[0][:], B[1][:, :], QSCALE, ropeS[:, cols], ALU.mult, ALU.mult, ["ropeS"], ["t2_0"], x=[BK[1]])
                            tt("pool", qrT[:, cols], t1[0][:], t2[0][:], ALU.add, ["t1_0", "t2_0"], ["qrT"])
                            tt("dve", t1[1][:], B[2][:, :], ropeC[:, cols], ALU.mult, ["ropeC"], ["t1_1"], x=[BK[2]])
                            tt("dve", t2[1][:], B[3][:, :], ropeS[:, cols], ALU.mult, ["ropeS"], ["t2_1"], x=[BK[3]])
                            tt("pool", krT[:, cols], t1[1][:], t2[1][:], ALU.add, ["t1_1", "t2_1"], ["krT"])
                        if stop_after == "P2b":
                            s.barrier()
                            return nc
                        for i in range(NTA):
                            lat = i < NT
                            bv = 4 + (i % 2)
                            bg = 2 + (i % 2)
                            srcT, rk, c0 = (aT, "aT", i * 128) if lat else (acT, "acT", (i - NT) * 128)
                            for kt in range(8):
                                mm(B[bv][:, 0:256], srcT[:, kt, c0:c0 + 128], wv[:, kt, :], kt == 0, kt == 7, [rk, "wv"], [], x=[BK[bv]])
                            cp("act", vtk[:, i, :], B[bv][:, 0:256], [], ["vtk"], x=[BK[bv]])
                            if lat:
                                for kt in range(8):
                                    mm(B[bg][:, 0:256], srcT[:, kt, c0:c0 + 128], wg[:, kt, :], kt == 0, kt == 7, [rk, "wg"], [], x=[BK[bg]])
                                act(sgt[:, i, :], B[bg][:, 0:256], AF.Silu, [], ["sgt"], x=[BK[bg]])
                            else:
                                for kt in range(8):
                                    mm(B[bg][:, 0:128], srcT[:, kt, c0:c0 + 128], wk[:, kt, :], kt == 0, kt == 7, [rk, "wk"], [], x=[BK[bg]])
                                cp("dve", krk[:, i, :], B[bg][:, 0:128], [], ["krk"], x=[BK[bg]])
                        for i in range(NT):
                            u = i % 2
                            tr(pT[u][:, 0, :], krT[:, i * 128:(i + 1) * 128], ident_b[:], ["krT", "ident_b"], [], x=[TK[u]])
                            cp("dve", krk[:, i, :], pT[u][:, 0, :], [], ["krk"], x=[TK[u]])
                        if stop_after == "P2c":
                            s.barrier()
                            return nc
                        if h + 1 < 4:
                            load_head_w(h + 1)
                        precast(12)
                        groups = [(0, 4), (4, 4), (8, 4), (12, 4), (16, 2)]
                        hc = slice(128 * h, 128 * (h + 1))
                        for (t0, nt_) in groups:
                            lat = t0 < NT
                            n = nt_ * 128
                            gc = slice(t0 * 128, t0 * 128 + n)
                            for d_ in range(2):
                                bz, bb, be = B[d_], B[2 + d_], B[4 + d_]
                                xz, xb, xe_ = [BK[d_]], [BK[2 + d_]], [BK[4 + d_]]
                                az_, ex_, rz_, L_ = t1[0], t1[1], t2[0], t2[1]
                                for tl in range(nt_):
                                    tokc = slice((t0 + tl) * 128, (t0 + tl + 1) * 128)
                                    zc = slice(tl * 128, (tl + 1) * 128)
                                    mm(bz[:, zc], alT[d_][0:16, tokc], wa2[0:16, d_, hc], True, False, ["alT%d" % d_, "wa2"], [], inc=False, x=xz)
                                    mm(bz[:, zc], ones_b[0:1, 0:128], ba2[0:1, d_, hc], False, True, ["ones_b", "ba2"], [], x=xz)
                                act(az_[:, 0:n], bz[:, 0:n], AF.Abs, [], ["t1_0"], x=xz)
                                ts("dve", rz_[:, 0:n], bz[:, 0:n], -1.0, 0.0, ALU.mult, ALU.max, [], ["t2_0"], x=xz)
                                act(ex_[:, 0:n], az_[:, 0:n], AF.Exp, ["t1_0"], ["t1_1"], scale=-1.0)
                                act(ex_[:, 0:n], ex_[:, 0:n], AF.Ln, ["t1_1", "one_c"], ["t1_1"], bias=one_c[:, 0:1])
                                tt("pool", L_[:, 0:n], rz_[:, 0:n], ex_[:, 0:n], ALU.add, ["t2_0", "t1_1"], ["t2_1"])
                                for tl in range(nt_):
                                    zc = slice(tl * 128, (tl + 1) * 128)
                                    mm(bb[:, zc], L_[:, zc], tri_f[:, 2 * d_, :], True, True, ["t2_1", "tri"], [], x=xb)
                                for tl in range(nt_):
                                    zc = slice(tl * 128, (tl + 1) * 128)
                                    mm(be[:, zc], tri_f[:, 2 * d_ + 1, :], L_[:, zc], True, True, ["t2_1", "tri"], [], x=xe_)
                                act(Dt[0][:, 0:n], bb[:, 0:n], AF.Exp, [], ["Dt0"], scale=-1.0 / 16, x=xb)
                                if lat:
                                    act(Di[0][:, 0:n], bb[:, 0:n], AF.Exp, [], ["Di0"], scale=1.0 / 16, x=xb)
                                act(EK[0][:, 0:n], be[:, 0:n], AF.Exp, [], ["EK0"], scale=-1.0 / 16, x=xe_)
                                dsrc = Dt[0][:, 63:n:64] if d_ == 0 else Dt[0][:, 0:n:64]
                                cp("pool", dec[:, d_, t0:t0 + nt_, :].rearrange("p t c -> p (t c)"), dsrc, ["Dt0"], ["dec"])
                                tt("pool", kd[d_][:, t0:t0 + nt_, :], krk[:, t0:t0 + nt_, :], EK[0][:, 0:n].rearrange("p (t c) -> p t c", c=128),
                                   ALU.mult, ["krk", "EK0"], ["kd%d" % d_])
                                if lat:
                                    tt("dve", qe[d_][:, gc], qrT[:, gc], Dt[0][:, 0:n], ALU.mult, ["qrT", "Dt0"], ["qe%d" % d_])
                                    tt("dve", ke[d_][:, gc], krT[:, gc], Di[0][:, 0:n], ALU.mult, ["krT", "Di0"], ["ke%d" % d_])
                        if stop_after == "P2d":
                            s.barrier()
                            return nc
                        ver = [0, 0]
                        for d_ in range(2):
                            s.op("pool", lambda e, d_=d_: e.memset(S32[d_][:], 0.0), writes=["S32_%d" % d_])
                            s.op("pool", lambda e, d_=d_: e.memset(S16[d_][0][:], 0.0), writes=["S16_%d_0" % d_])

                        def kvmm(d_, i, half):
                            rows = slice(64 * half, 64 * half + 64)
                            bk = 2 + 2 * d_ + half
                            mm(B[bk][:, 0:256], kd[d_][rows, i, :], vtk[rows, i, :], True, True, ["kd%d" % d_, "vtk"], [], x=[BK[bk]])

                        def upd(d_, i, half):
                            bk = 2 + 2 * d_ + half
                            pkv = B[bk][:, 0:256]
                            nv = (ver[d_] + 1) % 2
                            stt("dve", S16[d_][nv][:], S32[d_][:], dec[:, d_, i, half:half + 1], pkv, ALU.mult, ALU.add,
                                ["S32_%d" % d_, "dec"], ["S16_%d_%d" % (d_, nv)], x=[BK[bk]])
                            stt("dve", S32[d_][:], S32[d_][:], dec[:, d_, i, half:half + 1], pkv, ALU.mult, ALU.add,
                                ["S32_%d" % d_, "dec"], ["S32_%d" % d_], x=[BK[bk]])
                            ver[d_] += 1

                        for i in (NT, NT + 1):
                            for half in (0, 1):
                                kvmm(0, i, half)
                                upd(0, i, half)
                        for i in (NT + 1, NT):
                            for half in (1, 0):
                                kvmm(1, i, half)
                                upd(1, i, half)
                        if smp == 0 and h == 0:
                            dump("s_f", S32[0][:], ["S32_0"])
                            dump("s_b", S32[1][:], ["S32_1"])
                        if stop_after == "P2e":
                            s.barrier()
                            return nc

                        done = [0] * NT

                        def lat_front(d_, i):
                            tokc = slice(i * 128, (i + 1) * 128)
                            pa = B[d_][:, 256:384]
                            po = B[d_][:, 0:256]
                            mm(pa, ke[d_][:, tokc], qe[d_][:, tokc], True, True, ["ke%d" % d_, "qe%d" % d_], [], x=[BK[d_]])
                            tt("dve", att[d_][:], pa, tri_f[:, 2 * d_, :], ALU.mult, ["tri"], ["att%d" % d_], x=[BK[d_]])
                            mm(po, att[d_][:], vtk[:, i, :], True, False, ["att%d" % d_, "vtk"], [], inc=False, x=[BK[d_]])

                        def lat_inter(d_, i, half, last):
                            po = B[d_][:, 0:256]
                            rows = slice(64 * half, 64 * half + 64)
                            c0 = i * 128 + 64 * half
                            cv = ver[d_] % 2
                            mm(po[rows, :], qe[d_][:, c0:c0 + 64], S16[d_][cv][:], False, last, ["qe%d" % d_, "S16_%d_%d" % (d_, cv)],
                               [], inc=True, x=[BK[d_]])

                        def lat_back(d_, i):
                            po = B[d_][:, 0:256]
                            xo = [BK[d_]]
                            tokc = slice(i * 128, (i + 1) * 128)
                            if done[i] == 0:
                                cp("act", oacc[:, i, :], po, [], ["oacc%d" % i], x=xo)
                                done[i] = 1
                                return
                            u = i % 2
                            tt("dve", oacc[:, i, :], po, oacc[:, i, :], ALU.add, ["oacc%d" % i], ["oacc%d" % i], x=xo)
                            s.op("pool", lambda e, u=u: e.memset(st2[u][:], 0.0), writes=["st2_%d" % u])
                            act(junk[:], oacc[:, i, :], AF.Square, ["oacc%d" % i, "st2_%d" % u], ["junk2", "st2_%d" % u],
                                accum_out=st2[u][:, 0:1])
                            act(st2[u][:, 1:2], st2[u][:, 0:1], AF.Sqrt, ["st2_%d" % u, "eps_c"], ["st2_%d" % u],
                                scale=1.0 / 256, bias=eps_c[:, 0:1])
                            s.op("dve", lambda e, u=u: e.reciprocal(out=st2[u][:, 2:3], in_=st2[u][:, 1:2]),
                                 reads=["st2_%d" % u], writes=["st2_%d" % u])
                            stt("dve", onr[u][:], oacc[:, i, :], st2[u][:, 2:3], gnb[:], ALU.mult, ALU.mult,
                                ["oacc%d" % i, "st2_%d" % u, "gnb"], ["onr%d" % u])
                            tt("pool", rbf[u][:], onr[u][:], sgt[:, i, :], ALU.mult, ["onr%d" % u, "sgt"], ["rbf%d" % u])
                            for j in range(2):
                                tr(pT[u][:, j, :], rbf[u][:, j * 128:(j + 1) * 128], ident_b[:], ["rbf%d" % u, "ident_b"], [], x=[TK[u]])
                            cp("act", oglT[:, 2 * h:2 * h + 2, tokc], pT[u][:, 0:2, :], [], ["oglT"], x=[TK[u]])

                        for j in range(NT):
                            fi, bi_ = j, NT - 1 - j
                            lat_front(0, fi)
                            lat_front(1, bi_)
                            kvmm(0, fi, 0)
                            kvmm(0, fi, 1)
                            kvmm(1, bi_, 1)
                            kvmm(1, bi_, 0)
                            lat_inter(0, fi, 0, False)
                            upd(0, fi, 0)
                            lat_inter(1, bi_, 1, False)
                            upd(1, bi_, 1)
                            lat_inter(0, fi, 1, True)
                            upd(0, fi, 1)
                            lat_inter(1, bi_, 0, True)
                            upd(1, bi_, 0)
                            lat_back(0, fi)
                            lat_back(1, bi_)
                    if smp == 0:
                        dump("oglT", oglT[:, :, :], ["oglT"])
                    s.barrier()
                    print("P2 done: deadlock-free", s.check_deadlock(), s.n_ins, s.n_wait, s.cnt)
                if stop_after == "P2":
                    s.barrier()
                    return nc

                onaT = sb(sa, "onaT", [128, 4, S], BF16)
                with ExitStack() as st:
                    wq3 = sb(st, "wq3", [128, 8, 128], BF16)
                    wk3 = sb(st, "wk3", [128, 8, 128], BF16)
                    wv3 = sb(st, "wv3", [128, 8, 128], BF16)
                    nam = sb(st, "nam", [128, 8, 256], F32)
                    nabt = [sb(st, "nabt%d" % i, [128, 2, 256], F32) for i in range(2)]
                    BMp = sb(st, "BMp", [128, 8, 2, 256], BF16)
                    qT3 = sb(st, "qT3", [128, S], BF16)
                    kT3 = sb(st, "kT3", [128, S + CT], BF16)
                    Ve = sb(st, "Ve", [128, 16, 128], BF16)
                    Vo = sb(st, "Vo", [128, 15, 128], BF16)
                    Vc = sb(st, "Vc", [128, 2, 128], BF16)
                    PT = [sb(st, "PT%d" % i, [128, 6, 2, 64], BF16) for i in range(2)]
                    rden = [sb(st, "rden%d" % i, [128, 128], F32) for i in range(2)]
                    SB = [pst(st, "p3s%d" % i, [128, 512]) for i in range(4)]
                    SK = ["p3S%d" % i for i in range(4)]
                    OB = [pst(st, "p3o%d" % i, [128, 512]) for i in range(2)]
                    OK_ = ["p3O%d" % i for i in range(2)]
                    PB = [pst(st, "p3p%d" % i, [128, 512]) for i in range(2)]
                    PK = ["p3P%d" % i for i in range(2)]
                    s.dma("sp", nam[:], nam_d, writes=["nam"])
                    for pr in range(4):
                        precast(12)
                        s.dma("pool", wq3[:], wview(w_in_d[:, C_NAQ + 128 * pr:C_NAQ + 128 * (pr + 1)]), writes=["wq3"])
                        s.dma("pool", wk3[:], wview(w_in_d[:, C_NAK + 128 * pr:C_NAK + 128 * (pr + 1)]), writes=["wk3"])
                        s.dma("pool", wv3[:], wview(w_in_d[:, C_NAV + 128 * pr:C_NAV + 128 * (pr + 1)]), writes=["wv3"])
                        for p in range(8):
                            s.dma("sp", nabt[p % 2][:], nab_d[pr, :, p, :, :], writes=["nabt%d" % (p % 2)])
                            for hh in range(2):
                                stt("dve", BMp[:, p, hh, :], nabt[p % 2][:, hh, :], 8.0, nam[:, p, :], ALU.mult, ALU.add,
                                    ["nabt%d" % (p % 2), "nam"], ["BMp"])
                        cnt_p = [0]

                        def proj(dst, lhs_fn, rhs_fn, m, n, rk, dk):
                            u = cnt_p[0] % 2
                            cnt_p[0] += 1
                            for kt in range(8):
                                mm(PB[u][0:m, 0:n], lhs_fn(kt), rhs_fn(kt), kt == 0, kt == 7, rk, [], x=[PK[u]])
                            cp("act" if u == 0 else "dve", dst, PB[u][0:m, 0:n], [], [dk], x=[PK[u]])

                        for tb in range(4):
                            cols = slice(tb * 512, (tb + 1) * 512)
                            proj(qT3[:, cols], lambda kt: wq3[:, kt, :], lambda kt, cols=cols: aT[:, kt, cols], 128, 512, ["wq3", "aT"], "qT3")
                            proj(kT3[:, cols], lambda kt: wk3[:, kt, :], lambda kt, cols=cols: aT[:, kt, cols], 128, 512, ["wk3", "aT"], "kT3")
                        proj(kT3[:, S:S + CT], lambda kt: wk3[:, kt, :], lambda kt: acT[:, kt, :], 128, CT, ["wk3", "acT"], "kT3")
                        for i in range(16):
                            proj(Ve[:, i, :], lambda kt, i=i: aT[:, kt, i * 128:(i + 1) * 128], lambda kt: wv3[:, kt, :], 128, 128, ["wv3", "aT"], "Ve")
                        for i in range(15):
                            proj(Vo[:, i, :], lambda kt, i=i: aT[:, kt, 64 + i * 128:64 + (i + 1) * 128], lambda kt: wv3[:, kt, :], 128, 128, ["wv3", "aT"], "Vo")
                        for i in range(2):
                            proj(Vc[:, i, :], lambda kt, i=i: acT[:, kt, i * 128:(i + 1) * 128], lambda kt: wv3[:, kt, :], 128, 128, ["wv3", "acT"], "Vc")

                        def scores(r):
                            rs = min(max(r - 4, 0), 24)
                            p = r - rs
                            buf = r % 2
                            qc = slice(r * 64, r * 64 + 64)
                            for hh in range(2):
                                pr_ = slice(64 * hh, 64 * hh + 64)
                                bank = SB[buf * 2 + hh]
                                xk = [SK[buf * 2 + hh]]
                                mm(bank[:, 0:256], ident_b[:], BMp[:, p, hh, :], True, False, ["ident_b", "BMp"], [], inc=False, x=xk)
                                for j in range(4):
                                    kc0 = (rs + 2 * j) * 64
                                    mm(bank[:, j * 64:(j + 1) * 64], kT3[pr_, kc0:kc0 + 128], qT3[pr_, qc], False, j == 3,
                                       ["kT3", "qT3"], [], inc=False, x=xk)
                                for jc in range(2):
                                    mm(bank[:, 256 + jc * 64:256 + (jc + 1) * 64], kT3[pr_, S + jc * 128:S + (jc + 1) * 128], qT3[pr_, qc],
                                       True, True, ["kT3", "qT3"], [], inc=(jc == 1), x=xk)
                                act(PT[buf][:, :, hh, :], bank[:, 0:384].rearrange("p (j q) -> p j q", q=64), AF.Exp,
                                    [], ["PT%d" % buf], scale=0.125, x=xk)

                        def pv(r):
                            rs = min(max(r - 4, 0), 24)
                            buf = r % 2
                            qc = slice(r * 64, r * 64 + 64)
                            ob = OB[buf]
                            xo = [OK_[buf]]
                            for hh in range(2):
                                hs = slice(64 * hh, 64 * hh + 64)
                                for j in range(6):
                                    if j < 4:
                                        if rs % 2 == 0:
                                            Vt, vk = Ve[:, (rs + 2 * j) // 2, hs], "Ve"
                                        else:
                                            Vt, vk = Vo[:, (rs + 2 * j - 1) // 2, hs], "Vo"
                                    else:
                                        Vt, vk = Vc[:, j - 4, hs], "Vc"
                                    mm(ob[hs, 0:64], Vt, PT[buf][:, j, hh, :], j == 0, j == 5, [vk, "PT%d" % buf], [],
                                       inc=(j == 5), x=xo)
                            for j in range(6):
                                mm(ob[:, 64:192], ones_b[:, :], PT[buf][:, j, :, :].rearrange("p h q -> p (h q)"), j == 0, j == 5,
                                   ["ones_b", "PT%d" % buf], [], inc=(j == 5), x=xo)
                            s.op("dve", lambda e: e.reciprocal(out=rden[buf][:], in_=ob[:, 64:192]), reads=[], writes=["rden%d" % buf], excl=xo)
                            for hh in range(2):
                                hs = slice(64 * hh, 64 * hh + 64)
                                tt("dve", onaT[hs, pr, qc], ob[hs, 0:64], rden[buf][hs, 64 * hh:64 * hh + 64], ALU.mult,
                                   ["rden%d" % buf], ["onaT"], x=xo)

                        for r in range(32):
                            scores(r)
                            if r > 0:
                                pv(r - 1)
                        pv(31)
                    if smp == 0:
                        dump("onaT", onaT[:, :, :], ["onaT"])
                    s.barrier()
                    print("P3 done: deadlock-free", s.check_deadlock(), s.n_ins, s.n_wait, s.cnt)
                if stop_after == "P3":
                    s.barrier()
                    return nc

                UT = sb(sa, "UT", [128, 8, S], BF16)
                with ExitStack() as st:
                    wna = [sb(st, "wna%d" % i, [128, 4, 128], BF16) for i in range(2)]
                    wgl = [sb(st, "wgl%d" % i, [128, 8, 128], BF16) for i in range(2)]
                    w8 = [sb(st, "w8_%d" % i, [128, 8, 128], BF16) for i in range(2)]
                    w9 = [sb(st, "w9_%d" % i, [128, 8, 128], BF16) for i in range(2)]
                    sg8 = [sb(st, "sg8_%d" % i, [128, 512], F32) for i in range(2)]
                    sg9 = [sb(st, "sg9_%d" % i, [128, 512], F32) for i in range(2)]
                    t1m = [sb(st, "t1m%d" % i, [128, 512], F32) for i in range(2)]
                    t2m = [sb(st, "t2m%d" % i, [128, 512], F32) for i in range(2)]
                    MB = [pst(st, "p4b%d" % i, [128, 512]) for i in range(8)]
                    MK = ["p4B%d" % i for i in range(8)]
                    for nt in range(8):
                        wb = nt % 2
                        ncol = slice(nt * 128, (nt + 1) * 128)
                        s.dma("pool", wna[wb][:], wview(w_nao_d[:, ncol]), writes=["wna%d" % wb])
                        s.dma("pool", wgl[wb][:], wview(w_glo_d[:, ncol]), writes=["wgl%d" % wb])
                        s.dma("pool", w8[wb][:], wview(w_in_d[:, C_M8 + nt * 128:C_M8 + (nt + 1) * 128]), writes=["w8_%d" % wb])
                        s.dma("pool", w9[wb][:], wview(w_in_d[:, C_M9 + nt * 128:C_M9 + (nt + 1) * 128]), writes=["w9_%d" % wb])
                        for tb in range(4):
                            cols = slice(tb * 512, (tb + 1) * 512)
                            u = (nt * 4 + tb) % 2
                            bA, bG8, bB, bG9 = [MB[4 * u + q] for q in range(4)]
                            kA, kG8, kB, kG9 = [[MK[4 * u + q]] for q in range(4)]
                            for kt in range(4):
                                mm(bA[:, :], wna[wb][:, kt, :], onaT[:, kt, cols], kt == 0, kt == 3, ["wna%d" % wb, "onaT"], [], x=kA)
                            for kt in range(8):
                                mm(bG8[:, :], w8[wb][:, kt, :], aT[:, kt, cols], kt == 0, kt == 7, ["w8_%d" % wb, "aT"], [], x=kG8)
                            for kt in range(8):
                                mm(bB[:, :], wgl[wb][:, kt, :], oglT[:, kt, cols], kt == 0, kt == 7, ["wgl%d" % wb, "oglT"], [], x=kB)
                            for kt in range(8):
                                mm(bG9[:, :], w9[wb][:, kt, :], aT[:, kt, cols], kt == 0, kt == 7, ["w9_%d" % wb, "aT"], [], x=kG9)
                            act(sg8[u][:], bG8[:, :], AF.Sigmoid, [], ["sg8_%d" % u], x=kG8)
                            act(sg9[u][:], bG9[:, :], AF.Sigmoid, [], ["sg9_%d" % u], x=kG9)
                            tt("dve", t1m[u][:], bA[:, :], sg8[u][:], ALU.mult, ["sg8_%d" % u], ["t1m%d" % u], x=kA)
                            tt("dve", t2m[u][:], bB[:, :], sg9[u][:], ALU.mult, ["sg9_%d" % u], ["t2m%d" % u], x=kB)
                            tt("pool", UT[:, nt, cols], t1m[u][:], t2m[u][:], ALU.add, ["t1m%d" % u, "t2m%d" % u], ["UT"])
                    if smp == 0:
                        dump("UT", UT[:, :, 0:256], ["UT"])
                    s.barrier()
                with ExitStack() as st:
                    wo = sb(st, "wo", [128, 8, D], BF16)
                    wtmp = [sb(st, "wtmp%d" % i, [128, D], F32) for i in range(2)]
                    g1b = sb(st, "g1b", [128, D], F32)
                    xt2 = [sb(st, "xt2_%d" % i, [128, D], F32) for i in range(2)]
                    h1t = [sb(st, "h1t%d" % i, [128, D], F32) for i in range(2)]
                    MB = [pst(st, "p4c%d" % i, [128, 512]) for i in range(4)]
                    MK = ["p4C%d" % i for i in range(4)]
                    s.dma("sp", g1b[:], modv_d[smp, 2 * D:3 * D].partition_broadcast(128), writes=["g1b"])
                    for kt in range(8):
                        s.dma("sp", wtmp[kt % 2][:], w_out_d[kt * 128:(kt + 1) * 128, :], writes=["wtmp%d" % (kt % 2)])
                        tt("pool", wo[:, kt, :], wtmp[kt % 2][:], g1b[:], ALU.mult, ["wtmp%d" % (kt % 2), "g1b"], ["wo"])
                    for i in range(NT):
                        u = i % 2
                        tokc = slice(i * 128, (i + 1) * 128)
                        s.dma("sp", xt2[u][:], x_d[smp, tokc, :], writes=["xt2_%d" % u])
                        for half in range(2):
                            hcol = slice(half * 512, (half + 1) * 512)
                            bk = MB[2 * u + half]
                            xk = [MK[2 * u + half]]
                            for kt in range(8):
                                mm(bk[:, :], UT[:, kt, tokc], wo[:, kt, hcol], kt == 0, kt == 7, ["UT", "wo"], [], x=xk)
                            tt("dve", h1t[u][:, hcol], bk[:, :], xt2[u][:, hcol], ALU.add, ["xt2_%d" % u], ["h1t%d" % u], x=xk)
                        s.dma("sp", h1_d[smp, tokc, :], h1t[u][:], reads=["h1t%d" % u], writes=["h1d"])
                        if smp == 0 and i < 2 and "h1" in dbg_d:
                            dump("h1", h1t[u][:], ["h1t%d" % u], dst=dbg_d["h1"][i * 128:(i + 1) * 128, :])
                    s.barrier()
                    print("P4 done: deadlock-free", s.check_deadlock(), s.n_ins, s.n_wait, s.cnt)
                if stop_after == "P4":
                    s.barrier()
                    return nc
            CAP = MOE_CAP
            NB = CAP // 128
            NSLOT = NEXP * CAP
            Xd = nc.dram_tensor("xd_scr%d" % smp, [NSLOT, D], BF16).ap()
            Yd = nc.dram_tensor("yd_scr%d" % smp, [NSLOT, D], BF16).ap()

            ind_hist = []
            IND_DEPTH = int(os.environ.get("IND_DEPTH", "1000"))

            def dma_fn(eng, fn, reads, writes):
                i_ = s.dnext
                s.dnext = (s.dnext + 1) % s.n_fg
                k_ = ("d", i_)
                waits = s._deps(eng, reads, writes)
                if s.dcnt[i_] > 0 and s.waited[eng].get(k_, -1) < s.dcnt[i_]:
                    s.waited[eng][k_] = s.dcnt[i_]
                    waits.append((k_, s.dcnt[i_]))
                if len(ind_hist) >= IND_DEPTH:
                    pk, pv = ind_hist[-IND_DEPTH]
                    if pk != k_ and s.waited[eng].get(pk, -1) < pv:
                        s.waited[eng][pk] = pv
                        waits.append((pk, pv))
                s.dcnt[i_] += 16
                tok = (k_, s.dcnt[i_])
                ind_hist.append(tok)
                s._emit1(eng, waits, fn, (k_, 16))
                s._commit(tok, reads, writes)

            with ExitStack() as sm:
                hres = sb(sm, "hres", [128, NT, D], F32)
                g2b = sb(sm, "g2b", [128, D], F32)
                gfb = sb(sm, "gfb", [128, D], F32)
                SLi = sb(sm, "SLi", [128, NT * 2], I32)
                w12 = sb(sm, "w12", [128, NT, 2], F32)
                s.dma("sp", g2b[:], modv_d[smp, 5 * D:6 * D].partition_broadcast(128), writes=["g2b"])
                s.dma("sp", gfb[:], g_fin_d.partition_broadcast(128), writes=["gfb"])
                with ExitStack() as st:
                    fbf = sb(st, "fbf", [128, NT, D], BF16)
                    M1a = sb(st, "M1a", [128, NT, 32], F32)
                    M2a = sb(st, "M2a", [128, NT, 32], F32)
                    Ma = sb(st, "Ma", [128, NT, 32], F32)
                    SLf = sb(st, "SLf", [128, NT, 2], F32)
                    ebase = sb(st, "ebase_sb", [128, 32], F32)
                    triS = sb(st, "triS", [128, 128], F32)
                    ones128 = sb(st, "ones128", [128, 128], F32)
                    s2b = sb(st, "s2b", [128, D], F32)
                    sh2b = sb(st, "sh2b", [128, D], F32)
                    gfn = sb(st, "gfn", [128, D], F32)
                    wrt = sb(st, "wrt", [128, 8, 36], F32)
                    brt = sb(st, "brt", [1, 36], F32)
                    ones_f = sb(st, "ones_f", [1, 128], F32)
                    xm3 = [sb(st, "xm3_%d" % i, [128, D], F32) for i in range(2)]
                    junk3 = sb(st, "junk3", [128, D], F32)
                    fT32 = [sb(st, "fT32_%d" % i, [128, 8, 128], F32) for i in range(2)]
                    LG = sb(st, "LG", [128, NT, 36], F32)
                    gm = sb(st, "gm", [128, NT, 8], F32)
                    ohg = sb(st, "ohg", [128, NT, 4], F32)
                    ge = sb(st, "ge", [128, NT, 4], F32)
                    big = sb(st, "bigr", [128, NT, 4, 8], F32)
                    leg = sb(st, "leg", [128, NT, 8], F32)
                    oh1 = sb(st, "oh1", [128, NT, 8], F32)
                    oh2 = sb(st, "oh2", [128, NT, 8], F32)
                    msk = sb(st, "msk", [128, NT, 8], F32)
                    rk = sb(st, "rk", [128, NT, 32], F32)
                    vm = sb(st, "vm", [128, NT, 32], F32)
                    st3 = [sb(st, "st3_%d" % i, [128, 4], F32) for i in range(2)]
                    pf = [[pst(st, "p5f%d_%d" % (i, j), [128, 4, 128]) for j in range(2)] for i in range(2)]
                    pfk = [["p5F%d_%d" % (i, j) for j in range(2)] for i in range(2)]
                    pl = [pst(st, "p5l%d" % i, [128, 512]) for i in range(2)]
                    plk = ["p5L%d" % i for i in range(2)]
                    s.dma("sp", gfn[:], g_ffn_d.partition_broadcast(128), writes=["gfn"])
                    s.dma("sp", s2b[:], modv_d[smp, 4 * D:5 * D].partition_broadcast(128), writes=["s2b"])
                    stt("dve", s2b[:], s2b[:], 1.0, gfn[:], ALU.add, ALU.mult, ["s2b", "gfn"], ["s2b"])
                    s.dma("sp", sh2b[:], modv_d[smp, 3 * D:4 * D].partition_broadcast(128), writes=["sh2b"])
                    s.dma("sp", wrt[:], wview(w_rt_d), writes=["wrt"])
                    s.dma("sp", brt[:], b_rt_d.partition_broadcast(1), writes=["brt"])
                    s.dma("sp", ebase[:], ebase_d, writes=["ebase"])
                    s.dma("sp", triS[:], tri_d[4], writes=["triS"])
                    s.op("pool", lambda e: e.memset(ones_f[:], 1.0), writes=["ones_f"])
                    s.op("pool", lambda e: e.memset(ones128[:], 1.0), writes=["ones128"])
                    for i in range(NT):
                        u = i % 2
                        tokc = slice(i * 128, (i + 1) * 128)
                        hk = "hres%d" % i
                        s.dma("sp", hres[:, i, :], h1_d[smp, tokc, :], reads=["h1d"], writes=[hk])
                        s.op("pool", lambda e, u=u: e.memset(st3[u][:], 0.0), writes=["st3_%d" % u])
                        act(junk3[:], hres[:, i, :], AF.Square, [hk, "st3_%d" % u], ["junk3", "st3_%d" % u], accum_out=st3[u][:, 0:1])
                        act(st3[u][:, 1:2], st3[u][:, 0:1], AF.Sqrt, ["st3_%d" % u, "eps_c"], ["st3_%d" % u], scale=1.0 / D, bias=eps_c[:, 0:1])
                        s.op("dve", lambda e, u=u: e.reciprocal(out=st3[u][:, 2:3], in_=st3[u][:, 1:2]), reads=["st3_%d" % u], writes=["st3_%d" % u])
                        stt("dve", xm3[u][:], hres[:, i, :], st3[u][:, 2:3], s2b[:], ALU.mult, ALU.mult, [hk, "st3_%d" % u, "s2b"], ["xm3_%d" % u])
                        tt("pool", xm3[u][:], xm3[u][:], sh2b[:], ALU.add, ["xm3_%d" % u, "sh2b"], ["xm3_%d" % u])
                        cp("pool", fbf[:, i, :], xm3[u][:], ["xm3_%d" % u], ["fbf%d" % i])
                        for kt in range(8):
                            tr(pf[u][kt // 4][:, kt % 4, :], xm3[u][:, kt * 128:(kt + 1) * 128], ident_f[:], ["xm3_%d" % u, "ident_f"], [], x=[pfk[u][kt // 4]])
                        for j in range(2):
                            cp("act" if j == 0 else "dve", fT32[u][:, 4 * j:4 * j + 4, :], pf[u][j][:, :, :], [], ["fT32_%d" % u], x=[pfk[u][j]])
                        for kt in range(8):
                            mm(pl[u][:, 0:36], fT32[u][:, kt, :], wrt[:, kt, :], kt == 0, False, ["fT32_%d" % u, "wrt"], [], inc=False, x=[plk[u]])
                        mm(pl[u][:, 0:36], ones_f[0:1, :], brt[0:1, :], False, True, ["ones_f", "brt"], [], x=[plk[u]])
                        cp("dve", LG[:, i, :], pl[u][:, 0:36], [], ["LG"], x=[plk[u]])
                    T_ = NT
                    dv = lambda fn: s.op("dve", fn, reads=["LG", "RT"], writes=["RT"])
                    bc = lambda ap, shp: ap.broadcast_to(shp)
                    lgg = LG[:, :, 0:4]
                    lge = LG[:, :, 4:36].rearrange("p t (g j) -> p t g j", j=8)
                    dv(lambda e: e.reduce_max(out=gm[:, :, 0], in_=lgg, axis=AX.X))
                    dv(lambda e: e.tensor_tensor(out=ohg[:], in0=lgg, in1=bc(gm[:, :, 0:1], [128, T_, 4]), op=ALU.is_equal))
                    dv(lambda e: e.tensor_tensor(out=ge[:], in0=lgg, in1=bc(gm[:, :, 0:1], [128, T_, 4]), op=ALU.subtract))
                    act(ge[:], ge[:], AF.Exp, ["RT"], ["RT"])
                    dv(lambda e: e.reduce_sum(out=gm[:, :, 1], in_=ge[:], axis=AX.X))
                    dv(lambda e: e.reciprocal(out=gm[:, :, 1], in_=gm[:, :, 1]))
                    dv(lambda e: e.tensor_tensor(out=big[:], in0=lge, in1=bc(ohg[:, :, :, None], [128, T_, 4, 8]), op=ALU.mult))
                    dv(lambda e: e.reduce_sum(out=leg[:], in_=big[:].rearrange("p t g j -> p t j g"), axis=AX.X))
                    dv(lambda e: e.reduce_max(out=gm[:, :, 2], in_=leg[:], axis=AX.X))
                    dv(lambda e: e.tensor_tensor(out=oh1[:], in0=leg[:], in1=bc(gm[:, :, 2:3], [128, T_, 8]), op=ALU.is_equal))
                    dv(lambda e: e.scalar_tensor_tensor(out=msk[:], in0=oh1[:], scalar=-1e30, in1=leg[:], op0=ALU.mult, op1=ALU.add))
                    dv(lambda e: e.reduce_max(out=gm[:, :, 3], in_=msk[:], axis=AX.X))
                    dv(lambda e: e.tensor_tensor(out=oh2[:], in0=msk[:], in1=bc(gm[:, :, 3:4], [128, T_, 8]), op=ALU.is_equal))
                    dv(lambda e: e.tensor_tensor(out=gm[:, :, 4], in0=gm[:, :, 3], in1=gm[:, :, 2], op=ALU.subtract))
                    act(gm[:, :, 4], gm[:, :, 4], AF.Exp, ["RT"], ["RT"])
                    dv(lambda e: e.tensor_scalar(out=gm[:, :, 5], in0=gm[:, :, 4], scalar1=1.0, scalar2=None, op0=ALU.add))
                    dv(lambda e: e.reciprocal(out=gm[:, :, 5], in_=gm[:, :, 5]))
                    s.op("dve", lambda e: e.tensor_tensor(out=w12[:, :, 0], in0=gm[:, :, 5], in1=gm[:, :, 1], op=ALU.mult), reads=["RT"], writes=["w12"])
                    s.op("dve", lambda e: e.tensor_tensor(out=w12[:, :, 1], in0=w12[:, :, 0], in1=gm[:, :, 4], op=ALU.mult), reads=["RT", "w12"], writes=["w12"])
                    m1v = M1a[:].rearrange("p t (g j) -> p t g j", j=8)
                    m2v = M2a[:].rearrange("p t (g j) -> p t g j", j=8)
                    s.op("dve", lambda e: e.tensor_tensor(out=m1v, in0=bc(ohg[:, :, :, None], [128, T_, 4, 8]), in1=bc(oh1[:, :, None, :], [128, T_, 4, 8]),
                                                          op=ALU.mult), reads=["RT"], writes=["M1a"])
                    s.op("dve", lambda e: e.tensor_tensor(out=m2v, in0=bc(ohg[:, :, :, None], [128, T_, 4, 8]), in1=bc(oh2[:, :, None, :], [128, T_, 4, 8]),
                                                          op=ALU.mult), reads=["RT"], writes=["M2a"])
                    tt("pool", Ma[:], M1a[:], M2a[:], ALU.add, ["M1a", "M2a"], ["Ma"])
                    for i in range(NT):
                        rc = slice(i * 32, (i + 1) * 32)
                        mm(pl[0][:, rc], triS[:], Ma[:, i, :], True, i == 0, ["triS", "Ma"], [], inc=(i == 0), x=[plk[0]])
                        for i2 in range(i):
                            mm(pl[0][:, rc], ones128[:], Ma[:, i2, :], False, i2 == i - 1, ["ones128", "Ma"], [], inc=(i2 == i - 1), x=[plk[0]])
                    dq = lambda fn, rd=(), wr=(), x=(): s.op("dve", fn, reads=["QT"] + list(rd), writes=["QT"] + list(wr), excl=x)
                    dq(lambda e: e.tensor_copy(out=rk[:], in_=pl[0][:, :].rearrange("p (t e) -> p t e", e=32)), x=[plk[0]])
                    dq(lambda e: e.tensor_scalar(out=vm[:], in0=rk[:], scalar1=float(CAP), scalar2=None, op0=ALU.is_lt))
                    dq(lambda e: e.tensor_tensor(out=rk[:], in0=rk[:], in1=bc(ebase[:, None, :], [128, T_, 32]), op=ALU.add), rd=["ebase"])
                    for k_, Mk, mk in ((0, M1a, "M1a"), (1, M2a, "M2a")):
                        dq(lambda e, Mk=Mk: e.tensor_tensor(out=big[:].rearrange("p t g j -> p t (g j)"), in0=Mk[:], in1=rk[:], op=ALU.mult), rd=[mk, "RT"], wr=["RT"])
                        dq(lambda e, k_=k_: e.reduce_sum(out=SLf[:, :, k_], in_=big[:].rearrange("p t g j -> p t (g j)"), axis=AX.X), rd=["RT"], wr=["SLf"])
                        dq(lambda e, Mk=Mk: e.tensor_tensor(out=big[:].rearrange("p t g j -> p t (g j)"), in0=Mk[:], in1=vm[:], op=ALU.mult), rd=[mk, "RT"], wr=["RT"])
                        dq(lambda e: e.reduce_sum(out=gm[:, :, 6], in_=big[:].rearrange("p t g j -> p t (g j)"), axis=AX.X), rd=["RT"], wr=["RT"])
                        dq(lambda e, k_=k_: e.tensor_tensor(out=w12[:, :, k_], in0=w12[:, :, k_], in1=gm[:, :, 6], op=ALU.mult), rd=["w12", "RT"], wr=["w12"])
                        dq(lambda e: e.tensor_scalar(out=gm[:, :, 7], in0=gm[:, :, 6], scalar1=-1.0e6, scalar2=1.0e6, op0=ALU.mult, op1=ALU.add), rd=["RT"], wr=["RT"])
                        dq(lambda e, k_=k_: e.tensor_tensor(out=SLf[:, :, k_], in0=SLf[:, :, k_], in1=gm[:, :, 7], op=ALU.add), rd=["RT", "SLf"], wr=["SLf"])
                    s.op("dve", lambda e: e.tensor_copy(out=SLi[:], in_=SLf[:].rearrange("p t k -> p (t k)")), reads=["SLf"], writes=["SLi"])
                    for i in range(NT):
                        for k_ in range(2):
                            dma_fn("pool", lambda e, k_=k_, i=i: e.indirect_dma_start(
                                out=Xd[:, :], out_offset=bass.IndirectOffsetOnAxis(ap=SLi[:, 2 * i + k_:2 * i + k_ + 1], axis=0),
                                in_=fbf[:, i, :], in_offset=None, bounds_check=bc_reg, oob_is_err=False),
                                ["SLi", "fbf%d" % i], ["Xd"])
                    if smp == 0:
                        dump("SLf", SLf[:, :, :], ["SLf"])
                        dump("w12", w12[:, :, :], ["w12"])
                    s.barrier()
                    print("P5 done: deadlock-free", s.check_deadlock(), s.n_ins, s.n_wait, s.cnt)
                if stop_after == "P5":
                    s.barrier()
                    return nc
                chunks = [(c0, min(c0 + 512, CAP)) for c0 in range(0, CAP, 512)]
                with ExitStack() as st:
                    weg = [sb(st, "weg%d" % i, [128, 8, DE], BF16) for i in range(2)]
                    weu = [sb(st, "weu%d" % i, [128, 8, DE], BF16) for i in range(2)]
                    wed = [sb(st, "wed%d" % i, [128, 4, D], BF16) for i in range(2)]
                    XeT = [sb(st, "XeT%d" % i, [128, 8, CAP], BF16) for i in range(2)]
                    hT = [sb(st, "hT%d" % i, [128, 4, CAP], BF16) for i in range(2)]
                    xe = [sb(st, "xe%d" % i, [128, D], BF16) for i in range(2)]
                    ye = [sb(st, "ye%d" % i, [128, D], BF16) for i in range(2)]
                    sgm = [sb(st, "sgm%d" % i, [128, 512], F32) for i in range(2)]
                    TX = [pst(st, "p6t%d" % i, [128, 8, 128], BF16) for i in range(2)]
                    GB = [pst(st, "p6g%d" % i, [128, 512]) for i in range(2)]
                    UB = [pst(st, "p6u%d" % i, [128, 512]) for i in range(2)]
                    YB = [pst(st, "p6y%d" % i, [128, 512]) for i in range(2)]
                    TXK = ["p6T%d" % i for i in range(2)]
                    GK = ["p6G%d" % i for i in range(2)]
                    UK = ["p6U%d" % i for i in range(2)]
                    YK = ["p6Y%d" % i for i in range(2)]
                    cnt6 = [0, 0, 0]

                    def load_w(e_):
                        wb = e_ % 2
                        s.dma("sp", weg[wb][:], wview(weg_b[e_]), reads=["wbg%d" % e_], writes=["weg%d" % wb])
                        s.dma("sp", weu[wb][:], wview(weu_b[e_]), reads=["wbu%d" % e_], writes=["weu%d" % wb])
                        s.dma("sp", wed[wb][:], wview(wed_b[e_]), reads=["wbd%d" % e_], writes=["wed%d" % wb])
                        for kt in range(4):
                            tt("pool", wed[wb][:, kt, :], wed[wb][:, kt, :], g2b[:], ALU.mult, ["wed%d" % wb, "g2b"], ["wed%d" % wb])

                    def ph_T(e_):
                        wb = e_ % 2
                        for blk in range(NB):
                            u = cnt6[0] % 2
                            cnt6[0] += 1
                            r0 = e_ * CAP + blk * 128
                            s.dma("sp", xe[u][:], Xd[r0:r0 + 128, :], writes=["xe%d" % u])
                            for kt in range(8):
                                tr(TX[u][:, kt, :], xe[u][:, kt * 128:(kt + 1) * 128], ident_b[:], ["xe%d" % u, "ident_b"], [], x=[TXK[u]])
                            cp("act" if u == 0 else "dve", XeT[wb][:, :, blk * 128:(blk + 1) * 128], TX[u][:, :, :], [], ["XeT%d" % wb], x=[TXK[u]])

                    def ph_GU(e_):
                        wb = e_ % 2
                        for nt in range(4):
                            ncol = slice(nt * 128, (nt + 1) * 128)
                            for (c0, c1) in chunks:
                                u = cnt6[1] % 2
                                cnt6[1] += 1
                                n = c1 - c0
                                for kt in range(8):
                                    mm(GB[u][:, 0:n], weg[wb][:, kt, ncol], XeT[wb][:, kt, c0:c1], kt == 0, kt == 7, ["weg%d" % wb, "XeT%d" % wb], [], x=[GK[u]])
                                for kt in range(8):
                                    mm(UB[u][:, 0:n], weu[wb][:, kt, ncol], XeT[wb][:, kt, c0:c1], kt == 0, kt == 7, ["weu%d" % wb, "XeT%d" % wb], [], x=[UK[u]])
                                act(sgm[u][:, 0:n], GB[u][:, 0:n], AF.Silu, [], ["sgm%d" % u], x=[GK[u]])
                                tt("dve", hT[wb][:, nt, c0:c1], sgm[u][:, 0:n], UB[u][:, 0:n], ALU.mult, ["sgm%d" % u], ["hT%d" % wb], x=[UK[u]])

                    def ph_D(e_):
                        wb = e_ % 2
                        for blk in range(NB):
                            yu = blk % 2
                            r0 = e_ * CAP + blk * 128
                            for half in range(2):
                                u = cnt6[2] % 2
                                cnt6[2] += 1
                                hcol = slice(half * 512, (half + 1) * 512)
                                for kt in range(4):
                                    mm(YB[u][:, :], hT[wb][:, kt, blk * 128:(blk + 1) * 128], wed[wb][:, kt, hcol], kt == 0, kt == 3,
                                       ["hT%d" % wb, "wed%d" % wb], [], x=[YK[u]])
                                cp("act" if half == 0 else "dve", ye[yu][:, hcol], YB[u][:, :], [], ["ye%d" % yu], x=[YK[u]])
                            s.dma("pool", Yd[r0:r0 + 128, :], ye[yu][:], reads=["ye%d" % yu])

                    precast(96)
                    load_w(0)
                    ph_T(0)
                    for e_ in range(NEXP):
                        if e_ + 1 < NEXP:
                            load_w(e_ + 1)
                        ph_GU(e_)
                        if e_ + 1 < NEXP:
                            ph_T(e_ + 1)
                        ph_D(e_)
                    s.barrier()
                    print("P6 done: deadlock-free", s.check_deadlock(), s.n_ins, s.n_wait, s.cnt)
                with ExitStack() as st:
                    yk = [[sb(st, "yk%d_%d" % (k_, i), [128, D], BF16) for i in range(2)] for k_ in range(2)]
                    ot = [sb(st, "ot%d" % i, [128, D], F32) for i in range(2)]
                    junk4 = sb(st, "junk4", [128, D], F32)
                    st4 = [sb(st, "st4_%d" % i, [128, 4], F32) for i in range(2)]
                    for k_ in range(2):
                        for i in range(2):
                            s.op("pool", lambda e, k_=k_, i=i: e.memset(yk[k_][i][:], 0.0), writes=["yk%d_%d" % (k_, i)])
                    for i in range(NT):
                        u = i % 2
                        tokc = slice(i * 128, (i + 1) * 128)
                        hk = "hres%d" % i
                        for k_ in range(2):
                            dma_fn("pool", lambda e, k_=k_, u=u, i=i: e.indirect_dma_start(
                                out=yk[k_][u][:, :], out_offset=None, in_=Yd[:, :],
                                in_offset=bass.IndirectOffsetOnAxis(ap=SLi[:, 2 * i + k_:2 * i + k_ + 1], axis=0), bounds_check=bc_reg, oob_is_err=False),
                                ["SLi"], ["yk%d_%d" % (k_, u)])
                            stt("dve", hres[:, i, :], yk[k_][u][:], w12[:, i, k_:k_ + 1], hres[:, i, :], ALU.mult, ALU.add,
                                ["yk%d_%d" % (k_, u), "w12", hk], [hk])
                        s.op("pool", lambda e, u=u: e.memset(st4[u][:], 0.0), writes=["st4_%d" % u])
                        act(junk4[:], hres[:, i, :], AF.Square, [hk, "st4_%d" % u], ["junk4", "st4_%d" % u], accum_out=st4[u][:, 0:1])
                        act(st4[u][:, 1:2], st4[u][:, 0:1], AF.Sqrt, ["st4_%d" % u, "eps_c"], ["st4_%d" % u], scale=1.0 / D, bias=eps_c[:, 0:1])
                        s.op("dve", lambda e, u=u: e.reciprocal(out=st4[u][:, 2:3], in_=st4[u][:, 1:2]), reads=["st4_%d" % u], writes=["st4_%d" % u])
                        stt("dve", ot[u][:], hres[:, i, :], st4[u][:, 2:3], gfb[:], ALU.mult, ALU.mult, [hk, "st4_%d" % u, "gfb"], ["ot%d" % u])
                        s.dma("sp", y_d[smp, tokc, :], ot[u][:], reads=["ot%d" % u])
                    s.barrier()
        s.barrier(final=True)
    return nc


def _consts():
    ident = np.eye(128, dtype=np.float32)
    idx = np.arange(128)
    same = (idx[:, None] // 64) == (idx[None, :] // 64)
    tri = np.zeros((6, 128, 128), np.float32)
    tri[0] = same & (idx[:, None] <= idx[None, :])
    tri[1] = same & (idx[:, None] > idx[None, :])
    tri[2] = same & (idx[:, None] >= idx[None, :])
    tri[3] = same & (idx[:, None] < idx[None, :])
    tri[4] = idx[:, None] < idx[None, :]
    tri[5] = 1.0
    t = np.arange(S)
    pos_r = (t // GW).astype(np.float32)
    pos_c = (t % GW).astype(np.float32)
    inv = (10000.0 ** (-np.arange(32, dtype=np.float32) / 32)).astype(np.float32)
    ang = np.zeros((128, S), np.float32)
    ang[0:32] = inv[:, None] * pos_r[None, :]
    ang[32:64] = inv[:, None] * pos_r[None, :]
    ang[64:96] = inv[:, None] * pos_c[None, :]
    ang[96:128] = inv[:, None] * pos_c[None, :]
    cos = np.cos(ang).astype(np.float32)
    sin = np.sin(ang).astype(np.float32)
    sgn = np.ones((128, 1), np.float32)
    sgn[0:32] = -1
    sgn[64:96] = -1
    sin = sin * sgn
    perm = np.concatenate([np.arange(32, 64), np.arange(0, 32), np.arange(96, 128), np.arange(64, 96)])
    return ident, tri, cos, sin, perm


def _na_tables(rpb):
    a = (np.arange(128) // 64)[:, None, None, None]
    kc = (np.arange(128) % 64)[:, None, None, None]
    p = np.arange(8)[None, :, None, None]
    j = np.arange(4)[None, None, :, None]
    qc = np.arange(64)[None, None, None, :]
    ridx = 2 * j + a - p + 7
    cstart = np.clip(qc - 8, 0, 48)
    valid = (kc >= cstart) & (kc < cstart + 16) & (ridx >= 0) & (ridx <= 14)
    valid = np.broadcast_to(valid, (128, 8, 4, 64))
    cidx = np.clip(kc - qc + 15, 0, 30)
    ridx_c = np.clip(ridx, 0, 14)
    ridx_b = np.broadcast_to(ridx_c, (128, 8, 4, 64))
    cidx_b = np.broadcast_to(cidx, (128, 8, 4, 64))
    g = rpb[:, ridx_b, cidx_b]
    g = np.where(valid[None], g, np.float32(0.0)).astype(np.float32)
    g = g.reshape(4, 2, 128, 8, 256).transpose(0, 2, 3, 1, 4)
    mask = np.where(valid, np.float32(0.0), np.float32(MASKV)).astype(np.float32).reshape(128, 8, 256)
    return np.ascontiguousarray(g), np.ascontiguousarray(mask)


_NC_CACHE = {}


def kernel(x, c, ctx, c_ctx, w_mod, b_mod, norm_attn_g, norm_ffn_g, w_in, w_gla_a2, b_gla_a2,
           gla_norm_g, na_rpb, w_na_o, w_gla_o, w_out, w_group, b_group, w_expert, b_expert,
           w_exp_gate, w_exp_up, w_exp_down, final_norm_g):
    f = lambda a: np.ascontiguousarray(np.asarray(a, dtype=np.float32))
    x, c, ctx, c_ctx = f(x), f(c), f(ctx), f(c_ctx)
    ident, tri, cos, sin, perm = _consts()
    w_in0 = f(w_in)[0]
    gq = w_in0[:, C_GQ:C_GQ + 512].reshape(D, 4, 128)[:, :, perm].reshape(D, 512)
    gk = w_in0[:, C_GK:C_GK + 512].reshape(D, 4, 128)[:, :, perm].reshape(D, 512)
    w_rope = np.ascontiguousarray(np.concatenate([gq, gk], axis=1))
    w_a2b = np.ascontiguousarray(np.concatenate(
        [f(w_gla_a2)[0].transpose(1, 0, 2), f(b_gla_a2)[0][None]], axis=0))
    nab, nam = _na_tables(f(na_rpb)[0])
    w_rt = np.ascontiguousarray(np.concatenate([f(w_group)[0], f(w_expert)[0]], axis=1))
    b_rt = np.ascontiguousarray(np.concatenate([f(b_group)[0], f(b_expert)[0]], axis=0))
    shared = {
        "w_mod": f(w_mod)[0], "b_mod": f(b_mod)[0], "g_attn": f(norm_attn_g)[0], "g_ffn": f(norm_ffn_g)[0],
        "g_fin": f(final_norm_g), "gla_g": f(gla_norm_g)[0], "w_in": w_in0, "w_rope": w_rope, "w_a2b": w_a2b,
        "w_na_o": f(w_na_o)[0], "w_gla_o": f(w_gla_o)[0], "w_out": f(w_out)[0], "w_rt": w_rt, "b_rt": b_rt,
        "w_eg": f(w_exp_gate)[0], "w_eu": f(w_exp_up)[0], "w_ed": f(w_exp_down)[0],
        "ident": ident, "tri": tri, "rope_cos": cos, "rope_sin": sin, "na_bias": nab, "na_mask": nam,
        "ebase": np.ascontiguousarray(np.broadcast_to((np.arange(NEXP, dtype=np.float32) * MOE_CAP)[None, :], (128, NEXP))),
    }
    n = 8
    NS = x.shape[0] // n
    if "nc" not in _NC_CACHE:
        _NC_CACHE["nc"] = build_nc(NS)
    nc = _NC_CACHE["nc"]
    in_maps = []
    for i in range(n):
        m = dict(shared)
        m["x"] = x[i * NS:(i + 1) * NS]
        m["ctx"] = ctx[i * NS:(i + 1) * NS]
        m["cc"] = np.ascontiguousarray(np.concatenate([c[i * NS:(i + 1) * NS], c_ctx[None]], axis=0))
        in_maps.append(m)
    res = run_bass_kernel_spmd(nc, in_maps, core_ids=list(range(n)))
    return np.concatenate([r["y"] for r in res.results], axis=0)
```

```python
import os
import numpy as np
import concourse.bass as bass
import concourse.mybir as mybir
from concourse.bass_utils import run_bass_kernel_spmd
from contextlib import ExitStack

F32 = mybir.dt.float32
BF16 = mybir.dt.bfloat16
I32 = mybir.dt.int32
ALU = mybir.AluOpType
AF = mybir.ActivationFunctionType
AX = mybir.AxisListType

D = 1024
S = 2048
CT = 256
NT = 16
NTC = 2
NTA = NT + NTC
GW = 64
EPS = 1e-6
NEXP = 32
DE = 512
C_NAQ, C_NAK, C_NAV, C_GQ, C_GK, C_GV, C_GG, C_AL, C_M8, C_M9 = 0, 512, 1024, 1536, 2048, 2560, 3584, 4608, 4640, 5664
MASKV = -30000.0
MOE_CAP = 640


class Sched:
    ENGS = ("pe", "act", "dve", "pool", "sp")
    HND = {"pe": "tensor", "act": "scalar", "dve": "vector", "pool": "gpsimd", "sp": "sync"}

    def __init__(self, nc, es, n_dma_sems=40):
        self.nc = nc
        self.sem = {e: es.enter_context(nc.semaphore("s_" + e)) for e in self.ENGS}
        self.cnt = {e: 0 for e in self.ENGS}
        self.pending = {e: False for e in self.ENGS}
        self.waited = {e: {} for e in self.ENGS}
        self.dsem = [es.enter_context(nc.semaphore("s_dma%d" % i)) for i in range(n_dma_sems)]
        self.dcnt = [0] * n_dma_sems
        self.dnext = 0
        self.n_fg = n_dma_sems - 8
        self.bnext = 0
        self.lastw = {}
        self.reads = {}
        self.semobj = {}
        for e in self.ENGS:
            self.semobj[("e", e)] = self.sem[e]
        for i, sm in enumerate(self.dsem):
            self.semobj[("d", i)] = sm
        self.n_ins = 0
        self.n_wait = 0

    def _deps(self, eng, reads, writes, excl=()):
        toks = {}

        def add(t):
            if t is None:
                return
            k, v = t
            if toks.get(k, -1) < v:
                toks[k] = v
        for b in reads:
            add(self.lastw.get(b))
        for b in writes:
            add(self.lastw.get(b))
            for t in self.reads.get(b, ()):
                add(t)
        for b in excl:
            t = self.lastw.get(b)
            if t is not None and t[0] != ("e", eng):
                add(t)
        out = []
        for k, v in toks.items():
            if k == ("e", eng):
                if eng == "pe":
                    continue
                if v <= self.cnt[eng] - 2:
                    continue
            if self.waited[eng].get(k, -1) >= v:
                continue
            self.waited[eng][k] = v
            out.append((k, v))
        return out

    def _commit(self, tok, reads, writes):
        for b in writes:
            self.lastw[b] = tok
            self.reads[b] = []
        for b in reads:
            self.reads.setdefault(b, []).append(tok)

    def check_deadlock(self):
        pos = {e: 0 for e in self.ENGS}
        val = {}
        prog = True
        while prog:
            prog = False
            for e in self.ENGS:
                lst = self.log[e]
                while pos[e] < len(lst):
                    waits, inc = lst[pos[e]]
                    if all(val.get(k, 0) >= v for k, v in waits):
                        if inc is not None:
                            val[inc[0]] = val.get(inc[0], 0) + inc[1]
                        pos[e] += 1
                        prog = True
                    else:
                        break
        stuck = {e: (pos[e], len(self.log[e])) for e in self.ENGS if pos[e] < len(self.log[e])}
        for e in stuck:
            waits, inc = self.log[e][pos[e]]
            print("STUCK", e, pos[e], [(k, v, val.get(k, 0)) for k, v in waits])
        return not stuck

    def _emit1(self, eng, waits, fn, inc):
        if not hasattr(self, "log"):
            self.log = {e: [] for e in self.ENGS}
        self.log[eng].append((list(waits), inc if fn is not None else None))
        engh = getattr(self.nc, self.HND[eng])
        for k, v in waits:
            engh.wait_ge(self.semobj[k], v)
            self.n_wait += 1
        if fn is None:
            return
        ins = fn(engh)
        if inc is not None:
            ins.then_inc(self.semobj[inc[0]], inc[1])
        self.n_ins += 1

    def op(self, eng, fn, reads=(), writes=(), inc=True, excl=()):
        waits = self._deps(eng, reads, writes, excl)
        if inc:
            self.cnt[eng] += 1
            tok = (("e", eng), self.cnt[eng])
            self.pending[eng] = False
        else:
            tok = (("e", eng), self.cnt[eng] + 1)
            self.pending[eng] = True
        self._emit1(eng, waits, fn, (("e", eng), 1) if inc else None)
        self._commit(tok, reads, writes)
        for b_ in excl:
            self.lastw[b_] = tok
        return tok

    def dma(self, eng, out, in_, reads=(), writes=(), bg=False, **kw):
        if bg:
            i = self.n_fg + self.bnext
            self.bnext = (self.bnext + 1) % 8
        else:
            i = self.dnext
            self.dnext = (self.dnext + 1) % self.n_fg
        k = ("d", i)
        waits = self._deps(eng, reads, writes)
        if self.dcnt[i] > 0 and self.waited[eng].get(k, -1) < self.dcnt[i]:
            self.waited[eng][k] = self.dcnt[i]
            waits.append((k, self.dcnt[i]))
        self.dcnt[i] += 16
        tok = (k, self.dcnt[i])
        self._emit1(eng, waits, lambda e: e.dma_start(out=out, in_=in_, **kw), (k, 16))
        self._commit(tok, reads, writes)
        return tok

    def barrier(self, final=False):
        assert not any(self.pending.values())
        allt = [(("e", e), self.cnt[e]) for e in self.ENGS if self.cnt[e] > 0]
        allt += [(("d", i), c) for i, c in enumerate(self.dcnt) if c > 0 and (i < self.n_fg or final)]
        for e in self.ENGS:
            waits = []
            for k, v in allt:
                if k == ("e", e):
                    continue
                if self.waited[e].get(k, -1) >= v:
                    continue
                self.waited[e][k] = v
                waits.append((k, v))
            self._emit1(e, waits, None, None)
        keep = {k: t for k, t in self.lastw.items() if t[0][0] == "d" and t[0][1] >= self.n_fg}
        self.lastw = {} if final else keep
        self.reads = {}


def build_nc(NS=2, dbg=None, stop_after=None):
    nc = bass.Bass("TRN2", target_bir_lowering=False)

    def din(name, shape, dt=F32):
        return nc.dram_tensor(name, list(shape), dt, kind="ExternalInput").ap()

    x_d = din("x", [NS, S, D])
    ctx_d = din("ctx", [NS, CT, D])
    cc_d = din("cc", [NS + 1, D])
    w_mod_d = din("w_mod", [D, 6 * D])
    b_mod_d = din("b_mod", [6 * D])
    g_attn_d = din("g_attn", [D])
    g_ffn_d = din("g_ffn", [D])
    g_fin_d = din("g_fin", [D])
    gla_g_d = din("gla_g", [256])
    w_in_d = din("w_in", [D, 6688])
    w_rope_d = din("w_rope", [D, 1024])
    w_a2b_d = din("w_a2b", [17, 2, 512])
    w_nao_d = din("w_na_o", [512, D])
    w_glo_d = din("w_gla_o", [D, D])
    w_out_d = din("w_out", [D, D])
    w_rt_d = din("w_rt", [D, 36])
    b_rt_d = din("b_rt", [36])
    w_eg_d = din("w_eg", [NEXP, D, DE])
    w_eu_d = din("w_eu", [NEXP, D, DE])
    w_ed_d = din("w_ed", [NEXP, DE, D])
    ident_d = din("ident", [128, 128])
    tri_d = din("tri", [6, 128, 128])
    ebase_d = din("ebase", [128, 32])
    cos_d = din("rope_cos", [128, S])
    sin_d = din("rope_sin", [128, S])
    nab_d = din("na_bias", [4, 128, 8, 2, 256])
    nam_d = din("na_mask", [128, 8, 256])
    y_d = nc.dram_tensor("y", [NS, S, D], F32, kind="ExternalOutput").ap()
    modv_d = nc.dram_tensor("modv", [NS + 1, 6 * D], F32).ap()
    dbg_d = {}
    if dbg:
        for name, shape in dbg.items():
            dbg_d[name] = nc.dram_tensor("dbg_" + name, list(shape), F32, kind="ExternalOutput").ap()

    with ExitStack() as es:
        s = Sched(nc, es)

        used_names = {}

        def uniq(name):
            k = used_names.get(name, 0)
            used_names[name] = k + 1
            return name if k == 0 else "%s_r%d" % (name, k)

        def sb(st, name, shape, dt):
            return st.enter_context(nc.sbuf_tensor(uniq(name), list(shape), dt))

        def pst(st, name, shape, dt=F32):
            return st.enter_context(nc.psum_tensor(uniq(name), list(shape), dt))

        def mm(out, lhsT, rhs, start, stop, reads, writes, inc=None, x=()):
            if inc is None:
                inc = stop
            s.op("pe", lambda e: e.matmul(out, lhsT, rhs, start=start, stop=stop),
                 reads=reads, writes=writes, inc=inc, excl=x)

        def tr(out, in_, idn, reads, writes, x=()):
            s.op("pe", lambda e: e.transpose(out, in_, idn), reads=reads, writes=writes, excl=x)

        def act(out, in_, func, reads, writes, x=(), **kw):
            s.op("act", lambda e: e.activation(out=out, in_=in_, func=func, **kw), reads=reads, writes=writes, excl=x)

        def tt(eng, out, in0, in1, op, reads, writes, x=()):
            s.op(eng, lambda e: e.tensor_tensor(out=out, in0=in0, in1=in1, op=op), reads=reads, writes=writes, excl=x)

        def ts(eng, out, in0, s1, s2, op0, op1, reads, writes, x=()):
            if s2 is None:
                s.op(eng, lambda e: e.tensor_scalar(out=out, in0=in0, scalar1=s1, scalar2=None, op0=op0),
                     reads=reads, writes=writes, excl=x)
            else:
                s.op(eng, lambda e: e.tensor_scalar(out=out, in0=in0, scalar1=s1, scalar2=s2, op0=op0, op1=op1),
                     reads=reads, writes=writes, excl=x)

        def stt(eng, out, in0, scalar, in1, op0, op1, reads, writes, x=()):
            s.op(eng, lambda e: e.scalar_tensor_tensor(out=out, in0=in0, scalar=scalar, in1=in1, op0=op0, op1=op1),
                 reads=reads, writes=writes, excl=x)

        def cp(eng, out, in_, reads, writes, x=()):
            if eng == "act":
                s.op("act", lambda e: e.copy(out=out, in_=in_), reads=reads, writes=writes, excl=x)
            else:
                s.op(eng, lambda e: e.tensor_copy(out=out, in_=in_), reads=reads, writes=writes, excl=x)

        def dump(name, src_ap, reads, dst=None):
            if name in dbg_d:
                s.dma("pool", dbg_d[name] if dst is None else dst, src_ap, reads=reads)

        def wview(ap2d):
            return ap2d.rearrange("(kt p) n -> p kt n", p=128)

        ident_f = sb(es, "ident_f", [128, 128], F32)
        ident_b = sb(es, "ident_b", [128, 128], BF16)
        tri_f = sb(es, "tri_f", [128, 4, 128], F32)
        ones_b = sb(es, "ones_b", [128, 128], BF16)
        eps_c = sb(es, "eps_c", [128, 1], F32)
        one_c = sb(es, "one_c", [128, 1], F32)
        s.dma("sp", ident_f[:], ident_d, writes=["ident_f"])
        s.dma("pool", ident_b[:], ident_d, writes=["ident_b"])
        s.dma("sp", tri_f[:], tri_d[0:4].rearrange("m s t -> s m t"), writes=["tri"])
        s.op("pool", lambda e: e.memset(ones_b[:], 1.0), writes=["ones_b"])
        s.op("pool", lambda e: e.memset(eps_c[:], EPS), writes=["eps_c"])
        s.op("pool", lambda e: e.memset(one_c[:], 1.0), writes=["one_c"])

        with ExitStack() as st:
            ccs = sb(st, "ccs", [NS + 1, D], F32)
            scT = sb(st, "scT", [128, 8, NS + 1], F32)
            modsb = sb(st, "modsb", [NS + 1, 6 * D], F32)
            bmod = sb(st, "bmod", [NS + 1, 6 * D], F32)
            wm = [sb(st, "wm%d" % i, [128, 8, 512], F32) for i in range(2)]
            psA = pst(st, "p0a", [128, 512])
            psB = [pst(st, "p0b%d" % i, [128, 512]) for i in range(2)]
            R = NS + 1
            s.dma("sp", ccs[:], cc_d, writes=["ccs"])
            s.dma("sp", bmod[:], b_mod_d.partition_broadcast(R), writes=["bmod"])
            act(ccs[:], ccs[:], AF.Silu, ["ccs"], ["ccs"])
            for kt in range(8):
                tr(psA[:, kt * R:(kt + 1) * R], ccs[:, kt * 128:(kt + 1) * 128], ident_f[0:R, 0:R],
                   ["ccs", "ident_f"], ["p0a"])
            cp("dve", scT[:].rearrange("p k r -> p (k r)"), psA[:, 0:8 * R], ["p0a"], ["scT"])
            for cb in range(12):
                w = wm[cb % 2]
                s.dma("sp", w[:], wview(w_mod_d[:, cb * 512:(cb + 1) * 512]), writes=["wm%d" % (cb % 2)])
                ps = psB[cb % 2]
                for kt in range(8):
                    mm(ps[0:R, :], scT[:, kt, :], w[:, kt, :], kt == 0, kt == 7,
                       ["scT", "wm%d" % (cb % 2)], ["p0b%d" % (cb % 2)])
                tt("dve", modsb[:, cb * 512:(cb + 1) * 512], ps[0:R, :], bmod[:, cb * 512:(cb + 1) * 512], ALU.add,
                   ["p0b%d" % (cb % 2), "bmod"], ["modsb"])
            s.dma("sp", modv_d, modsb[:], reads=["modsb"], writes=["modv"])
            dump("modv", modsb[:], ["modsb"])
            s.barrier()
        if stop_after == "P0":
            s.barrier()
            return nc

        QSCALE = 128.0 ** -0.5
        weg_b = nc.dram_tensor("weg_bf", [NEXP, D, DE], BF16).ap()
        weu_b = nc.dram_tensor("weu_bf", [NEXP, D, DE], BF16).ap()
        wed_b = nc.dram_tensor("wed_bf", [NEXP, DE, D], BF16).ap()

        def _precast_gen():
            for e_ in range(NEXP):
                for nm, dst, srcw, r in (("g", weg_b, w_eg_d, 4), ("u", weu_b, w_eu_d, 4), ("d", wed_b, w_ed_d, 2)):
                    s.dma("pool", dst[e_].rearrange("(a r) n -> a (r n)", r=r), srcw[e_].rearrange("(a r) n -> a (r n)", r=r),
                          writes=["wb%s%d" % (nm, e_)], bg=True)
                    yield
        _pc = _precast_gen()

        def precast(n):
            for _ in range(n):
                next(_pc, None)
        bc_reg = nc.gpsimd.alloc_register("bcreg")
        nc.gpsimd.reg_mov(bc_reg, NEXP * MOE_CAP - 1)
        h1_d = nc.dram_tensor("h1_scr", [NS, S, D], F32).ap()

        def bank_set(st, pfx):
            return [pst(st, "%s%d" % (pfx, i), [128, 512]) for i in range(7)]

        for smp in range(NS):
            with ExitStack() as sa:
                aT = sb(sa, "aT", [128, 8, S], BF16)
                acT = sb(sa, "acT", [128, 8, CT], BF16)
                oglT = sb(sa, "oglT", [128, 8, S], BF16)

                with ExitStack() as st:
                    s1b = sb(st, "s1b", [128, D], F32)
                    sh1b = sb(st, "sh1b", [128, D], F32)
                    s1c = sb(st, "s1c", [128, D], F32)
                    sh1c = sb(st, "sh1c", [128, D], F32)
                    gab = sb(st, "gab", [128, D], F32)
                    tmpv = sb(st, "tmpv", [128, D], F32)
                    xt = [sb(st, "xt%d" % i, [128, D], F32) for i in range(2)]
                    xm = [sb(st, "xm%d" % i, [128, D], F32) for i in range(2)]
                    xn = [sb(st, "xn%d" % i, [128, D], BF16) for i in range(2)]
                    stat = [sb(st, "stat%d" % i, [128, 4], F32) for i in range(2)]
                    psT = [pst(st, "p1t%d" % i, [128, 8, 128], BF16) for i in range(2)]
                    s.dma("sp", gab[:], g_attn_d.partition_broadcast(128), writes=["gab"])
                    for row, s1, sh, nm in ((smp, s1b, sh1b, "l"), (NS, s1c, sh1c, "c")):
                        s.dma("sp", tmpv[:], modv_d[row, D:2 * D].partition_broadcast(128), writes=["tmpv"])
                        stt("dve", s1[:], tmpv[:], 1.0, gab[:], ALU.add, ALU.mult, ["tmpv", "gab"], ["s1" + nm])
                        s.dma("sp", sh[:], modv_d[row, 0:D].partition_broadcast(128), writes=["sh1" + nm])
                    for i in range(NTA):
                        b = i % 2
                        lat = i < NT
                        src = x_d[smp, i * 128:(i + 1) * 128, :] if lat else ctx_d[smp, (i - NT) * 128:(i - NT + 1) * 128, :]
                        nm = "l" if lat else "c"
                        s1, sh = (s1b, sh1b) if lat else (s1c, sh1c)
                        s.dma("sp", xt[b][:], src, writes=["xt%d" % b])
                        s.op("pool", lambda e, b=b: e.memset(stat[b][:], 0.0), writes=["stat%d" % b])
                        act(xm[b][:], xt[b][:], AF.Square, ["xt%d" % b, "stat%d" % b], ["xm%d" % b, "stat%d" % b],
                            accum_out=stat[b][:, 0:1])
                        act(stat[b][:, 1:2], stat[b][:, 0:1], AF.Sqrt, ["stat%d" % b, "eps_c"], ["stat%d" % b],
                            scale=1.0 / D, bias=eps_c[:, 0:1])
                        s.op("dve", lambda e, b=b: e.reciprocal(out=stat[b][:, 2:3], in_=stat[b][:, 1:2]),
                             reads=["stat%d" % b], writes=["stat%d" % b])
                        stt("dve", xm[b][:], xt[b][:], stat[b][:, 2:3], s1[:], ALU.mult, ALU.mult,
                            ["xt%d" % b, "stat%d" % b, "s1" + nm], ["xm%d" % b])
                        tt("pool", xn[b][:], xm[b][:], sh[:], ALU.add, ["xm%d" % b, "sh1" + nm], ["xn%d" % b])
                        for kt in range(8):
                            tr(psT[b][:, kt, :], xn[b][:, kt * 128:(kt + 1) * 128], ident_b[:],
                               ["xn%d" % b, "ident_b"], ["p1t%d" % b])
                        if lat:
                            cp("act", aT[:, :, i * 128:(i + 1) * 128], psT[b][:, :, :], ["p1t%d" % b], ["aT"])
                        else:
                            cp("act", acT[:, :, (i - NT) * 128:(i - NT + 1) * 128], psT[b][:, :, :], ["p1t%d" % b], ["acT"])
                    if smp == 0:
                        dump("aT", aT[:, :, 0:256], ["aT"])
                    s.barrier()
                if stop_after == "P1":
                    s.barrier()
                    return nc

                with ExitStack() as st:
                    ropeC = sb(st, "ropeC", [128, S], BF16)
                    ropeS = sb(st, "ropeS", [128, S], BF16)
                    alT = [sb(st, "alT%d" % d_, [16, S + CT], BF16) for d_ in range(2)]
                    wal = sb(st, "wal", [128, 8, 32], BF16)
                    wa2 = sb(st, "wa2", [16, 2, 512], BF16)
                    ba2 = sb(st, "ba2", [1, 2, 512], BF16)
                    gnb = sb(st, "gnb", [128, 256], F32)
                    wq = sb(st, "wq", [128, 8, 128], BF16)
                    wqs = sb(st, "wqs", [128, 8, 128], BF16)
                    wk = sb(st, "wk", [128, 8, 128], BF16)
                    wks = sb(st, "wks", [128, 8, 128], BF16)
                    wv = sb(st, "wv", [128, 8, 256], BF16)
                    wg = sb(st, "wg", [128, 8, 256], BF16)
                    qrT = sb(st, "qrT", [128, S], BF16)
                    krT = sb(st, "krT", [128, S], BF16)
                    krk = sb(st, "krk", [128, NTA, 128], BF16)
                    vtk = sb(st, "vtk", [128, NTA, 256], BF16)
                    sgt = sb(st, "sgt", [128, NT, 256], BF16)
                    qe = [sb(st, "qe%d" % d_, [128, S], BF16) for d_ in range(2)]
                    ke = [sb(st, "ke%d" % d_, [128, S], BF16) for d_ in range(2)]
                    kd = [sb(st, "kd%d" % d_, [128, NTA, 128], BF16) for d_ in range(2)]
                    dec = sb(st, "dec", [128, 2, NTA, 2], F32)
                    oacc = sb(st, "oacc", [128, NT, 256], F32)
                    S32 = [[sb(st, "S32_%d_%d" % (d_, v_), [128, 256], F32) for v_ in range(2)] for d_ in range(2)]
                    attf = [sb(st, "attf%d" % i, [128, 128], BF16) for i in range(2)]
                    trib = sb(st, "trib", [128, 2, 128], BF16)
                    S16 = [[sb(st, "S16_%d_%d" % (d_, v_), [128, 256], BF16) for v_ in range(2)] for d_ in range(2)]
                    t1 = [sb(st, "t1_%d" % i, [128, 512], F32) for i in range(2)]
                    t2 = [sb(st, "t2_%d" % i, [128, 512], F32) for i in range(2)]
                    otmp = [t1[i][:, 0:256] for i in range(2)]
                    onr = [t2[i][:, 0:256] for i in range(2)]
                    Dt = [sb(st, "Dt0", [128, 512], F32)]
                    Di = [sb(st, "Di0", [128, 512], F32)]
                    EK = [sb(st, "EK0", [128, 512], F32)]
                    att = [sb(st, "att%d" % i, [128, 128], BF16) for i in range(2)]
                    junk = sb(st, "junk2", [128, 256], F32)
                    rbf = [sb(st, "rbf%d" % i, [128, 256], BF16) for i in range(2)]
                    st2 = [sb(st, "st2_%d" % i, [128, 4], F32) for i in range(2)]
                    B = [pst(st, "p2b%d" % i, [128, 512]) for i in range(6)]
                    BK = ["p2B%d" % i for i in range(6)]
                    pT = [pst(st, "p2t%d" % i, [128, 8, 128], BF16) for i in range(2)]
                    TK = ["p2T0", "p2T1"]

                    s.dma("pool", ropeC[:], cos_d, writes=["ropeC"])
                    s.dma("pool", ropeS[:], sin_d, writes=["ropeS"])
                    s.dma("pool", wal[:], wview(w_in_d[:, C_AL:C_AL + 32]), writes=["wal"])
                    s.dma("pool", wa2[:], w_a2b_d[0:16], writes=["wa2"])
                    s.dma("pool", ba2[:], w_a2b_d[16:17], writes=["ba2"])
                    s.dma("sp", gnb[:], gla_g_d.partition_broadcast(128), writes=["gnb"])
                    for tb in range(5):
                        if tb < 4:
                            rhs_of = lambda kt, tb=tb: aT[:, kt, tb * 512:(tb + 1) * 512]
                            n, c0, rk = 512, tb * 512, "aT"
                        else:
                            rhs_of = lambda kt: acT[:, kt, :]
                            n, c0, rk = CT, S, "acT"
                        for d_ in range(2):
                            for kt in range(8):
                                mm(B[d_][0:16, 0:n], wal[:, kt, d_ * 16:(d_ + 1) * 16], rhs_of(kt), kt == 0, kt == 7,
                                   ["wal", rk], [], x=[BK[d_]])
                            cp("act" if d_ == 0 else "dve", alT[d_][:, c0:c0 + n], B[d_][0:16, 0:n], [], ["alT%d" % d_], x=[BK[d_]])
                    if stop_after == "P2a":
                        s.barrier()
                        return nc

                    def load_head_w(h):
                        s.dma("pool", wq[:], wview(w_in_d[:, C_GQ + 128 * h:C_GQ + 128 * (h + 1)]), writes=["wq"])
                        s.dma("pool", wqs[:], wview(w_rope_d[:, 128 * h:128 * (h + 1)]), writes=["wqs"])
                        s.dma("pool", wk[:], wview(w_in_d[:, C_GK + 128 * h:C_GK + 128 * (h + 1)]), writes=["wk"])
                        s.dma("pool", wks[:], wview(w_rope_d[:, 512 + 128 * h:512 + 128 * (h + 1)]), writes=["wks"])
                        s.dma("pool", wv[:], wview(w_in_d[:, C_GV + 256 * h:C_GV + 256 * (h + 1)]), writes=["wv"])
                        s.dma("pool", wg[:], wview(w_in_d[:, C_GG + 256 * h:C_GG + 256 * (h + 1)]), writes=["wg"])

                    load_head_w(0)
                    for h in range(4):
                        for tb in range(4):
                            cols = slice(tb * 512, (tb + 1) * 512)
                            for bi, (w, wn) in enumerate(((wq, "wq"), (wqs, "wqs"), (wk, "wk"), (wks, "wks"))):
                                for kt in range(8):
                                    mm(B[bi][:, :], w[:, kt, :], aT[:, kt, cols], kt == 0, kt == 7, [wn, "aT"], [], x=[BK[bi]])
                            stt("dve", t1[0][:], B[0][:, :], QSCALE, ropeC[:, cols], ALU.mult, ALU.mult, ["ropeC"], ["t1_0"], x=[BK[0]])
                            stt("dve", t2[0][:], B[1][:, :], QSCALE, ropeS[:, cols], ALU.mult, ALU.mult, ["ropeS"], ["t2_0"], x=[BK[1]])
                            tt("pool", qrT[:, cols], t1[0][:], t2[0][:], ALU.add, ["t1_0", "t2_0"], ["qrT"])
                            tt("dve", t1[1][:], B[2][:, :], ropeC[:, cols], ALU.mult, ["ropeC"], ["t1_1"], x=[BK[2]])
                            tt("dve", t2[1][:], B[3][:, :], ropeS[:, cols], ALU.mult, ["ropeS"], ["t2_1"], x=[BK[3]])
                            tt("pool", krT[:, cols], t1[1][:], t2[1][:], ALU.add, ["t1_1", "t2_1"], ["krT"])
                        if stop_after == "P2b":
                            s.barrier()
                            return nc
                        for i in range(NTA):
                            lat = i < NT
                            bv = 4 + (i % 2)
                            bg = 2 + (i % 2)
                            srcT, rk, c0 = (aT, "aT", i * 128) if lat else (acT, "acT", (i - NT) * 128)
                            for kt in range(8):
                                mm(B[bv][:, 0:256], srcT[:, kt, c0:c0 + 128], wv[:, kt, :], kt == 0, kt == 7, [rk, "wv"], [], x=[BK[bv]])
                            cp("act", vtk[:, i, :], B[bv][:, 0:256], [], ["vtk"], x=[BK[bv]])
                            if lat:
                                for kt in range(8):
                                    mm(B[bg][:, 0:256], srcT[:, kt, c0:c0 + 128], wg[:, kt, :], kt == 0, kt == 7, [rk, "wg"], [], x=[BK[bg]])
                                act(sgt[:, i, :], B[bg][:, 0:256], AF.Silu, [], ["sgt"], x=[BK[bg]])
                            else:
                                for kt in range(8):
                                    mm(B[bg][:, 0:128], srcT[:, kt, c0:c0 + 128], wk[:, kt, :], kt == 0, kt == 7, [rk, "wk"], [], x=[BK[bg]])
                                cp("dve", krk[:, i, :], B[bg][:, 0:128], [], ["krk"], x=[BK[bg]])
                        for i in range(NT):
                            u = i % 2
                            tr(pT[u][:, 0, :], krT[:, i * 128:(i + 1) * 128], ident_b[:], ["krT", "ident_b"], [], x=[TK[u]])
                            cp("dve", krk[:, i, :], pT[u][:, 0, :], [], ["krk"], x=[TK[u]])
                        if stop_after == "P2c":
                            s.barrier()
                            return nc
                        if h + 1 < 4:
                            load_head_w(h + 1)
                        groups = [(0, 4), (4, 4), (8, 4), (12, 4), (16, 2)]
                        hc = slice(128 * h, 128 * (h + 1))
                        for (t0, nt_) in groups:
                            lat = t0 < NT
                            n = nt_ * 128
                            gc = slice(t0 * 128, t0 * 128 + n)
                            for d_ in range(2):
                                bz, bb, be = B[d_], B[2 + d_], B[4 + d_]
                                xz, xb, xe_ = [BK[d_]], [BK[2 + d_]], [BK[4 + d_]]
                                az_, ex_, rz_, L_ = t1[0], t1[1], t2[0], t2[1]
                                for tl in range(nt_):
                                    tokc = slice((t0 + tl) * 128, (t0 + tl + 1) * 128)
                                    zc = slice(tl * 128, (tl + 1) * 128)
                                    mm(bz[:, zc], alT[d_][0:16, tokc], wa2[0:16, d_, hc], True, False, ["alT%d" % d_, "wa2"], [], inc=False, x=xz)
                                    mm(bz[:, zc], ones_b[0:1, 0:128], ba2[0:1, d_, hc], False, True, ["ones_b", "ba2"], [], x=xz)
                                act(az_[:, 0:n], bz[:, 0:n], AF.Abs, [], ["t1_0"], x=xz)
                                ts("dve", rz_[:, 0:n], bz[:, 0:n], -1.0, 0.0, ALU.mult, ALU.max, [], ["t2_0"], x=xz)
                                act(ex_[:, 0:n], az_[:, 0:n], AF.Exp, ["t1_0"], ["t1_1"], scale=-1.0)
                                act(ex_[:, 0:n], ex_[:, 0:n], AF.Ln, ["t1_1", "one_c"], ["t1_1"], bias=one_c[:, 0:1])
                                tt("pool", L_[:, 0:n], rz_[:, 0:n], ex_[:, 0:n], ALU.add, ["t2_0", "t1_1"], ["t2_1"])
                                for tl in range(nt_):
                                    zc = slice(tl * 128, (tl + 1) * 128)
                                    mm(bb[:, zc], L_[:, zc], tri_f[:, 2 * d_, :], True, True, ["t2_1", "tri"], [], x=xb)
                                for tl in range(nt_):
                                    zc = slice(tl * 128, (tl + 1) * 128)
                                    mm(be[:, zc], tri_f[:, 2 * d_ + 1, :], L_[:, zc], True, True, ["t2_1", "tri"], [], x=xe_)
                                act(Dt[0][:, 0:n], bb[:, 0:n], AF.Exp, [], ["Dt0"], scale=-1.0 / 16, x=xb)
                                if lat:
                                    act(Di[0][:, 0:n], bb[:, 0:n], AF.Exp, [], ["Di0"], scale=1.0 / 16, x=xb)
                                act(EK[0][:, 0:n], be[:, 0:n], AF.Exp, [], ["EK0"], scale=-1.0 / 16, x=xe_)
                                dsrc = Dt[0][:, 63:n:64] if d_ == 0 else Dt[0][:, 0:n:64]
                                cp("pool", dec[:, d_, t0:t0 + nt_, :].rearrange("p t c -> p (t c)"), dsrc, ["Dt0"], ["dec"])
                                tt("pool", kd[d_][:, t0:t0 + nt_, :], krk[:, t0:t0 + nt_, :], EK[0][:, 0:n].rearrange("p (t c) -> p t c", c=128),
                                   ALU.mult, ["krk", "EK0"], ["kd%d" % d_])
                                if lat:
                                    tt("dve", qe[d_][:, gc], qrT[:, gc], Dt[0][:, 0:n], ALU.mult, ["qrT", "Dt0"], ["qe%d" % d_])
                                    tt("dve", ke[d_][:, gc], krT[:, gc], Di[0][:, 0:n], ALU.mult, ["krT", "Di0"], ["ke%d" % d_])
                        if stop_after == "P2d":
                            s.barrier()
                            return nc
                        ver = [0, 0]
                        for d_ in range(2):
                            s.op("pool", lambda e, d_=d_: e.memset(S32[d_][0][:], 0.0), writes=["S32_%d_0" % d_])
                            s.op("pool", lambda e, d_=d_: e.memset(S16[d_][0][:], 0.0), writes=["S16_%d_0" % d_])
                            cp("pool", trib[:, d_, :], tri_f[:, 2 * d_, :], ["tri"], ["trib"])

                        def kvmm(d_, i, half, par):
                            rows = slice(64 * half, 64 * half + 64)
                            bk = 2 + 2 * half + par
                            mm(B[bk][:, 256 * d_:256 * d_ + 256], kd[d_][rows, i, :], vtk[rows, i, :], True, True,
                               ["kd%d" % d_, "vtk"], [], x=[BK[bk]])

                        def upd(d_, i, half, par):
                            bk = 2 + 2 * half + par
                            pkv = B[bk][:, 256 * d_:256 * d_ + 256]
                            cv = ver[d_] % 2
                            nv = (ver[d_] + 1) % 2
                            stt("dve", S16[d_][nv][:], S32[d_][cv][:], dec[:, d_, i, half:half + 1], pkv, ALU.mult, ALU.add,
                                ["S32_%d_%d" % (d_, cv), "dec"], ["S16_%d_%d" % (d_, nv)], x=[BK[bk]])
                            stt("dve", S32[d_][nv][:], S32[d_][cv][:], dec[:, d_, i, half:half + 1], pkv, ALU.mult, ALU.add,
                                ["S32_%d_%d" % (d_, cv), "dec"], ["S32_%d_%d" % (d_, nv)], x=[BK[bk]])
                            ver[d_] += 1

                        for n_, i in enumerate((NT, NT + 1)):
                            for half in (0, 1):
                                kvmm(0, i, half, n_ % 2)
                            for half in (0, 1):
                                upd(0, i, half, n_ % 2)
                        for n_, i in enumerate((NT + 1, NT)):
                            for half in (1, 0):
                                kvmm(1, i, half, n_ % 2)
                            for half in (1, 0):
                                upd(1, i, half, n_ % 2)
                        if smp == 0 and h == 0:
                            dump("s_f", S32[0][ver[0] % 2][:], ["S32_0_%d" % (ver[0] % 2)])
                            dump("s_b", S32[1][ver[1] % 2][:], ["S32_1_%d" % (ver[1] % 2)])
                        if stop_after == "P2e":
                            s.barrier()
                            return nc

                        done = [0] * NT

                        def lat_att(d_, i):
                            tokc = slice(i * 128, (i + 1) * 128)
                            pa = B[d_][:, 256:384]
                            mm(pa, ke[d_][:, tokc], qe[d_][:, tokc], True, True, ["ke%d" % d_, "qe%d" % d_], [], x=[BK[d_]])
                            cp("act", attf[d_][:], pa, [], ["attf%d" % d_], x=[BK[d_]])
                            tt("pool", att[d_][:], attf[d_][:], trib[:, d_, :], ALU.mult, ["attf%d" % d_, "trib"], ["att%d" % d_])

                        def lat_po(d_, i):
                            po = B[d_][:, 0:256]
                            mm(po, att[d_][:], vtk[:, i, :], True, False, ["att%d" % d_, "vtk"], [], inc=False, x=[BK[d_]])

                        def lat_inter(d_, i, half, last):
                            po = B[d_][:, 0:256]
                            rows = slice(64 * half, 64 * half + 64)
                            c0 = i * 128 + 64 * half
                            cv = ver[d_] % 2
                            mm(po[rows, :], qe[d_][:, c0:c0 + 64], S16[d_][cv][:], False, last, ["qe%d" % d_, "S16_%d_%d" % (d_, cv)],
                               [], inc=True, x=[BK[d_]])

                        def lat_back(d_, i):
                            po = B[d_][:, 0:256]
                            xo = [BK[d_]]
                            tokc = slice(i * 128, (i + 1) * 128)
                            if done[i] == 0:
                                cp("act", oacc[:, i, :], po, [], ["oacc%d" % i], x=xo)
                                done[i] = 1
                                return
                            u = i % 2
                            cp("act", otmp[u], po, [], ["t1_%d" % u], x=xo)
                            tt("pool", oacc[:, i, :], otmp[u], oacc[:, i, :], ALU.add, ["t1_%d" % u, "oacc%d" % i], ["oacc%d" % i])
                            s.op("pool", lambda e, u=u: e.memset(st2[u][:], 0.0), writes=["st2_%d" % u])
                            act(junk[:], oacc[:, i, :], AF.Square, ["oacc%d" % i, "st2_%d" % u], ["junk2", "st2_%d" % u],
                                accum_out=st2[u][:, 0:1])
                            act(st2[u][:, 1:2], st2[u][:, 0:1], AF.Sqrt, ["st2_%d" % u, "eps_c"], ["st2_%d" % u],
                                scale=1.0 / 256, bias=eps_c[:, 0:1])
                            s.op("dve", lambda e, u=u: e.reciprocal(out=st2[u][:, 2:3], in_=st2[u][:, 1:2]),
                                 reads=["st2_%d" % u], writes=["st2_%d" % u])
                            act(onr[u], oacc[:, i, :], AF.Copy, ["oacc%d" % i, "st2_%d" % u], ["t2_%d" % u], scale=st2[u][:, 2:3])
                            tt("pool", onr[u], onr[u], gnb[:], ALU.mult, ["t2_%d" % u, "gnb"], ["t2_%d" % u])
                            tt("pool", rbf[u][:], onr[u], sgt[:, i, :], ALU.mult, ["t2_%d" % u, "sgt"], ["rbf%d" % u])
                            for j in range(2):
                                tr(pT[u][:, j, :], rbf[u][:, j * 128:(j + 1) * 128], ident_b[:], ["rbf%d" % u, "ident_b"], [], x=[TK[u]])
                            cp("act", oglT[:, 2 * h:2 * h + 2, tokc], pT[u][:, 0:2, :], [], ["oglT"], x=[TK[u]])

                        lat_att(0, 0)
                        lat_att(1, NT - 1)
                        kvmm(0, 0, 0, 0)
                        kvmm(0, 0, 1, 0)
                        kvmm(1, NT - 1, 1, 0)
                        kvmm(1, NT - 1, 0, 0)
                        for j in range(NT):
                            fi, bi_ = j, NT - 1 - j
                            par = j % 2
                            precast(1)
                            lat_po(0, fi)
                            lat_po(1, bi_)
                            lat_inter(0, fi, 0, False)
                            lat_inter(1, bi_, 1, False)
                            upd(0, fi, 0, par)
                            upd(1, bi_, 1, par)
                            lat_inter(0, fi, 1, True)
                            lat_inter(1, bi_, 0, True)
                            upd(0, fi, 1, par)
                            upd(1, bi_, 0, par)
                            lat_back(0, fi)
                            lat_back(1, bi_)
                            if j + 1 < NT:
                                lat_att(0, fi + 1)
                                lat_att(1, bi_ - 1)
                                kvmm(0, fi + 1, 0, 1 - par)
                                kvmm(0, fi + 1, 1, 1 - par)
                                kvmm(1, bi_ - 1, 1, 1 - par)
                                kvmm(1, bi_ - 1, 0, 1 - par)
                    if smp == 0:
                        dump("oglT", oglT[:, :, :], ["oglT"])
                    s.barrier()
                    print("P2 done: deadlock-free", s.check_deadlock(), s.n_ins, s.n_wait, s.cnt)
                if stop_after == "P2":
                    s.barrier()
                    return nc

                onaT = sb(sa, "onaT", [128, 4, S], BF16)
                with ExitStack() as st:
                    wq3 = sb(st, "wq3", [128, 8, 128], BF16)
                    wk3 = sb(st, "wk3", [128, 8, 128], BF16)
                    wv3 = sb(st, "wv3", [128, 8, 128], BF16)
                    nam = sb(st, "nam", [128, 8, 256], F32)
                    nabt = [sb(st, "nabt%d" % i, [128, 2, 256], F32) for i in range(2)]
                    BMp = sb(st, "BMp", [128, 8, 2, 256], BF16)
                    qT3 = sb(st, "qT3", [128, S], BF16)
                    kT3 = sb(st, "kT3", [128, S + CT], BF16)
                    Ve = sb(st, "Ve", [128, 16, 128], BF16)
                    Vo = sb(st, "Vo", [128, 15, 128], BF16)
                    Vc = sb(st, "Vc", [128, 2, 128], BF16)
                    PT = [sb(st, "PT%d" % i, [128, 6, 2, 64], BF16) for i in range(2)]
                    rden = [sb(st, "rden%d" % i, [128, 128], F32) for i in range(2)]
                    SB = [pst(st, "p3s%d" % i, [128, 512]) for i in range(4)]
                    SK = ["p3S%d" % i for i in range(4)]
                    OB = [pst(st, "p3o%d" % i, [128, 512]) for i in range(2)]
                    OK_ = ["p3O%d" % i for i in range(2)]
                    PB = [pst(st, "p3p%d" % i, [128, 512]) for i in range(2)]
                    PK = ["p3P%d" % i for i in range(2)]
                    s.dma("sp", nam[:], nam_d, writes=["nam"])
                    for pr in range(4):
                        s.dma("pool", wq3[:], wview(w_in_d[:, C_NAQ + 128 * pr:C_NAQ + 128 * (pr + 1)]), writes=["wq3"])
                        s.dma("pool", wk3[:], wview(w_in_d[:, C_NAK + 128 * pr:C_NAK + 128 * (pr + 1)]), writes=["wk3"])
                        s.dma("pool", wv3[:], wview(w_in_d[:, C_NAV + 128 * pr:C_NAV + 128 * (pr + 1)]), writes=["wv3"])
                        for p in range(8):
                            s.dma("sp", nabt[p % 2][:], nab_d[pr, :, p, :, :], writes=["nabt%d" % (p % 2)])
                            for hh in range(2):
                                stt("dve", BMp[:, p, hh, :], nabt[p % 2][:, hh, :], 8.0, nam[:, p, :], ALU.mult, ALU.add,
                                    ["nabt%d" % (p % 2), "nam"], ["BMp"])
                        cnt_p = [0]

                        def proj(dst, lhs_fn, rhs_fn, m, n, rk, dk):
                            u = cnt_p[0] % 2
                            cnt_p[0] += 1
                            for kt in range(8):
                                mm(PB[u][0:m, 0:n], lhs_fn(kt), rhs_fn(kt), kt == 0, kt == 7, rk, [], x=[PK[u]])
                            cp("act" if u == 0 else "dve", dst, PB[u][0:m, 0:n], [], [dk], x=[PK[u]])

                        for tb in range(4):
                            cols = slice(tb * 512, (tb + 1) * 512)
                            proj(qT3[:, cols], lambda kt: wq3[:, kt, :], lambda kt, cols=cols: aT[:, kt, cols], 128, 512, ["wq3", "aT"], "qT3")
                            proj(kT3[:, cols], lambda kt: wk3[:, kt, :], lambda kt, cols=cols: aT[:, kt, cols], 128, 512, ["wk3", "aT"], "kT3")
                        proj(kT3[:, S:S + CT], lambda kt: wk3[:, kt, :], lambda kt: acT[:, kt, :], 128, CT, ["wk3", "acT"], "kT3")
                        for i in range(16):
                            proj(Ve[:, i, :], lambda kt, i=i: aT[:, kt, i * 128:(i + 1) * 128], lambda kt: wv3[:, kt, :], 128, 128, ["wv3", "aT"], "Ve")
                        for i in range(15):
                            proj(Vo[:, i, :], lambda kt, i=i: aT[:, kt, 64 + i * 128:64 + (i + 1) * 128], lambda kt: wv3[:, kt, :], 128, 128, ["wv3", "aT"], "Vo")
                        for i in range(2):
                            proj(Vc[:, i, :], lambda kt, i=i: acT[:, kt, i * 128:(i + 1) * 128], lambda kt: wv3[:, kt, :], 128, 128, ["wv3", "acT"], "Vc")

                        def scores(r):
                            rs = min(max(r - 4, 0), 24)
                            p = r - rs
                            buf = r % 2
                            qc = slice(r * 64, r * 64 + 64)
                            for hh in range(2):
                                pr_ = slice(64 * hh, 64 * hh + 64)
                                bank = SB[buf * 2 + hh]
                                xk = [SK[buf * 2 + hh]]
                                mm(bank[:, 0:256], ident_b[:], BMp[:, p, hh, :], True, False, ["ident_b", "BMp"], [], inc=False, x=xk)
                                for j in range(4):
                                    kc0 = (rs + 2 * j) * 64
                                    mm(bank[:, j * 64:(j + 1) * 64], kT3[pr_, kc0:kc0 + 128], qT3[pr_, qc], False, j == 3,
                                       ["kT3", "qT3"], [], inc=False, x=xk)
                                for jc in range(2):
                                    mm(bank[:, 256 + jc * 64:256 + (jc + 1) * 64], kT3[pr_, S + jc * 128:S + (jc + 1) * 128], qT3[pr_, qc],
                                       True, True, ["kT3", "qT3"], [], inc=(jc == 1), x=xk)
                                act(PT[buf][:, :, hh, :], bank[:, 0:384].rearrange("p (j q) -> p j q", q=64), AF.Exp,
                                    [], ["PT%d" % buf], scale=0.125, x=xk)

                        def pv(r):
                            rs = min(max(r - 4, 0), 24)
                            buf = r % 2
                            qc = slice(r * 64, r * 64 + 64)
                            ob = OB[buf]
                            xo = [OK_[buf]]
                            for hh in range(2):
                                hs = slice(64 * hh, 64 * hh + 64)
                                for j in range(6):
                                    if j < 4:
                                        if rs % 2 == 0:
                                            Vt, vk = Ve[:, (rs + 2 * j) // 2, hs], "Ve"
                                        else:
                                            Vt, vk = Vo[:, (rs + 2 * j - 1) // 2, hs], "Vo"
                                    else:
                                        Vt, vk = Vc[:, j - 4, hs], "Vc"
                                    mm(ob[hs, 0:64], Vt, PT[buf][:, j, hh, :], j == 0, j == 5, [vk, "PT%d" % buf], [],
                                       inc=(j == 5), x=xo)
                            for j in range(6):
                                mm(ob[:, 64:192], ones_b[:, :], PT[buf][:, j, :, :].rearrange("p h q -> p (h q)"), j == 0, j == 5,
                                   ["ones_b", "PT%d" % buf], [], inc=(j == 5), x=xo)
                            s.op("dve", lambda e: e.reciprocal(out=rden[buf][:], in_=ob[:, 64:192]), reads=[], writes=["rden%d" % buf], excl=xo)
                            for hh in range(2):
                                hs = slice(64 * hh, 64 * hh + 64)
                                tt("dve", onaT[hs, pr, qc], ob[hs, 0:64], rden[buf][hs, 64 * hh:64 * hh + 64], ALU.mult,
                                   ["rden%d" % buf], ["onaT"], x=xo)

                        for r in range(32):
                            if r < 8:
                                precast(1)
                            scores(r)
                            if r > 0:
                                pv(r - 1)
                        pv(31)
                    if smp == 0:
                        dump("onaT", onaT[:, :, :], ["onaT"])
                    s.barrier()
                    print("P3 done: deadlock-free", s.check_deadlock(), s.n_ins, s.n_wait, s.cnt)
                if stop_after == "P3":
                    s.barrier()
                    return nc

                UT = sb(sa, "UT", [128, 8, S], BF16)
                with ExitStack() as st:
                    wna = [sb(st, "wna%d" % i, [128, 4, 128], BF16) for i in range(2)]
                    wgl = [sb(st, "wgl%d" % i, [128, 8, 128], BF16) for i in range(2)]
                    w8 = [sb(st, "w8_%d" % i, [128, 8, 128], BF16) for i in range(2)]
                    w9 = [sb(st, "w9_%d" % i, [128, 8, 128], BF16) for i in range(2)]
                    sg8 = [sb(st, "sg8_%d" % i, [128, 512], F32) for i in range(2)]
                    sg9 = [sb(st, "sg9_%d" % i, [128, 512], F32) for i in range(2)]
                    t1m = [sb(st, "t1m%d" % i, [128, 512], F32) for i in range(2)]
                    t2m = [sb(st, "t2m%d" % i, [128, 512], F32) for i in range(2)]
                    MB = [pst(st, "p4b%d" % i, [128, 512]) for i in range(8)]
                    MK = ["p4B%d" % i for i in range(8)]
                    for nt in range(8):
                        wb = nt % 2
                        ncol = slice(nt * 128, (nt + 1) * 128)
                        s.dma("pool", wna[wb][:], wview(w_nao_d[:, ncol]), writes=["wna%d" % wb])
                        s.dma("pool", wgl[wb][:], wview(w_glo_d[:, ncol]), writes=["wgl%d" % wb])
                        s.dma("pool", w8[wb][:], wview(w_in_d[:, C_M8 + nt * 128:C_M8 + (nt + 1) * 128]), writes=["w8_%d" % wb])
                        s.dma("pool", w9[wb][:], wview(w_in_d[:, C_M9 + nt * 128:C_M9 + (nt + 1) * 128]), writes=["w9_%d" % wb])
                        for tb in range(4):
                            cols = slice(tb * 512, (tb + 1) * 512)
                            u = (nt * 4 + tb) % 2
                            bA, bG8, bB, bG9 = [MB[4 * u + q] for q in range(4)]
                            kA, kG8, kB, kG9 = [[MK[4 * u + q]] for q in range(4)]
                            for kt in range(4):
                                mm(bA[:, :], wna[wb][:, kt, :], onaT[:, kt, cols], kt == 0, kt == 3, ["wna%d" % wb, "onaT"], [], x=kA)
                            for kt in range(8):
                                mm(bG8[:, :], w8[wb][:, kt, :], aT[:, kt, cols], kt == 0, kt == 7, ["w8_%d" % wb, "aT"], [], x=kG8)
                            for kt in range(8):
                                mm(bB[:, :], wgl[wb][:, kt, :], oglT[:, kt, cols], kt == 0, kt == 7, ["wgl%d" % wb, "oglT"], [], x=kB)
                            for kt in range(8):
                                mm(bG9[:, :], w9[wb][:, kt, :], aT[:, kt, cols], kt == 0, kt == 7, ["w9_%d" % wb, "aT"], [], x=kG9)
                            act(sg8[u][:], bG8[:, :], AF.Sigmoid, [], ["sg8_%d" % u], x=kG8)
                            act(sg9[u][:], bG9[:, :], AF.Sigmoid, [], ["sg9_%d" % u], x=kG9)
                            tt("dve", t1m[u][:], bA[:, :], sg8[u][:], ALU.mult, ["sg8_%d" % u], ["t1m%d" % u], x=kA)
                            tt("dve", t2m[u][:], bB[:, :], sg9[u][:], ALU.mult, ["sg9_%d" % u], ["t2m%d" % u], x=kB)
                            tt("pool", UT[:, nt, cols], t1m[u][:], t2m[u][:], ALU.add, ["t1m%d" % u, "t2m%d" % u], ["UT"])
                    if smp == 0:
                        dump("UT", UT[:, :, 0:256], ["UT"])
                    s.barrier()
                with ExitStack() as st:
                    wo = sb(st, "wo", [128, 8, D], BF16)
                    wtmp = [sb(st, "wtmp%d" % i, [128, D], F32) for i in range(2)]
                    g1b = sb(st, "g1b", [128, D], F32)
                    xt2 = [sb(st, "xt2_%d" % i, [128, D], F32) for i in range(2)]
                    h1t = [sb(st, "h1t%d" % i, [128, D], F32) for i in range(2)]
                    MB = [pst(st, "p4c%d" % i, [128, 512]) for i in range(4)]
                    MK = ["p4C%d" % i for i in range(4)]
                    s.dma("sp", g1b[:], modv_d[smp, 2 * D:3 * D].partition_broadcast(128), writes=["g1b"])
                    for kt in range(8):
                        s.dma("sp", wtmp[kt % 2][:], w_out_d[kt * 128:(kt + 1) * 128, :], writes=["wtmp%d" % (kt % 2)])
                        tt("pool", wo[:, kt, :], wtmp[kt % 2][:], g1b[:], ALU.mult, ["wtmp%d" % (kt % 2), "g1b"], ["wo"])
                    for i in range(NT):
                        u = i % 2
                        tokc = slice(i * 128, (i + 1) * 128)
                        s.dma("sp", xt2[u][:], x_d[smp, tokc, :], writes=["xt2_%d" % u])
                        for half in range(2):
                            hcol = slice(half * 512, (half + 1) * 512)
                            bk = MB[2 * u + half]
                            xk = [MK[2 * u + half]]
                            for kt in range(8):
                                mm(bk[:, :], UT[:, kt, tokc], wo[:, kt, hcol], kt == 0, kt == 7, ["UT", "wo"], [], x=xk)
                            tt("dve", h1t[u][:, hcol], bk[:, :], xt2[u][:, hcol], ALU.add, ["xt2_%d" % u], ["h1t%d" % u], x=xk)
                        s.dma("sp", h1_d[smp, tokc, :], h1t[u][:], reads=["h1t%d" % u], writes=["h1d"])
                        if smp == 0 and i < 2 and "h1" in dbg_d:
                            dump("h1", h1t[u][:], ["h1t%d" % u], dst=dbg_d["h1"][i * 128:(i + 1) * 128, :])
                    s.barrier()
                    print("P4 done: deadlock-free", s.check_deadlock(), s.n_ins, s.n_wait, s.cnt)
                if stop_after == "P4":
                    s.barrier()
                    return nc
            CAP = MOE_CAP
            NB = CAP // 128
            NSLOT = NEXP * CAP
            Xd = nc.dram_tensor("xd_scr%d" % smp, [NSLOT, D], BF16).ap()
            Yd = nc.dram_tensor("yd_scr%d" % smp, [NSLOT, D], BF16).ap()

            ind_hist = []
            IND_DEPTH = int(os.environ.get("IND_DEPTH", "1000"))

            def dma_fn(eng, fn, reads, writes):
                i_ = s.dnext
                s.dnext = (s.dnext + 1) % s.n_fg
                k_ = ("d", i_)
                waits = s._deps(eng, reads, writes)
                if s.dcnt[i_] > 0 and s.waited[eng].get(k_, -1) < s.dcnt[i_]:
                    s.waited[eng][k_] = s.dcnt[i_]
                    waits.append((k_, s.dcnt[i_]))
                if len(ind_hist) >= IND_DEPTH:
                    pk, pv = ind_hist[-IND_DEPTH]
                    if pk != k_ and s.waited[eng].get(pk, -1) < pv:
                        s.waited[eng][pk] = pv
                        waits.append((pk, pv))
                s.dcnt[i_] += 16
                tok = (k_, s.dcnt[i_])
                ind_hist.append(tok)
                s._emit1(eng, waits, fn, (k_, 16))
                s._commit(tok, reads, writes)

            with ExitStack() as sm:
                hres = sb(sm, "hres", [128, NT, D], F32)
                g2b = sb(sm, "g2b", [128, D], F32)
                gfb = sb(sm, "gfb", [128, D], F32)
                SLi = sb(sm, "SLi", [128, NT * 2], I32)
                w12 = sb(sm, "w12", [128, NT, 2], F32)
                s.dma("sp", g2b[:], modv_d[smp, 5 * D:6 * D].partition_broadcast(128), writes=["g2b"])
                s.dma("sp", gfb[:], g_fin_d.partition_broadcast(128), writes=["gfb"])
                with ExitStack() as st:
                    fbf = sb(st, "fbf", [128, NT, D], BF16)
                    M1a = sb(st, "M1a", [128, NT, 32], F32)
                    M2a = sb(st, "M2a", [128, NT, 32], F32)
                    Ma = sb(st, "Ma", [128, NT, 32], F32)
                    SLf = sb(st, "SLf", [128, NT, 2], F32)
                    ebase = sb(st, "ebase_sb", [128, 32], F32)
                    triS = sb(st, "triS", [128, 128], F32)
                    ones128 = sb(st, "ones128", [128, 128], F32)
                    s2b = sb(st, "s2b", [128, D], F32)
                    sh2b = sb(st, "sh2b", [128, D], F32)
                    gfn = sb(st, "gfn", [128, D], F32)
                    wrt = sb(st, "wrt", [128, 8, 36], F32)
                    brt = sb(st, "brt", [1, 36], F32)
                    ones_f = sb(st, "ones_f", [1, 128], F32)
                    xm3 = [sb(st, "xm3_%d" % i, [128, D], F32) for i in range(2)]
                    junk3 = sb(st, "junk3", [128, D], F32)
                    fT32 = [sb(st, "fT32_%d" % i, [128, 8, 128], F32) for i in range(2)]
                    LG = sb(st, "LG", [128, NT, 36], F32)
                    gm = sb(st, "gm", [128, NT, 8], F32)
                    ohg = sb(st, "ohg", [128, NT, 4], F32)
                    ge = sb(st, "ge", [128, NT, 4], F32)
                    big = sb(st, "bigr", [128, NT, 4, 8], F32)
                    leg = sb(st, "leg", [128, NT, 8], F32)
                    oh1 = sb(st, "oh1", [128, NT, 8], F32)
                    oh2 = sb(st, "oh2", [128, NT, 8], F32)
                    msk = sb(st, "msk", [128, NT, 8], F32)
                    rk = sb(st, "rk", [128, NT, 32], F32)
                    vm = sb(st, "vm", [128, NT, 32], F32)
                    st3 = [sb(st, "st3_%d" % i, [128, 4], F32) for i in range(2)]
                    pf = [[pst(st, "p5f%d_%d" % (i, j), [128, 4, 128]) for j in range(2)] for i in range(2)]
                    pfk = [["p5F%d_%d" % (i, j) for j in range(2)] for i in range(2)]
                    pl = [pst(st, "p5l%d" % i, [128, 512]) for i in range(2)]
                    plk = ["p5L%d" % i for i in range(2)]
                    s.dma("sp", gfn[:], g_ffn_d.partition_broadcast(128), writes=["gfn"])
                    s.dma("sp", s2b[:], modv_d[smp, 4 * D:5 * D].partition_broadcast(128), writes=["s2b"])
                    stt("dve", s2b[:], s2b[:], 1.0, gfn[:], ALU.add, ALU.mult, ["s2b", "gfn"], ["s2b"])
                    s.dma("sp", sh2b[:], modv_d[smp, 3 * D:4 * D].partition_broadcast(128), writes=["sh2b"])
                    s.dma("sp", wrt[:], wview(w_rt_d), writes=["wrt"])
                    s.dma("sp", brt[:], b_rt_d.partition_broadcast(1), writes=["brt"])
                    s.dma("sp", ebase[:], ebase_d, writes=["ebase"])
                    s.dma("sp", triS[:], tri_d[4], writes=["triS"])
                    s.op("pool", lambda e: e.memset(ones_f[:], 1.0), writes=["ones_f"])
                    s.op("pool", lambda e: e.memset(ones128[:], 1.0), writes=["ones128"])
                    for i in range(NT):
                        u = i % 2
                        tokc = slice(i * 128, (i + 1) * 128)
                        hk = "hres%d" % i
                        s.dma("sp", hres[:, i, :], h1_d[smp, tokc, :], reads=["h1d"], writes=[hk])
                        s.op("pool", lambda e, u=u: e.memset(st3[u][:], 0.0), writes=["st3_%d" % u])
                        act(junk3[:], hres[:, i, :], AF.Square, [hk, "st3_%d" % u], ["junk3", "st3_%d" % u], accum_out=st3[u][:, 0:1])
                        act(st3[u][:, 1:2], st3[u][:, 0:1], AF.Sqrt, ["st3_%d" % u, "eps_c"], ["st3_%d" % u], scale=1.0 / D, bias=eps_c[:, 0:1])
                        s.op("dve", lambda e, u=u: e.reciprocal(out=st3[u][:, 2:3], in_=st3[u][:, 1:2]), reads=["st3_%d" % u], writes=["st3_%d" % u])
                        stt("dve", xm3[u][:], hres[:, i, :], st3[u][:, 2:3], s2b[:], ALU.mult, ALU.mult, [hk, "st3_%d" % u, "s2b"], ["xm3_%d" % u])
                        tt("pool", xm3[u][:], xm3[u][:], sh2b[:], ALU.add, ["xm3_%d" % u, "sh2b"], ["xm3_%d" % u])
                        cp("pool", fbf[:, i, :], xm3[u][:], ["xm3_%d" % u], ["fbf%d" % i])
                        for kt in range(8):
                            tr(pf[u][kt // 4][:, kt % 4, :], xm3[u][:, kt * 128:(kt + 1) * 128], ident_f[:], ["xm3_%d" % u, "ident_f"], [], x=[pfk[u][kt // 4]])
                        for j in range(2):
                            cp("act" if j == 0 else "dve", fT32[u][:, 4 * j:4 * j + 4, :], pf[u][j][:, :, :], [], ["fT32_%d" % u], x=[pfk[u][j]])
                        for kt in range(8):
                            mm(pl[u][:, 0:36], fT32[u][:, kt, :], wrt[:, kt, :], kt == 0, False, ["fT32_%d" % u, "wrt"], [], inc=False, x=[plk[u]])
                        mm(pl[u][:, 0:36], ones_f[0:1, :], brt[0:1, :], False, True, ["ones_f", "brt"], [], x=[plk[u]])
                        cp("dve", LG[:, i, :], pl[u][:, 0:36], [], ["LG"], x=[plk[u]])
                    T_ = NT
                    dv = lambda fn: s.op("dve", fn, reads=["LG", "RT"], writes=["RT"])
                    bc = lambda ap, shp: ap.broadcast_to(shp)
                    lgg = LG[:, :, 0:4]
                    lge = LG[:, :, 4:36].rearrange("p t (g j) -> p t g j", j=8)
                    dv(lambda e: e.reduce_max(out=gm[:, :, 0], in_=lgg, axis=AX.X))
                    dv(lambda e: e.tensor_tensor(out=ohg[:], in0=lgg, in1=bc(gm[:, :, 0:1], [128, T_, 4]), op=ALU.is_equal))
                    dv(lambda e: e.tensor_tensor(out=ge[:], in0=lgg, in1=bc(gm[:, :, 0:1], [128, T_, 4]), op=ALU.subtract))
                    act(ge[:], ge[:], AF.Exp, ["RT"], ["RT"])
                    dv(lambda e: e.reduce_sum(out=gm[:, :, 1], in_=ge[:], axis=AX.X))
                    dv(lambda e: e.reciprocal(out=gm[:, :, 1], in_=gm[:, :, 1]))
                    dv(lambda e: e.tensor_tensor(out=big[:], in0=lge, in1=bc(ohg[:, :, :, None], [128, T_, 4, 8]), op=ALU.mult))
                    dv(lambda e: e.reduce_sum(out=leg[:], in_=big[:].rearrange("p t g j -> p t j g"), axis=AX.X))
                    dv(lambda e: e.reduce_max(out=gm[:, :, 2], in_=leg[:], axis=AX.X))
                    dv(lambda e: e.tensor_tensor(out=oh1[:], in0=leg[:], in1=bc(gm[:, :, 2:3], [128, T_, 8]), op=ALU.is_equal))
                    dv(lambda e: e.scalar_tensor_tensor(out=msk[:], in0=oh1[:], scalar=-1e30, in1=leg[:], op0=ALU.mult, op1=ALU.add))
                    dv(lambda e: e.reduce_max(out=gm[:, :, 3], in_=msk[:], axis=AX.X))
                    dv(lambda e: e.tensor_tensor(out=oh2[:], in0=msk[:], in1=bc(gm[:, :, 3:4], [128, T_, 8]), op=ALU.is_equal))
                    dv(lambda e: e.tensor_tensor(out=gm[:, :, 4], in0=gm[:, :, 3], in1=gm[:, :, 2], op=ALU.subtract))
                    act(gm[:, :, 4], gm[:, :, 4], AF.Exp, ["RT"], ["RT"])
                    dv(lambda e: e.tensor_scalar(out=gm[:, :, 5], in0=gm[:, :, 4], scalar1=1.0, scalar2=None, op0=ALU.add))
                    dv(lambda e: e.reciprocal(out=gm[:, :, 5], in_=gm[:, :, 5]))
                    s.op("dve", lambda e: e.tensor_tensor(out=w12[:, :, 0], in0=gm[:, :, 5], in1=gm[:, :, 1], op=ALU.mult), reads=["RT"], writes=["w12"])
                    s.op("dve", lambda e: e.tensor_tensor(out=w12[:, :, 1], in0=w12[:, :, 0], in1=gm[:, :, 4], op=ALU.mult), reads=["RT", "w12"], writes=["w12"])
                    m1v = M1a[:].rearrange("p t (g j) -> p t g j", j=8)
                    m2v = M2a[:].rearrange("p t (g j) -> p t g j", j=8)
                    s.op("dve", lambda e: e.tensor_tensor(out=m1v, in0=bc(ohg[:, :, :, None], [128, T_, 4, 8]), in1=bc(oh1[:, :, None, :], [128, T_, 4, 8]),
                                                          op=ALU.mult), reads=["RT"], writes=["M1a"])
                    s.op("dve", lambda e: e.tensor_tensor(out=m2v, in0=bc(ohg[:, :, :, None], [128, T_, 4, 8]), in1=bc(oh2[:, :, None, :], [128, T_, 4, 8]),
                                                          op=ALU.mult), reads=["RT"], writes=["M2a"])
                    tt("pool", Ma[:], M1a[:], M2a[:], ALU.add, ["M1a", "M2a"], ["Ma"])
                    for i in range(NT):
                        rc = slice(i * 32, (i + 1) * 32)
                        mm(pl[0][:, rc], triS[:], Ma[:, i, :], True, i == 0, ["triS", "Ma"], [], inc=(i == 0), x=[plk[0]])
                        for i2 in range(i):
                            mm(pl[0][:, rc], ones128[:], Ma[:, i2, :], False, i2 == i - 1, ["ones128", "Ma"], [], inc=(i2 == i - 1), x=[plk[0]])
                    dq = lambda fn, rd=(), wr=(), x=(): s.op("dve", fn, reads=["QT"] + list(rd), writes=["QT"] + list(wr), excl=x)
                    dq(lambda e: e.tensor_copy(out=rk[:], in_=pl[0][:, :].rearrange("p (t e) -> p t e", e=32)), x=[plk[0]])
                    dq(lambda e: e.tensor_scalar(out=vm[:], in0=rk[:], scalar1=float(CAP), scalar2=None, op0=ALU.is_lt))
                    dq(lambda e: e.tensor_tensor(out=rk[:], in0=rk[:], in1=bc(ebase[:, None, :], [128, T_, 32]), op=ALU.add), rd=["ebase"])
                    for k_, Mk, mk in ((0, M1a, "M1a"), (1, M2a, "M2a")):
                        dq(lambda e, Mk=Mk: e.tensor_tensor(out=big[:].rearrange("p t g j -> p t (g j)"), in0=Mk[:], in1=rk[:], op=ALU.mult), rd=[mk, "RT"], wr=["RT"])
                        dq(lambda e, k_=k_: e.reduce_sum(out=SLf[:, :, k_], in_=big[:].rearrange("p t g j -> p t (g j)"), axis=AX.X), rd=["RT"], wr=["SLf"])
                        dq(lambda e, Mk=Mk: e.tensor_tensor(out=big[:].rearrange("p t g j -> p t (g j)"), in0=Mk[:], in1=vm[:], op=ALU.mult), rd=[mk, "RT"], wr=["RT"])
                        dq(lambda e: e.reduce_sum(out=gm[:, :, 6], in_=big[:].rearrange("p t g j -> p t (g j)"), axis=AX.X), rd=["RT"], wr=["RT"])
                        dq(lambda e, k_=k_: e.tensor_tensor(out=w12[:, :, k_], in0=w12[:, :, k_], in1=gm[:, :, 6], op=ALU.mult), rd=["w12", "RT"], wr=["w12"])
                        dq(lambda e: e.tensor_scalar(out=gm[:, :, 7], in0=gm[:, :, 6], scalar1=-1.0e6, scalar2=1.0e6, op0=ALU.mult, op1=ALU.add), rd=["RT"], wr=["RT"])
                        dq(lambda e, k_=k_: e.tensor_tensor(out=SLf[:, :, k_], in0=SLf[:, :, k_], in1=gm[:, :, 7], op=ALU.add), rd=["RT", "SLf"], wr=["SLf"])
                    s.op("dve", lambda e: e.tensor_copy(out=SLi[:], in_=SLf[:].rearrange("p t k -> p (t k)")), reads=["SLf"], writes=["SLi"])
                    for i in range(NT):
                        for k_ in range(2):
                            dma_fn("pool", lambda e, k_=k_, i=i: e.indirect_dma_start(
                                out=Xd[:, :], out_offset=bass.IndirectOffsetOnAxis(ap=SLi[:, 2 * i + k_:2 * i + k_ + 1], axis=0),
                                in_=fbf[:, i, :], in_offset=None, bounds_check=bc_reg, oob_is_err=False),
                                ["SLi", "fbf%d" % i], ["Xd"])
                    if smp == 0:
                        dump("SLf", SLf[:, :, :], ["SLf"])
                        dump("w12", w12[:, :, :], ["w12"])
                    s.barrier()
                    print("P5 done: deadlock-free", s.check_deadlock(), s.n_ins, s.n_wait, s.cnt)
                if stop_after == "P5":
                    s.barrier()
                    return nc
                chunks = [(c0, min(c0 + 512, CAP)) for c0 in range(0, CAP, 512)]
                with ExitStack() as st:
                    weg = [sb(st, "weg%d" % i, [128, 8, DE], BF16) for i in range(2)]
                    weu = [sb(st, "weu%d" % i, [128, 8, DE], BF16) for i in range(2)]
                    wed = [sb(st, "wed%d" % i, [128, 4, D], BF16) for i in range(2)]
                    XeT = [sb(st, "XeT%d" % i, [128, 8, CAP], BF16) for i in range(2)]
                    hT = [sb(st, "hT%d" % i, [128, 4, CAP], BF16) for i in range(2)]
                    xe = [sb(st, "xe%d" % i, [128, D], BF16) for i in range(2)]
                    ye = [sb(st, "ye%d" % i, [128, D], BF16) for i in range(2)]
                    sgm = [sb(st, "sgm%d" % i, [128, 512], F32) for i in range(2)]
                    TX = [pst(st, "p6t%d" % i, [128, 8, 128], BF16) for i in range(2)]
                    GB = [pst(st, "p6g%d" % i, [128, 512]) for i in range(2)]
                    UB = [pst(st, "p6u%d" % i, [128, 512]) for i in range(2)]
                    YB = [pst(st, "p6y%d" % i, [128, 512]) for i in range(2)]
                    TXK = ["p6T%d" % i for i in range(2)]
                    GK = ["p6G%d" % i for i in range(2)]
                    UK = ["p6U%d" % i for i in range(2)]
                    YK = ["p6Y%d" % i for i in range(2)]
                    cnt6 = [0, 0, 0]

                    def load_w(e_):
                        wb = e_ % 2
                        s.dma("sp", weg[wb][:], wview(weg_b[e_]), reads=["wbg%d" % e_], writes=["weg%d" % wb])
                        s.dma("sp", weu[wb][:], wview(weu_b[e_]), reads=["wbu%d" % e_], writes=["weu%d" % wb])
                        s.dma("sp", wed[wb][:], wview(wed_b[e_]), reads=["wbd%d" % e_], writes=["wed%d" % wb])
                        for kt in range(4):
                            tt("pool", wed[wb][:, kt, :], wed[wb][:, kt, :], g2b[:], ALU.mult, ["wed%d" % wb, "g2b"], ["wed%d" % wb])

                    def ph_T(e_):
                        wb = e_ % 2
                        for blk in range(NB):
                            u = cnt6[0] % 2
                            cnt6[0] += 1
                            r0 = e_ * CAP + blk * 128
                            s.dma("sp", xe[u][:], Xd[r0:r0 + 128, :], writes=["xe%d" % u])
                            for kt in range(8):
                                tr(TX[u][:, kt, :], xe[u][:, kt * 128:(kt + 1) * 128], ident_b[:], ["xe%d" % u, "ident_b"], [], x=[TXK[u]])
                            cp("act" if u == 0 else "dve", XeT[wb][:, :, blk * 128:(blk + 1) * 128], TX[u][:, :, :], [], ["XeT%d" % wb], x=[TXK[u]])

                    def ph_GU(e_):
                        wb = e_ % 2
                        for nt in range(4):
                            ncol = slice(nt * 128, (nt + 1) * 128)
                            for (c0, c1) in chunks:
                                u = cnt6[1] % 2
                                cnt6[1] += 1
                                n = c1 - c0
                                for kt in range(8):
                                    mm(GB[u][:, 0:n], weg[wb][:, kt, ncol], XeT[wb][:, kt, c0:c1], kt == 0, kt == 7, ["weg%d" % wb, "XeT%d" % wb], [], x=[GK[u]])
                                for kt in range(8):
                                    mm(UB[u][:, 0:n], weu[wb][:, kt, ncol], XeT[wb][:, kt, c0:c1], kt == 0, kt == 7, ["weu%d" % wb, "XeT%d" % wb], [], x=[UK[u]])
                                act(sgm[u][:, 0:n], GB[u][:, 0:n], AF.Silu, [], ["sgm%d" % u], x=[GK[u]])
                                tt("dve", hT[wb][:, nt, c0:c1], sgm[u][:, 0:n], UB[u][:, 0:n], ALU.mult, ["sgm%d" % u], ["hT%d" % wb], x=[UK[u]])

                    def ph_D(e_):
                        wb = e_ % 2
                        for blk in range(NB):
                            yu = blk % 2
                            r0 = e_ * CAP + blk * 128
                            for half in range(2):
                                u = cnt6[2] % 2
                                cnt6[2] += 1
                                hcol = slice(half * 512, (half + 1) * 512)
                                for kt in range(4):
                                    mm(YB[u][:, :], hT[wb][:, kt, blk * 128:(blk + 1) * 128], wed[wb][:, kt, hcol], kt == 0, kt == 3,
                                       ["hT%d" % wb, "wed%d" % wb], [], x=[YK[u]])
                                cp("act" if half == 0 else "dve", ye[yu][:, hcol], YB[u][:, :], [], ["ye%d" % yu], x=[YK[u]])
                            s.dma("pool", Yd[r0:r0 + 128, :], ye[yu][:], reads=["ye%d" % yu])

                    precast(96)
                    load_w(0)
                    ph_T(0)
                    for e_ in range(NEXP):
                        if e_ + 1 < NEXP:
                            load_w(e_ + 1)
                        ph_GU(e_)
                        if e_ + 1 < NEXP:
                            ph_T(e_ + 1)
                        ph_D(e_)
                    s.barrier()
                    print("P6 done: deadlock-free", s.check_deadlock(), s.n_ins, s.n_wait, s.cnt)
                with ExitStack() as st:
                    yk = [[sb(st, "yk%d_%d" % (k_, i), [128, D], BF16) for i in range(2)] for k_ in range(2)]
                    ot = [sb(st, "ot%d" % i, [128, D], F32) for i in range(2)]
                    junk4 = sb(st, "junk4", [128, D], F32)
                    st4 = [sb(st, "st4_%d" % i, [128, 4], F32) for i in range(2)]
                    for k_ in range(2):
                        for i in range(2):
                            s.op("pool", lambda e, k_=k_, i=i: e.memset(yk[k_][i][:], 0.0), writes=["yk%d_%d" % (k_, i)])
                    for i in range(NT):
                        u = i % 2
                        tokc = slice(i * 128, (i + 1) * 128)
                        hk = "hres%d" % i
                        for k_ in range(2):
                            dma_fn("pool", lambda e, k_=k_, u=u, i=i: e.indirect_dma_start(
                                out=yk[k_][u][:, :], out_offset=None, in_=Yd[:, :],
                                in_offset=bass.IndirectOffsetOnAxis(ap=SLi[:, 2 * i + k_:2 * i + k_ + 1], axis=0), bounds_check=bc_reg, oob_is_err=False),
                                ["SLi"], ["yk%d_%d" % (k_, u)])
                            stt("dve", hres[:, i, :], yk[k_][u][:], w12[:, i, k_:k_ + 1], hres[:, i, :], ALU.mult, ALU.add,
                                ["yk%d_%d" % (k_, u), "w12", hk], [hk])
                        s.op("pool", lambda e, u=u: e.memset(st4[u][:], 0.0), writes=["st4_%d" % u])
                        act(junk4[:], hres[:, i, :], AF.Square, [hk, "st4_%d" % u], ["junk4", "st4_%d" % u], accum_out=st4[u][:, 0:1])
                        act(st4[u][:, 1:2], st4[u][:, 0:1], AF.Sqrt, ["st4_%d" % u, "eps_c"], ["st4_%d" % u], scale=1.0 / D, bias=eps_c[:, 0:1])
                        s.op("dve", lambda e, u=u: e.reciprocal(out=st4[u][:, 2:3], in_=st4[u][:, 1:2]), reads=["st4_%d" % u], writes=["st4_%d" % u])
                        stt("dve", ot[u][:], hres[:, i, :], st4[u][:, 2:3], gfb[:], ALU.mult, ALU.mult, [hk, "st4_%d" % u, "gfb"], ["ot%d" % u])
                        s.dma("sp", y_d[smp, tokc, :], ot[u][:], reads=["ot%d" % u])
                    s.barrier()
        s.barrier(final=True)
    return nc


def _consts():
    ident = np.eye(128, dtype=np.float32)
    idx = np.arange(128)
    same = (idx[:, None] // 64) == (idx[None, :] // 64)
    tri = np.zeros((6, 128, 128), np.float32)
    tri[0] = same & (idx[:, None] <= idx[None, :])
    tri[1] = same & (idx[:, None] > idx[None, :])
    tri[2] = same & (idx[:, None] >= idx[None, :])
    tri[3] = same & (idx[:, None] < idx[None, :])
    tri[4] = idx[:, None] < idx[None, :]
    tri[5] = 1.0
    t = np.arange(S)
    pos_r = (t // GW).astype(np.float32)
    pos_c = (t % GW).astype(np.float32)
    inv = (10000.0 ** (-np.arange(32, dtype=np.float32) / 32)).astype(np.float32)
    ang = np.zeros((128, S), np.float32)
    ang[0:32] = inv[:, None] * pos_r[None, :]
    ang[32:64] = inv[:, None] * pos_r[None, :]
    ang[64:96] = inv[:, None] * pos_c[None, :]
    ang[96:128] = inv[:, None] * pos_c[None, :]
    cos = np.cos(ang).astype(np.float32)
    sin = np.sin(ang).astype(np.float32)
    sgn = np.ones((128, 1), np.float32)
    sgn[0:32] = -1
    sgn[64:96] = -1
    sin = sin * sgn
    perm = np.concatenate([np.arange(32, 64), np.arange(0, 32), np.arange(96, 128), np.arange(64, 96)])
    return ident, tri, cos, sin, perm


def _na_tables(rpb):
    a = (np.arange(128) // 64)[:, None, None, None]
    kc = (np.arange(128) % 64)[:, None, None, None]
    p = np.arange(8)[None, :, None, None]
    j = np.arange(4)[None, None, :, None]
    qc = np.arange(64)[None, None, None, :]
    ridx = 2 * j + a - p + 7
    cstart = np.clip(qc - 8, 0, 48)
    valid = (kc >= cstart) & (kc < cstart + 16) & (ridx >= 0) & (ridx <= 14)
    valid = np.broadcast_to(valid, (128, 8, 4, 64))
    cidx = np.clip(kc - qc + 15, 0, 30)
    ridx_c = np.clip(ridx, 0, 14)
    ridx_b = np.broadcast_to(ridx_c, (128, 8, 4, 64))
    cidx_b = np.broadcast_to(cidx, (128, 8, 4, 64))
    g = rpb[:, ridx_b, cidx_b]
    g = np.where(valid[None], g, np.float32(0.0)).astype(np.float32)
    g = g.reshape(4, 2, 128, 8, 256).transpose(0, 2, 3, 1, 4)
    mask = np.where(valid, np.float32(0.0), np.float32(MASKV)).astype(np.float32).reshape(128, 8, 256)
    return np.ascontiguousarray(g), np.ascontiguousarray(mask)


_NC_CACHE = {}


def kernel(x, c, ctx, c_ctx, w_mod, b_mod, norm_attn_g, norm_ffn_g, w_in, w_gla_a2, b_gla_a2,
           gla_norm_g, na_rpb, w_na_o, w_gla_o, w_out, w_group, b_group, w_expert, b_expert,
           w_exp_gate, w_exp_up, w_exp_down, final_norm_g):
    f = lambda a: np.ascontiguousarray(np.asarray(a, dtype=np.float32))
    x, c, ctx, c_ctx = f(x), f(c), f(ctx), f(c_ctx)
    ident, tri, cos, sin, perm = _consts()
    w_in0 = f(w_in)[0]
    gq = w_in0[:, C_GQ:C_GQ + 512].reshape(D, 4, 128)[:, :, perm].reshape(D, 512)
    gk = w_in0[:, C_GK:C_GK + 512].reshape(D, 4, 128)[:, :, perm].reshape(D, 512)
    w_rope = np.ascontiguousarray(np.concatenate([gq, gk], axis=1))
    w_a2b = np.ascontiguousarray(np.concatenate(
        [f(w_gla_a2)[0].transpose(1, 0, 2), f(b_gla_a2)[0][None]], axis=0))
    nab, nam = _na_tables(f(na_rpb)[0])
    w_rt = np.ascontiguousarray(np.concatenate([f(w_group)[0], f(w_expert)[0]], axis=1))
    b_rt = np.ascontiguousarray(np.concatenate([f(b_group)[0], f(b_expert)[0]], axis=0))
    shared = {
        "w_mod": f(w_mod)[0], "b_mod": f(b_mod)[0], "g_attn": f(norm_attn_g)[0], "g_ffn": f(norm_ffn_g)[0],
        "g_fin": f(final_norm_g), "gla_g": f(gla_norm_g)[0], "w_in": w_in0, "w_rope": w_rope, "w_a2b": w_a2b,
        "w_na_o": f(w_na_o)[0], "w_gla_o": f(w_gla_o)[0], "w_out": f(w_out)[0], "w_rt": w_rt, "b_rt": b_rt,
        "w_eg": f(w_exp_gate)[0], "w_eu": f(w_exp_up)[0], "w_ed": f(w_exp_down)[0],
        "ident": ident, "tri": tri, "rope_cos": cos, "rope_sin": sin, "na_bias": nab, "na_mask": nam,
        "ebase": np.ascontiguousarray(np.broadcast_to((np.arange(NEXP, dtype=np.float32) * MOE_CAP)[None, :], (128, NEXP))),
    }
    n = 8
    NS = x.shape[0] // n
    if "nc" not in _NC_CACHE:
        _NC_CACHE["nc"] = build_nc(NS)
    nc = _NC_CACHE["nc"]
    in_maps = []
    for i in range(n):
        m = dict(shared)
        m["x"] = x[i * NS:(i + 1) * NS]
        m["ctx"] = ctx[i * NS:(i + 1) * NS]
        m["cc"] = np.ascontiguousarray(np.concatenate([c[i * NS:(i + 1) * NS], c_ctx[None]], axis=0))
        in_maps.append(m)
    res = run_bass_kernel_spmd(nc, in_maps, core_ids=list(range(n)))
    return np.concatenate([r["y"] for r in res.results], axis=0)
```

```python
import os
import numpy as np
import concourse.bass as bass
import concourse.mybir as mybir
from concourse.bass_utils import run_bass_kernel_spmd
from contextlib import ExitStack

F32 = mybir.dt.float32
BF16 = mybir.dt.bfloat16
I32 = mybir.dt.int32
ALU = mybir.AluOpType
AF = mybir.ActivationFunctionType
AX = mybir.AxisListType

D = 1024
S = 2048
CT = 256
NT = 16
NTC = 2
NTA = NT + NTC
GW = 64
EPS = 1e-6
NEXP = 32
DE = 512
C_NAQ, C_NAK, C_NAV, C_GQ, C_GK, C_GV, C_GG, C_AL, C_M8, C_M9 = 0, 512, 1024, 1536, 2048, 2560, 3584, 4608, 4640, 5664
MASKV = -30000.0
MOE_CAP = 640


class Sched:
    ENGS = ("pe", "act", "dve", "pool", "sp")
    HND = {"pe": "tensor", "act": "scalar", "dve": "vector", "pool": "gpsimd", "sp": "sync"}

    def __init__(self, nc, es, n_dma_sems=40):
        self.nc = nc
        self.sem = {e: es.enter_context(nc.semaphore("s_" + e)) for e in self.ENGS}
        self.cnt = {e: 0 for e in self.ENGS}
        self.pending = {e: False for e in self.ENGS}
        self.waited = {e: {} for e in self.ENGS}
        self.dsem = [es.enter_context(nc.semaphore("s_dma%d" % i)) for i in range(n_dma_sems)]
        self.dcnt = [0] * n_dma_sems
        self.dnext = 0
        self.n_fg = n_dma_sems - 8
        self.bnext = 0
        self.lastw = {}
        self.reads = {}
        self.semobj = {}
        for e in self.ENGS:
            self.semobj[("e", e)] = self.sem[e]
        for i, sm in enumerate(self.dsem):
            self.semobj[("d", i)] = sm
        self.n_ins = 0
        self.n_wait = 0

    def _deps(self, eng, reads, writes, excl=()):
        toks = {}

        def add(t):
            if t is None:
                return
            k, v = t
            if toks.get(k, -1) < v:
                toks[k] = v
        for b in reads:
            add(self.lastw.get(b))
        for b in writes:
            add(self.lastw.get(b))
            for t in self.reads.get(b, ()):
                add(t)
        for b in excl:
            t = self.lastw.get(b)
            if t is not None and t[0] != ("e", eng):
                add(t)
        out = []
        for k, v in toks.items():
            if k == ("e", eng):
                if eng == "pe":
                    continue
                if v <= self.cnt[eng] - 2:
                    continue
            if self.waited[eng].get(k, -1) >= v:
                continue
            self.waited[eng][k] = v
            out.append((k, v))
        return out

    def _commit(self, tok, reads, writes):
        for b in writes:
            self.lastw[b] = tok
            self.reads[b] = []
        for b in reads:
            self.reads.setdefault(b, []).append(tok)

    def check_deadlock(self):
        pos = {e: 0 for e in self.ENGS}
        val = {}
        prog = True
        while prog:
            prog = False
            for e in self.ENGS:
                lst = self.log[e]
                while pos[e] < len(lst):
                    waits, inc = lst[pos[e]]
                    if all(val.get(k, 0) >= v for k, v in waits):
                        if inc is not None:
                            val[inc[0]] = val.get(inc[0], 0) + inc[1]
                        pos[e] += 1
                        prog = True
                    else:
                        break
        stuck = {e: (pos[e], len(self.log[e])) for e in self.ENGS if pos[e] < len(self.log[e])}
        for e in stuck:
            waits, inc = self.log[e][pos[e]]
            print("STUCK", e, pos[e], [(k, v, val.get(k, 0)) for k, v in waits])
        return not stuck

    def _emit1(self, eng, waits, fn, inc):
        if not hasattr(self, "log"):
            self.log = {e: [] for e in self.ENGS}
        self.log[eng].append((list(waits), inc if fn is not None else None))
        engh = getattr(self.nc, self.HND[eng])
        for k, v in waits:
            engh.wait_ge(self.semobj[k], v)
            self.n_wait += 1
        if fn is None:
            return
        ins = fn(engh)
        if inc is not None:
            ins.then_inc(self.semobj[inc[0]], inc[1])
        self.n_ins += 1

    def op(self, eng, fn, reads=(), writes=(), inc=True, excl=()):
        waits = self._deps(eng, reads, writes, excl)
        if inc:
            self.cnt[eng] += 1
            tok = (("e", eng), self.cnt[eng])
            self.pending[eng] = False
        else:
            tok = (("e", eng), self.cnt[eng] + 1)
            self.pending[eng] = True
        self._emit1(eng, waits, fn, (("e", eng), 1) if inc else None)
        self._commit(tok, reads, writes)
        for b_ in excl:
            self.lastw[b_] = tok
        return tok

    def dma(self, eng, out, in_, reads=(), writes=(), bg=False, **kw):
        if bg:
            i = self.n_fg + self.bnext
            self.bnext = (self.bnext + 1) % 8
        else:
            i = self.dnext
            self.dnext = (self.dnext + 1) % self.n_fg
        k = ("d", i)
        waits = self._deps(eng, reads, writes)
        if self.dcnt[i] > 0 and self.waited[eng].get(k, -1) < self.dcnt[i]:
            self.waited[eng][k] = self.dcnt[i]
            waits.append((k, self.dcnt[i]))
        self.dcnt[i] += 16
        tok = (k, self.dcnt[i])
        self._emit1(eng, waits, lambda e: e.dma_start(out=out, in_=in_, **kw), (k, 16))
        self._commit(tok, reads, writes)
        return tok

    def barrier(self, final=False):
        assert not any(self.pending.values())
        allt = [(("e", e), self.cnt[e]) for e in self.ENGS if self.cnt[e] > 0]
        allt += [(("d", i), c) for i, c in enumerate(self.dcnt) if c > 0 and (i < self.n_fg or final)]
        for e in self.ENGS:
            waits = []
            for k, v in allt:
                if k == ("e", e):
                    continue
                if self.waited[e].get(k, -1) >= v:
                    continue
                self.waited[e][k] = v
                waits.append((k, v))
            self._emit1(e, waits, None, None)
        keep = {k: t for k, t in self.lastw.items() if t[0][0] == "d" and t[0][1] >= self.n_fg}
        self.lastw = {} if final else keep
        self.reads = {}


def build_nc(NS=2, dbg=None, stop_after=None):
    nc = bass.Bass("TRN2", target_bir_lowering=False)

    def din(name, shape, dt=F32):
        return nc.dram_tensor(name, list(shape), dt, kind="ExternalInput").ap()

    x_d = din("x", [NS, S, D])
    ctx_d = din("ctx", [NS, CT, D])
    cc_d = din("cc", [NS + 1, D])
    w_mod_d = din("w_mod", [D, 6 * D])
    b_mod_d = din("b_mod", [6 * D])
    g_attn_d = din("g_attn", [D])
    g_ffn_d = din("g_ffn", [D])
    g_fin_d = din("g_fin", [D])
    gla_g_d = din("gla_g", [256])
    w_in_d = din("w_in", [D, 6688])
    w_rope_d = din("w_rope", [D, 1024])
    w_a2b_d = din("w_a2b", [17, 2, 512])
    w_nao_d = din("w_na_o", [512, D])
    w_glo_d = din("w_gla_o", [D, D])
    w_out_d = din("w_out", [D, D])
    w_rt_d = din("w_rt", [D, 36])
    b_rt_d = din("b_rt", [36])
    w_eg_d = din("w_eg", [NEXP, D, DE])
    w_eu_d = din("w_eu", [NEXP, D, DE])
    w_ed_d = din("w_ed", [NEXP, DE, D])
    ident_d = din("ident", [128, 128])
    tri_d = din("tri", [6, 128, 128])
    ebase_d = din("ebase", [128, 32])
    cos_d = din("rope_cos", [128, S])
    sin_d = din("rope_sin", [128, S])
    nab_d = din("na_bias", [4, 128, 8, 2, 256])
    nam_d = din("na_mask", [128, 8, 256])
    y_d = nc.dram_tensor("y", [NS, S, D], F32, kind="ExternalOutput").ap()
    modv_d = nc.dram_tensor("modv", [NS + 1, 6 * D], F32).ap()
    dbg_d = {}
    if dbg:
        for name, shape in dbg.items():
            dbg_d[name] = nc.dram_tensor("dbg_" + name, list(shape), F32, kind="ExternalOutput").ap()

    with ExitStack() as es:
        s = Sched(nc, es)

        used_names = {}

        def uniq(name):
            k = used_names.get(name, 0)
            used_names[name] = k + 1
            return name if k == 0 else "%s_r%d" % (name, k)

        def sb(st, name, shape, dt):
            return st.enter_context(nc.sbuf_tensor(uniq(name), list(shape), dt))

        def pst(st, name, shape, dt=F32):
            return st.enter_context(nc.psum_tensor(uniq(name), list(shape), dt))

        def mm(out, lhsT, rhs, start, stop, reads, writes, inc=None, x=()):
            if inc is None:
                inc = stop
            s.op("pe", lambda e: e.matmul(out, lhsT, rhs, start=start, stop=stop),
                 reads=reads, writes=writes, inc=inc, excl=x)

        def tr(out, in_, idn, reads, writes, x=()):
            s.op("pe", lambda e: e.transpose(out, in_, idn), reads=reads, writes=writes, excl=x)

        def act(out, in_, func, reads, writes, x=(), **kw):
            s.op("act", lambda e: e.activation(out=out, in_=in_, func=func, **kw), reads=reads, writes=writes, excl=x)

        def tt(eng, out, in0, in1, op, reads, writes, x=()):
            s.op(eng, lambda e: e.tensor_tensor(out=out, in0=in0, in1=in1, op=op), reads=reads, writes=writes, excl=x)

        def ts(eng, out, in0, s1, s2, op0, op1, reads, writes, x=()):
            if s2 is None:
                s.op(eng, lambda e: e.tensor_scalar(out=out, in0=in0, scalar1=s1, scalar2=None, op0=op0),
                     reads=reads, writes=writes, excl=x)
            else:
                s.op(eng, lambda e: e.tensor_scalar(out=out, in0=in0, scalar1=s1, scalar2=s2, op0=op0, op1=op1),
                     reads=reads, writes=writes, excl=x)

        def stt(eng, out, in0, scalar, in1, op0, op1, reads, writes, x=()):
            s.op(eng, lambda e: e.scalar_tensor_tensor(out=out, in0=in0, scalar=scalar, in1=in1, op0=op0, op1=op1),
                 reads=reads, writes=writes, excl=x)

        def cp(eng, out, in_, reads, writes, x=()):
            if eng == "act":
                s.op("act", lambda e: e.copy(out=out, in_=in_), reads=reads, writes=writes, excl=x)
            else:
                s.op(eng, lambda e: e.tensor_copy(out=out, in_=in_), reads=reads, writes=writes, excl=x)

        def dump(name, src_ap, reads, dst=None):
            if name in dbg_d:
                s.dma("pool", dbg_d[name] if dst is None else dst, src_ap, reads=reads)

        def wview(ap2d):
            return ap2d.rearrange("(kt p) n -> p kt n", p=128)

        ident_f = sb(es, "ident_f", [128, 128], F32)
        ident_b = sb(es, "ident_b", [128, 128], BF16)
        tri_f = sb(es, "tri_f", [128, 4, 128], F32)
        ones_b = sb(es, "ones_b", [128, 128], BF16)
        eps_c = sb(es, "eps_c", [128, 1], F32)
        one_c = sb(es, "one_c", [128, 1], F32)
        s.dma("sp", ident_f[:], ident_d, writes=["ident_f"])
        s.dma("pool", ident_b[:], ident_d, writes=["ident_b"])
        s.dma("sp", tri_f[:], tri_d[0:4].rearrange("m s t -> s m t"), writes=["tri"])
        s.op("pool", lambda e: e.memset(ones_b[:], 1.0), writes=["ones_b"])
        s.op("pool", lambda e: e.memset(eps_c[:], EPS), writes=["eps_c"])
        s.op("pool", lambda e: e.memset(one_c[:], 1.0), writes=["one_c"])

        with ExitStack() as st:
            ccs = sb(st, "ccs", [NS + 1, D], F32)
            scT = sb(st, "scT", [128, 8, NS + 1], F32)
            modsb = sb(st, "modsb", [NS + 1, 6 * D], F32)
            bmod = sb(st, "bmod", [NS + 1, 6 * D], F32)
            wm = [sb(st, "wm%d" % i, [128, 8, 512], F32) for i in range(2)]
            psA = pst(st, "p0a", [128, 512])
            psB = [pst(st, "p0b%d" % i, [128, 512]) for i in range(2)]
            R = NS + 1
            s.dma("sp", ccs[:], cc_d, writes=["ccs"])
            s.dma("sp", bmod[:], b_mod_d.partition_broadcast(R), writes=["bmod"])
            act(ccs[:], ccs[:], AF.Silu, ["ccs"], ["ccs"])
            for kt in range(8):
                tr(psA[:, kt * R:(kt + 1) * R], ccs[:, kt * 128:(kt + 1) * 128], ident_f[0:R, 0:R],
                   ["ccs", "ident_f"], ["p0a"])
            cp("dve", scT[:].rearrange("p k r -> p (k r)"), psA[:, 0:8 * R], ["p0a"], ["scT"])
            for cb in range(12):
                w = wm[cb % 2]
                s.dma("sp", w[:], wview(w_mod_d[:, cb * 512:(cb + 1) * 512]), writes=["wm%d" % (cb % 2)])
                ps = psB[cb % 2]
                for kt in range(8):
                    mm(ps[0:R, :], scT[:, kt, :], w[:, kt, :], kt == 0, kt == 7,
                       ["scT", "wm%d" % (cb % 2)], ["p0b%d" % (cb % 2)])
                tt("dve", modsb[:, cb * 512:(cb + 1) * 512], ps[0:R, :], bmod[:, cb * 512:(cb + 1) * 512], ALU.add,
                   ["p0b%d" % (cb % 2), "bmod"], ["modsb"])
            s.dma("sp", modv_d, modsb[:], reads=["modsb"], writes=["modv"])
            dump("modv", modsb[:], ["modsb"])
            s.barrier()
        if stop_after == "P0":
            s.barrier()
            return nc

        QSCALE = 128.0 ** -0.5
        weg_b = nc.dram_tensor("weg_bf", [NEXP, D, DE], BF16).ap()
        weu_b = nc.dram_tensor("weu_bf", [NEXP, D, DE], BF16).ap()
        wed_b = nc.dram_tensor("wed_bf", [NEXP, DE, D], BF16).ap()

        wna_b = nc.dram_tensor("wna_bf", [512, D], BF16).ap()
        wgl_b = nc.dram_tensor("wgl_bf", [D, D], BF16).ap()
        wm8_b = nc.dram_tensor("wm8_bf", [D, D], BF16).ap()
        wm9_b = nc.dram_tensor("wm9_bf", [D, D], BF16).ap()

        def precast_m1():
            s.dma("pool", wna_b, w_nao_d, writes=["wnab"], bg=True)
            s.dma("pool", wgl_b, w_glo_d, writes=["wglb"], bg=True)
            s.dma("pool", wm8_b, w_in_d[:, C_M8:C_M8 + D], writes=["wm8b"], bg=True)
            s.dma("pool", wm9_b, w_in_d[:, C_M9:C_M9 + D], writes=["wm9b"], bg=True)

        def _precast_gen():
            for e_ in range(NEXP):
                for nm, dst, srcw, r in (("g", weg_b, w_eg_d, 4), ("u", weu_b, w_eu_d, 4), ("d", wed_b, w_ed_d, 2)):
                    s.dma("pool", dst[e_].rearrange("(a r) n -> a (r n)", r=r), srcw[e_].rearrange("(a r) n -> a (r n)", r=r),
                          writes=["wb%s%d" % (nm, e_)], bg=True)
                    yield
        _pc = _precast_gen()

        def precast(n):
            for _ in range(n):
                next(_pc, None)
        bc_reg = nc.gpsimd.alloc_register("bcreg")
        nc.gpsimd.reg_mov(bc_reg, NEXP * MOE_CAP - 1)
        h1_d = nc.dram_tensor("h1_scr", [NS, S, D], F32).ap()

        def bank_set(st, pfx):
            return [pst(st, "%s%d" % (pfx, i), [128, 512]) for i in range(7)]

        for smp in range(NS):
            with ExitStack() as sa:
                aT = sb(sa, "aT", [128, 8, S], BF16)
                acT = sb(sa, "acT", [128, 8, CT], BF16)
                oglT = sb(sa, "oglT", [128, 8, S], BF16)

                with ExitStack() as st:
                    s1b = sb(st, "s1b", [128, D], F32)
                    sh1b = sb(st, "sh1b", [128, D], F32)
                    s1c = sb(st, "s1c", [128, D], F32)
                    sh1c = sb(st, "sh1c", [128, D], F32)
                    gab = sb(st, "gab", [128, D], F32)
                    tmpv = sb(st, "tmpv", [128, D], F32)
                    xt = [sb(st, "xt%d" % i, [128, D], F32) for i in range(2)]
                    xm = [sb(st, "xm%d" % i, [128, D], F32) for i in range(2)]
                    xn = [sb(st, "xn%d" % i, [128, D], BF16) for i in range(2)]
                    stat = [sb(st, "stat%d" % i, [128, 4], F32) for i in range(2)]
                    psT = [pst(st, "p1t%d" % i, [128, 8, 128], BF16) for i in range(2)]
                    s.dma("sp", gab[:], g_attn_d.partition_broadcast(128), writes=["gab"])
                    if smp == 0:
                        precast_m1()
                    for row, s1, sh, nm in ((smp, s1b, sh1b, "l"), (NS, s1c, sh1c, "c")):
                        s.dma("sp", tmpv[:], modv_d[row, D:2 * D].partition_broadcast(128), writes=["tmpv"])
                        stt("dve", s1[:], tmpv[:], 1.0, gab[:], ALU.add, ALU.mult, ["tmpv", "gab"], ["s1" + nm])
                        s.dma("sp", sh[:], modv_d[row, 0:D].partition_broadcast(128), writes=["sh1" + nm])
                    for i in range(NTA):
                        b = i % 2
                        lat = i < NT
                        src = x_d[smp, i * 128:(i + 1) * 128, :] if lat else ctx_d[smp, (i - NT) * 128:(i - NT + 1) * 128, :]
                        nm = "l" if lat else "c"
                        s1, sh = (s1b, sh1b) if lat else (s1c, sh1c)
                        s.dma("sp", xt[b][:], src, writes=["xt%d" % b])
                        s.op("pool", lambda e, b=b: e.memset(stat[b][:], 0.0), writes=["stat%d" % b])
                        act(xm[b][:], xt[b][:], AF.Square, ["xt%d" % b, "stat%d" % b], ["xm%d" % b, "stat%d" % b],
                            accum_out=stat[b][:, 0:1])
                        act(stat[b][:, 1:2], stat[b][:, 0:1], AF.Sqrt, ["stat%d" % b, "eps_c"], ["stat%d" % b],
                            scale=1.0 / D, bias=eps_c[:, 0:1])
                        s.op("dve", lambda e, b=b: e.reciprocal(out=stat[b][:, 2:3], in_=stat[b][:, 1:2]),
                             reads=["stat%d" % b], writes=["stat%d" % b])
                        stt("dve", xm[b][:], xt[b][:], stat[b][:, 2:3], s1[:], ALU.mult, ALU.mult,
                            ["xt%d" % b, "stat%d" % b, "s1" + nm], ["xm%d" % b])
                        tt("pool", xn[b][:], xm[b][:], sh[:], ALU.add, ["xm%d" % b, "sh1" + nm], ["xn%d" % b])
                        for kt in range(8):
                            tr(psT[b][:, kt, :], xn[b][:, kt * 128:(kt + 1) * 128], ident_b[:],
                               ["xn%d" % b, "ident_b"], ["p1t%d" % b])
                        if lat:
                            cp("act", aT[:, :, i * 128:(i + 1) * 128], psT[b][:, :, :], ["p1t%d" % b], ["aT"])
                        else:
                            cp("act", acT[:, :, (i - NT) * 128:(i - NT + 1) * 128], psT[b][:, :, :], ["p1t%d" % b], ["acT"])
                    if smp == 0:
                        dump("aT", aT[:, :, 0:256], ["aT"])
                    s.barrier()
                if stop_after == "P1":
                    s.barrier()
                    return nc

                with ExitStack() as st:
                    ropeC = sb(st, "ropeC", [128, S], BF16)
                    ropeS = sb(st, "ropeS", [128, S], BF16)
                    alT = [sb(st, "alT%d" % d_, [16, S + CT], BF16) for d_ in range(2)]
                    wal = sb(st, "wal", [128, 8, 32], BF16)
                    wa2 = sb(st, "wa2", [16, 2, 512], BF16)
                    ba2 = sb(st, "ba2", [1, 2, 512], BF16)
                    gnb = sb(st, "gnb", [128, 256], F32)
                    wq = sb(st, "wq", [128, 8, 128], BF16)
                    wqs = sb(st, "wqs", [128, 8, 128], BF16)
                    wk = sb(st, "wk", [128, 8, 128], BF16)
                    wks = sb(st, "wks", [128, 8, 128], BF16)
                    wv = sb(st, "wv", [128, 8, 256], BF16)
                    wg = sb(st, "wg", [128, 8, 256], BF16)
                    qrT = sb(st, "qrT", [128, S], BF16)
                    krT = sb(st, "krT", [128, S], BF16)
                    krk = sb(st, "krk", [128, NTA, 128], BF16)
                    vtk = sb(st, "vtk", [128, NTA, 256], BF16)
                    sgt = sb(st, "sgt", [128, NT, 256], BF16)
                    qe = [sb(st, "qe%d" % d_, [128, S], BF16) for d_ in range(2)]
                    ke = [sb(st, "ke%d" % d_, [128, S], BF16) for d_ in range(2)]
                    kd = [sb(st, "kd%d" % d_, [128, NTA, 128], BF16) for d_ in range(2)]
                    dec = sb(st, "dec", [128, 2, NTA, 2], F32)
                    oacc = sb(st, "oacc", [128, NT, 256], F32)
                    S32 = [[sb(st, "S32_%d_%d" % (d_, v_), [128, 256], F32) for v_ in range(2)] for d_ in range(2)]
                    attf = [sb(st, "attf%d" % i, [128, 128], BF16) for i in range(2)]
                    trib = sb(st, "trib", [128, 2, 128], BF16)
                    S16 = [[sb(st, "S16_%d_%d" % (d_, v_), [128, 256], BF16) for v_ in range(2)] for d_ in range(2)]
                    t1 = [sb(st, "t1_%d" % i, [128, 512], F32) for i in range(2)]
                    t2 = [sb(st, "t2_%d" % i, [128, 512], F32) for i in range(2)]
                    otmp = [t1[i][:, 0:256] for i in range(2)]
                    onr = [t2[i][:, 0:256] for i in range(2)]
                    Dt = [sb(st, "Dt0", [128, 512], F32)]
                    Di = [sb(st, "Di0", [128, 512], F32)]
                    EK = [sb(st, "EK0", [128, 512], F32)]
                    att = [sb(st, "att%d" % i, [128, 128], BF16) for i in range(2)]
                    junk = sb(st, "junk2", [128, 256], F32)
                    rbf = [sb(st, "rbf%d" % i, [128, 256], BF16) for i in range(2)]
                    st2 = [sb(st, "st2_%d" % i, [128, 4], F32) for i in range(2)]
                    B = [pst(st, "p2b%d" % i, [128, 512]) for i in range(6)]
                    BK = ["p2B%d" % i for i in range(6)]
                    pT = [pst(st, "p2t%d" % i, [128, 8, 128], BF16) for i in range(2)]
                    TK = ["p2T0", "p2T1"]

                    s.dma("pool", ropeC[:], cos_d, writes=["ropeC"])
                    s.dma("pool", ropeS[:], sin_d, writes=["ropeS"])
                    s.dma("pool", wal[:], wview(w_in_d[:, C_AL:C_AL + 32]), writes=["wal"])
                    s.dma("pool", wa2[:], w_a2b_d[0:16], writes=["wa2"])
                    s.dma("pool", ba2[:], w_a2b_d[16:17], writes=["ba2"])
                    s.dma("sp", gnb[:], gla_g_d.partition_broadcast(128), writes=["gnb"])
                    for tb in range(5):
                        if tb < 4:
                            rhs_of = lambda kt, tb=tb: aT[:, kt, tb * 512:(tb + 1) * 512]
                            n, c0, rk = 512, tb * 512, "aT"
                        else:
                            rhs_of = lambda kt: acT[:, kt, :]
                            n, c0, rk = CT, S, "acT"
                        for d_ in range(2):
                            for kt in range(8):
                                mm(B[d_][0:16, 0:n], wal[:, kt, d_ * 16:(d_ + 1) * 16], rhs_of(kt), kt == 0, kt == 7,
                                   ["wal", rk], [], x=[BK[d_]])
                            cp("act" if d_ == 0 else "dve", alT[d_][:, c0:c0 + n], B[d_][0:16, 0:n], [], ["alT%d" % d_], x=[BK[d_]])
                    if stop_after == "P2a":
                        s.barrier()
                        return nc

                    def load_head_w(h):
                        s.dma("pool", wq[:], wview(w_in_d[:, C_GQ + 128 * h:C_GQ + 128 * (h + 1)]), writes=["wq"])
                        s.dma("pool", wqs[:], wview(w_rope_d[:, 128 * h:128 * (h + 1)]), writes=["wqs"])
                        s.dma("pool", wk[:], wview(w_in_d[:, C_GK + 128 * h:C_GK + 128 * (h + 1)]), writes=["wk"])
                        s.dma("pool", wks[:], wview(w_rope_d[:, 512 + 128 * h:512 + 128 * (h + 1)]), writes=["wks"])
                        s.dma("pool", wv[:], wview(w_in_d[:, C_GV + 256 * h:C_GV + 256 * (h + 1)]), writes=["wv"])
                        s.dma("pool", wg[:], wview(w_in_d[:, C_GG + 256 * h:C_GG + 256 * (h + 1)]), writes=["wg"])

                    load_head_w(0)
                    for h in range(4):
                        for tb in range(4):
                            cols = slice(tb * 512, (tb + 1) * 512)
                            for bi, (w, wn) in enumerate(((wq, "wq"), (wqs, "wqs"), (wk, "wk"), (wks, "wks"))):
                                for kt in range(8):
                                    mm(B[bi][:, :], w[:, kt, :], aT[:, kt, cols], kt == 0, kt == 7, [wn, "aT"], [], x=[BK[bi]])
                            stt("dve", t1[0][:], B[0][:, :], QSCALE, ropeC[:, cols], ALU.mult, ALU.mult, ["ropeC"], ["t1_0"], x=[BK[0]])
                            stt("dve", t2[0][:], B[1][:, :], QSCALE, ropeS[:, cols], ALU.mult, ALU.mult, ["ropeS"], ["t2_0"], x=[BK[1]])
                            tt("pool", qrT[:, cols], t1[0][:], t2[0][:], ALU.add, ["t1_0", "t2_0"], ["qrT"])
                            tt("dve", t1[1][:], B[2][:, :], ropeC[:, cols], ALU.mult, ["ropeC"], ["t1_1"], x=[BK[2]])
                            tt("dve", t2[1][:], B[3][:, :], ropeS[:, cols], ALU.mult, ["ropeS"], ["t2_1"], x=[BK[3]])
                            tt("pool", krT[:, cols], t1[1][:], t2[1][:], ALU.add, ["t1_1", "t2_1"], ["krT"])
                        if stop_after == "P2b":
                            s.barrier()
                            return nc
                        for i in range(NTA):
                            lat = i < NT
                            bv = 4 + (i % 2)
                            bg = 2 + (i % 2)
                            srcT, rk, c0 = (aT, "aT", i * 128) if lat else (acT, "acT", (i - NT) * 128)
                            for kt in range(8):
                                mm(B[bv][:, 0:256], srcT[:, kt, c0:c0 + 128], wv[:, kt, :], kt == 0, kt == 7, [rk, "wv"], [], x=[BK[bv]])
                            cp("act", vtk[:, i, :], B[bv][:, 0:256], [], ["vtk"], x=[BK[bv]])
                            if lat:
                                for kt in range(8):
                                    mm(B[bg][:, 0:256], srcT[:, kt, c0:c0 + 128], wg[:, kt, :], kt == 0, kt == 7, [rk, "wg"], [], x=[BK[bg]])
                                act(sgt[:, i, :], B[bg][:, 0:256], AF.Silu, [], ["sgt"], x=[BK[bg]])
                            else:
                                for kt in range(8):
                                    mm(B[bg][:, 0:128], srcT[:, kt, c0:c0 + 128], wk[:, kt, :], kt == 0, kt == 7, [rk, "wk"], [], x=[BK[bg]])
                                cp("dve", krk[:, i, :], B[bg][:, 0:128], [], ["krk"], x=[BK[bg]])
                        for i in range(NT):
                            u = i % 2
                            tr(pT[u][:, 0, :], krT[:, i * 128:(i + 1) * 128], ident_b[:], ["krT", "ident_b"], [], x=[TK[u]])
                            cp("dve", krk[:, i, :], pT[u][:, 0, :], [], ["krk"], x=[TK[u]])
                        if stop_after == "P2c":
                            s.barrier()
                            return nc
                        if h + 1 < 4:
                            load_head_w(h + 1)
                        groups = [(0, 4), (4, 4), (8, 4), (12, 4), (16, 2)]
                        hc = slice(128 * h, 128 * (h + 1))
                        for (t0, nt_) in groups:
                            lat = t0 < NT
                            n = nt_ * 128
                            gc = slice(t0 * 128, t0 * 128 + n)
                            for d_ in range(2):
                                bz, bb, be = B[d_], B[2 + d_], B[4 + d_]
                                xz, xb, xe_ = [BK[d_]], [BK[2 + d_]], [BK[4 + d_]]
                                az_, ex_, rz_, L_ = t1[0], t1[1], t2[0], t2[1]
                                for tl in range(nt_):
                                    tokc = slice((t0 + tl) * 128, (t0 + tl + 1) * 128)
                                    zc = slice(tl * 128, (tl + 1) * 128)
                                    mm(bz[:, zc], alT[d_][0:16, tokc], wa2[0:16, d_, hc], True, False, ["alT%d" % d_, "wa2"], [], inc=False, x=xz)
                                    mm(bz[:, zc], ones_b[0:1, 0:128], ba2[0:1, d_, hc], False, True, ["ones_b", "ba2"], [], x=xz)
                                act(az_[:, 0:n], bz[:, 0:n], AF.Abs, [], ["t1_0"], x=xz)
                                ts("dve", rz_[:, 0:n], bz[:, 0:n], -1.0, 0.0, ALU.mult, ALU.max, [], ["t2_0"], x=xz)
                                act(ex_[:, 0:n], az_[:, 0:n], AF.Exp, ["t1_0"], ["t1_1"], scale=-1.0)
                                act(ex_[:, 0:n], ex_[:, 0:n], AF.Ln, ["t1_1", "one_c"], ["t1_1"], bias=one_c[:, 0:1])
                                tt("pool", L_[:, 0:n], rz_[:, 0:n], ex_[:, 0:n], ALU.add, ["t2_0", "t1_1"], ["t2_1"])
                                for tl in range(nt_):
                                    zc = slice(tl * 128, (tl + 1) * 128)
                                    mm(bb[:, zc], L_[:, zc], tri_f[:, 2 * d_, :], True, True, ["t2_1", "tri"], [], x=xb)
                                for tl in range(nt_):
                                    zc = slice(tl * 128, (tl + 1) * 128)
                                    mm(be[:, zc], tri_f[:, 2 * d_ + 1, :], L_[:, zc], True, True, ["t2_1", "tri"], [], x=xe_)
                                act(Dt[0][:, 0:n], bb[:, 0:n], AF.Exp, [], ["Dt0"], scale=-1.0 / 16, x=xb)
                                if lat:
                                    act(Di[0][:, 0:n], bb[:, 0:n], AF.Exp, [], ["Di0"], scale=1.0 / 16, x=xb)
                                act(EK[0][:, 0:n], be[:, 0:n], AF.Exp, [], ["EK0"], scale=-1.0 / 16, x=xe_)
                                dsrc = Dt[0][:, 63:n:64] if d_ == 0 else Dt[0][:, 0:n:64]
                                cp("pool", dec[:, d_, t0:t0 + nt_, :].rearrange("p t c -> p (t c)"), dsrc, ["Dt0"], ["dec"])
                                tt("pool", kd[d_][:, t0:t0 + nt_, :], krk[:, t0:t0 + nt_, :], EK[0][:, 0:n].rearrange("p (t c) -> p t c", c=128),
                                   ALU.mult, ["krk", "EK0"], ["kd%d" % d_])
                                if lat:
                                    tt("dve", qe[d_][:, gc], qrT[:, gc], Dt[0][:, 0:n], ALU.mult, ["qrT", "Dt0"], ["qe%d" % d_])
                                    tt("dve", ke[d_][:, gc], krT[:, gc], Di[0][:, 0:n], ALU.mult, ["krT", "Di0"], ["ke%d" % d_])
                        if stop_after == "P2d":
                            s.barrier()
                            return nc
                        ver = [0, 0]
                        for d_ in range(2):
                            s.op("pool", lambda e, d_=d_: e.memset(S32[d_][0][:], 0.0), writes=["S32_%d_0" % d_])
                            s.op("pool", lambda e, d_=d_: e.memset(S16[d_][0][:], 0.0), writes=["S16_%d_0" % d_])
                            cp("pool", trib[:, d_, :], tri_f[:, 2 * d_, :], ["tri"], ["trib"])

                        def kvmm(d_, i, half, par):
                            rows = slice(64 * half, 64 * half + 64)
                            bk = 2 + 2 * half + par
                            mm(B[bk][:, 256 * d_:256 * d_ + 256], kd[d_][rows, i, :], vtk[rows, i, :], True, True,
                               ["kd%d" % d_, "vtk"], [], x=[BK[bk]])

                        def upd(d_, i, half, par):
                            bk = 2 + 2 * half + par
                            pkv = B[bk][:, 256 * d_:256 * d_ + 256]
                            cv = ver[d_] % 2
                            nv = (ver[d_] + 1) % 2
                            stt("dve", S16[d_][nv][:], S32[d_][cv][:], dec[:, d_, i, half:half + 1], pkv, ALU.mult, ALU.add,
                                ["S32_%d_%d" % (d_, cv), "dec"], ["S16_%d_%d" % (d_, nv)], x=[BK[bk]])
                            stt("dve", S32[d_][nv][:], S32[d_][cv][:], dec[:, d_, i, half:half + 1], pkv, ALU.mult, ALU.add,
                                ["S32_%d_%d" % (d_, cv), "dec"], ["S32_%d_%d" % (d_, nv)], x=[BK[bk]])
                            ver[d_] += 1

                        for n_, i in enumerate((NT, NT + 1)):
                            for half in (0, 1):
                                kvmm(0, i, half, n_ % 2)
                            for half in (0, 1):
                                upd(0, i, half, n_ % 2)
                        for n_, i in enumerate((NT + 1, NT)):
                            for half in (1, 0):
                                kvmm(1, i, half, n_ % 2)
                            for half in (1, 0):
                                upd(1, i, half, n_ % 2)
                        if smp == 0 and h == 0:
                            dump("s_f", S32[0][ver[0] % 2][:], ["S32_0_%d" % (ver[0] % 2)])
                            dump("s_b", S32[1][ver[1] % 2][:], ["S32_1_%d" % (ver[1] % 2)])
                        if stop_after == "P2e":
                            s.barrier()
                            return nc

                        done = [0] * NT

                        def lat_att(d_, i):
                            tokc = slice(i * 128, (i + 1) * 128)
                            pa = B[d_][:, 256:384]
                            mm(pa, ke[d_][:, tokc], qe[d_][:, tokc], True, True, ["ke%d" % d_, "qe%d" % d_], [], x=[BK[d_]])
                            cp("act", attf[d_][:], pa, [], ["attf%d" % d_], x=[BK[d_]])
                            tt("pool", att[d_][:], attf[d_][:], trib[:, d_, :], ALU.mult, ["attf%d" % d_, "trib"], ["att%d" % d_])

                        def lat_po(d_, i):
                            po = B[d_][:, 0:256]
                            mm(po, att[d_][:], vtk[:, i, :], True, False, ["att%d" % d_, "vtk"], [], inc=False, x=[BK[d_]])

                        def lat_inter(d_, i, half, last):
                            po = B[d_][:, 0:256]
                            rows = slice(64 * half, 64 * half + 64)
                            c0 = i * 128 + 64 * half
                            cv = ver[d_] % 2
                            mm(po[rows, :], qe[d_][:, c0:c0 + 64], S16[d_][cv][:], False, last, ["qe%d" % d_, "S16_%d_%d" % (d_, cv)],
                               [], inc=True, x=[BK[d_]])

                        def lat_back(d_, i):
                            po = B[d_][:, 0:256]
                            xo = [BK[d_]]
                            tokc = slice(i * 128, (i + 1) * 128)
                            if done[i] == 0:
                                cp("act", oacc[:, i, :], po, [], ["oacc%d" % i], x=xo)
                                done[i] = 1
                                return
                            u = i % 2
                            cp("act", otmp[u], po, [], ["t1_%d" % u], x=xo)
                            tt("pool", oacc[:, i, :], otmp[u], oacc[:, i, :], ALU.add, ["t1_%d" % u, "oacc%d" % i], ["oacc%d" % i])
                            s.op("pool", lambda e, u=u: e.memset(st2[u][:], 0.0), writes=["st2_%d" % u])
                            act(junk[:], oacc[:, i, :], AF.Square, ["oacc%d" % i, "st2_%d" % u], ["junk2", "st2_%d" % u],
                                accum_out=st2[u][:, 0:1])
                            act(st2[u][:, 1:2], st2[u][:, 0:1], AF.Sqrt, ["st2_%d" % u, "eps_c"], ["st2_%d" % u],
                                scale=1.0 / 256, bias=eps_c[:, 0:1])
                            s.op("dve", lambda e, u=u: e.reciprocal(out=st2[u][:, 2:3], in_=st2[u][:, 1:2]),
                                 reads=["st2_%d" % u], writes=["st2_%d" % u])
                            act(onr[u], oacc[:, i, :], AF.Copy, ["oacc%d" % i, "st2_%d" % u], ["t2_%d" % u], scale=st2[u][:, 2:3])
                            tt("pool", onr[u], onr[u], gnb[:], ALU.mult, ["t2_%d" % u, "gnb"], ["t2_%d" % u])
                            tt("pool", rbf[u][:], onr[u], sgt[:, i, :], ALU.mult, ["t2_%d" % u, "sgt"], ["rbf%d" % u])
                            for j in range(2):
                                tr(pT[u][:, j, :], rbf[u][:, j * 128:(j + 1) * 128], ident_b[:], ["rbf%d" % u, "ident_b"], [], x=[TK[u]])
                            cp("act", oglT[:, 2 * h:2 * h + 2, tokc], pT[u][:, 0:2, :], [], ["oglT"], x=[TK[u]])

                        lat_att(0, 0)
                        lat_att(1, NT - 1)
                        kvmm(0, 0, 0, 0)
                        kvmm(0, 0, 1, 0)
                        kvmm(1, NT - 1, 1, 0)
                        kvmm(1, NT - 1, 0, 0)
                        for j in range(NT):
                            fi, bi_ = j, NT - 1 - j
                            par = j % 2
                            precast(1)
                            lat_po(0, fi)
                            lat_po(1, bi_)
                            lat_inter(0, fi, 0, False)
                            lat_inter(1, bi_, 1, False)
                            upd(0, fi, 0, par)
                            upd(1, bi_, 1, par)
                            lat_inter(0, fi, 1, True)
                            lat_inter(1, bi_, 0, True)
                            upd(0, fi, 1, par)
                            upd(1, bi_, 0, par)
                            lat_back(0, fi)
                            lat_back(1, bi_)
                            if j + 1 < NT:
                                lat_att(0, fi + 1)
                                lat_att(1, bi_ - 1)
                                kvmm(0, fi + 1, 0, 1 - par)
                                kvmm(0, fi + 1, 1, 1 - par)
                                kvmm(1, bi_ - 1, 1, 1 - par)
                                kvmm(1, bi_ - 1, 0, 1 - par)
                    if smp == 0:
                        dump("oglT", oglT[:, :, :], ["oglT"])
                    s.barrier()
                    print("P2 done: deadlock-free", s.check_deadlock(), s.n_ins, s.n_wait, s.cnt)
                if stop_after == "P2":
                    s.barrier()
                    return nc

                onaT = sb(sa, "onaT", [128, 4, S], BF16)
                with ExitStack() as st:
                    wq3 = sb(st, "wq3", [128, 8, 128], BF16)
                    wk3 = sb(st, "wk3", [128, 8, 128], BF16)
                    wv3 = sb(st, "wv3", [128, 8, 128], BF16)
                    nam = sb(st, "nam", [128, 8, 256], F32)
                    nabt = [sb(st, "nabt%d" % i, [128, 2, 256], F32) for i in range(2)]
                    BMp = sb(st, "BMp", [128, 8, 2, 256], BF16)
                    qT3 = sb(st, "qT3", [128, S], BF16)
                    kT3 = sb(st, "kT3", [128, S + CT], BF16)
                    Ve = sb(st, "Ve", [128, 16, 128], BF16)
                    Vo = sb(st, "Vo", [128, 15, 128], BF16)
                    Vc = sb(st, "Vc", [128, 2, 128], BF16)
                    PT = [sb(st, "PT%d" % i, [128, 6, 2, 64], BF16) for i in range(2)]
                    rden = [sb(st, "rden%d" % i, [128, 128], F32) for i in range(2)]
                    SB = [pst(st, "p3s%d" % i, [128, 512]) for i in range(4)]
                    SK = ["p3S%d" % i for i in range(4)]
                    OB = [pst(st, "p3o%d" % i, [128, 512]) for i in range(2)]
                    OK_ = ["p3O%d" % i for i in range(2)]
                    PB = [pst(st, "p3p%d" % i, [128, 512]) for i in range(2)]
                    PK = ["p3P%d" % i for i in range(2)]
                    s.dma("sp", nam[:], nam_d, writes=["nam"])
                    def load_pair_w(pr):
                        s.dma("pool", wq3[:], wview(w_in_d[:, C_NAQ + 128 * pr:C_NAQ + 128 * (pr + 1)]), writes=["wq3"])
                        s.dma("pool", wk3[:], wview(w_in_d[:, C_NAK + 128 * pr:C_NAK + 128 * (pr + 1)]), writes=["wk3"])
                        s.dma("pool", wv3[:], wview(w_in_d[:, C_NAV + 128 * pr:C_NAV + 128 * (pr + 1)]), writes=["wv3"])

                    load_pair_w(0)
                    for pr in range(4):
                        for p in range(8):
                            s.dma("sp", nabt[p % 2][:], nab_d[pr, :, p, :, :], writes=["nabt%d" % (p % 2)])
                            for hh in range(2):
                                stt("dve", BMp[:, p, hh, :], nabt[p % 2][:, hh, :], 8.0, nam[:, p, :], ALU.mult, ALU.add,
                                    ["nabt%d" % (p % 2), "nam"], ["BMp"])
                        cnt_p = [0]

                        def proj(dst, lhs_fn, rhs_fn, m, n, rk, dk):
                            u = cnt_p[0] % 2
                            cnt_p[0] += 1
                            for kt in range(8):
                                mm(PB[u][0:m, 0:n], lhs_fn(kt), rhs_fn(kt), kt == 0, kt == 7, rk, [], x=[PK[u]])
                            cp("act" if u == 0 else "dve", dst, PB[u][0:m, 0:n], [], [dk], x=[PK[u]])

                        for tb in range(4):
                            cols = slice(tb * 512, (tb + 1) * 512)
                            proj(qT3[:, cols], lambda kt: wq3[:, kt, :], lambda kt, cols=cols: aT[:, kt, cols], 128, 512, ["wq3", "aT"], "qT3")
                            proj(kT3[:, cols], lambda kt: wk3[:, kt, :], lambda kt, cols=cols: aT[:, kt, cols], 128, 512, ["wk3", "aT"], "kT3")
                        proj(kT3[:, S:S + CT], lambda kt: wk3[:, kt, :], lambda kt: acT[:, kt, :], 128, CT, ["wk3", "acT"], "kT3")
                        for i in range(16):
                            proj(Ve[:, i, :], lambda kt, i=i: aT[:, kt, i * 128:(i + 1) * 128], lambda kt: wv3[:, kt, :], 128, 128, ["wv3", "aT"], "Ve")
                        for i in range(15):
                            proj(Vo[:, i, :], lambda kt, i=i: aT[:, kt, 64 + i * 128:64 + (i + 1) * 128], lambda kt: wv3[:, kt, :], 128, 128, ["wv3", "aT"], "Vo")
                        for i in range(2):
                            proj(Vc[:, i, :], lambda kt, i=i: acT[:, kt, i * 128:(i + 1) * 128], lambda kt: wv3[:, kt, :], 128, 128, ["wv3", "acT"], "Vc")

                        if pr + 1 < 4:
                            load_pair_w(pr + 1)

                        def scores(r):
                            rs = min(max(r - 4, 0), 24)
                            p = r - rs
                            buf = r % 2
                            qc = slice(r * 64, r * 64 + 64)
                            for hh in range(2):
                                pr_ = slice(64 * hh, 64 * hh + 64)
                                bank = SB[buf * 2 + hh]
                                xk = [SK[buf * 2 + hh]]
                                mm(bank[:, 0:256], ident_b[:], BMp[:, p, hh, :], True, False, ["ident_b", "BMp"], [], inc=False, x=xk)
                                for j in range(4):
                                    kc0 = (rs + 2 * j) * 64
                                    mm(bank[:, j * 64:(j + 1) * 64], kT3[pr_, kc0:kc0 + 128], qT3[pr_, qc], False, j == 3,
                                       ["kT3", "qT3"], [], inc=False, x=xk)
                                for jc in range(2):
                                    mm(bank[:, 256 + jc * 64:256 + (jc + 1) * 64], kT3[pr_, S + jc * 128:S + (jc + 1) * 128], qT3[pr_, qc],
                                       True, True, ["kT3", "qT3"], [], inc=(jc == 1), x=xk)
                                act(PT[buf][:, :, hh, :], bank[:, 0:384].rearrange("p (j q) -> p j q", q=64), AF.Exp,
                                    [], ["PT%d" % buf], scale=0.125, x=xk)

                        def pv(r):
                            rs = min(max(r - 4, 0), 24)
                            buf = r % 2
                            qc = slice(r * 64, r * 64 + 64)
                            ob = OB[buf]
                            xo = [OK_[buf]]
                            for hh in range(2):
                                hs = slice(64 * hh, 64 * hh + 64)
                                for j in range(6):
                                    if j < 4:
                                        if rs % 2 == 0:
                                            Vt, vk = Ve[:, (rs + 2 * j) // 2, hs], "Ve"
                                        else:
                                            Vt, vk = Vo[:, (rs + 2 * j - 1) // 2, hs], "Vo"
                                    else:
                                        Vt, vk = Vc[:, j - 4, hs], "Vc"
                                    mm(ob[hs, 0:64], Vt, PT[buf][:, j, hh, :], j == 0, j == 5, [vk, "PT%d" % buf], [],
                                       inc=(j == 5), x=xo)
                            for j in range(6):
                                mm(ob[:, 64:192], ones_b[:, :], PT[buf][:, j, :, :].rearrange("p h q -> p (h q)"), j == 0, j == 5,
                                   ["ones_b", "PT%d" % buf], [], inc=(j == 5), x=xo)
                            s.op("dve", lambda e: e.reciprocal(out=rden[buf][:], in_=ob[:, 64:192]), reads=[], writes=["rden%d" % buf], excl=xo)
                            for hh in range(2):
                                hs = slice(64 * hh, 64 * hh + 64)
                                tt("dve", onaT[hs, pr, qc], ob[hs, 0:64], rden[buf][hs, 64 * hh:64 * hh + 64], ALU.mult,
                                   ["rden%d" % buf], ["onaT"], x=xo)

                        for r in range(32):
                            if r < 8:
                                precast(1)
                            scores(r)
                            if r > 0:
                                pv(r - 1)
                        pv(31)
                    if smp == 0:
                        dump("onaT", onaT[:, :, :], ["onaT"])
                    s.barrier()
                    print("P3 done: deadlock-free", s.check_deadlock(), s.n_ins, s.n_wait, s.cnt)
                if stop_after == "P3":
                    s.barrier()
                    return nc

                UT = sb(sa, "UT", [128, 8, S], BF16)
                with ExitStack() as st:
                    wna = [sb(st, "wna%d" % i, [128, 4, 128], BF16) for i in range(2)]
                    wgl = [sb(st, "wgl%d" % i, [128, 8, 128], BF16) for i in range(2)]
                    w8 = [sb(st, "w8_%d" % i, [128, 8, 128], BF16) for i in range(2)]
                    w9 = [sb(st, "w9_%d" % i, [128, 8, 128], BF16) for i in range(2)]
                    sg8 = [sb(st, "sg8_%d" % i, [128, 512], F32) for i in range(2)]
                    sg9 = [sb(st, "sg9_%d" % i, [128, 512], F32) for i in range(2)]
                    t1m = [sb(st, "t1m%d" % i, [128, 512], F32) for i in range(2)]
                    t2m = [sb(st, "t2m%d" % i, [128, 512], F32) for i in range(2)]
                    MB = [pst(st, "p4b%d" % i, [128, 512]) for i in range(8)]
                    MK = ["p4B%d" % i for i in range(8)]
                    for nt in range(8):
                        wb = nt % 2
                        ncol = slice(nt * 128, (nt + 1) * 128)
                        s.dma("sp", wna[wb][:], wview(wna_b[:, ncol]), reads=["wnab"], writes=["wna%d" % wb])
                        s.dma("sp", wgl[wb][:], wview(wgl_b[:, ncol]), reads=["wglb"], writes=["wgl%d" % wb])
                        s.dma("sp", w8[wb][:], wview(wm8_b[:, ncol]), reads=["wm8b"], writes=["w8_%d" % wb])
                        s.dma("sp", w9[wb][:], wview(wm9_b[:, ncol]), reads=["wm9b"], writes=["w9_%d" % wb])
                        for tb in range(4):
                            cols = slice(tb * 512, (tb + 1) * 512)
                            u = (nt * 4 + tb) % 2
                            bA, bG8, bB, bG9 = [MB[4 * u + q] for q in range(4)]
                            kA, kG8, kB, kG9 = [[MK[4 * u + q]] for q in range(4)]
                            for kt in range(4):
                                mm(bA[:, :], wna[wb][:, kt, :], onaT[:, kt, cols], kt == 0, kt == 3, ["wna%d" % wb, "onaT"], [], x=kA)
                            for kt in range(8):
                                mm(bG8[:, :], w8[wb][:, kt, :], aT[:, kt, cols], kt == 0, kt == 7, ["w8_%d" % wb, "aT"], [], x=kG8)
                            for kt in range(8):
                                mm(bB[:, :], wgl[wb][:, kt, :], oglT[:, kt, cols], kt == 0, kt == 7, ["wgl%d" % wb, "oglT"], [], x=kB)
                            for kt in range(8):
                                mm(bG9[:, :], w9[wb][:, kt, :], aT[:, kt, cols], kt == 0, kt == 7, ["w9_%d" % wb, "aT"], [], x=kG9)
                            act(sg8[u][:], bG8[:, :], AF.Sigmoid, [], ["sg8_%d" % u], x=kG8)
                            act(sg9[u][:], bG9[:, :], AF.Sigmoid, [], ["sg9_%d" % u], x=kG9)
                            tt("dve", t1m[u][:], bA[:, :], sg8[u][:], ALU.mult, ["sg8_%d" % u], ["t1m%d" % u], x=kA)
                            tt("dve", t2m[u][:], bB[:, :], sg9[u][:], ALU.mult, ["sg9_%d" % u], ["t2m%d" % u], x=kB)
                            tt("pool", UT[:, nt, cols], t1m[u][:], t2m[u][:], ALU.add, ["t1m%d" % u, "t2m%d" % u], ["UT"])
                    if smp == 0:
                        dump("UT", UT[:, :, 0:256], ["UT"])
                    s.barrier()
                with ExitStack() as st:
                    wo = sb(st, "wo", [128, 8, D], BF16)
                    wtmp = [sb(st, "wtmp%d" % i, [128, D], F32) for i in range(2)]
                    g1b = sb(st, "g1b", [128, D], F32)
                    xt2 = [sb(st, "xt2_%d" % i, [128, D], F32) for i in range(2)]
                    h1t = [sb(st, "h1t%d" % i, [128, D], F32) for i in range(2)]
                    MB = [pst(st, "p4c%d" % i, [128, 512]) for i in range(4)]
                    MK = ["p4C%d" % i for i in range(4)]
                    s.dma("sp", g1b[:], modv_d[smp, 2 * D:3 * D].partition_broadcast(128), writes=["g1b"])
                    for kt in range(8):
                        s.dma("sp", wtmp[kt % 2][:], w_out_d[kt * 128:(kt + 1) * 128, :], writes=["wtmp%d" % (kt % 2)])
                        tt("pool", wo[:, kt, :], wtmp[kt % 2][:], g1b[:], ALU.mult, ["wtmp%d" % (kt % 2), "g1b"], ["wo"])
                    for i in range(NT):
                        u = i % 2
                        tokc = slice(i * 128, (i + 1) * 128)
                        s.dma("sp", xt2[u][:], x_d[smp, tokc, :], writes=["xt2_%d" % u])
                        for half in range(2):
                            hcol = slice(half * 512, (half + 1) * 512)
                            bk = MB[2 * u + half]
                            xk = [MK[2 * u + half]]
                            for kt in range(8):
                                mm(bk[:, :], UT[:, kt, tokc], wo[:, kt, hcol], kt == 0, kt == 7, ["UT", "wo"], [], x=xk)
                            tt("dve", h1t[u][:, hcol], bk[:, :], xt2[u][:, hcol], ALU.add, ["xt2_%d" % u], ["h1t%d" % u], x=xk)
                        s.dma("sp", h1_d[smp, tokc, :], h1t[u][:], reads=["h1t%d" % u], writes=["h1d"])
                        if smp == 0 and i < 2 and "h1" in dbg_d:
                            dump("h1", h1t[u][:], ["h1t%d" % u], dst=dbg_d["h1"][i * 128:(i + 1) * 128, :])
                    s.barrier()
                    print("P4 done: deadlock-free", s.check_deadlock(), s.n_ins, s.n_wait, s.cnt)
                if stop_after == "P4":
                    s.barrier()
                    return nc
            CAP = MOE_CAP
            NB = CAP // 128
            NSLOT = NEXP * CAP
            Xd = nc.dram_tensor("xd_scr%d" % smp, [NSLOT, D], BF16).ap()
            Yd = nc.dram_tensor("yd_scr%d" % smp, [NSLOT, D], BF16).ap()

            ind_hist = []
            IND_DEPTH = int(os.environ.get("IND_DEPTH", "1000"))

            def dma_fn(eng, fn, reads, writes):
                i_ = s.dnext
                s.dnext = (s.dnext + 1) % s.n_fg
                k_ = ("d", i_)
                waits = s._deps(eng, reads, writes)
                if s.dcnt[i_] > 0 and s.waited[eng].get(k_, -1) < s.dcnt[i_]:
                    s.waited[eng][k_] = s.dcnt[i_]
                    waits.append((k_, s.dcnt[i_]))
                if len(ind_hist) >= IND_DEPTH:
                    pk, pv = ind_hist[-IND_DEPTH]
                    if pk != k_ and s.waited[eng].get(pk, -1) < pv:
                        s.waited[eng][pk] = pv
                        waits.append((pk, pv))
                s.dcnt[i_] += 16
                tok = (k_, s.dcnt[i_])
                ind_hist.append(tok)
                s._emit1(eng, waits, fn, (k_, 16))
                s._commit(tok, reads, writes)

            with ExitStack() as sm:
                hres = sb(sm, "hres", [128, NT, D], F32)
                g2b = sb(sm, "g2b", [128, D], F32)
                gfb = sb(sm, "gfb", [128, D], F32)
                SLi = sb(sm, "SLi", [128, NT * 2], I32)
                w12 = sb(sm, "w12", [128, NT, 2], F32)
                s.dma("sp", g2b[:], modv_d[smp, 5 * D:6 * D].partition_broadcast(128), writes=["g2b"])
                s.dma("sp", gfb[:], g_fin_d.partition_broadcast(128), writes=["gfb"])
                with ExitStack() as st:
                    fbf = sb(st, "fbf", [128, NT, D], BF16)
                    M1a = sb(st, "M1a", [128, NT, 32], F32)
                    M2a = sb(st, "M2a", [128, NT, 32], F32)
                    Ma = sb(st, "Ma", [128, NT, 32], F32)
                    SLf = sb(st, "SLf", [128, NT, 2], F32)
                    ebase = sb(st, "ebase_sb", [128, 32], F32)
                    triS = sb(st, "triS", [128, 128], F32)
                    ones128 = sb(st, "ones128", [128, 128], F32)
                    s2b = sb(st, "s2b", [128, D], F32)
                    sh2b = sb(st, "sh2b", [128, D], F32)
                    gfn = sb(st, "gfn", [128, D], F32)
                    wrt = sb(st, "wrt", [128, 8, 36], F32)
                    brt = sb(st, "brt", [1, 36], F32)
                    ones_f = sb(st, "ones_f", [1, 128], F32)
                    xm3 = [sb(st, "xm3_%d" % i, [128, D], F32) for i in range(2)]
                    junk3 = sb(st, "junk3", [128, D], F32)
                    fT32 = [sb(st, "fT32_%d" % i, [128, 8, 128], F32) for i in range(2)]
                    LG = sb(st, "LG", [128, NT, 36], F32)
                    gm = sb(st, "gm", [128, NT, 8], F32)
                    ohg = sb(st, "ohg", [128, NT, 4], F32)
                    ge = sb(st, "ge", [128, NT, 4], F32)
                    big = sb(st, "bigr", [128, NT, 4, 8], F32)
                    leg = sb(st, "leg", [128, NT, 8], F32)
                    oh1 = sb(st, "oh1", [128, NT, 8], F32)
                    oh2 = sb(st, "oh2", [128, NT, 8], F32)
                    msk = sb(st, "msk", [128, NT, 8], F32)
                    rk = sb(st, "rk", [128, NT, 32], F32)
                    vm = sb(st, "vm", [128, NT, 32], F32)
                    st3 = [sb(st, "st3_%d" % i, [128, 4], F32) for i in range(2)]
                    pf = [[pst(st, "p5f%d_%d" % (i, j), [128, 4, 128]) for j in range(2)] for i in range(2)]
                    pfk = [["p5F%d_%d" % (i, j) for j in range(2)] for i in range(2)]
                    pl = [pst(st, "p5l%d" % i, [128, 512]) for i in range(2)]
                    plk = ["p5L%d" % i for i in range(2)]
                    s.dma("sp", gfn[:], g_ffn_d.partition_broadcast(128), writes=["gfn"])
                    s.dma("sp", s2b[:], modv_d[smp, 4 * D:5 * D].partition_broadcast(128), writes=["s2b"])
                    stt("dve", s2b[:], s2b[:], 1.0, gfn[:], ALU.add, ALU.mult, ["s2b", "gfn"], ["s2b"])
                    s.dma("sp", sh2b[:], modv_d[smp, 3 * D:4 * D].partition_broadcast(128), writes=["sh2b"])
                    s.dma("sp", wrt[:], wview(w_rt_d), writes=["wrt"])
                    s.dma("sp", brt[:], b_rt_d.partition_broadcast(1), writes=["brt"])
                    s.dma("sp", ebase[:], ebase_d, writes=["ebase"])
                    s.dma("sp", triS[:], tri_d[4], writes=["triS"])
                    s.op("pool", lambda e: e.memset(ones_f[:], 1.0), writes=["ones_f"])
                    s.op("pool", lambda e: e.memset(ones128[:], 1.0), writes=["ones128"])
                    for i in range(NT):
                        u = i % 2
                        tokc = slice(i * 128, (i + 1) * 128)
                        hk = "hres%d" % i
                        s.dma("sp", hres[:, i, :], h1_d[smp, tokc, :], reads=["h1d"], writes=[hk])
                        s.op("pool", lambda e, u=u: e.memset(st3[u][:], 0.0), writes=["st3_%d" % u])
                        act(junk3[:], hres[:, i, :], AF.Square, [hk, "st3_%d" % u], ["junk3", "st3_%d" % u], accum_out=st3[u][:, 0:1])
                        act(st3[u][:, 1:2], st3[u][:, 0:1], AF.Sqrt, ["st3_%d" % u, "eps_c"], ["st3_%d" % u], scale=1.0 / D, bias=eps_c[:, 0:1])
                        s.op("dve", lambda e, u=u: e.reciprocal(out=st3[u][:, 2:3], in_=st3[u][:, 1:2]), reads=["st3_%d" % u], writes=["st3_%d" % u])
                        stt("dve", xm3[u][:], hres[:, i, :], st3[u][:, 2:3], s2b[:], ALU.mult, ALU.mult, [hk, "st3_%d" % u, "s2b"], ["xm3_%d" % u])
                        tt("pool", xm3[u][:], xm3[u][:], sh2b[:], ALU.add, ["xm3_%d" % u, "sh2b"], ["xm3_%d" % u])
                        cp("pool", fbf[:, i, :], xm3[u][:], ["xm3_%d" % u], ["fbf%d" % i])
                        for kt in range(8):
                            tr(pf[u][kt // 4][:, kt % 4, :], xm3[u][:, kt * 128:(kt + 1) * 128], ident_f[:], ["xm3_%d" % u, "ident_f"], [], x=[pfk[u][kt // 4]])
                        for j in range(2):
                            cp("act" if j == 0 else "dve", fT32[u][:, 4 * j:4 * j + 4, :], pf[u][j][:, :, :], [], ["fT32_%d" % u], x=[pfk[u][j]])
                        for kt in range(8):
                            mm(pl[u][:, 0:36], fT32[u][:, kt, :], wrt[:, kt, :], kt == 0, False, ["fT32_%d" % u, "wrt"], [], inc=False, x=[plk[u]])
                        mm(pl[u][:, 0:36], ones_f[0:1, :], brt[0:1, :], False, True, ["ones_f", "brt"], [], x=[plk[u]])
                        cp("dve", LG[:, i, :], pl[u][:, 0:36], [], ["LG"], x=[plk[u]])
                    T_ = NT
                    dv = lambda fn: s.op("dve", fn, reads=["LG", "RT"], writes=["RT"])
                    bc = lambda ap, shp: ap.broadcast_to(shp)
                    lgg = LG[:, :, 0:4]
                    lge = LG[:, :, 4:36].rearrange("p t (g j) -> p t g j", j=8)
                    dv(lambda e: e.reduce_max(out=gm[:, :, 0], in_=lgg, axis=AX.X))
                    dv(lambda e: e.tensor_tensor(out=ohg[:], in0=lgg, in1=bc(gm[:, :, 0:1], [128, T_, 4]), op=ALU.is_equal))
                    dv(lambda e: e.tensor_tensor(out=ge[:], in0=lgg, in1=bc(gm[:, :, 0:1], [128, T_, 4]), op=ALU.subtract))
                    act(ge[:], ge[:], AF.Exp, ["RT"], ["RT"])
                    dv(lambda e: e.reduce_sum(out=gm[:, :, 1], in_=ge[:], axis=AX.X))
                    dv(lambda e: e.reciprocal(out=gm[:, :, 1], in_=gm[:, :, 1]))
                    dv(lambda e: e.tensor_tensor(out=big[:], in0=lge, in1=bc(ohg[:, :, :, None], [128, T_, 4, 8]), op=ALU.mult))
                    dv(lambda e: e.reduce_sum(out=leg[:], in_=big[:].rearrange("p t g j -> p t j g"), axis=AX.X))
                    dv(lambda e: e.reduce_max(out=gm[:, :, 2], in_=leg[:], axis=AX.X))
                    dv(lambda e: e.tensor_tensor(out=oh1[:], in0=leg[:], in1=bc(gm[:, :, 2:3], [128, T_, 8]), op=ALU.is_equal))
                    dv(lambda e: e.scalar_tensor_tensor(out=msk[:], in0=oh1[:], scalar=-1e30, in1=leg[:], op0=ALU.mult, op1=ALU.add))
                    dv(lambda e: e.reduce_max(out=gm[:, :, 3], in_=msk[:], axis=AX.X))
                    dv(lambda e: e.tensor_tensor(out=oh2[:], in0=msk[:], in1=bc(gm[:, :, 3:4], [128, T_, 8]), op=ALU.is_equal))
                    dv(lambda e: e.tensor_tensor(out=gm[:, :, 4], in0=gm[:, :, 3], in1=gm[:, :, 2], op=ALU.subtract))
                    act(gm[:, :, 4], gm[:, :, 4], AF.Exp, ["RT"], ["RT"])
                    dv(lambda e: e.tensor_scalar(out=gm[:, :, 5], in0=gm[:, :, 4], scalar1=1.0, scalar2=None, op0=ALU.add))
                    dv(lambda e: e.reciprocal(out=gm[:, :, 5], in_=gm[:, :, 5]))
                    s.op("dve", lambda e: e.tensor_tensor(out=w12[:, :, 0], in0=gm[:, :, 5], in1=gm[:, :, 1], op=ALU.mult), reads=["RT"], writes=["w12"])
                    s.op("dve", lambda e: e.tensor_tensor(out=w12[:, :, 1], in0=w12[:, :, 0], in1=gm[:, :, 4], op=ALU.mult), reads=["RT", "w12"], writes=["w12"])
                    m1v = M1a[:].rearrange("p t (g j) -> p t g j", j=8)
                    m2v = M2a[:].rearrange("p t (g j) -> p t g j", j=8)
                    s.op("dve", lambda e: e.tensor_tensor(out=m1v, in0=bc(ohg[:, :, :, None], [128, T_, 4, 8]), in1=bc(oh1[:, :, None, :], [128, T_, 4, 8]),
                                                          op=ALU.mult), reads=["RT"], writes=["M1a"])
                    s.op("dve", lambda e: e.tensor_tensor(out=m2v, in0=bc(ohg[:, :, :, None], [128, T_, 4, 8]), in1=bc(oh2[:, :, None, :], [128, T_, 4, 8]),
                                                          op=ALU.mult), reads=["RT"], writes=["M2a"])
                    tt("pool", Ma[:], M1a[:], M2a[:], ALU.add, ["M1a", "M2a"], ["Ma"])
                    for i in range(NT):
                        rc = slice(i * 32, (i + 1) * 32)
                        mm(pl[0][:, rc], triS[:], Ma[:, i, :], True, i == 0, ["triS", "Ma"], [], inc=(i == 0), x=[plk[0]])
                        for i2 in range(i):
                            mm(pl[0][:, rc], ones128[:], Ma[:, i2, :], False, i2 == i - 1, ["ones128", "Ma"], [], inc=(i2 == i - 1), x=[plk[0]])
                    dq = lambda fn, rd=(), wr=(), x=(): s.op("dve", fn, reads=["QT"] + list(rd), writes=["QT"] + list(wr), excl=x)
                    dq(lambda e: e.tensor_copy(out=rk[:], in_=pl[0][:, :].rearrange("p (t e) -> p t e", e=32)), x=[plk[0]])
                    dq(lambda e: e.tensor_scalar(out=vm[:], in0=rk[:], scalar1=float(CAP), scalar2=None, op0=ALU.is_lt))
                    dq(lambda e: e.tensor_tensor(out=rk[:], in0=rk[:], in1=bc(ebase[:, None, :], [128, T_, 32]), op=ALU.add), rd=["ebase"])
                    for k_, Mk, mk in ((0, M1a, "M1a"), (1, M2a, "M2a")):
                        dq(lambda e, Mk=Mk: e.tensor_tensor(out=big[:].rearrange("p t g j -> p t (g j)"), in0=Mk[:], in1=rk[:], op=ALU.mult), rd=[mk, "RT"], wr=["RT"])
                        dq(lambda e, k_=k_: e.reduce_sum(out=SLf[:, :, k_], in_=big[:].rearrange("p t g j -> p t (g j)"), axis=AX.X), rd=["RT"], wr=["SLf"])
                        dq(lambda e, Mk=Mk: e.tensor_tensor(out=big[:].rearrange("p t g j -> p t (g j)"), in0=Mk[:], in1=vm[:], op=ALU.mult), rd=[mk, "RT"], wr=["RT"])
                        dq(lambda e: e.reduce_sum(out=gm[:, :, 6], in_=big[:].rearrange("p t g j -> p t (g j)"), axis=AX.X), rd=["RT"], wr=["RT"])
                        dq(lambda e, k_=k_: e.tensor_tensor(out=w12[:, :, k_], in0=w12[:, :, k_], in1=gm[:, :, 6], op=ALU.mult), rd=["w12", "RT"], wr=["w12"])
                        dq(lambda e: e.tensor_scalar(out=gm[:, :, 7], in0=gm[:, :, 6], scalar1=-1.0e6, scalar2=1.0e6, op0=ALU.mult, op1=ALU.add), rd=["RT"], wr=["RT"])
                        dq(lambda e, k_=k_: e.tensor_tensor(out=SLf[:, :, k_], in0=SLf[:, :, k_], in1=gm[:, :, 7], op=ALU.add), rd=["RT", "SLf"], wr=["SLf"])
                    s.op("dve", lambda e: e.tensor_copy(out=SLi[:], in_=SLf[:].rearrange("p t k -> p (t k)")), reads=["SLf"], writes=["SLi"])
                    for i in range(NT):
                        for k_ in range(2):
                            dma_fn("pool", lambda e, k_=k_, i=i: e.indirect_dma_start(
                                out=Xd[:, :], out_offset=bass.IndirectOffsetOnAxis(ap=SLi[:, 2 * i + k_:2 * i + k_ + 1], axis=0),
                                in_=fbf[:, i, :], in_offset=None, bounds_check=bc_reg, oob_is_err=False),
                                ["SLi", "fbf%d" % i], ["Xd"])
                    if smp == 0:
                        dump("SLf", SLf[:, :, :], ["SLf"])
                        dump("w12", w12[:, :, :], ["w12"])
                    s.barrier()
                    print("P5 done: deadlock-free", s.check_deadlock(), s.n_ins, s.n_wait, s.cnt)
                if stop_after == "P5":
                    s.barrier()
                    return nc
                chunks = [(c0, min(c0 + 512, CAP)) for c0 in range(0, CAP, 512)]
                with ExitStack() as st:
                    weg = [sb(st, "weg%d" % i, [128, 8, DE], BF16) for i in range(2)]
                    weu = [sb(st, "weu%d" % i, [128, 8, DE], BF16) for i in range(2)]
                    wed = [sb(st, "wed%d" % i, [128, 4, D], BF16) for i in range(2)]
                    XeT = [sb(st, "XeT%d" % i, [128, 8, CAP], BF16) for i in range(2)]
                    hT = [sb(st, "hT%d" % i, [128, 4, CAP], BF16) for i in range(2)]
                    NXB = 2 * NB
                    xe = [sb(st, "xe%d" % i, [128, D], BF16) for i in range(NXB)]
                    ye = [sb(st, "ye%d" % i, [128, D], BF16) for i in range(NXB)]
                    sgm = [sb(st, "sgm%d" % i, [128, 512], F32) for i in range(2)]
                    TX = [pst(st, "p6t%d" % i, [128, 8, 128], BF16) for i in range(2)]
                    GB = [pst(st, "p6g%d" % i, [128, 512]) for i in range(2)]
                    UB = [pst(st, "p6u%d" % i, [128, 512]) for i in range(2)]
                    YB = [pst(st, "p6y%d" % i, [128, 512]) for i in range(2)]
                    TXK = ["p6T%d" % i for i in range(2)]
                    GK = ["p6G%d" % i for i in range(2)]
                    UK = ["p6U%d" % i for i in range(2)]
                    YK = ["p6Y%d" % i for i in range(2)]
                    cnt6 = [0, 0, 0]

                    def load_w(e_):
                        wb = e_ % 2
                        s.dma("sp", weg[wb][:], wview(weg_b[e_]), reads=["wbg%d" % e_], writes=["weg%d" % wb])
                        s.dma("sp", weu[wb][:], wview(weu_b[e_]), reads=["wbu%d" % e_], writes=["weu%d" % wb])
                        s.dma("sp", wed[wb][:], wview(wed_b[e_]), reads=["wbd%d" % e_], writes=["wed%d" % wb])
                        for kt in range(4):
                            tt("pool", wed[wb][:, kt, :], wed[wb][:, kt, :], g2b[:], ALU.mult, ["wed%d" % wb, "g2b"], ["wed%d" % wb])

                    cntx = [0]

                    def load_x(e_):
                        for blk in range(NB):
                            xb = cntx[0] % NXB
                            cntx[0] += 1
                            r0 = e_ * CAP + blk * 128
                            s.dma("sp", xe[xb][:], Xd[r0:r0 + 128, :], writes=["xe%d" % xb])

                    def ph_T(e_):
                        wb = e_ % 2
                        for blk in range(NB):
                            u = cnt6[0] % 2
                            xb = cnt6[0] % NXB
                            cnt6[0] += 1
                            for kt in range(8):
                                tr(TX[u][:, kt, :], xe[xb][:, kt * 128:(kt + 1) * 128], ident_b[:], ["xe%d" % xb, "ident_b"], [], x=[TXK[u]])
                            cp("act" if u == 0 else "dve", XeT[wb][:, :, blk * 128:(blk + 1) * 128], TX[u][:, :, :], [], ["XeT%d" % wb], x=[TXK[u]])

                    def ph_GU(e_):
                        wb = e_ % 2
                        for nt in range(4):
                            ncol = slice(nt * 128, (nt + 1) * 128)
                            for (c0, c1) in chunks:
                                u = cnt6[1] % 2
                                cnt6[1] += 1
                                n = c1 - c0
                                for kt in range(8):
                                    mm(GB[u][:, 0:n], weg[wb][:, kt, ncol], XeT[wb][:, kt, c0:c1], kt == 0, kt == 7, ["weg%d" % wb, "XeT%d" % wb], [], x=[GK[u]])
                                for kt in range(8):
                                    mm(UB[u][:, 0:n], weu[wb][:, kt, ncol], XeT[wb][:, kt, c0:c1], kt == 0, kt == 7, ["weu%d" % wb, "XeT%d" % wb], [], x=[UK[u]])
                                act(sgm[u][:, 0:n], GB[u][:, 0:n], AF.Silu, [], ["sgm%d" % u], x=[GK[u]])
                                tt("dve", hT[wb][:, nt, c0:c1], sgm[u][:, 0:n], UB[u][:, 0:n], ALU.mult, ["sgm%d" % u], ["hT%d" % wb], x=[UK[u]])

                    def ph_D(e_):
                        wb = e_ % 2
                        for blk in range(NB):
                            yu = (e_ * NB + blk) % NXB
                            r0 = e_ * CAP + blk * 128
                            for half in range(2):
                                u = cnt6[2] % 2
                                cnt6[2] += 1
                                hcol = slice(half * 512, (half + 1) * 512)
                                for kt in range(4):
                                    mm(YB[u][:, :], hT[wb][:, kt, blk * 128:(blk + 1) * 128], wed[wb][:, kt, hcol], kt == 0, kt == 3,
                                       ["hT%d" % wb, "wed%d" % wb], [], x=[YK[u]])
                                cp("act" if half == 0 else "dve", ye[yu][:, hcol], YB[u][:, :], [], ["ye%d" % yu], x=[YK[u]])
                            s.dma("pool", Yd[r0:r0 + 128, :], ye[yu][:], reads=["ye%d" % yu])

                    precast(96)
                    load_w(0)
                    load_x(0)
                    load_x(1)
                    ph_T(0)
                    for e_ in range(NEXP):
                        if e_ + 1 < NEXP:
                            load_w(e_ + 1)
                        if e_ + 2 < NEXP:
                            load_x(e_ + 2)
                        ph_GU(e_)
                        if e_ + 1 < NEXP:
                            ph_T(e_ + 1)
                        ph_D(e_)
                    s.barrier()
                    print("P6 done: deadlock-free", s.check_deadlock(), s.n_ins, s.n_wait, s.cnt)
                with ExitStack() as st:
                    yk = [[sb(st, "yk%d_%d" % (k_, i), [128, D], BF16) for i in range(2)] for k_ in range(2)]
                    ot = [sb(st, "ot%d" % i, [128, D], F32) for i in range(2)]
                    junk4 = sb(st, "junk4", [128, D], F32)
                    st4 = [sb(st, "st4_%d" % i, [128, 4], F32) for i in range(2)]
                    for k_ in range(2):
                        for i in range(2):
                            s.op("pool", lambda e, k_=k_, i=i: e.memset(yk[k_][i][:], 0.0), writes=["yk%d_%d" % (k_, i)])
                    for i in range(NT):
                        u = i % 2
                        tokc = slice(i * 128, (i + 1) * 128)
                        hk = "hres%d" % i
                        for k_ in range(2):
                            dma_fn("pool", lambda e, k_=k_, u=u, i=i: e.indirect_dma_start(
                                out=yk[k_][u][:, :], out_offset=None, in_=Yd[:, :],
                                in_offset=bass.IndirectOffsetOnAxis(ap=SLi[:, 2 * i + k_:2 * i + k_ + 1], axis=0), bounds_check=bc_reg, oob_is_err=False),
                                ["SLi"], ["yk%d_%d" % (k_, u)])
                            stt("dve", hres[:, i, :], yk[k_][u][:], w12[:, i, k_:k_ + 1], hres[:, i, :], ALU.mult, ALU.add,
                                ["yk%d_%d" % (k_, u), "w12", hk], [hk])
                        s.op("pool", lambda e, u=u: e.memset(st4[u][:], 0.0), writes=["st4_%d" % u])
                        act(junk4[:], hres[:, i, :], AF.Square, [hk, "st4_%d" % u], ["junk4", "st4_%d" % u], accum_out=st4[u][:, 0:1])
                        act(st4[u][:, 1:2], st4[u][:, 0:1], AF.Sqrt, ["st4_%d" % u, "eps_c"], ["st4_%d" % u], scale=1.0 / D, bias=eps_c[:, 0:1])
                        s.op("dve", lambda e, u=u: e.reciprocal(out=st4[u][:, 2:3], in_=st4[u][:, 1:2]), reads=["st4_%d" % u], writes=["st4_%d" % u])
                        stt("dve", ot[u][:], hres[:, i, :], st4[u][:, 2:3], gfb[:], ALU.mult, ALU.mult, [hk, "st4_%d" % u, "gfb"], ["ot%d" % u])
                        s.dma("sp", y_d[smp, tokc, :], ot[u][:], reads=["ot%d" % u])
                    s.barrier()
        s.barrier(final=True)
    return nc


def _consts():
    ident = np.eye(128, dtype=np.float32)
    idx = np.arange(128)
    same = (idx[:, None] // 64) == (idx[None, :] // 64)
    tri = np.zeros((6, 128, 128), np.float32)
    tri[0] = same & (idx[:, None] <= idx[None, :])
    tri[1] = same & (idx[:, None] > idx[None, :])
    tri[2] = same & (idx[:, None] >= idx[None, :])
    tri[3] = same & (idx[:, None] < idx[None, :])
    tri[4] = idx[:, None] < idx[None, :]
    tri[5] = 1.0
    t = np.arange(S)
    pos_r = (t // GW).astype(np.float32)
    pos_c = (t % GW).astype(np.float32)
    inv = (10000.0 ** (-np.arange(32, dtype=np.float32) / 32)).astype(np.float32)
    ang = np.zeros((128, S), np.float32)
    ang[0:32] = inv[:, None] * pos_r[None, :]
    ang[32:64] = inv[:, None] * pos_r[None, :]
    ang[64:96] = inv[:, None] * pos_c[None, :]
    ang[96:128] = inv[:, None] * pos_c[None, :]
    cos = np.cos(ang).astype(np.float32)
    sin = np.sin(ang).astype(np.float32)
    sgn = np.ones((128, 1), np.float32)
    sgn[0:32] = -1
    sgn[64:96] = -1
    sin = sin * sgn
    perm = np.concatenate([np.arange(32, 64), np.arange(0, 32), np.arange(96, 128), np.arange(64, 96)])
    return ident, tri, cos, sin, perm


def _na_tables(rpb):
    a = (np.arange(128) // 64)[:, None, None, None]
    kc = (np.arange(128) % 64)[:, None, None, None]
    p = np.arange(8)[None, :, None, None]
    j = np.arange(4)[None, None, :, None]
    qc = np.arange(64)[None, None, None, :]
    ridx = 2 * j + a - p + 7
    cstart = np.clip(qc - 8, 0, 48)
    valid = (kc >= cstart) & (kc < cstart + 16) & (ridx >= 0) & (ridx <= 14)
    valid = np.broadcast_to(valid, (128, 8, 4, 64))
    cidx = np.clip(kc - qc + 15, 0, 30)
    ridx_c = np.clip(ridx, 0, 14)
    ridx_b = np.broadcast_to(ridx_c, (128, 8, 4, 64))
    cidx_b = np.broadcast_to(cidx, (128, 8, 4, 64))
    g = rpb[:, ridx_b, cidx_b]
    g = np.where(valid[None], g, np.float32(0.0)).astype(np.float32)
    g = g.reshape(4, 2, 128, 8, 256).transpose(0, 2, 3, 1, 4)
    mask = np.where(valid, np.float32(0.0), np.float32(MASKV)).astype(np.float32).reshape(128, 8, 256)
    return np.ascontiguousarray(g), np.ascontiguousarray(mask)


_NC_CACHE = {}


def kernel(x, c, ctx, c_ctx, w_mod, b_mod, norm_attn_g, norm_ffn_g, w_in, w_gla_a2, b_gla_a2,
           gla_norm_g, na_rpb, w_na_o, w_gla_o, w_out, w_group, b_group, w_expert, b_expert,
           w_exp_gate, w_exp_up, w_exp_down, final_norm_g):
    f = lambda a: np.ascontiguousarray(np.asarray(a, dtype=np.float32))
    x, c, ctx, c_ctx = f(x), f(c), f(ctx), f(c_ctx)
    ident, tri, cos, sin, perm = _consts()
    w_in0 = f(w_in)[0]
    gq = w_in0[:, C_GQ:C_GQ + 512].reshape(D, 4, 128)[:, :, perm].reshape(D, 512)
    gk = w_in0[:, C_GK:C_GK + 512].reshape(D, 4, 128)[:, :, perm].reshape(D, 512)
    w_rope = np.ascontiguousarray(np.concatenate([gq, gk], axis=1))
    w_a2b = np.ascontiguousarray(np.concatenate(
        [f(w_gla_a2)[0].transpose(1, 0, 2), f(b_gla_a2)[0][None]], axis=0))
    nab, nam = _na_tables(f(na_rpb)[0])
    w_rt = np.ascontiguousarray(np.concatenate([f(w_group)[0], f(w_expert)[0]], axis=1))
    b_rt = np.ascontiguousarray(np.concatenate([f(b_group)[0], f(b_expert)[0]], axis=0))
    shared = {
        "w_mod": f(w_mod)[0], "b_mod": f(b_mod)[0], "g_attn": f(norm_attn_g)[0], "g_ffn": f(norm_ffn_g)[0],
        "g_fin": f(final_norm_g), "gla_g": f(gla_norm_g)[0], "w_in": w_in0, "w_rope": w_rope, "w_a2b": w_a2b,
        "w_na_o": f(w_na_o)[0], "w_gla_o": f(w_gla_o)[0], "w_out": f(w_out)[0], "w_rt": w_rt, "b_rt": b_rt,
        "w_eg": f(w_exp_gate)[0], "w_eu": f(w_exp_up)[0], "w_ed": f(w_exp_down)[0],
        "ident": ident, "tri": tri, "rope_cos": cos, "rope_sin": sin, "na_bias": nab, "na_mask": nam,
        "ebase": np.ascontiguousarray(np.broadcast_to((np.arange(NEXP, dtype=np.float32) * MOE_CAP)[None, :], (128, NEXP))),
    }
    n = 8
    NS = x.shape[0] // n
    if "nc" not in _NC_CACHE:
        _NC_CACHE["nc"] = build_nc(NS)
    nc = _NC_CACHE["nc"]
    in_maps = []
    for i in range(n):
        m = dict(shared)
        m["x"] = x[i * NS:(i + 1) * NS]
        m["ctx"] = ctx[i * NS:(i + 1) * NS]
        m["cc"] = np.ascontiguousarray(np.concatenate([c[i * NS:(i + 1) * NS], c_ctx[None]], axis=0))
        in_maps.append(m)
    res = run_bass_kernel_spmd(nc, in_maps, core_ids=list(range(n)))
    return np.concatenate([r["y"] for r in res.results], axis=0)
```

```python
import os
import numpy as np
import concourse.bass as bass
import concourse.mybir as mybir
from concourse.bass_utils import run_bass_kernel_spmd
from contextlib import ExitStack

F32 = mybir.dt.float32
BF16 = mybir.dt.bfloat16
I32 = mybir.dt.int32
ALU = mybir.AluOpType
AF = mybir.ActivationFunctionType
AX = mybir.AxisListType

D = 1024
S = 2048
CT = 256
NT = 16
NTC = 2
NTA = NT + NTC
GW = 64
EPS = 1e-6
NEXP = 32
DE = 512
C_NAQ, C_NAK, C_NAV, C_GQ, C_GK, C_GV, C_GG, C_AL, C_M8, C_M9 = 0, 512, 1024, 1536, 2048, 2560, 3584, 4608, 4640, 5664
MASKV = -30000.0
MOE_CAP = 512


class Sched:
    ENGS = ("pe", "act", "dve", "pool", "sp")
    HND = {"pe": "tensor", "act": "scalar", "dve": "vector", "pool": "gpsimd", "sp": "sync"}

    def __init__(self, nc, es, n_dma_sems=40):
        self.nc = nc
        self.sem = {e: es.enter_context(nc.semaphore("s_" + e)) for e in self.ENGS}
        self.cnt = {e: 0 for e in self.ENGS}
        self.pending = {e: False for e in self.ENGS}
        self.waited = {e: {} for e in self.ENGS}
        self.dsem = [es.enter_context(nc.semaphore("s_dma%d" % i)) for i in range(n_dma_sems)]
        self.dcnt = [0] * n_dma_sems
        self.dnext = 0
        self.n_fg = n_dma_sems - 8
        self.bnext = 0
        self.lastw = {}
        self.reads = {}
        self.semobj = {}
        for e in self.ENGS:
            self.semobj[("e", e)] = self.sem[e]
        for i, sm in enumerate(self.dsem):
            self.semobj[("d", i)] = sm
        self.n_ins = 0
        self.n_wait = 0

    def _deps(self, eng, reads, writes, excl=()):
        toks = {}

        def add(t):
            if t is None:
                return
            k, v = t
            if toks.get(k, -1) < v:
                toks[k] = v
        for b in reads:
            add(self.lastw.get(b))
        for b in writes:
            add(self.lastw.get(b))
            for t in self.reads.get(b, ()):
                add(t)
        for b in excl:
            t = self.lastw.get(b)
            if t is not None and t[0] != ("e", eng):
                add(t)
        out = []
        for k, v in toks.items():
            if k == ("e", eng):
                if eng == "pe":
                    continue
                if v <= self.cnt[eng] - 2:
                    continue
            if self.waited[eng].get(k, -1) >= v:
                continue
            self.waited[eng][k] = v
            out.append((k, v))
        return out

    def _commit(self, tok, reads, writes):
        for b in writes:
            self.lastw[b] = tok
            self.reads[b] = []
        for b in reads:
            self.reads.setdefault(b, []).append(tok)

    def check_deadlock(self):
        pos = {e: 0 for e in self.ENGS}
        val = {}
        prog = True
        while prog:
            prog = False
            for e in self.ENGS:
                lst = self.log[e]
                while pos[e] < len(lst):
                    waits, inc = lst[pos[e]]
                    if all(val.get(k, 0) >= v for k, v in waits):
                        if inc is not None:
                            val[inc[0]] = val.get(inc[0], 0) + inc[1]
                        pos[e] += 1
                        prog = True
                    else:
                        break
        stuck = {e: (pos[e], len(self.log[e])) for e in self.ENGS if pos[e] < len(self.log[e])}
        for e in stuck:
            waits, inc = self.log[e][pos[e]]
            print("STUCK", e, pos[e], [(k, v, val.get(k, 0)) for k, v in waits])
        return not stuck

    def _emit1(self, eng, waits, fn, inc):
        if not hasattr(self, "log"):
            self.log = {e: [] for e in self.ENGS}
        self.log[eng].append((list(waits), inc if fn is not None else None))
        engh = getattr(self.nc, self.HND[eng])
        for k, v in waits:
            engh.wait_ge(self.semobj[k], v)
            self.n_wait += 1
        if fn is None:
            return
        ins = fn(engh)
        if inc is not None:
            ins.then_inc(self.semobj[inc[0]], inc[1])
        self.n_ins += 1

    def op(self, eng, fn, reads=(), writes=(), inc=True, excl=()):
        waits = self._deps(eng, reads, writes, excl)
        if inc:
            self.cnt[eng] += 1
            tok = (("e", eng), self.cnt[eng])
            self.pending[eng] = False
        else:
            tok = (("e", eng), self.cnt[eng] + 1)
            self.pending[eng] = True
        self._emit1(eng, waits, fn, (("e", eng), 1) if inc else None)
        self._commit(tok, reads, writes)
        for b_ in excl:
            self.lastw[b_] = tok
        return tok

    def dma(self, eng, out, in_, reads=(), writes=(), bg=False, **kw):
        if bg:
            i = self.n_fg + self.bnext
            self.bnext = (self.bnext + 1) % 8
        else:
            i = self.dnext
            self.dnext = (self.dnext + 1) % self.n_fg
        k = ("d", i)
        waits = self._deps(eng, reads, writes)
        if self.dcnt[i] > 0 and self.waited[eng].get(k, -1) < self.dcnt[i]:
            self.waited[eng][k] = self.dcnt[i]
            waits.append((k, self.dcnt[i]))
        self.dcnt[i] += 16
        tok = (k, self.dcnt[i])
        self._emit1(eng, waits, lambda e: e.dma_start(out=out, in_=in_, **kw), (k, 16))
        self._commit(tok, reads, writes)
        return tok

    def barrier(self, final=False):
        assert not any(self.pending.values())
        allt = [(("e", e), self.cnt[e]) for e in self.ENGS if self.cnt[e] > 0]
        allt += [(("d", i), c) for i, c in enumerate(self.dcnt) if c > 0 and (i < self.n_fg or final)]
        for e in self.ENGS:
            waits = []
            for k, v in allt:
                if k == ("e", e):
                    continue
                if self.waited[e].get(k, -1) >= v:
                    continue
                self.waited[e][k] = v
                waits.append((k, v))
            self._emit1(e, waits, None, None)
        keep = {k: t for k, t in self.lastw.items() if t[0][0] == "d" and t[0][1] >= self.n_fg}
        self.lastw = {} if final else keep
        self.reads = {}


def build_nc(NS=2, dbg=None, stop_after=None):
    nc = bass.Bass("TRN2", target_bir_lowering=False)

    def din(name, shape, dt=F32):
        return nc.dram_tensor(name, list(shape), dt, kind="ExternalInput").ap()

    x_d = din("x", [NS, S, D])
    ctx_d = din("ctx", [NS, CT, D])
    cc_d = din("cc", [NS + 1, D])
    w_mod_d = din("w_mod", [D, 6 * D])
    b_mod_d = din("b_mod", [6 * D])
    g_attn_d = din("g_attn", [D])
    g_ffn_d = din("g_ffn", [D])
    g_fin_d = din("g_fin", [D])
    gla_g_d = din("gla_g", [256])
    w_in_d = din("w_in", [D, 6688])
    w_rope_d = din("w_rope", [D, 1024])
    w_a2b_d = din("w_a2b", [17, 2, 512])
    w_nao_d = din("w_na_o", [512, D])
    w_glo_d = din("w_gla_o", [D, D])
    w_out_d = din("w_out", [D, D])
    w_rt_d = din("w_rt", [D, 36])
    b_rt_d = din("b_rt", [36])
    w_eg_d = din("w_eg", [NEXP, D, DE])
    w_eu_d = din("w_eu", [NEXP, D, DE])
    w_ed_d = din("w_ed", [NEXP, DE, D])
    ident_d = din("ident", [128, 128])
    tri_d = din("tri", [6, 128, 128])
    ebase_d = din("ebase", [128, 32])
    cos_d = din("rope_cos", [128, S])
    sin_d = din("rope_sin", [128, S])
    nab_d = din("na_bias", [4, 128, 8, 2, 256])
    nam_d = din("na_mask", [128, 8, 256])
    y_d = nc.dram_tensor("y", [NS, S, D], F32, kind="ExternalOutput").ap()
    modv_d = nc.dram_tensor("modv", [NS + 1, 6 * D], F32).ap()
    dbg_d = {}
    if dbg:
        for name, shape in dbg.items():
            dbg_d[name] = nc.dram_tensor("dbg_" + name, list(shape), F32, kind="ExternalOutput").ap()

    with ExitStack() as es:
        s = Sched(nc, es)

        used_names = {}

        def uniq(name):
            k = used_names.get(name, 0)
            used_names[name] = k + 1
            return name if k == 0 else "%s_r%d" % (name, k)

        def sb(st, name, shape, dt):
            return st.enter_context(nc.sbuf_tensor(uniq(name), list(shape), dt))

        def pst(st, name, shape, dt=F32):
            return st.enter_context(nc.psum_tensor(uniq(name), list(shape), dt))

        def mm(out, lhsT, rhs, start, stop, reads, writes, inc=None, x=()):
            if inc is None:
                inc = stop
            s.op("pe", lambda e: e.matmul(out, lhsT, rhs, start=start, stop=stop),
                 reads=reads, writes=writes, inc=inc, excl=x)

        def tr(out, in_, idn, reads, writes, x=()):
            s.op("pe", lambda e: e.transpose(out, in_, idn), reads=reads, writes=writes, excl=x)

        def act(out, in_, func, reads, writes, x=(), **kw):
            s.op("act", lambda e: e.activation(out=out, in_=in_, func=func, **kw), reads=reads, writes=writes, excl=x)

        def tt(eng, out, in0, in1, op, reads, writes, x=()):
            s.op(eng, lambda e: e.tensor_tensor(out=out, in0=in0, in1=in1, op=op), reads=reads, writes=writes, excl=x)

        def ts(eng, out, in0, s1, s2, op0, op1, reads, writes, x=()):
            if s2 is None:
                s.op(eng, lambda e: e.tensor_scalar(out=out, in0=in0, scalar1=s1, scalar2=None, op0=op0),
                     reads=reads, writes=writes, excl=x)
            else:
                s.op(eng, lambda e: e.tensor_scalar(out=out, in0=in0, scalar1=s1, scalar2=s2, op0=op0, op1=op1),
                     reads=reads, writes=writes, excl=x)

        def stt(eng, out, in0, scalar, in1, op0, op1, reads, writes, x=()):
            s.op(eng, lambda e: e.scalar_tensor_tensor(out=out, in0=in0, scalar=scalar, in1=in1, op0=op0, op1=op1),
                 reads=reads, writes=writes, excl=x)

        def cp(eng, out, in_, reads, writes, x=()):
            if eng == "act":
                s.op("act", lambda e: e.copy(out=out, in_=in_), reads=reads, writes=writes, excl=x)
            else:
                s.op(eng, lambda e: e.tensor_copy(out=out, in_=in_), reads=reads, writes=writes, excl=x)

        def dump(name, src_ap, reads, dst=None):
            if name in dbg_d:
                s.dma("pool", dbg_d[name] if dst is None else dst, src_ap, reads=reads)

        def wview(ap2d):
            return ap2d.rearrange("(kt p) n -> p kt n", p=128)

        ident_f = sb(es, "ident_f", [128, 128], F32)
        ident_b = sb(es, "ident_b", [128, 128], BF16)
        tri_f = sb(es, "tri_f", [128, 4, 128], F32)
        ones_b = sb(es, "ones_b", [128, 128], BF16)
        eps_c = sb(es, "eps_c", [128, 1], F32)
        one_c = sb(es, "one_c", [128, 1], F32)
        s.dma("sp", ident_f[:], ident_d, writes=["ident_f"])
        s.dma("pool", ident_b[:], ident_d, writes=["ident_b"])
        s.dma("sp", tri_f[:], tri_d[0:4].rearrange("m s t -> s m t"), writes=["tri"])
        s.op("pool", lambda e: e.memset(ones_b[:], 1.0), writes=["ones_b"])
        s.op("pool", lambda e: e.memset(eps_c[:], EPS), writes=["eps_c"])
        s.op("pool", lambda e: e.memset(one_c[:], 1.0), writes=["one_c"])

        with ExitStack() as st:
            ccs = sb(st, "ccs", [NS + 1, D], F32)
            scT = sb(st, "scT", [128, 8, NS + 1], F32)
            modsb = sb(st, "modsb", [NS + 1, 6 * D], F32)
            bmod = sb(st, "bmod", [NS + 1, 6 * D], F32)
            wm = [sb(st, "wm%d" % i, [128, 8, 512], F32) for i in range(2)]
            psA = pst(st, "p0a", [128, 512])
            psB = [pst(st, "p0b%d" % i, [128, 512]) for i in range(2)]
            R = NS + 1
            s.dma("sp", ccs[:], cc_d, writes=["ccs"])
            s.dma("sp", bmod[:], b_mod_d.partition_broadcast(R), writes=["bmod"])
            act(ccs[:], ccs[:], AF.Silu, ["ccs"], ["ccs"])
            for kt in range(8):
                tr(psA[:, kt * R:(kt + 1) * R], ccs[:, kt * 128:(kt + 1) * 128], ident_f[0:R, 0:R],
                   ["ccs", "ident_f"], ["p0a"])
            cp("dve", scT[:].rearrange("p k r -> p (k r)"), psA[:, 0:8 * R], ["p0a"], ["scT"])
            for cb in range(12):
                w = wm[cb % 2]
                s.dma("sp", w[:], wview(w_mod_d[:, cb * 512:(cb + 1) * 512]), writes=["wm%d" % (cb % 2)])
                ps = psB[cb % 2]
                for kt in range(8):
                    mm(ps[0:R, :], scT[:, kt, :], w[:, kt, :], kt == 0, kt == 7,
                       ["scT", "wm%d" % (cb % 2)], ["p0b%d" % (cb % 2)])
                tt("dve", modsb[:, cb * 512:(cb + 1) * 512], ps[0:R, :], bmod[:, cb * 512:(cb + 1) * 512], ALU.add,
                   ["p0b%d" % (cb % 2), "bmod"], ["modsb"])
            s.dma("sp", modv_d, modsb[:], reads=["modsb"], writes=["modv"])
            dump("modv", modsb[:], ["modsb"])
            s.barrier()
        if stop_after == "P0":
            s.barrier()
            return nc

        QSCALE = 128.0 ** -0.5
        weg_b = nc.dram_tensor("weg_bf", [NEXP, D, DE], BF16).ap()
        weu_b = nc.dram_tensor("weu_bf", [NEXP, D, DE], BF16).ap()
        wed_b = nc.dram_tensor("wed_bf", [NEXP, DE, D], BF16).ap()

        wna_b = nc.dram_tensor("wna_bf", [512, D], BF16).ap()
        wgl_b = nc.dram_tensor("wgl_bf", [D, D], BF16).ap()
        wm8_b = nc.dram_tensor("wm8_bf", [D, D], BF16).ap()
        wm9_b = nc.dram_tensor("wm9_bf", [D, D], BF16).ap()

        def precast_m1():
            s.dma("pool", wna_b, w_nao_d, writes=["wnab"], bg=True)
            s.dma("pool", wgl_b, w_glo_d, writes=["wglb"], bg=True)
            s.dma("pool", wm8_b, w_in_d[:, C_M8:C_M8 + D], writes=["wm8b"], bg=True)
            s.dma("pool", wm9_b, w_in_d[:, C_M9:C_M9 + D], writes=["wm9b"], bg=True)

        def _precast_gen():
            for e_ in range(NEXP):
                for nm, dst, srcw, r in (("g", weg_b, w_eg_d, 4), ("u", weu_b, w_eu_d, 4), ("d", wed_b, w_ed_d, 2)):
                    s.dma("pool", dst[e_].rearrange("(a r) n -> a (r n)", r=r), srcw[e_].rearrange("(a r) n -> a (r n)", r=r),
                          writes=["wb%s%d" % (nm, e_)], bg=True)
                    yield
        _pc = _precast_gen()

        def precast(n):
            for _ in range(n):
                next(_pc, None)
        bc_reg = nc.gpsimd.alloc_register("bcreg")
        nc.gpsimd.reg_mov(bc_reg, NEXP * MOE_CAP - 1)
        h1_d = nc.dram_tensor("h1_scr", [NS, S, D], F32).ap()

        def bank_set(st, pfx):
            return [pst(st, "%s%d" % (pfx, i), [128, 512]) for i in range(7)]

        for smp in range(NS):
            with ExitStack() as sa:
                aT = sb(sa, "aT", [128, 8, S], BF16)
                acT = sb(sa, "acT", [128, 8, CT], BF16)
                oglT = sb(sa, "oglT", [128, 8, S], BF16)

                with ExitStack() as st:
                    s1b = sb(st, "s1b", [128, D], F32)
                    sh1b = sb(st, "sh1b", [128, D], F32)
                    s1c = sb(st, "s1c", [128, D], F32)
                    sh1c = sb(st, "sh1c", [128, D], F32)
                    gab = sb(st, "gab", [128, D], F32)
                    tmpv = sb(st, "tmpv", [128, D], F32)
                    xt = [sb(st, "xt%d" % i, [128, D], F32) for i in range(2)]
                    xm = [sb(st, "xm%d" % i, [128, D], F32) for i in range(2)]
                    xn = [sb(st, "xn%d" % i, [128, D], BF16) for i in range(2)]
                    stat = [sb(st, "stat%d" % i, [128, 4], F32) for i in range(2)]
                    psT = [pst(st, "p1t%d" % i, [128, 8, 128], BF16) for i in range(2)]
                    s.dma("sp", gab[:], g_attn_d.partition_broadcast(128), writes=["gab"])
                    if smp == 0:
                        precast_m1()
                    for row, s1, sh, nm in ((smp, s1b, sh1b, "l"), (NS, s1c, sh1c, "c")):
                        s.dma("sp", tmpv[:], modv_d[row, D:2 * D].partition_broadcast(128), writes=["tmpv"])
                        stt("dve", s1[:], tmpv[:], 1.0, gab[:], ALU.add, ALU.mult, ["tmpv", "gab"], ["s1" + nm])
                        s.dma("sp", sh[:], modv_d[row, 0:D].partition_broadcast(128), writes=["sh1" + nm])
                    for i in range(NTA):
                        b = i % 2
                        lat = i < NT
                        src = x_d[smp, i * 128:(i + 1) * 128, :] if lat else ctx_d[smp, (i - NT) * 128:(i - NT + 1) * 128, :]
                        nm = "l" if lat else "c"
                        s1, sh = (s1b, sh1b) if lat else (s1c, sh1c)
                        s.dma("sp", xt[b][:], src, writes=["xt%d" % b])
                        s.op("pool", lambda e, b=b: e.memset(stat[b][:], 0.0), writes=["stat%d" % b])
                        act(xm[b][:], xt[b][:], AF.Square, ["xt%d" % b, "stat%d" % b], ["xm%d" % b, "stat%d" % b],
                            accum_out=stat[b][:, 0:1])
                        act(stat[b][:, 1:2], stat[b][:, 0:1], AF.Sqrt, ["stat%d" % b, "eps_c"], ["stat%d" % b],
                            scale=1.0 / D, bias=eps_c[:, 0:1])
                        s.op("dve", lambda e, b=b: e.reciprocal(out=stat[b][:, 2:3], in_=stat[b][:, 1:2]),
                             reads=["stat%d" % b], writes=["stat%d" % b])
                        stt("dve", xm[b][:], xt[b][:], stat[b][:, 2:3], s1[:], ALU.mult, ALU.mult,
                            ["xt%d" % b, "stat%d" % b, "s1" + nm], ["xm%d" % b])
                        tt("pool", xn[b][:], xm[b][:], sh[:], ALU.add, ["xm%d" % b, "sh1" + nm], ["xn%d" % b])
                        for kt in range(8):
                            tr(psT[b][:, kt, :], xn[b][:, kt * 128:(kt + 1) * 128], ident_b[:],
                               ["xn%d" % b, "ident_b"], ["p1t%d" % b])
                        if lat:
                            cp("act", aT[:, :, i * 128:(i + 1) * 128], psT[b][:, :, :], ["p1t%d" % b], ["aT"])
                        else:
                            cp("act", acT[:, :, (i - NT) * 128:(i - NT + 1) * 128], psT[b][:, :, :], ["p1t%d" % b], ["acT"])
                    if smp == 0:
                        dump("aT", aT[:, :, 0:256], ["aT"])
                    s.barrier()
                if stop_after == "P1":
                    s.barrier()
                    return nc

                with ExitStack() as st:
                    ropeC = sb(st, "ropeC", [128, S], BF16)
                    ropeS = sb(st, "ropeS", [128, S], BF16)
                    alT = [sb(st, "alT%d" % d_, [16, S + CT], BF16) for d_ in range(2)]
                    wal = sb(st, "wal", [128, 8, 32], BF16)
                    wa2 = sb(st, "wa2", [16, 2, 512], BF16)
                    ba2 = sb(st, "ba2", [1, 2, 512], BF16)
                    gnb = sb(st, "gnb", [128, 256], F32)
                    wq = sb(st, "wq", [128, 8, 128], BF16)
                    wqs = sb(st, "wqs", [128, 8, 128], BF16)
                    wk = sb(st, "wk", [128, 8, 128], BF16)
                    wks = sb(st, "wks", [128, 8, 128], BF16)
                    wv = sb(st, "wv", [128, 8, 256], BF16)
                    wg = sb(st, "wg", [128, 8, 256], BF16)
                    qrT = sb(st, "qrT", [128, S], BF16)
                    krT = sb(st, "krT", [128, S], BF16)
                    krk = sb(st, "krk", [128, NTA, 128], BF16)
                    vtk = sb(st, "vtk", [128, NTA, 256], BF16)
                    sgt = sb(st, "sgt", [128, NT, 256], BF16)
                    qe = [sb(st, "qe%d" % d_, [128, S], BF16) for d_ in range(2)]
                    ke = [sb(st, "ke%d" % d_, [128, S], BF16) for d_ in range(2)]
                    kd = [sb(st, "kd%d" % d_, [128, NTA, 128], BF16) for d_ in range(2)]
                    dec = sb(st, "dec", [128, 2, NTA, 2], F32)
                    oacc = sb(st, "oacc", [128, NT, 256], F32)
                    S32 = [[sb(st, "S32_%d_%d" % (d_, v_), [128, 256], F32) for v_ in range(2)] for d_ in range(2)]
                    attf = [sb(st, "attf%d" % i, [128, 128], BF16) for i in range(2)]
                    trib = sb(st, "trib", [128, 2, 128], BF16)
                    S16 = [[sb(st, "S16_%d_%d" % (d_, v_), [128, 256], BF16) for v_ in range(2)] for d_ in range(2)]
                    t1 = [sb(st, "t1_%d" % i, [128, 512], F32) for i in range(2)]
                    t2 = [sb(st, "t2_%d" % i, [128, 512], F32) for i in range(2)]
                    otmp = [t1[i][:, 0:256] for i in range(2)]
                    onr = [t2[i][:, 0:256] for i in range(2)]
                    Dt = [sb(st, "Dt0", [128, 512], F32)]
                    Di = [sb(st, "Di0", [128, 512], F32)]
                    EK = [sb(st, "EK0", [128, 512], F32)]
                    att = [sb(st, "att%d" % i, [128, 128], BF16) for i in range(2)]
                    junk = sb(st, "junk2", [128, 256], F32)
                    rbf = [sb(st, "rbf%d" % i, [128, 256], BF16) for i in range(2)]
                    st2 = [sb(st, "st2_%d" % i, [128, 4], F32) for i in range(2)]
                    B = [pst(st, "p2b%d" % i, [128, 512]) for i in range(6)]
                    BK = ["p2B%d" % i for i in range(6)]
                    pT = [pst(st, "p2t%d" % i, [128, 8, 128], BF16) for i in range(2)]
                    TK = ["p2T0", "p2T1"]

                    s.dma("pool", ropeC[:], cos_d, writes=["ropeC"])
                    s.dma("pool", ropeS[:], sin_d, writes=["ropeS"])
                    s.dma("pool", wal[:], wview(w_in_d[:, C_AL:C_AL + 32]), writes=["wal"])
                    s.dma("pool", wa2[:], w_a2b_d[0:16], writes=["wa2"])
                    s.dma("pool", ba2[:], w_a2b_d[16:17], writes=["ba2"])
                    s.dma("sp", gnb[:], gla_g_d.partition_broadcast(128), writes=["gnb"])
                    for tb in range(5):
                        if tb < 4:
                            rhs_of = lambda kt, tb=tb: aT[:, kt, tb * 512:(tb + 1) * 512]
                            n, c0, rk = 512, tb * 512, "aT"
                        else:
                            rhs_of = lambda kt: acT[:, kt, :]
                            n, c0, rk = CT, S, "acT"
                        for d_ in range(2):
                            for kt in range(8):
                                mm(B[d_][0:16, 0:n], wal[:, kt, d_ * 16:(d_ + 1) * 16], rhs_of(kt), kt == 0, kt == 7,
                                   ["wal", rk], [], x=[BK[d_]])
                            cp("act" if d_ == 0 else "dve", alT[d_][:, c0:c0 + n], B[d_][0:16, 0:n], [], ["alT%d" % d_], x=[BK[d_]])
                    if stop_after == "P2a":
                        s.barrier()
                        return nc

                    def load_head_w(h):
                        s.dma("pool", wq[:], wview(w_in_d[:, C_GQ + 128 * h:C_GQ + 128 * (h + 1)]), writes=["wq"])
                        s.dma("pool", wqs[:], wview(w_rope_d[:, 128 * h:128 * (h + 1)]), writes=["wqs"])
                        s.dma("pool", wk[:], wview(w_in_d[:, C_GK + 128 * h:C_GK + 128 * (h + 1)]), writes=["wk"])
                        s.dma("pool", wks[:], wview(w_rope_d[:, 512 + 128 * h:512 + 128 * (h + 1)]), writes=["wks"])
                        s.dma("pool", wv[:], wview(w_in_d[:, C_GV + 256 * h:C_GV + 256 * (h + 1)]), writes=["wv"])
                        s.dma("pool", wg[:], wview(w_in_d[:, C_GG + 256 * h:C_GG + 256 * (h + 1)]), writes=["wg"])

                    load_head_w(0)
                    for h in range(4):
                        for tb in range(4):
                            cols = slice(tb * 512, (tb + 1) * 512)
                            for bi, (w, wn) in enumerate(((wq, "wq"), (wqs, "wqs"), (wk, "wk"), (wks, "wks"))):
                                for kt in range(8):
                                    mm(B[bi][:, :], w[:, kt, :], aT[:, kt, cols], kt == 0, kt == 7, [wn, "aT"], [], x=[BK[bi]])
                            stt("dve", t1[0][:], B[0][:, :], QSCALE, ropeC[:, cols], ALU.mult, ALU.mult, ["ropeC"], ["t1_0"], x=[BK[0]])
                            stt("dve", t2[0][:], B[1][:, :], QSCALE, ropeS[:, cols], ALU.mult, ALU.mult, ["ropeS"], ["t2_0"], x=[BK[1]])
                            tt("pool", qrT[:, cols], t1[0][:], t2[0][:], ALU.add, ["t1_0", "t2_0"], ["qrT"])
                            tt("dve", t1[1][:], B[2][:, :], ropeC[:, cols], ALU.mult, ["ropeC"], ["t1_1"], x=[BK[2]])
                            tt("dve", t2[1][:], B[3][:, :], ropeS[:, cols], ALU.mult, ["ropeS"], ["t2_1"], x=[BK[3]])
                            tt("pool", krT[:, cols], t1[1][:], t2[1][:], ALU.add, ["t1_1", "t2_1"], ["krT"])
                        if stop_after == "P2b":
                            s.barrier()
                            return nc
                        for i in range(NTA):
                            lat = i < NT
                            bv = 4 + (i % 2)
                            bg = 2 + (i % 2)
                            srcT, rk, c0 = (aT, "aT", i * 128) if lat else (acT, "acT", (i - NT) * 128)
                            for kt in range(8):
                                mm(B[bv][:, 0:256], srcT[:, kt, c0:c0 + 128], wv[:, kt, :], kt == 0, kt == 7, [rk, "wv"], [], x=[BK[bv]])
                            cp("act", vtk[:, i, :], B[bv][:, 0:256], [], ["vtk"], x=[BK[bv]])
                            if lat:
                                for kt in range(8):
                                    mm(B[bg][:, 0:256], srcT[:, kt, c0:c0 + 128], wg[:, kt, :], kt == 0, kt == 7, [rk, "wg"], [], x=[BK[bg]])
                                act(sgt[:, i, :], B[bg][:, 0:256], AF.Silu, [], ["sgt"], x=[BK[bg]])
                            else:
                                for kt in range(8):
                                    mm(B[bg][:, 0:128], srcT[:, kt, c0:c0 + 128], wk[:, kt, :], kt == 0, kt == 7, [rk, "wk"], [], x=[BK[bg]])
                                cp("dve", krk[:, i, :], B[bg][:, 0:128], [], ["krk"], x=[BK[bg]])
                        for i in range(NT):
                            u = i % 2
                            tr(pT[u][:, 0, :], krT[:, i * 128:(i + 1) * 128], ident_b[:], ["krT", "ident_b"], [], x=[TK[u]])
                            cp("dve", krk[:, i, :], pT[u][:, 0, :], [], ["krk"], x=[TK[u]])
                        if stop_after == "P2c":
                            s.barrier()
                            return nc
                        if h + 1 < 4:
                            load_head_w(h + 1)
                        groups = [(0, 4), (4, 4), (8, 4), (12, 4), (16, 2)]
                        hc = slice(128 * h, 128 * (h + 1))
                        for (t0, nt_) in groups:
                            lat = t0 < NT
                            n = nt_ * 128
                            gc = slice(t0 * 128, t0 * 128 + n)
                            for d_ in range(2):
                                bz, bb, be = B[d_], B[2 + d_], B[4 + d_]
                                xz, xb, xe_ = [BK[d_]], [BK[2 + d_]], [BK[4 + d_]]
                                az_, ex_, rz_, L_ = t1[0], t1[1], t2[0], t2[1]
                                for tl in range(nt_):
                                    tokc = slice((t0 + tl) * 128, (t0 + tl + 1) * 128)
                                    zc = slice(tl * 128, (tl + 1) * 128)
                                    mm(bz[:, zc], alT[d_][0:16, tokc], wa2[0:16, d_, hc], True, False, ["alT%d" % d_, "wa2"], [], inc=False, x=xz)
                                    mm(bz[:, zc], ones_b[0:1, 0:128], ba2[0:1, d_, hc], False, True, ["ones_b", "ba2"], [], x=xz)
                                act(az_[:, 0:n], bz[:, 0:n], AF.Abs, [], ["t1_0"], x=xz)
                                ts("dve", rz_[:, 0:n], bz[:, 0:n], -1.0, 0.0, ALU.mult, ALU.max, [], ["t2_0"], x=xz)
                                act(ex_[:, 0:n], az_[:, 0:n], AF.Exp, ["t1_0"], ["t1_1"], scale=-1.0)
                                act(ex_[:, 0:n], ex_[:, 0:n], AF.Ln, ["t1_1", "one_c"], ["t1_1"], bias=one_c[:, 0:1])
                                tt("pool", L_[:, 0:n], rz_[:, 0:n], ex_[:, 0:n], ALU.add, ["t2_0", "t1_1"], ["t2_1"])
                                for tl in range(nt_):
                                    zc = slice(tl * 128, (tl + 1) * 128)
                                    mm(bb[:, zc], L_[:, zc], tri_f[:, 2 * d_, :], True, True, ["t2_1", "tri"], [], x=xb)
                                for tl in range(nt_):
                                    zc = slice(tl * 128, (tl + 1) * 128)
                                    mm(be[:, zc], tri_f[:, 2 * d_ + 1, :], L_[:, zc], True, True, ["t2_1", "tri"], [], x=xe_)
                                act(Dt[0][:, 0:n], bb[:, 0:n], AF.Exp, [], ["Dt0"], scale=-1.0 / 16, x=xb)
                                if lat:
                                    act(Di[0][:, 0:n], bb[:, 0:n], AF.Exp, [], ["Di0"], scale=1.0 / 16, x=xb)
                                act(EK[0][:, 0:n], be[:, 0:n], AF.Exp, [], ["EK0"], scale=-1.0 / 16, x=xe_)
                                dsrc = Dt[0][:, 63:n:64] if d_ == 0 else Dt[0][:, 0:n:64]
                                cp("pool", dec[:, d_, t0:t0 + nt_, :].rearrange("p t c -> p (t c)"), dsrc, ["Dt0"], ["dec"])
                                tt("pool", kd[d_][:, t0:t0 + nt_, :], krk[:, t0:t0 + nt_, :], EK[0][:, 0:n].rearrange("p (t c) -> p t c", c=128),
                                   ALU.mult, ["krk", "EK0"], ["kd%d" % d_])
                                if lat:
                                    tt("dve", qe[d_][:, gc], qrT[:, gc], Dt[0][:, 0:n], ALU.mult, ["qrT", "Dt0"], ["qe%d" % d_])
                                    tt("dve", ke[d_][:, gc], krT[:, gc], Di[0][:, 0:n], ALU.mult, ["krT", "Di0"], ["ke%d" % d_])
                        if stop_after == "P2d":
                            s.barrier()
                            return nc
                        ver = [0, 0]
                        for d_ in range(2):
                            s.op("pool", lambda e, d_=d_: e.memset(S32[d_][0][:], 0.0), writes=["S32_%d_0" % d_])
                            s.op("pool", lambda e, d_=d_: e.memset(S16[d_][0][:], 0.0), writes=["S16_%d_0" % d_])
                            cp("pool", trib[:, d_, :], tri_f[:, 2 * d_, :], ["tri"], ["trib"])

                        def kvmm(d_, i, half, par):
                            rows = slice(64 * half, 64 * half + 64)
                            bk = 2 + 2 * half + par
                            mm(B[bk][:, 256 * d_:256 * d_ + 256], kd[d_][rows, i, :], vtk[rows, i, :], True, True,
                               ["kd%d" % d_, "vtk"], [], x=[BK[bk]])

                        def upd(d_, i, half, par):
                            bk = 2 + 2 * half + par
                            pkv = B[bk][:, 256 * d_:256 * d_ + 256]
                            cv = ver[d_] % 2
                            nv = (ver[d_] + 1) % 2
                            stt("dve", S16[d_][nv][:], S32[d_][cv][:], dec[:, d_, i, half:half + 1], pkv, ALU.mult, ALU.add,
                                ["S32_%d_%d" % (d_, cv), "dec"], ["S16_%d_%d" % (d_, nv)], x=[BK[bk]])
                            stt("dve", S32[d_][nv][:], S32[d_][cv][:], dec[:, d_, i, half:half + 1], pkv, ALU.mult, ALU.add,
                                ["S32_%d_%d" % (d_, cv), "dec"], ["S32_%d_%d" % (d_, nv)], x=[BK[bk]])
                            ver[d_] += 1

                        for n_, i in enumerate((NT, NT + 1)):
                            for half in (0, 1):
                                kvmm(0, i, half, n_ % 2)
                            for half in (0, 1):
                                upd(0, i, half, n_ % 2)
                        for n_, i in enumerate((NT + 1, NT)):
                            for half in (1, 0):
                                kvmm(1, i, half, n_ % 2)
                            for half in (1, 0):
                                upd(1, i, half, n_ % 2)
                        if smp == 0 and h == 0:
                            dump("s_f", S32[0][ver[0] % 2][:], ["S32_0_%d" % (ver[0] % 2)])
                            dump("s_b", S32[1][ver[1] % 2][:], ["S32_1_%d" % (ver[1] % 2)])
                        if stop_after == "P2e":
                            s.barrier()
                            return nc

                        done = [0] * NT

                        def lat_att(d_, i):
                            tokc = slice(i * 128, (i + 1) * 128)
                            pa = B[d_][:, 256:384]
                            mm(pa, ke[d_][:, tokc], qe[d_][:, tokc], True, True, ["ke%d" % d_, "qe%d" % d_], [], x=[BK[d_]])
                            cp("act", attf[d_][:], pa, [], ["attf%d" % d_], x=[BK[d_]])
                            tt("pool", att[d_][:], attf[d_][:], trib[:, d_, :], ALU.mult, ["attf%d" % d_, "trib"], ["att%d" % d_])

                        def lat_po(d_, i):
                            po = B[d_][:, 0:256]
                            mm(po, att[d_][:], vtk[:, i, :], True, False, ["att%d" % d_, "vtk"], [], inc=False, x=[BK[d_]])

                        def lat_inter(d_, i, half, last):
                            po = B[d_][:, 0:256]
                            rows = slice(64 * half, 64 * half + 64)
                            c0 = i * 128 + 64 * half
                            cv = ver[d_] % 2
                            mm(po[rows, :], qe[d_][:, c0:c0 + 64], S16[d_][cv][:], False, last, ["qe%d" % d_, "S16_%d_%d" % (d_, cv)],
                               [], inc=True, x=[BK[d_]])

                        def lat_back(d_, i):
                            po = B[d_][:, 0:256]
                            xo = [BK[d_]]
                            tokc = slice(i * 128, (i + 1) * 128)
                            if done[i] == 0:
                                cp("act", oacc[:, i, :], po, [], ["oacc%d" % i], x=xo)
                                done[i] = 1
                                return
                            u = i % 2
                            cp("act", otmp[u], po, [], ["t1_%d" % u], x=xo)
                            tt("pool", oacc[:, i, :], otmp[u], oacc[:, i, :], ALU.add, ["t1_%d" % u, "oacc%d" % i], ["oacc%d" % i])
                            s.op("pool", lambda e, u=u: e.memset(st2[u][:], 0.0), writes=["st2_%d" % u])
                            act(junk[:], oacc[:, i, :], AF.Square, ["oacc%d" % i, "st2_%d" % u], ["junk2", "st2_%d" % u],
                                accum_out=st2[u][:, 0:1])
                            act(st2[u][:, 1:2], st2[u][:, 0:1], AF.Sqrt, ["st2_%d" % u, "eps_c"], ["st2_%d" % u],
                                scale=1.0 / 256, bias=eps_c[:, 0:1])
                            s.op("dve", lambda e, u=u: e.reciprocal(out=st2[u][:, 2:3], in_=st2[u][:, 1:2]),
                                 reads=["st2_%d" % u], writes=["st2_%d" % u])
                            act(onr[u], oacc[:, i, :], AF.Copy, ["oacc%d" % i, "st2_%d" % u], ["t2_%d" % u], scale=st2[u][:, 2:3])
                            tt("pool", onr[u], onr[u], gnb[:], ALU.mult, ["t2_%d" % u, "gnb"], ["t2_%d" % u])
                            tt("pool", rbf[u][:], onr[u], sgt[:, i, :], ALU.mult, ["t2_%d" % u, "sgt"], ["rbf%d" % u])
                            for j in range(2):
                                tr(pT[u][:, j, :], rbf[u][:, j * 128:(j + 1) * 128], ident_b[:], ["rbf%d" % u, "ident_b"], [], x=[TK[u]])
                            cp("act", oglT[:, 2 * h:2 * h + 2, tokc], pT[u][:, 0:2, :], [], ["oglT"], x=[TK[u]])

                        lat_att(0, 0)
                        lat_att(1, NT - 1)
                        kvmm(0, 0, 0, 0)
                        kvmm(0, 0, 1, 0)
                        kvmm(1, NT - 1, 1, 0)
                        kvmm(1, NT - 1, 0, 0)
                        for j in range(NT):
                            fi, bi_ = j, NT - 1 - j
                            par = j % 2
                            precast(1)
                            lat_po(0, fi)
                            lat_po(1, bi_)
                            lat_inter(0, fi, 0, False)
                            lat_inter(1, bi_, 1, False)
                            upd(0, fi, 0, par)
                            upd(1, bi_, 1, par)
                            lat_inter(0, fi, 1, True)
                            lat_inter(1, bi_, 0, True)
                            upd(0, fi, 1, par)
                            upd(1, bi_, 0, par)
                            lat_back(0, fi)
                            lat_back(1, bi_)
                            if j + 1 < NT:
                                lat_att(0, fi + 1)
                                lat_att(1, bi_ - 1)
                                kvmm(0, fi + 1, 0, 1 - par)
                                kvmm(0, fi + 1, 1, 1 - par)
                                kvmm(1, bi_ - 1, 1, 1 - par)
                                kvmm(1, bi_ - 1, 0, 1 - par)
                    if smp == 0:
                        dump("oglT", oglT[:, :, :], ["oglT"])
                    s.barrier()
                    print("P2 done: deadlock-free", s.check_deadlock(), s.n_ins, s.n_wait, s.cnt)
                if stop_after == "P2":
                    s.barrier()
                    return nc

                onaT = sb(sa, "onaT", [128, 4, S], BF16)
                with ExitStack() as st:
                    wq3 = sb(st, "wq3", [128, 8, 128], BF16)
                    wk3 = sb(st, "wk3", [128, 8, 128], BF16)
                    wv3 = sb(st, "wv3", [128, 8, 128], BF16)
                    nam = sb(st, "nam", [128, 8, 256], F32)
                    nabt = [sb(st, "nabt%d" % i, [128, 2, 256], F32) for i in range(2)]
                    BMp = sb(st, "BMp", [128, 8, 4, 2, 64], BF16)
                    qbd = sb(st, "qbd", [128, 32, 2, 64], BF16)
                    kT3 = sb(st, "kT3", [128, S + CT], BF16)
                    Ve = sb(st, "Ve", [128, 16, 2, 128], BF16)
                    Vo = sb(st, "Vo", [128, 15, 2, 128], BF16)
                    Vc = sb(st, "Vc", [128, 2, 2, 128], BF16)
                    PT = [sb(st, "PT%d" % i, [128, 6, 2, 64], BF16) for i in range(2)]
                    rden = [sb(st, "rden%d" % i, [128, 128], F32) for i in range(2)]
                    SB = [pst(st, "p3s%d" % i, [128, 512]) for i in range(4)]
                    SK = ["p3S%d" % i for i in range(4)]
                    OB = [pst(st, "p3o%d" % i, [128, 512]) for i in range(2)]
                    OK_ = ["p3O%d" % i for i in range(2)]
                    PB = [pst(st, "p3p%d" % i, [128, 512]) for i in range(2)]
                    PK = ["p3P%d" % i for i in range(2)]
                    s.dma("sp", nam[:], nam_d, writes=["nam"])
                    s.op("pool", lambda e: e.memset(qbd[:], 0.0), writes=["qbd"])
                    s.op("pool", lambda e: e.memset(Ve[:, :, :, 64:128], 1.0), writes=["Ve"])
                    s.op("pool", lambda e: e.memset(Vo[:, :, :, 64:128], 1.0), writes=["Vo"])
                    s.op("pool", lambda e: e.memset(Vc[:, :, :, 64:128], 1.0), writes=["Vc"])

                    def load_pair_w(pr):
                        s.dma("pool", wq3[:], wview(w_in_d[:, C_NAQ + 128 * pr:C_NAQ + 128 * (pr + 1)]), writes=["wq3"])
                        s.dma("pool", wk3[:], wview(w_in_d[:, C_NAK + 128 * pr:C_NAK + 128 * (pr + 1)]), writes=["wk3"])
                        s.dma("pool", wv3[:], wview(w_in_d[:, C_NAV + 128 * pr:C_NAV + 128 * (pr + 1)]), writes=["wv3"])

                    load_pair_w(0)
                    for pr in range(4):
                        for p in range(8):
                            s.dma("sp", nabt[p % 2][:], nab_d[pr, :, p, :, :], writes=["nabt%d" % (p % 2)])
                            for hh in range(2):
                                stt("dve", BMp[:, p, :, hh, :], nabt[p % 2][:, hh, :].rearrange("k (j q) -> k j q", q=64), 8.0,
                                    nam[:, p, :].rearrange("k (j q) -> k j q", q=64), ALU.mult, ALU.add,
                                    ["nabt%d" % (p % 2), "nam"], ["BMp"])
                        cnt_p = [0]

                        def proj(lhs_fn, rhs_fn, m, n, rk, evac):
                            u = cnt_p[0] % 2
                            cnt_p[0] += 1
                            for kt in range(8):
                                mm(PB[u][0:m, 0:n], lhs_fn(kt), rhs_fn(kt), kt == 0, kt == 7, rk, [], x=[PK[u]])
                            evac(PB[u], [PK[u]], "act" if u == 0 else "dve")

                        for tb in range(4):
                            cols = slice(tb * 512, (tb + 1) * 512)

                            def ev_q(ps, xk, eng, tb=tb):
                                for hh in range(2):
                                    hs = slice(64 * hh, 64 * hh + 64)
                                    cp(eng, qbd[hs, tb * 8:(tb + 1) * 8, hh, :], ps[hs, 0:512].rearrange("p (r q) -> p r q", q=64), [], ["qbd"], x=xk)
                            proj(lambda kt: wq3[:, kt, :], lambda kt, cols=cols: aT[:, kt, cols], 128, 512, ["wq3", "aT"], ev_q)
                            proj(lambda kt: wk3[:, kt, :], lambda kt, cols=cols: aT[:, kt, cols], 128, 512, ["wk3", "aT"],
                                 lambda ps, xk, eng, cols=cols: cp(eng, kT3[:, cols], ps[:, 0:512], [], ["kT3"], x=xk))
                        proj(lambda kt: wk3[:, kt, :], lambda kt: acT[:, kt, :], 128, CT, ["wk3", "acT"],
                             lambda ps, xk, eng: cp(eng, kT3[:, S:S + CT], ps[:, 0:CT], [], ["kT3"], x=xk))
                        for (Vt_, nt_, off, srcT, rk, vk) in ((Ve, 16, 0, aT, "aT", "Ve"), (Vo, 15, 64, aT, "aT", "Vo"), (Vc, 2, 0, acT, "acT", "Vc")):
                            for i in range(nt_):
                                proj(lambda kt, i=i, off=off, srcT=srcT: srcT[:, kt, off + i * 128:off + (i + 1) * 128], lambda kt: wv3[:, kt, :],
                                     128, 128, ["wv3", rk],
                                     lambda ps, xk, eng, i=i, Vt_=Vt_, vk=vk: cp(eng, Vt_[:, i, :, 0:64], ps[:, 0:128].rearrange("p (h d) -> p h d", d=64),
                                                                                  [], [vk], x=xk))
                        if pr + 1 < 4:
                            load_pair_w(pr + 1)

                        def scores(r):
                            rs = min(max(r - 4, 0), 24)
                            p = r - rs
                            buf = r % 2
                            bw, bc_ = SB[buf * 2], SB[buf * 2 + 1]
                            xw, xc = [SK[buf * 2]], [SK[buf * 2 + 1]]
                            qr = qbd[:, r, :, :].rearrange("p h q -> p (h q)")
                            mm(bw[:, 0:512], ident_b[:], BMp[:, p, :, :, :].rearrange("k j h q -> k (j h q)"), True, False,
                               ["ident_b", "BMp"], [], inc=False, x=xw)
                            for j in range(4):
                                kc0 = (rs + 2 * j) * 64
                                mm(bw[:, j * 128:(j + 1) * 128], kT3[:, kc0:kc0 + 128], qr, False, j == 3, ["kT3", "qbd"], [], inc=(j == 3), x=xw)
                            for jc in range(2):
                                mm(bc_[:, jc * 128:(jc + 1) * 128], kT3[:, S + jc * 128:S + (jc + 1) * 128], qr, True, True,
                                   ["kT3", "qbd"], [], inc=(jc == 1), x=xc)
                            act(PT[buf][:, 0:4, :, :].rearrange("k j h q -> k (j h q)"), bw[:, 0:512], AF.Exp, [], ["PT%d" % buf], scale=0.125, x=xw)
                            act(PT[buf][:, 4:6, :, :].rearrange("k j h q -> k (j h q)"), bc_[:, 0:256], AF.Exp, [], ["PT%d" % buf], scale=0.125, x=xc)

                        def pv(r):
                            rs = min(max(r - 4, 0), 24)
                            buf = r % 2
                            qc = slice(r * 64, r * 64 + 64)
                            ob = OB[buf]
                            xo = [OK_[buf]]
                            for hh in range(2):
                                for j in range(6):
                                    if j < 4:
                                        if rs % 2 == 0:
                                            Vt, vk = Ve[:, (rs + 2 * j) // 2, hh, :], "Ve"
                                        else:
                                            Vt, vk = Vo[:, (rs + 2 * j - 1) // 2, hh, :], "Vo"
                                    else:
                                        Vt, vk = Vc[:, j - 4, hh, :], "Vc"
                                    mm(ob[:, 64 * hh:64 * hh + 64], Vt, PT[buf][:, j, hh, :], j == 0, j == 5, [vk, "PT%d" % buf], [],
                                       inc=(j == 5), x=xo)
                            s.op("dve", lambda e: e.reciprocal(out=rden[buf][64:128, :], in_=ob[64:128, 0:128]), reads=[], writes=["rden%d" % buf], excl=xo)
                            for hh in range(2):
                                tt("dve", onaT[64 * hh:64 * hh + 64, pr, qc], ob[0:64, 64 * hh:64 * hh + 64], rden[buf][64:128, 64 * hh:64 * hh + 64],
                                   ALU.mult, ["rden%d" % buf], ["onaT"], x=xo)

                        for r in range(32):
                            if r < 8:
                                precast(1)
                            scores(r)
                            if r > 0:
                                pv(r - 1)
                        pv(31)
                    if smp == 0:
                        dump("onaT", onaT[:, :, :], ["onaT"])
                    s.barrier()
                    print("P3 done: deadlock-free", s.check_deadlock(), s.n_ins, s.n_wait, s.cnt)
                if stop_after == "P3":
                    s.barrier()
                    return nc

                UT = sb(sa, "UT", [128, 8, S], BF16)
                with ExitStack() as st:
                    wna = [sb(st, "wna%d" % i, [128, 4, 128], BF16) for i in range(2)]
                    wgl = [sb(st, "wgl%d" % i, [128, 8, 128], BF16) for i in range(2)]
                    w8 = [sb(st, "w8_%d" % i, [128, 8, 128], BF16) for i in range(2)]
                    w9 = [sb(st, "w9_%d" % i, [128, 8, 128], BF16) for i in range(2)]
                    sg8 = [sb(st, "sg8_%d" % i, [128, 512], F32) for i in range(2)]
                    sg9 = [sb(st, "sg9_%d" % i, [128, 512], F32) for i in range(2)]
                    t1m = [sb(st, "t1m%d" % i, [128, 512], F32) for i in range(2)]
                    t2m = [sb(st, "t2m%d" % i, [128, 512], F32) for i in range(2)]
                    MB = [pst(st, "p4b%d" % i, [128, 512]) for i in range(8)]
                    MK = ["p4B%d" % i for i in range(8)]
                    for nt in range(8):
                        wb = nt % 2
                        ncol = slice(nt * 128, (nt + 1) * 128)
                        s.dma("sp", wna[wb][:], wview(wna_b[:, ncol]), reads=["wnab"], writes=["wna%d" % wb])
                        s.dma("sp", wgl[wb][:], wview(wgl_b[:, ncol]), reads=["wglb"], writes=["wgl%d" % wb])
                        s.dma("sp", w8[wb][:], wview(wm8_b[:, ncol]), reads=["wm8b"], writes=["w8_%d" % wb])
                        s.dma("sp", w9[wb][:], wview(wm9_b[:, ncol]), reads=["wm9b"], writes=["w9_%d" % wb])
                        for tb in range(4):
                            cols = slice(tb * 512, (tb + 1) * 512)
                            u = (nt * 4 + tb) % 2
                            bA, bG8, bB, bG9 = [MB[4 * u + q] for q in range(4)]
                            kA, kG8, kB, kG9 = [[MK[4 * u + q]] for q in range(4)]
                            for kt in range(4):
                                mm(bA[:, :], wna[wb][:, kt, :], onaT[:, kt, cols], kt == 0, kt == 3, ["wna%d" % wb, "onaT"], [], x=kA)
                            for kt in range(8):
                                mm(bG8[:, :], w8[wb][:, kt, :], aT[:, kt, cols], kt == 0, kt == 7, ["w8_%d" % wb, "aT"], [], x=kG8)
                            for kt in range(8):
                                mm(bB[:, :], wgl[wb][:, kt, :], oglT[:, kt, cols], kt == 0, kt == 7, ["wgl%d" % wb, "oglT"], [], x=kB)
                            for kt in range(8):
                                mm(bG9[:, :], w9[wb][:, kt, :], aT[:, kt, cols], kt == 0, kt == 7, ["w9_%d" % wb, "aT"], [], x=kG9)
                            act(sg8[u][:], bG8[:, :], AF.Sigmoid, [], ["sg8_%d" % u], x=kG8)
                            act(sg9[u][:], bG9[:, :], AF.Sigmoid, [], ["sg9_%d" % u], x=kG9)
                            tt("dve", t1m[u][:], bA[:, :], sg8[u][:], ALU.mult, ["sg8_%d" % u], ["t1m%d" % u], x=kA)
                            tt("dve", t2m[u][:], bB[:, :], sg9[u][:], ALU.mult, ["sg9_%d" % u], ["t2m%d" % u], x=kB)
                            tt("pool", UT[:, nt, cols], t1m[u][:], t2m[u][:], ALU.add, ["t1m%d" % u, "t2m%d" % u], ["UT"])
                    if smp == 0:
                        dump("UT", UT[:, :, 0:256], ["UT"])
                    s.barrier()
                with ExitStack() as st:
                    wo = sb(st, "wo", [128, 8, D], BF16)
                    wtmp = [sb(st, "wtmp%d" % i, [128, D], F32) for i in range(2)]
                    g1b = sb(st, "g1b", [128, D], F32)
                    xt2 = [sb(st, "xt2_%d" % i, [128, D], F32) for i in range(2)]
                    h1t = [sb(st, "h1t%d" % i, [128, D], F32) for i in range(2)]
                    MB = [pst(st, "p4c%d" % i, [128, 512]) for i in range(4)]
                    MK = ["p4C%d" % i for i in range(4)]
                    s.dma("sp", g1b[:], modv_d[smp, 2 * D:3 * D].partition_broadcast(128), writes=["g1b"])
                    for kt in range(8):
                        s.dma("sp", wtmp[kt % 2][:], w_out_d[kt * 128:(kt + 1) * 128, :], writes=["wtmp%d" % (kt % 2)])
                        tt("pool", wo[:, kt, :], wtmp[kt % 2][:], g1b[:], ALU.mult, ["wtmp%d" % (kt % 2), "g1b"], ["wo"])
                    for i in range(NT):
                        u = i % 2
                        tokc = slice(i * 128, (i + 1) * 128)
                        s.dma("sp", xt2[u][:], x_d[smp, tokc, :], writes=["xt2_%d" % u])
                        for half in range(2):
                            hcol = slice(half * 512, (half + 1) * 512)
                            bk = MB[2 * u + half]
                            xk = [MK[2 * u + half]]
                            for kt in range(8):
                                mm(bk[:, :], UT[:, kt, tokc], wo[:, kt, hcol], kt == 0, kt == 7, ["UT", "wo"], [], x=xk)
                            tt("dve", h1t[u][:, hcol], bk[:, :], xt2[u][:, hcol], ALU.add, ["xt2_%d" % u], ["h1t%d" % u], x=xk)
                        s.dma("sp", h1_d[smp, tokc, :], h1t[u][:], reads=["h1t%d" % u], writes=["h1d"])
                        if smp == 0 and i < 2 and "h1" in dbg_d:
                            dump("h1", h1t[u][:], ["h1t%d" % u], dst=dbg_d["h1"][i * 128:(i + 1) * 128, :])
                    s.barrier()
                    print("P4 done: deadlock-free", s.check_deadlock(), s.n_ins, s.n_wait, s.cnt)
                if stop_after == "P4":
                    s.barrier()
                    return nc
            CAP = MOE_CAP
            NB = CAP // 128
            NSLOT = NEXP * CAP
            Xd = nc.dram_tensor("xd_scr%d" % smp, [NSLOT, D], BF16).ap()
            Yd = nc.dram_tensor("yd_scr%d" % smp, [NSLOT, D], BF16).ap()

            ind_hist = []
            IND_DEPTH = int(os.environ.get("IND_DEPTH", "1000"))

            def dma_fn(eng, fn, reads, writes):
                i_ = s.dnext
                s.dnext = (s.dnext + 1) % s.n_fg
                k_ = ("d", i_)
                waits = s._deps(eng, reads, writes)
                if s.dcnt[i_] > 0 and s.waited[eng].get(k_, -1) < s.dcnt[i_]:
                    s.waited[eng][k_] = s.dcnt[i_]
                    waits.append((k_, s.dcnt[i_]))
                if len(ind_hist) >= IND_DEPTH:
                    pk, pv = ind_hist[-IND_DEPTH]
                    if pk != k_ and s.waited[eng].get(pk, -1) < pv:
                        s.waited[eng][pk] = pv
                        waits.append((pk, pv))
                s.dcnt[i_] += 16
                tok = (k_, s.dcnt[i_])
                ind_hist.append(tok)
                s._emit1(eng, waits, fn, (k_, 16))
                s._commit(tok, reads, writes)

            with ExitStack() as sm:
                hres = sb(sm, "hres", [128, NT, D], F32)
                g2b = sb(sm, "g2b", [128, D], F32)
                gfb = sb(sm, "gfb", [128, D], F32)
                SLi = sb(sm, "SLi", [128, NT * 2], I32)
                w12 = sb(sm, "w12", [128, NT, 2], F32)
                s.dma("sp", g2b[:], modv_d[smp, 5 * D:6 * D].partition_broadcast(128), writes=["g2b"])
                s.dma("sp", gfb[:], g_fin_d.partition_broadcast(128), writes=["gfb"])
                with ExitStack() as st:
                    fbf = sb(st, "fbf", [128, NT, D], BF16)
                    M1a = sb(st, "M1a", [128, NT, 32], F32)
                    M2a = sb(st, "M2a", [128, NT, 32], F32)
                    Ma = sb(st, "Ma", [128, NT, 32], F32)
                    SLf = sb(st, "SLf", [128, NT, 2], F32)
                    ebase = sb(st, "ebase_sb", [128, 32], F32)
                    triS = sb(st, "triS", [128, 128], F32)
                    ones128 = sb(st, "ones128", [128, 128], F32)
                    s2b = sb(st, "s2b", [128, D], F32)
                    sh2b = sb(st, "sh2b", [128, D], F32)
                    gfn = sb(st, "gfn", [128, D], F32)
                    wrt = sb(st, "wrt", [128, 8, 36], F32)
                    brt = sb(st, "brt", [1, 36], F32)
                    ones_f = sb(st, "ones_f", [1, 128], F32)
                    xm3 = [sb(st, "xm3_%d" % i, [128, D], F32) for i in range(2)]
                    junk3 = sb(st, "junk3", [128, D], F32)
                    fT32 = [sb(st, "fT32_%d" % i, [128, 8, 128], F32) for i in range(2)]
                    LG = sb(st, "LG", [128, NT, 36], F32)
                    gm = sb(st, "gm", [128, NT, 8], F32)
                    ohg = sb(st, "ohg", [128, NT, 4], F32)
                    ge = sb(st, "ge", [128, NT, 4], F32)
                    big = sb(st, "bigr", [128, NT, 4, 8], F32)
                    leg = sb(st, "leg", [128, NT, 8], F32)
                    oh1 = sb(st, "oh1", [128, NT, 8], F32)
                    oh2 = sb(st, "oh2", [128, NT, 8], F32)
                    msk = sb(st, "msk", [128, NT, 8], F32)
                    rk = sb(st, "rk", [128, NT, 32], F32)
                    vm = sb(st, "vm", [128, NT, 32], F32)
                    st3 = [sb(st, "st3_%d" % i, [128, 4], F32) for i in range(2)]
                    pf = [[pst(st, "p5f%d_%d" % (i, j), [128, 4, 128]) for j in range(2)] for i in range(2)]
                    pfk = [["p5F%d_%d" % (i, j) for j in range(2)] for i in range(2)]
                    pl = [pst(st, "p5l%d" % i, [128, 512]) for i in range(2)]
                    plk = ["p5L%d" % i for i in range(2)]
                    s.dma("sp", gfn[:], g_ffn_d.partition_broadcast(128), writes=["gfn"])
                    s.dma("sp", s2b[:], modv_d[smp, 4 * D:5 * D].partition_broadcast(128), writes=["s2b"])
                    stt("dve", s2b[:], s2b[:], 1.0, gfn[:], ALU.add, ALU.mult, ["s2b", "gfn"], ["s2b"])
                    s.dma("sp", sh2b[:], modv_d[smp, 3 * D:4 * D].partition_broadcast(128), writes=["sh2b"])
                    s.dma("sp", wrt[:], wview(w_rt_d), writes=["wrt"])
                    s.dma("sp", brt[:], b_rt_d.partition_broadcast(1), writes=["brt"])
                    s.dma("sp", ebase[:], ebase_d, writes=["ebase"])
                    s.dma("sp", triS[:], tri_d[4], writes=["triS"])
                    s.op("pool", lambda e: e.memset(ones_f[:], 1.0), writes=["ones_f"])
                    s.op("pool", lambda e: e.memset(ones128[:], 1.0), writes=["ones128"])
                    for i in range(NT):
                        u = i % 2
                        tokc = slice(i * 128, (i + 1) * 128)
                        hk = "hres%d" % i
                        s.dma("sp", hres[:, i, :], h1_d[smp, tokc, :], reads=["h1d"], writes=[hk])
                        s.op("pool", lambda e, u=u: e.memset(st3[u][:], 0.0), writes=["st3_%d" % u])
                        act(junk3[:], hres[:, i, :], AF.Square, [hk, "st3_%d" % u], ["junk3", "st3_%d" % u], accum_out=st3[u][:, 0:1])
                        act(st3[u][:, 1:2], st3[u][:, 0:1], AF.Sqrt, ["st3_%d" % u, "eps_c"], ["st3_%d" % u], scale=1.0 / D, bias=eps_c[:, 0:1])
                        s.op("dve", lambda e, u=u: e.reciprocal(out=st3[u][:, 2:3], in_=st3[u][:, 1:2]), reads=["st3_%d" % u], writes=["st3_%d" % u])
                        stt("dve", xm3[u][:], hres[:, i, :], st3[u][:, 2:3], s2b[:], ALU.mult, ALU.mult, [hk, "st3_%d" % u, "s2b"], ["xm3_%d" % u])
                        tt("pool", xm3[u][:], xm3[u][:], sh2b[:], ALU.add, ["xm3_%d" % u, "sh2b"], ["xm3_%d" % u])
                        cp("pool", fbf[:, i, :], xm3[u][:], ["xm3_%d" % u], ["fbf%d" % i])
                        for kt in range(8):
                            tr(pf[u][kt // 4][:, kt % 4, :], xm3[u][:, kt * 128:(kt + 1) * 128], ident_f[:], ["xm3_%d" % u, "ident_f"], [], x=[pfk[u][kt // 4]])
                        for j in range(2):
                            cp("act" if j == 0 else "dve", fT32[u][:, 4 * j:4 * j + 4, :], pf[u][j][:, :, :], [], ["fT32_%d" % u], x=[pfk[u][j]])
                        for kt in range(8):
                            mm(pl[u][:, 0:36], fT32[u][:, kt, :], wrt[:, kt, :], kt == 0, False, ["fT32_%d" % u, "wrt"], [], inc=False, x=[plk[u]])
                        mm(pl[u][:, 0:36], ones_f[0:1, :], brt[0:1, :], False, True, ["ones_f", "brt"], [], x=[plk[u]])
                        cp("dve", LG[:, i, :], pl[u][:, 0:36], [], ["LG"], x=[plk[u]])
                    T_ = NT
                    dv = lambda fn: s.op("dve", fn, reads=["LG", "RT"], writes=["RT"])
                    bc = lambda ap, shp: ap.broadcast_to(shp)
                    lgg = LG[:, :, 0:4]
                    lge = LG[:, :, 4:36].rearrange("p t (g j) -> p t g j", j=8)
                    dv(lambda e: e.reduce_max(out=gm[:, :, 0], in_=lgg, axis=AX.X))
                    dv(lambda e: e.tensor_tensor(out=ohg[:], in0=lgg, in1=bc(gm[:, :, 0:1], [128, T_, 4]), op=ALU.is_equal))
                    dv(lambda e: e.tensor_tensor(out=ge[:], in0=lgg, in1=bc(gm[:, :, 0:1], [128, T_, 4]), op=ALU.subtract))
                    act(ge[:], ge[:], AF.Exp, ["RT"], ["RT"])
                    dv(lambda e: e.reduce_sum(out=gm[:, :, 1], in_=ge[:], axis=AX.X))
                    dv(lambda e: e.reciprocal(out=gm[:, :, 1], in_=gm[:, :, 1]))
                    dv(lambda e: e.tensor_tensor(out=big[:], in0=lge, in1=bc(ohg[:, :, :, None], [128, T_, 4, 8]), op=ALU.mult))
                    dv(lambda e: e.reduce_sum(out=leg[:], in_=big[:].rearrange("p t g j -> p t j g"), axis=AX.X))
                    dv(lambda e: e.reduce_max(out=gm[:, :, 2], in_=leg[:], axis=AX.X))
                    dv(lambda e: e.tensor_tensor(out=oh1[:], in0=leg[:], in1=bc(gm[:, :, 2:3], [128, T_, 8]), op=ALU.is_equal))
                    dv(lambda e: e.scalar_tensor_tensor(out=msk[:], in0=oh1[:], scalar=-1e30, in1=leg[:], op0=ALU.mult, op1=ALU.add))
                    dv(lambda e: e.reduce_max(out=gm[:, :, 3], in_=msk[:], axis=AX.X))
                    dv(lambda e: e.tensor_tensor(out=oh2[:], in0=msk[:], in1=bc(gm[:, :, 3:4], [128, T_, 8]), op=ALU.is_equal))
                    dv(lambda e: e.tensor_tensor(out=gm[:, :, 4], in0=gm[:, :, 3], in1=gm[:, :, 2], op=ALU.subtract))
                    act(gm[:, :, 4], gm[:, :, 4], AF.Exp, ["RT"], ["RT"])
                    dv(lambda e: e.tensor_scalar(out=gm[:, :, 5], in0=gm[:, :, 4], scalar1=1.0, scalar2=None, op0=ALU.add))
                    dv(lambda e: e.reciprocal(out=gm[:, :, 5], in_=gm[:, :, 5]))
                    s.op("dve", lambda e: e.tensor_tensor(out=w12[:, :, 0], in0=gm[:, :, 5], in1=gm[:, :, 1], op=ALU.mult), reads=["RT"], writes=["w12"])
                    s.op("dve", lambda e: e.tensor_tensor(out=w12[:, :, 1], in0=w12[:, :, 0], in1=gm[:, :, 4], op=ALU.mult), reads=["RT", "w12"], writes=["w12"])
                    m1v = M1a[:].rearrange("p t (g j) -> p t g j", j=8)
                    m2v = M2a[:].rearrange("p t (g j) -> p t g j", j=8)
                    s.op("dve", lambda e: e.tensor_tensor(out=m1v, in0=bc(ohg[:, :, :, None], [128, T_, 4, 8]), in1=bc(oh1[:, :, None, :], [128, T_, 4, 8]),
                                                          op=ALU.mult), reads=["RT"], writes=["M1a"])
                    s.op("dve", lambda e: e.tensor_tensor(out=m2v, in0=bc(ohg[:, :, :, None], [128, T_, 4, 8]), in1=bc(oh2[:, :, None, :], [128, T_, 4, 8]),
                                                          op=ALU.mult), reads=["RT"], writes=["M2a"])
                    tt("pool", Ma[:], M1a[:], M2a[:], ALU.add, ["M1a", "M2a"], ["Ma"])
                    for i in range(NT):
                        rc = slice(i * 32, (i + 1) * 32)
                        mm(pl[0][:, rc], triS[:], Ma[:, i, :], True, i == 0, ["triS", "Ma"], [], inc=(i == 0), x=[plk[0]])
                        for i2 in range(i):
                            mm(pl[0][:, rc], ones128[:], Ma[:, i2, :], False, i2 == i - 1, ["ones128", "Ma"], [], inc=(i2 == i - 1), x=[plk[0]])
                    dq = lambda fn, rd=(), wr=(), x=(): s.op("dve", fn, reads=["QT"] + list(rd), writes=["QT"] + list(wr), excl=x)
                    dq(lambda e: e.tensor_copy(out=rk[:], in_=pl[0][:, :].rearrange("p (t e) -> p t e", e=32)), x=[plk[0]])
                    dq(lambda e: e.tensor_scalar(out=vm[:], in0=rk[:], scalar1=float(CAP), scalar2=None, op0=ALU.is_lt))
                    dq(lambda e: e.tensor_tensor(out=rk[:], in0=rk[:], in1=bc(ebase[:, None, :], [128, T_, 32]), op=ALU.add), rd=["ebase"])
                    for k_, Mk, mk in ((0, M1a, "M1a"), (1, M2a, "M2a")):
                        dq(lambda e, Mk=Mk: e.tensor_tensor(out=big[:].rearrange("p t g j -> p t (g j)"), in0=Mk[:], in1=rk[:], op=ALU.mult), rd=[mk, "RT"], wr=["RT"])
                        dq(lambda e, k_=k_: e.reduce_sum(out=SLf[:, :, k_], in_=big[:].rearrange("p t g j -> p t (g j)"), axis=AX.X), rd=["RT"], wr=["SLf"])
                        dq(lambda e, Mk=Mk: e.tensor_tensor(out=big[:].rearrange("p t g j -> p t (g j)"), in0=Mk[:], in1=vm[:], op=ALU.mult), rd=[mk, "RT"], wr=["RT"])
                        dq(lambda e: e.reduce_sum(out=gm[:, :, 6], in_=big[:].rearrange("p t g j -> p t (g j)"), axis=AX.X), rd=["RT"], wr=["RT"])
                        dq(lambda e, k_=k_: e.tensor_tensor(out=w12[:, :, k_], in0=w12[:, :, k_], in1=gm[:, :, 6], op=ALU.mult), rd=["w12", "RT"], wr=["w12"])
                        dq(lambda e: e.tensor_scalar(out=gm[:, :, 7], in0=gm[:, :, 6], scalar1=-1.0e6, scalar2=1.0e6, op0=ALU.mult, op1=ALU.add), rd=["RT"], wr=["RT"])
                        dq(lambda e, k_=k_: e.tensor_tensor(out=SLf[:, :, k_], in0=SLf[:, :, k_], in1=gm[:, :, 7], op=ALU.add), rd=["RT", "SLf"], wr=["SLf"])
                    s.op("dve", lambda e: e.tensor_copy(out=SLi[:], in_=SLf[:].rearrange("p t k -> p (t k)")), reads=["SLf"], writes=["SLi"])
                    for i in range(NT):
                        for k_ in range(2):
                            dma_fn("pool", lambda e, k_=k_, i=i: e.indirect_dma_start(
                                out=Xd[:, :], out_offset=bass.IndirectOffsetOnAxis(ap=SLi[:, 2 * i + k_:2 * i + k_ + 1], axis=0),
                                in_=fbf[:, i, :], in_offset=None, bounds_check=bc_reg, oob_is_err=False),
                                ["SLi", "fbf%d" % i], ["Xd"])
                    if smp == 0:
                        dump("SLf", SLf[:, :, :], ["SLf"])
                        dump("w12", w12[:, :, :], ["w12"])
                    s.barrier()
                    print("P5 done: deadlock-free", s.check_deadlock(), s.n_ins, s.n_wait, s.cnt)
                if stop_after == "P5":
                    s.barrier()
                    return nc
                chunks = [(c0, min(c0 + 512, CAP)) for c0 in range(0, CAP, 512)]
                with ExitStack() as st:
                    weg = [sb(st, "weg%d" % i, [128, 8, DE], BF16) for i in range(2)]
                    weu = [sb(st, "weu%d" % i, [128, 8, DE], BF16) for i in range(2)]
                    wed = [sb(st, "wed%d" % i, [128, 4, D], BF16) for i in range(2)]
                    XeT = [sb(st, "XeT%d" % i, [128, 8, CAP], BF16) for i in range(2)]
                    hT = [sb(st, "hT%d" % i, [128, 4, CAP], BF16) for i in range(2)]
                    NXB = 2 * NB
                    xe = [sb(st, "xe%d" % i, [128, D], BF16) for i in range(NXB)]
                    ye = [sb(st, "ye%d" % i, [128, D], BF16) for i in range(NXB)]
                    sgm = [sb(st, "sgm%d" % i, [128, 512], F32) for i in range(2)]
                    TX = [pst(st, "p6t%d" % i, [128, 8, 128], BF16) for i in range(2)]
                    GB = [pst(st, "p6g%d" % i, [128, 512]) for i in range(2)]
                    UB = [pst(st, "p6u%d" % i, [128, 512]) for i in range(2)]
                    YB = [pst(st, "p6y%d" % i, [128, 512]) for i in range(2)]
                    TXK = ["p6T%d" % i for i in range(2)]
                    GK = ["p6G%d" % i for i in range(2)]
                    UK = ["p6U%d" % i for i in range(2)]
                    YK = ["p6Y%d" % i for i in range(2)]
                    cnt6 = [0, 0, 0]

                    def load_gu(e_):
                        wb = e_ % 2
                        s.dma("sp", weg[wb][:], wview(weg_b[e_]), reads=["wbg%d" % e_], writes=["weg%d" % wb])
                        s.dma("sp", weu[wb][:], wview(weu_b[e_]), reads=["wbu%d" % e_], writes=["weu%d" % wb])

                    def load_d(e_):
                        wb = e_ % 2
                        s.dma("sp", wed[wb][:], wview(wed_b[e_]), reads=["wbd%d" % e_], writes=["wed%d" % wb])
                        for kt in range(4):
                            tt("pool", wed[wb][:, kt, :], wed[wb][:, kt, :], g2b[:], ALU.mult, ["wed%d" % wb, "g2b"], ["wed%d" % wb])

                    cntx = [0]

                    def load_x(e_):
                        for blk in range(NB):
                            xb = cntx[0] % NXB
                            cntx[0] += 1
                            r0 = e_ * CAP + blk * 128
                            s.dma("sp", xe[xb][:], Xd[r0:r0 + 128, :], writes=["xe%d" % xb])

                    def ph_T(e_):
                        wb = e_ % 2
                        for blk in range(NB):
                            u = cnt6[0] % 2
                            xb = cnt6[0] % NXB
                            cnt6[0] += 1
                            for kt in range(8):
                                tr(TX[u][:, kt, :], xe[xb][:, kt * 128:(kt + 1) * 128], ident_b[:], ["xe%d" % xb, "ident_b"], [], x=[TXK[u]])
                            cp("act" if u == 0 else "dve", XeT[wb][:, :, blk * 128:(blk + 1) * 128], TX[u][:, :, :], [], ["XeT%d" % wb], x=[TXK[u]])

                    def ph_GU(e_):
                        wb = e_ % 2
                        for nt in range(4):
                            ncol = slice(nt * 128, (nt + 1) * 128)
                            for (c0, c1) in chunks:
                                u = cnt6[1] % 2
                                cnt6[1] += 1
                                n = c1 - c0
                                for kt in range(8):
                                    mm(GB[u][:, 0:n], weg[wb][:, kt, ncol], XeT[wb][:, kt, c0:c1], kt == 0, kt == 7, ["weg%d" % wb, "XeT%d" % wb], [], x=[GK[u]])
                                for kt in range(8):
                                    mm(UB[u][:, 0:n], weu[wb][:, kt, ncol], XeT[wb][:, kt, c0:c1], kt == 0, kt == 7, ["weu%d" % wb, "XeT%d" % wb], [], x=[UK[u]])
                                act(sgm[u][:, 0:n], GB[u][:, 0:n], AF.Silu, [], ["sgm%d" % u], x=[GK[u]])
                                tt("dve", hT[wb][:, nt, c0:c1], sgm[u][:, 0:n], UB[u][:, 0:n], ALU.mult, ["sgm%d" % u], ["hT%d" % wb], x=[UK[u]])
                                yield

                    def ph_D(e_):
                        wb = e_ % 2
                        for blk in range(NB):
                            yu = (e_ * NB + blk) % NXB
                            r0 = e_ * CAP + blk * 128
                            for half in range(2):
                                u = cnt6[2] % 2
                                cnt6[2] += 1
                                hcol = slice(half * 512, (half + 1) * 512)
                                for kt in range(4):
                                    mm(YB[u][:, :], hT[wb][:, kt, blk * 128:(blk + 1) * 128], wed[wb][:, kt, hcol], kt == 0, kt == 3,
                                       ["hT%d" % wb, "wed%d" % wb], [], x=[YK[u]])
                                cp("act" if half == 0 else "dve", ye[yu][:, hcol], YB[u][:, :], [], ["ye%d" % yu], x=[YK[u]])
                                if half == 1:
                                    s.dma("pool", Yd[r0:r0 + 128, :], ye[yu][:], reads=["ye%d" % yu])
                                yield

                    precast(96)
                    load_gu(0)
                    load_d(0)
                    load_x(0)
                    load_gu(1)
                    load_x(1)
                    ph_T(0)
                    def drain(g):
                        for _ in g:
                            pass

                    def interleave(g1, g2):
                        a1 = a2 = True
                        while a1 or a2:
                            if a1:
                                a1 = next(g1, "end") != "end"
                            if a2:
                                a2 = next(g2, "end") != "end"

                    drain(ph_GU(0))
                    for e_ in range(NEXP):
                        if e_ + 2 < NEXP:
                            load_gu(e_ + 2)
                        if e_ + 1 < NEXP:
                            load_d(e_ + 1)
                        if e_ + 2 < NEXP:
                            load_x(e_ + 2)
                        if e_ + 1 < NEXP:
                            ph_T(e_ + 1)
                            interleave(ph_GU(e_ + 1), ph_D(e_))
                        else:
                            drain(ph_D(e_))
                    s.barrier()
                    print("P6 done: deadlock-free", s.check_deadlock(), s.n_ins, s.n_wait, s.cnt)
                with ExitStack() as st:
                    yk = [[sb(st, "yk%d_%d" % (k_, i), [128, D], BF16) for i in range(2)] for k_ in range(2)]
                    ot = [sb(st, "ot%d" % i, [128, D], F32) for i in range(2)]
                    junk4 = sb(st, "junk4", [128, D], F32)
                    st4 = [sb(st, "st4_%d" % i, [128, 4], F32) for i in range(2)]
                    for k_ in range(2):
                        for i in range(2):
                            s.op("pool", lambda e, k_=k_, i=i: e.memset(yk[k_][i][:], 0.0), writes=["yk%d_%d" % (k_, i)])
                    for i in range(NT):
                        u = i % 2
                        tokc = slice(i * 128, (i + 1) * 128)
                        hk = "hres%d" % i
                        for k_ in range(2):
                            dma_fn("pool", lambda e, k_=k_, u=u, i=i: e.indirect_dma_start(
                                out=yk[k_][u][:, :], out_offset=None, in_=Yd[:, :],
                                in_offset=bass.IndirectOffsetOnAxis(ap=SLi[:, 2 * i + k_:2 * i + k_ + 1], axis=0), bounds_check=bc_reg, oob_is_err=False),
                                ["SLi"], ["yk%d_%d" % (k_, u)])
                            stt("dve", hres[:, i, :], yk[k_][u][:], w12[:, i, k_:k_ + 1], hres[:, i, :], ALU.mult, ALU.add,
                                ["yk%d_%d" % (k_, u), "w12", hk], [hk])
                        s.op("pool", lambda e, u=u: e.memset(st4[u][:], 0.0), writes=["st4_%d" % u])
                        act(junk4[:], hres[:, i, :], AF.Square, [hk, "st4_%d" % u], ["junk4", "st4_%d" % u], accum_out=st4[u][:, 0:1])
                        act(st4[u][:, 1:2], st4[u][:, 0:1], AF.Sqrt, ["st4_%d" % u, "eps_c"], ["st4_%d" % u], scale=1.0 / D, bias=eps_c[:, 0:1])
                        s.op("dve", lambda e, u=u: e.reciprocal(out=st4[u][:, 2:3], in_=st4[u][:, 1:2]), reads=["st4_%d" % u], writes=["st4_%d" % u])
                        stt("dve", ot[u][:], hres[:, i, :], st4[u][:, 2:3], gfb[:], ALU.mult, ALU.mult, [hk, "st4_%d" % u, "gfb"], ["ot%d" % u])
                        s.dma("sp", y_d[smp, tokc, :], ot[u][:], reads=["ot%d" % u])
                    s.barrier()
        s.barrier(final=True)
    return nc


def _consts():
    ident = np.eye(128, dtype=np.float32)
    idx = np.arange(128)
    same = (idx[:, None] // 64) == (idx[None, :] // 64)
    tri = np.zeros((6, 128, 128), np.float32)
    tri[0] = same & (idx[:, None] <= idx[None, :])
    tri[1] = same & (idx[:, None] > idx[None, :])
    tri[2] = same & (idx[:, None] >= idx[None, :])
    tri[3] = same & (idx[:, None] < idx[None, :])
    tri[4] = idx[:, None] < idx[None, :]
    tri[5] = 1.0
    t = np.arange(S)
    pos_r = (t // GW).astype(np.float32)
    pos_c = (t % GW).astype(np.float32)
    inv = (10000.0 ** (-np.arange(32, dtype=np.float32) / 32)).astype(np.float32)
    ang = np.zeros((128, S), np.float32)
    ang[0:32] = inv[:, None] * pos_r[None, :]
    ang[32:64] = inv[:, None] * pos_r[None, :]
    ang[64:96] = inv[:, None] * pos_c[None, :]
    ang[96:128] = inv[:, None] * pos_c[None, :]
    cos = np.cos(ang).astype(np.float32)
    sin = np.sin(ang).astype(np.float32)
    sgn = np.ones((128, 1), np.float32)
    sgn[0:32] = -1
    sgn[64:96] = -1
    sin = sin * sgn
    perm = np.concatenate([np.arange(32, 64), np.arange(0, 32), np.arange(96, 128), np.arange(64, 96)])
    return ident, tri, cos, sin, perm


def _na_tables(rpb):
    a = (np.arange(128) // 64)[:, None, None, None]
    kc = (np.arange(128) % 64)[:, None, None, None]
    p = np.arange(8)[None, :, None, None]
    j = np.arange(4)[None, None, :, None]
    qc = np.arange(64)[None, None, None, :]
    ridx = 2 * j + a - p + 7
    cstart = np.clip(qc - 8, 0, 48)
    valid = (kc >= cstart) & (kc < cstart + 16) & (ridx >= 0) & (ridx <= 14)
    valid = np.broadcast_to(valid, (128, 8, 4, 64))
    cidx = np.clip(kc - qc + 15, 0, 30)
    ridx_c = np.clip(ridx, 0, 14)
    ridx_b = np.broadcast_to(ridx_c, (128, 8, 4, 64))
    cidx_b = np.broadcast_to(cidx, (128, 8, 4, 64))
    g = rpb[:, ridx_b, cidx_b]
    g = np.where(valid[None], g, np.float32(0.0)).astype(np.float32)
    g = g.reshape(4, 2, 128, 8, 256).transpose(0, 2, 3, 1, 4)
    mask = np.where(valid, np.float32(0.0), np.float32(MASKV)).astype(np.float32).reshape(128, 8, 256)
    return np.ascontiguousarray(g), np.ascontiguousarray(mask)


_NC_CACHE = {}


def kernel(x, c, ctx, c_ctx, w_mod, b_mod, norm_attn_g, norm_ffn_g, w_in, w_gla_a2, b_gla_a2,
           gla_norm_g, na_rpb, w_na_o, w_gla_o, w_out, w_group, b_group, w_expert, b_expert,
           w_exp_gate, w_exp_up, w_exp_down, final_norm_g):
    f = lambda a: np.ascontiguousarray(np.asarray(a, dtype=np.float32))
    x, c, ctx, c_ctx = f(x), f(c), f(ctx), f(c_ctx)
    ident, tri, cos, sin, perm = _consts()
    w_in0 = f(w_in)[0]
    gq = w_in0[:, C_GQ:C_GQ + 512].reshape(D, 4, 128)[:, :, perm].reshape(D, 512)
    gk = w_in0[:, C_GK:C_GK + 512].reshape(D, 4, 128)[:, :, perm].reshape(D, 512)
    w_rope = np.ascontiguousarray(np.concatenate([gq, gk], axis=1))
    w_a2b = np.ascontiguousarray(np.concatenate(
        [f(w_gla_a2)[0].transpose(1, 0, 2), f(b_gla_a2)[0][None]], axis=0))
    nab, nam = _na_tables(f(na_rpb)[0])
    w_rt = np.ascontiguousarray(np.concatenate([f(w_group)[0], f(w_expert)[0]], axis=1))
    b_rt = np.ascontiguousarray(np.concatenate([f(b_group)[0], f(b_expert)[0]], axis=0))
    shared = {
        "w_mod": f(w_mod)[0], "b_mod": f(b_mod)[0], "g_attn": f(norm_attn_g)[0], "g_ffn": f(norm_ffn_g)[0],
        "g_fin": f(final_norm_g), "gla_g": f(gla_norm_g)[0], "w_in": w_in0, "w_rope": w_rope, "w_a2b": w_a2b,
        "w_na_o": f(w_na_o)[0], "w_gla_o": f(w_gla_o)[0], "w_out": f(w_out)[0], "w_rt": w_rt, "b_rt": b_rt,
        "w_eg": f(w_exp_gate)[0], "w_eu": f(w_exp_up)[0], "w_ed": f(w_exp_down)[0],
        "ident": ident, "tri": tri, "rope_cos": cos, "rope_sin": sin, "na_bias": nab, "na_mask": nam,
        "ebase": np.ascontiguousarray(np.broadcast_to((np.arange(NEXP, dtype=np.float32) * MOE_CAP)[None, :], (128, NEXP))),
    }
    n = 8
    NS = x.shape[0] // n
    if "nc" not in _NC_CACHE:
        _NC_CACHE["nc"] = build_nc(NS)
    nc = _NC_CACHE["nc"]
    in_maps = []
    for i in range(n):
        m = dict(shared)
        m["x"] = x[i * NS:(i + 1) * NS]
        m["ctx"] = ctx[i * NS:(i + 1) * NS]
        m["cc"] = np.ascontiguousarray(np.concatenate([c[i * NS:(i + 1) * NS], c_ctx[None]], axis=0))
        in_maps.append(m)
    res = run_bass_kernel_spmd(nc, in_maps, core_ids=list(range(n)))
    return np.concatenate([r["y"] for r in res.results], axis=0)
```
